# Optimizing a Trainium2 kernel written in Bass

```python
import math
import jax, jax.numpy as jnp
from jax import lax
import numpy as np

D_MODEL = 1024
BATCH = 2
SEQ = 16384
DEPTH = 2

N_MIXERS = 2
EPS = 1e-6
PLE_DIM = 256
SSD_EXPAND = 2
SSD_D_INNER = SSD_EXPAND * D_MODEL
SSD_HEAD_DIM = 64
SSD_N_HEADS = SSD_D_INNER // SSD_HEAD_DIM
SSD_N_GROUPS = 4
SSD_D_STATE = 128
SSD_CONV = 4
SSD_CHUNK = 128
SSD_CONV_DIM = SSD_D_INNER + 2 * SSD_N_GROUPS * SSD_D_STATE
SSD_IN_DIM = SSD_D_INNER + SSD_CONV_DIM + SSD_N_HEADS
SB_HEAD_DIM = 128
SB_N_HEADS = D_MODEL // SB_HEAD_DIM
SB_Q_BLOCK = 128
MOE_GROUPS = 8
MOE_EXPERTS_PER_GROUP = 8
MOE_EXPERTS = MOE_GROUPS * MOE_EXPERTS_PER_GROUP
MOE_TOP_K = 2
MOE_D_FF = 512
MOE_ROW_BLOCK = 128

N_SSD_LAYERS = (DEPTH + N_MIXERS - 1) // N_MIXERS
N_SB_LAYERS = DEPTH // N_MIXERS

kernel_name = "hybrid_ssd_stickbreaking_hmoe"


def rms_norm(x, g):
    xf = x.astype(jnp.float32)
    y = xf * lax.rsqrt(jnp.mean(xf * xf, axis=-1, keepdims=True) + EPS)
    return (y * g.astype(jnp.float32)).astype(x.dtype)


def causal_depthwise_conv(x, w, b):
    C = x.shape[-1]
    y = lax.conv_general_dilated(
        x, w[:, None, :].astype(x.dtype), window_strides=(1,),
        padding=[(SSD_CONV - 1, 0)], dimension_numbers=('NWC', 'WIO', 'NWC'),
        feature_group_count=C)
    return y + b.astype(x.dtype)


def ssd_chunked_scan(x, dt, A, Bm, Cm):
    Bsz, L, H, P = x.shape
    G, N, Q = SSD_N_GROUPS, SSD_D_STATE, SSD_CHUNK
    R = H // G
    nc = L // Q
    xc = (x * dt[..., None]).reshape(Bsz, nc, Q, G, R, P)
    acum = jnp.cumsum((dt * A).reshape(Bsz, nc, Q, H), axis=2)
    Bc = Bm.reshape(Bsz, nc, Q, G, N)
    Cc = Cm.reshape(Bsz, nc, Q, G, N)
    causal = jnp.tril(jnp.ones((Q, Q), dtype=bool))
    seg = acum[:, :, :, None, :] - acum[:, :, None, :, :]
    decay = jnp.exp(jnp.where(causal[None, None, :, :, None], seg, -jnp.inf))
    decay = decay.reshape(Bsz, nc, Q, Q, G, R)
    cb = jnp.einsum('bclgn,bcsgn->bclsg', Cc, Bc)
    w_ls = cb[..., None] * decay
    y_diag = jnp.einsum('bclsgr,bcsgrp->bclgrp', w_ls, xc)
    decay_to_end = jnp.exp(acum[:, :, -1:, :] - acum).reshape(Bsz, nc, Q, G, R)
    states = jnp.einsum('bcsgn,bcsgrp->bcgrpn', Bc, xc * decay_to_end[..., None])
    chunk_decay = jnp.exp(acum[:, :, -1, :]).reshape(Bsz, nc, G, R)

    def step(h, inp):
        s_c, a_c = inp
        return h * a_c[..., None, None] + s_c, h

    h0 = jnp.zeros((Bsz, G, R, P, N), states.dtype)
    _, states_in = lax.scan(step, h0, (jnp.swapaxes(states, 0, 1), jnp.swapaxes(chunk_decay, 0, 1)))
    states_in = jnp.swapaxes(states_in, 0, 1)
    decay_in = jnp.exp(acum).reshape(Bsz, nc, Q, G, R)
    y_off = jnp.einsum('bclgn,bcgrpn->bclgrp', Cc, states_in) * decay_in[..., None]
    return (y_diag + y_off).reshape(Bsz, L, H, P)


def ssd_mixer(u, w_in, conv_w, conv_b, dt_bias, a_log, d_skip, gnorm, w_out):
    Bsz, L, _ = u.shape
    zxbcdt = u @ w_in
    z = zxbcdt[..., :SSD_D_INNER]
    xbc = zxbcdt[..., SSD_D_INNER:SSD_D_INNER + SSD_CONV_DIM]
    dt_raw = zxbcdt[..., SSD_D_INNER + SSD_CONV_DIM:]
    xbc = jax.nn.silu(causal_depthwise_conv(xbc, conv_w, conv_b))
    gn = SSD_N_GROUPS * SSD_D_STATE
    xs = xbc[..., :SSD_D_INNER].reshape(Bsz, L, SSD_N_HEADS, SSD_HEAD_DIM)
    Bm = xbc[..., SSD_D_INNER:SSD_D_INNER + gn].reshape(Bsz, L, SSD_N_GROUPS, SSD_D_STATE)
    Cm = xbc[..., SSD_D_INNER + gn:].reshape(Bsz, L, SSD_N_GROUPS, SSD_D_STATE)
    dt = jax.nn.softplus(dt_raw.astype(jnp.float32) + dt_bias.astype(jnp.float32))
    A = -jnp.exp(a_log.astype(jnp.float32))
    y = ssd_chunked_scan(xs, dt, A, Bm, Cm)
    y = y + xs * d_skip[:, None]
    yz = (y.reshape(Bsz, L, SSD_D_INNER) * jax.nn.silu(z)).astype(jnp.float32)
    yg = yz.reshape(Bsz, L, SSD_N_GROUPS, SSD_D_INNER // SSD_N_GROUPS)
    yg = yg * lax.rsqrt(jnp.mean(yg * yg, axis=-1, keepdims=True) + EPS)
    yn = (yg.reshape(Bsz, L, SSD_D_INNER) * gnorm.astype(jnp.float32)).astype(u.dtype)
    return yn @ w_out


def stick_breaking_attention(u, w_qkv, w_o):
    Bsz, L, _ = u.shape
    Qb = SB_Q_BLOCK
    qkv = (u @ w_qkv).reshape(Bsz, L, 3, SB_N_HEADS, SB_HEAD_DIM)
    q = qkv[:, :, 0].transpose(0, 2, 1, 3) * (1.0 / math.sqrt(SB_HEAD_DIM))
    k = qkv[:, :, 1].transpose(0, 2, 1, 3)
    v = qkv[:, :, 2].transpose(0, 2, 1, 3)
    nb = L // Qb
    pos = jnp.arange(Qb)
    tri = (pos[:, None] >= pos[None, :]).astype(jnp.float32)
    diag_before = pos[None, :] < pos[:, None]
    outs = []
    for bi in range(nb):
        nk = bi + 1
        Lk = nk * Qb
        q_blk = q[:, :, bi * Qb:(bi + 1) * Qb]
        z = jnp.einsum('bhqd,bhkd->bhqk', q_blk, k[:, :, :Lk]).astype(jnp.float32)
        before = jnp.concatenate([jnp.ones((Qb, bi * Qb), bool), diag_before], axis=1)
        log_keep = jnp.where(before, -jax.nn.softplus(z), 0.0).reshape(Bsz, SB_N_HEADS, Qb, nk, Qb)
        within = jnp.einsum('bhqns,sj->bhqnj', log_keep, tri, precision=lax.Precision.HIGHEST)
        later = (jnp.arange(nk)[:, None] > jnp.arange(nk)[None, :]).astype(jnp.float32)
        offset = jnp.einsum('bhqm,mn->bhqn', within[..., 0], later, precision=lax.Precision.HIGHEST)
        log_a = z + (within + offset[..., None]).reshape(Bsz, SB_N_HEADS, Qb, Lk)
        a = jnp.exp(jnp.where(before, log_a, -jnp.inf))
        outs.append(jnp.einsum('bhqk,bhkd->bhqd', a.astype(v.dtype), v[:, :, :Lk]))
    o = jnp.concatenate(outs, axis=2)
    o = o.transpose(0, 2, 1, 3).reshape(Bsz, L, SB_N_HEADS * SB_HEAD_DIM)
    return o @ w_o


def hier_moe(h, w_rg, b_rg, w_re, b_re, w_gate, w_up, w_down):
    Bsz, L, D = h.shape
    T = Bsz * L
    ht = h.reshape(T, D)
    g_logits = (ht @ w_rg + b_rg).astype(jnp.float32)
    g_prob = jax.nn.softmax(g_logits, axis=-1)
    g_top, g_idx = lax.top_k(g_logits, 1)
    g_w = jnp.take_along_axis(g_prob, g_idx, axis=1)[:, 0]
    g_idx = g_idx[:, 0]
    e_logits = (ht @ w_re + b_re).astype(jnp.float32).reshape(T, MOE_GROUPS, MOE_EXPERTS_PER_GROUP)
    e_sel = jnp.take_along_axis(e_logits, g_idx[:, None, None], axis=1)[:, 0]
    e_top, e_idx = lax.top_k(e_sel, MOE_TOP_K)
    gates = g_w[:, None] * jax.nn.softmax(e_top, axis=-1)
    expert = g_idx[:, None] * MOE_EXPERTS_PER_GROUP + e_idx
    TK = T * MOE_TOP_K
    flat_e = expert.reshape(TK)
    flat_g = gates.reshape(TK)
    flat_tok = jnp.arange(TK) // MOE_TOP_K
    order = jnp.argsort(flat_e, stable=True)
    se, stok, sg = flat_e[order], flat_tok[order], flat_g[order]
    counts = jnp.bincount(flat_e, length=MOE_EXPERTS)
    padded = ((counts + MOE_ROW_BLOCK - 1) // MOE_ROW_BLOCK) * MOE_ROW_BLOCK
    pad_end = jnp.cumsum(padded)
    pad_start = pad_end - padded
    start = jnp.cumsum(counts) - counts
    dest = pad_start[se] + jnp.arange(TK) - start[se]
    n_rows = -(-TK // MOE_ROW_BLOCK) * MOE_ROW_BLOCK + MOE_EXPERTS * MOE_ROW_BLOCK
    n_blk = n_rows // MOE_ROW_BLOCK
    row_tok = jnp.full((n_rows,), T, jnp.int32).at[dest].set(stok.astype(jnp.int32))
    row_gate = jnp.zeros((n_rows,), jnp.float32).at[dest].set(sg)
    blk_expert = jnp.minimum(
        jnp.searchsorted(pad_end, jnp.arange(n_blk) * MOE_ROW_BLOCK, side='right'),
        MOE_EXPERTS - 1)
    h_pad = jnp.concatenate([ht, jnp.zeros((1, D), ht.dtype)], axis=0)
    x_rows = h_pad[row_tok].reshape(n_blk, MOE_ROW_BLOCK, D)

    def expert_block(args):
        xb, e = args
        return (jax.nn.silu(xb @ w_gate[e]) * (xb @ w_up[e])) @ w_down[e]

    y_rows = lax.map(expert_block, (x_rows, blk_expert)).reshape(n_rows, D)
    y_rows = y_rows * row_gate[:, None].astype(y_rows.dtype)
    out = jnp.zeros((T + 1, D), y_rows.dtype).at[row_tok].add(y_rows)[:T]
    return out.reshape(Bsz, L, D).astype(h.dtype)


def setup_inputs(seed: int = 0) -> dict:
    key = jax.random.key(seed)
    ks = iter(jax.random.split(key, 40))

    def nrm(shape, scale):
        return jax.random.normal(next(ks), shape, jnp.float32) * scale

    ns, nb = N_SSD_LAYERS, N_SB_LAYERS
    x = nrm((BATCH, SEQ, D_MODEL), 1.0)
    p = nrm((DEPTH, BATCH, SEQ, PLE_DIM), 1.0)
    ssd_norm = 1.0 + nrm((ns, D_MODEL), 0.02)
    ssd_w_in = nrm((ns, D_MODEL, SSD_IN_DIM), D_MODEL ** -0.5)
    ssd_conv_w = nrm((ns, SSD_CONV, SSD_CONV_DIM), SSD_CONV ** -0.5)
    ssd_conv_b = nrm((ns, SSD_CONV_DIM), 0.01)
    dt0 = jnp.exp(jax.random.uniform(next(ks), (ns, SSD_N_HEADS), jnp.float32,
                                     minval=math.log(1e-3), maxval=math.log(1e-1)))
    ssd_dt_bias = dt0 + jnp.log(-jnp.expm1(-dt0))
    ssd_a_log = jnp.log(jax.random.uniform(next(ks), (ns, SSD_N_HEADS), jnp.float32, minval=1.0, maxval=16.0))
    ssd_d = 1.0 + nrm((ns, SSD_N_HEADS), 0.1)
    ssd_gnorm = 1.0 + nrm((ns, SSD_D_INNER), 0.02)
    ssd_w_out = nrm((ns, SSD_D_INNER, D_MODEL), SSD_D_INNER ** -0.5)
    sb_norm = 1.0 + nrm((nb, D_MODEL), 0.02)
    sb_w_qkv = nrm((nb, D_MODEL, 3 * SB_N_HEADS * SB_HEAD_DIM), D_MODEL ** -0.5)
    sb_w_o = nrm((nb, SB_N_HEADS * SB_HEAD_DIM, D_MODEL), D_MODEL ** -0.5)
    moe_norm = 1.0 + nrm((DEPTH, D_MODEL), 0.02)
    moe_w_rg = nrm((DEPTH, D_MODEL, MOE_GROUPS), D_MODEL ** -0.5)
    moe_b_rg = nrm((DEPTH, MOE_GROUPS), 0.01)
    moe_w_re = nrm((DEPTH, D_MODEL, MOE_EXPERTS), D_MODEL ** -0.5)
    moe_b_re = nrm((DEPTH, MOE_EXPERTS), 0.01)
    moe_w_gate = nrm((DEPTH, MOE_EXPERTS, D_MODEL, MOE_D_FF), D_MODEL ** -0.5)
    moe_w_up = nrm((DEPTH, MOE_EXPERTS, D_MODEL, MOE_D_FF), D_MODEL ** -0.5)
    moe_w_down = nrm((DEPTH, MOE_EXPERTS, MOE_D_FF, D_MODEL), MOE_D_FF ** -0.5)
    ple_norm = 1.0 + nrm((DEPTH, D_MODEL), 0.02)
    ple_w_gate = nrm((DEPTH, D_MODEL, D_MODEL), D_MODEL ** -0.5)
    ple_b_gate = nrm((DEPTH, D_MODEL), 0.01)
    ple_w_proj = nrm((DEPTH, PLE_DIM, D_MODEL), PLE_DIM ** -0.5)
    final_norm = 1.0 + nrm((D_MODEL,), 0.02)
    return {"x": x, "p": p,
            "ssd_norm": ssd_norm, "ssd_w_in": ssd_w_in, "ssd_conv_w": ssd_conv_w, "ssd_conv_b": ssd_conv_b,
            "ssd_dt_bias": ssd_dt_bias, "ssd_a_log": ssd_a_log, "ssd_d": ssd_d, "ssd_gnorm": ssd_gnorm,
            "ssd_w_out": ssd_w_out,
            "sb_norm": sb_norm, "sb_w_qkv": sb_w_qkv, "sb_w_o": sb_w_o,
            "moe_norm": moe_norm, "moe_w_rg": moe_w_rg, "moe_b_rg": moe_b_rg, "moe_w_re": moe_w_re,
            "moe_b_re": moe_b_re, "moe_w_gate": moe_w_gate, "moe_w_up": moe_w_up, "moe_w_down": moe_w_down,
            "ple_norm": ple_norm, "ple_w_gate": ple_w_gate, "ple_b_gate": ple_b_gate, "ple_w_proj": ple_w_proj,
            "final_norm": final_norm}


def reference(x, p, ssd_norm, ssd_w_in, ssd_conv_w, ssd_conv_b, ssd_dt_bias, ssd_a_log, ssd_d, ssd_gnorm,
              ssd_w_out, sb_norm, sb_w_qkv, sb_w_o, moe_norm, moe_w_rg, moe_b_rg, moe_w_re, moe_b_re,
              moe_w_gate, moe_w_up, moe_w_down, ple_norm, ple_w_gate, ple_b_gate, ple_w_proj, final_norm):
    h = x
    for i in range(DEPTH):
        j = i // N_MIXERS
        if i % N_MIXERS == 0:
            h = h + ssd_mixer(rms_norm(h, ssd_norm[j]), ssd_w_in[j], ssd_conv_w[j], ssd_conv_b[j],
                              ssd_dt_bias[j], ssd_a_log[j], ssd_d[j], ssd_gnorm[j], ssd_w_out[j])
        else:
            h = h + stick_breaking_attention(rms_norm(h, sb_norm[j]), sb_w_qkv[j], sb_w_o[j])
        h = h + hier_moe(rms_norm(h, moe_norm[i]), moe_w_rg[i], moe_b_rg[i], moe_w_re[i], moe_b_re[i],
                         moe_w_gate[i], moe_w_up[i], moe_w_down[i])
        gate = jax.nn.sigmoid((rms_norm(h, ple_norm[i]) @ ple_w_gate[i] + ple_b_gate[i]).astype(jnp.float32))
        h = h + (gate * (p[i] @ ple_w_proj[i]).astype(jnp.float32)).astype(h.dtype)
    return rms_norm(h, final_norm)
```

```python
import numpy as np
import ml_dtypes
from contextlib import ExitStack
import concourse.bass as bass
import concourse.mybir as mybir
from concourse.bass_utils import run_bass_kernel_spmd

F32 = mybir.dt.float32
BF16 = mybir.dt.bfloat16
AF = mybir.ActivationFunctionType
ALU = mybir.AluOpType
AX = mybir.AxisListType
EPS = 1e-6
NPBF = ml_dtypes.bfloat16


class T:
    __slots__ = ("ap", "w", "r", "dsem", "dcnt", "name")

    def __init__(self, ap, name=""):
        self.ap = ap
        self.w = None
        self.r = {}
        self.dsem = None
        self.dcnt = 0
        self.name = name


class K:
    def __init__(self, nc, stack):
        self.nc = nc
        self.stack = stack
        self.eng = {"PE": nc.tensor, "ACT": nc.scalar, "DVE": nc.vector, "POOL": nc.gpsimd, "SP": nc.sync}
        self.sem = {}
        self.cnt = {}
        for e in ("PE", "ACT", "DVE", "POOL"):
            self.sem[e] = stack.enter_context(nc.semaphore("s_" + e))
            self.cnt[e] = 0
        self.seen = {e: {} for e in self.eng}
        self.semobj = dict(self.sem)
        self.ndsem = 0
        self.ninst = 0
        self.nalloc = 0

    def sb(self, shape, dt, name=None):
        self.nalloc += 1
        return self.stack.enter_context(self.nc.sbuf_tensor(name or ("sb%d" % self.nalloc), list(shape), dt))

    def ps(self, shape, dt, name=None):
        self.nalloc += 1
        return self.stack.enter_context(self.nc.psum_tensor(name or ("ps%d" % self.nalloc), list(shape), dt))

    def sbt(self, shape, dt, name=None):
        return T(self.sb(shape, dt, name)[:])

    def _need(self, e, key, val):
        if val <= 0 or self.seen[e].get(key, 0) >= val:
            return
        self.eng[e].wait_ge(self.semobj[key], val)
        self.seen[e][key] = val

    def op(self, e, fn, reads=(), writes=(), pe_acc=False):
        for t in reads:
            if t.w is not None:
                self._need(e, *t.w)
        for t in writes:
            if t.w is not None and not (pe_acc and t.w[0] == "PE"):
                self._need(e, *t.w)
            for kk, v in t.r.items():
                self._need(e, kk, v)
        ins = fn(self.eng[e])
        self.cnt[e] += 1
        c = self.cnt[e]
        ins.then_inc(self.sem[e], 1)
        for t in reads:
            t.r[e] = c
        for t in writes:
            t.w = (e, c)
            t.r = {}
        self.ninst += 1
        return ins

    def dma(self, q, out_ap, in_ap, reads=(), writes=(), part=False, **kw):
        assert len(writes) == 1
        t = writes[0]
        if t.dsem is None:
            key = "d%d" % self.ndsem
            t.dsem = self.stack.enter_context(self.nc.semaphore(key))
            self.semobj[key] = t.dsem
            t.name = key
            self.ndsem += 1
        key = t.name
        for r in reads:
            if r.w is not None:
                self._need(q, *r.w)
        if t.w is not None and not (part and t.w[0] == key):
            self._need(q, *t.w)
        for kk, v in t.r.items():
            self._need(q, kk, v)
        ins = self.eng[q].dma_start(out=out_ap, in_=in_ap, **kw)
        t.dcnt += 16
        ins.then_inc(t.dsem, 16)
        for r in reads:
            r.r[key] = max(r.r.get(key, 0), t.dcnt)
        t.w = (key, t.dcnt)
        if not part:
            t.r = {}
        self.ninst += 1
        return ins

    def alias(self, new, old):
        for n in new:
            for o in old:
                for kk, v in o.r.items():
                    n.r[kk] = max(n.r.get(kk, 0), v)
                if o.w is not None:
                    n.r[o.w[0]] = max(n.r.get(o.w[0], 0), o.w[1])

    def finish(self, e, tiles):
        for t in tiles:
            if t.w is not None:
                self._need(e, *t.w)


def v3(ap, a):
    return ap.rearrange("p (a n) -> p a n", a=a)


def build_tok(NT, TH, KA, mode, NE=64):
    nc = bass.Bass("TRN2", target_bir_lowering=False)

    def din(name, shape, dt=F32):
        return nc.dram_tensor(name, list(shape), dt, kind="ExternalInput").ap()

    def dout(name, shape, dt=F32):
        return nc.dram_tensor(name, list(shape), dt, kind="ExternalOutput").ap()

    hT = din("hT", [1024, NT])
    aT = din("aT", [KA, NT], BF16)
    w_a = din("w_a", [KA, 1024])
    vecs = din("vecs", [128, 32])
    wr = din("wr", [1024, 72])
    br = din("br", [1, 72])
    wg = din("wg", [64, 1024, 512])
    wu = din("wu", [64, 1024, 512])
    wd = din("wd", [64, 512, 1024])
    plg = din("plg", [1024, 1024])
    plp = din("plp", [256, 1024])
    pT = din("pT", [256, NT])
    ident_d = din("ident", [128, 128])
    if mode == "A":
        wqkv = din("wqkv", [1024, 3072])
        hT_o = dout("hT_o", [1024, NT])
        qT_o = dout("qT_o", [1024, NT], BF16)
        kT_o = dout("kT_o", [1024, NT], BF16)
        v_o = dout("v_o", [NT, 1024], BF16)
    else:
        out_o = dout("out_o", [1024, NT])
    KAC = KA // 128
    NTL = TH // 512
    NH = NT // TH

    with ExitStack() as st:
        k = K(nc, st)
        h_sb = k.sb([128, 8 * TH], F32, "h_sb")
        h3 = v3(h_sb[:], 8)
        HT = [[T(h3[:, d, t * 512:(t + 1) * 512]) for d in range(8)] for t in range(NTL)]
        u_sb = k.sb([128, 8 * TH], BF16, "u_sb")
        u3 = v3(u_sb[:], 8)
        UT = [T(u3[:, :, t * 512:(t + 1) * 512]) for t in range(NTL)]
        WB = [k.sbt([128, 12288], BF16, "wb%d" % i) for i in range(2)]
        GT = k.sbt([64, TH], F32, "gt")
        NAB = max(1, min(2, (8 * TH) // (KAC * 512)))
        ABS = [T(u_sb[:, i * KAC * 512:(i + 1) * KAC * 512]) for i in range(NAB)]
        PB = k.sbt([128, 2 * 512], BF16, "pb")
        SQ = k.sbt([128, 8 * 512], BF16, "sq")
        LNV = k.sbt([128, 512], F32, "lnv")
        RSTD = k.sbt([128, 512], F32, "rstd")
        vec = k.sbt([128, 32], F32, "vec_sb")
        ident = k.sbt([128, 128], F32, "ident_sb")
        ones_bf = k.sbt([128, 128], BF16, "ones")
        wr_sb = k.sbt([128, 8 * 72], F32, "wr_sb")
        br_bc = k.sbt([128, 72], F32, "brbc")
        S_ = [k.sbt([128, 512], BF16, "s%d" % i) for i in range(2)]
        TT = [k.sbt([128, 512], BF16, "tt%d" % i) for i in range(2)]
        HS = [[k.sbt([128, 512], BF16, "hs%d_%d" % (i, f)) for f in range(4)] for i in range(2)]
        EB = [k.sbt([64, 128], F32, "eb%d" % i) for i in range(2)]
        STG = [k.sbt([128, 512], BF16, "stg%d" % i) for i in range(2)]
        STF = [k.sbt([128, 512], F32, "stf%d" % i) for i in range(2)]
        r_lg = k.sbt([128, 72], F32, "r_lg")
        r_a = k.sbt([128, 8], F32, "r_a")
        r_goh = k.sbt([128, 8], F32, "r_goh")
        r_t64 = k.sbt([128, 64], F32, "r_t64")
        r_es = k.sbt([128, 8], F32, "r_es")
        r_es2 = k.sbt([128, 8], F32, "r_es2")
        r_oh1 = k.sbt([128, 8], F32, "r_oh1")
        r_oh2 = k.sbt([128, 8], F32, "r_oh2")
        r_ew = k.sbt([128, 8], F32, "r_ew")
        r_G = k.sbt([128, 64], F32, "r_G")
        r_s = [k.sbt([128, 1], F32, "r_s%d" % i) for i in range(12)]
        BK = [T(k.ps([128, 512], F32, "bk%d" % i)[:]) for i in range(8)]
        PG, PU, PD, PBK = BK[0:2], BK[2:4], BK[4:6], BK[6:8]

        k.dma("SP", vec.ap, vecs, writes=[vec])
        k.dma("SP", ident.ap, ident_d, writes=[ident])
        k.dma("SP", v3(wr_sb.ap, 8), wr.rearrange("(a p) n -> p a n", p=128), writes=[wr_sb])
        k.dma("SP", br_bc.ap, br.partition_broadcast(128).rearrange("p o n -> p (o n)"), writes=[br_bc])
        k.op("DVE", lambda e: e.memset(ones_bf.ap, 1.0), writes=[ones_bf])
        for kc in range(8):
            k.op("DVE", lambda e: e.tensor_scalar(out=wr_sb.ap[:, kc * 72:(kc + 1) * 72], in0=wr_sb.ap[:, kc * 72:(kc + 1) * 72],
                                                  scalar1=vec.ap[:, kc:kc + 1], scalar2=None, op0=ALU.mult),
                 reads=[vec, wr_sb], writes=[wr_sb])

        def rmsnorm(src_tiles, src_ap, gcol, dst_t, dst_ap, bank):
            k.op("ACT", lambda e: e.activation(out=v3(SQ.ap, 8), in_=src_ap, func=AF.Square), reads=src_tiles, writes=[SQ])
            for kc in range(8):
                k.op("PE", lambda e: e.matmul(bank.ap, ones_bf.ap, v3(SQ.ap, 8)[:, kc, :], start=(kc == 0), stop=(kc == 7)),
                     reads=[SQ, ones_bf], writes=[bank], pe_acc=(kc > 0))
            k.op("ACT", lambda e: e.activation(out=LNV.ap, in_=bank.ap, func=AF.Ln, scale=1.0 / 1024, bias=EPS), reads=[bank], writes=[LNV])
            k.op("ACT", lambda e: e.activation(out=RSTD.ap, in_=LNV.ap, func=AF.Exp, scale=-0.5), reads=[LNV], writes=[RSTD])
            for kc in range(8):
                k.op("DVE", lambda e: e.scalar_tensor_tensor(out=dst_ap[:, kc, :], in0=src_ap[:, kc, :], scalar=vec.ap[:, gcol + kc:gcol + kc + 1],
                                                             in1=RSTD.ap, op0=ALU.mult, op1=ALU.mult),
                     reads=list(src_tiles) + [RSTD, vec], writes=[dst_t])

        if mode == "A":
            qT_t, kT_t, v_t, ho_t = T(qT_o), T(kT_o), T(v_o), T(hT_o)
        else:
            out_t = T(out_o)
        for hf in range(NH):
            c0 = hf * TH
            k.alias(ABS, UT)
            for kp in range(KAC // 8):
                k.dma("POOL", v3(WB[kp].ap[:, 0:8192], 8), w_a[kp * 1024:(kp + 1) * 1024, :].rearrange("(a p) n -> p a n", p=128), writes=[WB[kp]])
            for t in range(NTL):
                cs = slice(c0 + t * 512, c0 + (t + 1) * 512)
                for d in range(8):
                    k.dma("SP", HT[t][d].ap, hT[d * 128:(d + 1) * 128, cs], writes=[HT[t][d]])
                AB = ABS[t % NAB]
                k.dma("SP", v3(AB.ap, KAC), aT[:, cs].rearrange("(a p) n -> p a n", p=128), writes=[AB])
                for d in range(8):
                    bank = PD[d % 2]
                    for kc in range(KAC):
                        wbt = WB[kc // 8]
                        k.op("PE", lambda e: e.matmul(bank.ap, v3(wbt.ap[:, 0:8192], 8)[:, kc % 8, d * 128:(d + 1) * 128], v3(AB.ap, KAC)[:, kc, :],
                                                      start=(kc == 0), stop=(kc == KAC - 1)),
                             reads=[wbt, AB], writes=[bank], pe_acc=(kc > 0))
                    k.op("DVE", lambda e: e.tensor_tensor(out=HT[t][d].ap, in0=HT[t][d].ap, in1=bank.ap, op=ALU.add),
                         reads=[bank, HT[t][d]], writes=[HT[t][d]])
            k.alias(UT, ABS)
            for t in range(NTL):
                hap = h3[:, :, t * 512:(t + 1) * 512]
                rmsnorm(HT[t], hap, 0, UT[t], UT[t].ap, PBK[0])
                for b in range(4):
                    bs = slice(t * 512 + b * 128, t * 512 + (b + 1) * 128)
                    plg_b, prs = PG[b % 2], PU[b % 2]
                    for kc in range(8):
                        k.op("PE", lambda e: e.matmul(plg_b.ap[:, 0:72], h3[:, kc, bs], wr_sb.ap[:, kc * 72:(kc + 1) * 72], start=(kc == 0), stop=(kc == 7)),
                             reads=[HT[t][kc], wr_sb], writes=[plg_b], pe_acc=(kc > 0))
                    k.op("PE", lambda e: e.matmul(prs.ap[:, 0:2], RSTD.ap[:, b * 128:(b + 1) * 128], ident.ap[:, 0:2], start=True, stop=True),
                         reads=[RSTD, ident], writes=[prs])
                    rt, gmax, ngmax, gsum, gw, m1, m2, dd, ex, w1, w2 = r_s[0:11]
                    k.op("DVE", lambda e: e.tensor_copy(out=rt.ap, in_=prs.ap[:, 0:1]), reads=[prs], writes=[rt])
                    k.op("DVE", lambda e: e.scalar_tensor_tensor(out=r_lg.ap, in0=plg_b.ap[:, 0:72], scalar=rt.ap, in1=br_bc.ap, op0=ALU.mult, op1=ALU.add),
                         reads=[plg_b, rt, br_bc], writes=[r_lg])
                    gl = r_lg.ap[:, 0:8]
                    el = r_lg.ap[:, 8:72]
                    k.op("DVE", lambda e: e.tensor_reduce(out=gmax.ap, in_=gl, axis=AX.X, op=ALU.max), reads=[r_lg], writes=[gmax])
                    k.op("DVE", lambda e: e.tensor_scalar(out=r_goh.ap, in0=gl, scalar1=gmax.ap, scalar2=None, op0=ALU.is_equal), reads=[r_lg, gmax], writes=[r_goh])
                    k.op("DVE", lambda e: e.tensor_scalar(out=ngmax.ap, in0=gmax.ap, scalar1=-1.0, scalar2=None, op0=ALU.mult), reads=[gmax], writes=[ngmax])
                    k.op("ACT", lambda e: e.activation(out=r_a.ap, in_=gl, func=AF.Exp, bias=ngmax.ap, scale=1.0, accum_out=gsum.ap), reads=[r_lg, ngmax], writes=[r_a, gsum])
                    k.op("DVE", lambda e: e.reciprocal(out=gw.ap, in_=gsum.ap), reads=[gsum], writes=[gw])
                    k.op("DVE", lambda e: e.tensor_tensor(out=v3(r_t64.ap, 8), in0=v3(el, 8), in1=r_goh.ap.unsqueeze(2).to_broadcast([128, 8, 8]), op=ALU.mult),
                         reads=[r_lg, r_goh], writes=[r_t64])
                    k.op("DVE", lambda e: e.tensor_reduce(out=r_es.ap, in_=v3(r_t64.ap, 8).rearrange("p g j -> p j g"), axis=AX.X, op=ALU.add), reads=[r_t64], writes=[r_es])
                    k.op("DVE", lambda e: e.tensor_reduce(out=m1.ap, in_=r_es.ap, axis=AX.X, op=ALU.max), reads=[r_es], writes=[m1])
                    k.op("DVE", lambda e: e.tensor_scalar(out=r_oh1.ap, in0=r_es.ap, scalar1=m1.ap, scalar2=None, op0=ALU.is_equal), reads=[r_es, m1], writes=[r_oh1])
                    k.op("DVE", lambda e: e.scalar_tensor_tensor(out=r_es2.ap, in0=r_oh1.ap, scalar=-1e30, in1=r_es.ap, op0=ALU.mult, op1=ALU.add), reads=[r_oh1, r_es], writes=[r_es2])
                    k.op("DVE", lambda e: e.tensor_reduce(out=m2.ap, in_=r_es2.ap, axis=AX.X, op=ALU.max), reads=[r_es2], writes=[m2])
                    k.op("DVE", lambda e: e.tensor_scalar(out=r_oh2.ap, in0=r_es2.ap, scalar1=m2.ap, scalar2=None, op0=ALU.is_equal), reads=[r_es2, m2], writes=[r_oh2])
                    k.op("DVE", lambda e: e.tensor_tensor(out=dd.ap, in0=m2.ap, in1=m1.ap, op=ALU.subtract), reads=[m1, m2], writes=[dd])
                    k.op("ACT", lambda e: e.activation(out=ex.ap, in_=dd.ap, func=AF.Exp), reads=[dd], writes=[ex])
                    k.op("DVE", lambda e: e.tensor_scalar(out=w1.ap, in0=ex.ap, scalar1=1.0, scalar2=None, op0=ALU.add), reads=[ex], writes=[w1])
                    k.op("DVE", lambda e: e.reciprocal(out=w1.ap, in_=w1.ap), reads=[w1], writes=[w1])
                    k.op("DVE", lambda e: e.tensor_tensor(out=w2.ap, in0=ex.ap, in1=w1.ap, op=ALU.mult), reads=[ex, w1], writes=[w2])
                    k.op("DVE", lambda e: e.tensor_tensor(out=w1.ap, in0=w1.ap, in1=gw.ap, op=ALU.mult), reads=[w1, gw], writes=[w1])
                    k.op("DVE", lambda e: e.tensor_tensor(out=w2.ap, in0=w2.ap, in1=gw.ap, op=ALU.mult), reads=[w2, gw], writes=[w2])
                    k.op("DVE", lambda e: e.tensor_scalar(out=r_ew.ap, in0=r_oh1.ap, scalar1=w1.ap, scalar2=None, op0=ALU.mult), reads=[r_oh1, w1], writes=[r_ew])
                    k.op("DVE", lambda e: e.scalar_tensor_tensor(out=r_ew.ap, in0=r_oh2.ap, scalar=w2.ap, in1=r_ew.ap, op0=ALU.mult, op1=ALU.add), reads=[r_oh2, w2, r_ew], writes=[r_ew])
                    k.op("DVE", lambda e: e.tensor_tensor(out=v3(r_G.ap, 8), in0=r_goh.ap.unsqueeze(2).to_broadcast([128, 8, 8]),
                                                          in1=r_ew.ap.unsqueeze(1).to_broadcast([128, 8, 8]), op=ALU.mult), reads=[r_goh, r_ew], writes=[r_G])
                    ptr = PD[b % 2]
                    k.op("PE", lambda e: e.transpose(ptr.ap[0:64, 0:128], r_G.ap, ident.ap), reads=[r_G, ident], writes=[ptr])
                    k.op("DVE", lambda e: e.tensor_copy(out=GT.ap[:, bs], in_=ptr.ap[0:64, 0:128]), reads=[ptr], writes=[GT])
            def load_expert(e_):
                wb = WB[e_ % 2]
                k.dma("POOL", v3(wb.ap[:, 0:4096], 8), wg[e_].rearrange("(a p) n -> p a n", p=128), writes=[wb])
                k.dma("POOL", v3(wb.ap[:, 4096:8192], 8), wu[e_].rearrange("(a p) n -> p a n", p=128), writes=[wb], part=True)
                k.dma("POOL", v3(wb.ap[:, 8192:12288], 4), wd[e_].rearrange("(a p) n -> p a n", p=128), writes=[wb], part=True)

            def emit_gu(e_, t, i):
                wb = WB[e_ % 2]
                Wg3 = v3(wb.ap[:, 0:4096], 8)
                Wu3 = v3(wb.ap[:, 4096:8192], 8)
                pb = PBK[i % 2]
                eb = EB[i % 2]
                k.op("DVE", lambda e: e.tensor_copy(out=eb.ap, in_=ident.ap[0:64, e_:e_ + 1].to_broadcast([64, 128])), reads=[ident], writes=[eb])
                k.op("PE", lambda e: e.matmul(pb.ap, eb.ap, GT.ap[:, t * 512:(t + 1) * 512], start=True, stop=True), reads=[eb, GT], writes=[pb])
                for f in range(4):
                    pg, pu = PG[f % 2], PU[f % 2]
                    for kc in range(8):
                        k.op("PE", lambda e: e.matmul(pg.ap, Wg3[:, kc, f * 128:(f + 1) * 128], UT[t].ap[:, kc, :], start=(kc == 0), stop=(kc == 7)),
                             reads=[wb, UT[t]], writes=[pg], pe_acc=(kc > 0))
                    for kc in range(8):
                        k.op("PE", lambda e: e.matmul(pu.ap, Wu3[:, kc, f * 128:(f + 1) * 128], UT[t].ap[:, kc, :], start=(kc == 0), stop=(kc == 7)),
                             reads=[wb, UT[t]], writes=[pu], pe_acc=(kc > 0))
                    s_, tt = S_[f % 2], TT[f % 2]
                    k.op("ACT", lambda e: e.activation(out=s_.ap, in_=pg.ap, func=AF.Silu), reads=[pg], writes=[s_])
                    k.op("DVE", lambda e: e.tensor_tensor(out=tt.ap, in0=s_.ap, in1=pu.ap, op=ALU.mult), reads=[s_, pu], writes=[tt])
                    k.op("DVE", lambda e: e.tensor_tensor(out=HS[i % 2][f].ap, in0=tt.ap, in1=pb.ap, op=ALU.mult), reads=[tt, pb], writes=[HS[i % 2][f]])

            def emit_dn(e_, t, i):
                wb = WB[e_ % 2]
                Wd3 = v3(wb.ap[:, 8192:12288], 4)
                for d in range(8):
                    pd = PD[d % 2]
                    for f in range(4):
                        k.op("PE", lambda e: e.matmul(pd.ap, Wd3[:, f, d * 128:(d + 1) * 128], HS[i % 2][f].ap, start=(f == 0), stop=(f == 3)),
                             reads=[wb, HS[i % 2][f]], writes=[pd], pe_acc=(f > 0))
                    k.op("DVE", lambda e: e.tensor_tensor(out=HT[t][d].ap, in0=HT[t][d].ap, in1=pd.ap, op=ALU.add), reads=[pd, HT[t][d]], writes=[HT[t][d]])

            seq = [(e_, t) for e_ in range(NE) for t in range(NTL)]
            load_expert(0)
            prev = None
            for i, (e_, t) in enumerate(seq):
                emit_gu(e_, t, i)
                if prev is not None:
                    emit_dn(*prev)
                if t == 0 and e_ + 1 < NE:
                    load_expert(e_ + 1)
                prev = (e_, t, i)
            emit_dn(*prev)
            k.dma("POOL", v3(WB[0].ap[:, 0:8192], 8), plg.rearrange("(a p) n -> p a n", p=128), writes=[WB[0]])
            k.dma("POOL", v3(WB[1].ap[:, 0:2048], 2), plp.rearrange("(a p) n -> p a n", p=128), writes=[WB[1]])
            for t in range(NTL):
                cs = slice(c0 + t * 512, c0 + (t + 1) * 512)
                hap = h3[:, :, t * 512:(t + 1) * 512]
                k.dma("POOL", v3(PB.ap, 2), pT[:, cs].rearrange("(a p) n -> p a n", p=128), writes=[PB])
                rmsnorm(HT[t], hap, 8, UT[t], UT[t].ap, PBK[0])
                for d in range(8):
                    pg, pu = PG[d % 2], PU[d % 2]
                    for kc in range(8):
                        k.op("PE", lambda e: e.matmul(pg.ap, v3(WB[0].ap[:, 0:8192], 8)[:, kc, d * 128:(d + 1) * 128], UT[t].ap[:, kc, :],
                                                      start=(kc == 0), stop=(kc == 7)), reads=[WB[0], UT[t]], writes=[pg], pe_acc=(kc > 0))
                    for kc in range(2):
                        k.op("PE", lambda e: e.matmul(pu.ap, v3(WB[1].ap[:, 0:2048], 2)[:, kc, d * 128:(d + 1) * 128], v3(PB.ap, 2)[:, kc, :],
                                                      start=(kc == 0), stop=(kc == 1)), reads=[WB[1], PB], writes=[pu], pe_acc=(kc > 0))
                    sf = STF[d % 2]
                    k.op("ACT", lambda e: e.activation(out=sf.ap, in_=pg.ap, func=AF.Sigmoid, bias=vec.ap[:, 16 + d:17 + d], scale=1.0), reads=[pg, vec], writes=[sf])
                    k.op("DVE", lambda e: e.tensor_tensor(out=sf.ap, in0=sf.ap, in1=pu.ap, op=ALU.mult), reads=[sf, pu], writes=[sf])
                    k.op("DVE", lambda e: e.tensor_tensor(out=HT[t][d].ap, in0=HT[t][d].ap, in1=sf.ap, op=ALU.add), reads=[sf, HT[t][d]], writes=[HT[t][d]])
            if mode == "A":
                k.dma("POOL", v3(WB[0].ap, 8), wqkv[:, 0:1536].rearrange("(a p) n -> p a n", p=128), writes=[WB[0]])
                k.dma("POOL", v3(WB[1].ap, 8), wqkv[:, 1536:3072].rearrange("(a p) n -> p a n", p=128), writes=[WB[1]])
                for t in range(NTL):
                    cs = slice(c0 + t * 512, c0 + (t + 1) * 512)
                    hap = h3[:, :, t * 512:(t + 1) * 512]
                    for d in range(8):
                        k.dma("SP", hT_o[d * 128:(d + 1) * 128, cs], HT[t][d].ap, reads=[HT[t][d]], writes=[ho_t], part=True)
                    rmsnorm(HT[t], hap, 24, UT[t], UT[t].ap, PBK[0])
                    U3 = UT[t].ap
                    for n in range(16):
                        col = n * 128
                        wbt = WB[0] if col < 1536 else WB[1]
                        lc = col if col < 1536 else col - 1536
                        bank = PG[n % 2]
                        for kc in range(8):
                            k.op("PE", lambda e: e.matmul(bank.ap, v3(wbt.ap, 8)[:, kc, lc:lc + 128], U3[:, kc, :], start=(kc == 0), stop=(kc == 7)),
                                 reads=[wbt, UT[t]], writes=[bank], pe_acc=(kc > 0))
                        sg = STG[n % 2]
                        sc = (1.0 / np.sqrt(128.0)) if n < 8 else 1.0
                        k.op("ACT", lambda e: e.activation(out=sg.ap, in_=bank.ap, func=AF.Identity, scale=float(sc)), reads=[bank], writes=[sg])
                        if n < 8:
                            k.dma("SP", qT_o[n * 128:(n + 1) * 128, cs], sg.ap, reads=[sg], writes=[qT_t], part=True)
                        else:
                            k.dma("SP", kT_o[(n - 8) * 128:(n - 7) * 128, cs], sg.ap, reads=[sg], writes=[kT_t], part=True)
                    for b in range(4):
                        for hh in range(2):
                            bank = PU[hh]
                            for kc in range(8):
                                k.op("PE", lambda e: e.matmul(bank.ap, U3[:, kc, b * 128:(b + 1) * 128], v3(WB[1].ap, 8)[:, kc, 512 + hh * 512:1024 + hh * 512],
                                                              start=(kc == 0), stop=(kc == 7)), reads=[WB[1], UT[t]], writes=[bank], pe_acc=(kc > 0))
                            sg = STG[hh]
                            k.op("ACT", lambda e: e.activation(out=sg.ap, in_=bank.ap, func=AF.Copy), reads=[bank], writes=[sg])
                            r0 = c0 + t * 512 + b * 128
                            k.dma("SP", v_o[r0:r0 + 128, hh * 512:(hh + 1) * 512], sg.ap, reads=[sg], writes=[v_t], part=True)
                fin = [qT_t, kT_t, v_t, ho_t]
            else:
                for t in range(NTL):
                    cs = slice(c0 + t * 512, c0 + (t + 1) * 512)
                    hap = h3[:, :, t * 512:(t + 1) * 512]
                    k.op("ACT", lambda e: e.activation(out=v3(SQ.ap, 8), in_=hap, func=AF.Square), reads=HT[t], writes=[SQ])
                    bank = PBK[0]
                    for kc in range(8):
                        k.op("PE", lambda e: e.matmul(bank.ap, ones_bf.ap, v3(SQ.ap, 8)[:, kc, :], start=(kc == 0), stop=(kc == 7)),
                             reads=[SQ, ones_bf], writes=[bank], pe_acc=(kc > 0))
                    k.op("ACT", lambda e: e.activation(out=LNV.ap, in_=bank.ap, func=AF.Ln, scale=1.0 / 1024, bias=EPS), reads=[bank], writes=[LNV])
                    k.op("ACT", lambda e: e.activation(out=RSTD.ap, in_=LNV.ap, func=AF.Exp, scale=-0.5), reads=[LNV], writes=[RSTD])
                    for d in range(8):
                        k.op("DVE", lambda e: e.scalar_tensor_tensor(out=HT[t][d].ap, in0=HT[t][d].ap, scalar=vec.ap[:, 24 + d:25 + d], in1=RSTD.ap,
                                                                     op0=ALU.mult, op1=ALU.mult), reads=[HT[t][d], RSTD, vec], writes=[HT[t][d]])
                        k.dma("SP", out_o[d * 128:(d + 1) * 128, cs], HT[t][d].ap, reads=[HT[t][d]], writes=[out_t], part=True)
                fin = [out_t]
            if hf == NH - 1:
                k.finish("SP", fin)
        print("tok ninst", k.ninst)
    return nc


def build_attn(L, NHD=2):
    nc = bass.Bass("TRN2", target_bir_lowering=False)

    def din(name, shape, dt=F32):
        return nc.dram_tensor(name, list(shape), dt, kind="ExternalInput").ap()

    qT = din("qT", [NHD, 128, L], BF16)
    kT = din("kT", [NHD, 128, L], BF16)
    vP = din("vP", [NHD, 128, L // 128, 128], BF16)
    cst = din("cst", [128, 256 + 4 * 512])
    oT = nc.dram_tensor("oT", [NHD, 128, L], BF16, kind="ExternalOutput").ap()
    NQT = L // 512
    NB = L // 128
    with ExitStack() as st:
        k = K(nc, st)
        Qs = k.sbt([128, L], BF16, "Qs")
        Ks = k.sbt([128, L], BF16, "Ks")
        Vs = k.sbt([128, L], BF16, "Vs")
        V3 = v3(Vs.ap, NB)
        cf = k.sbt([128, 256 + 2048], F32, "cf")
        trin = k.sbt([128, 128], BF16, "trin")
        onen = k.sbt([128, 128], BF16, "onen")
        m01 = k.sbt([128, 2048], BF16, "m01")
        mneg = k.sbt([128, 2048], F32, "mneg")
        E_ = [k.sbt([128, 512], F32, "E%d" % i) for i in range(2)]
        SPf = [k.sbt([128, 512], BF16, "SPf%d" % i) for i in range(2)]
        SPm = [k.sbt([128, 512], BF16, "SPm%d" % i) for i in range(2)]
        TMP = [k.sbt([128, 512], F32, "TMP%d" % i) for i in range(2)]
        AT = [k.sbt([128, 512], BF16, "AT%d" % i) for i in range(2)]
        OFF = k.sbt([128, 512], F32, "OFF")
        OST = [k.sbt([128, 512], BF16, "OST%d" % i) for i in range(2)]
        BK = [T(k.ps([128, 512], F32, "bk%d" % i)[:]) for i in range(8)]
        PA, PBb, PO = BK[0:2], BK[2:4], BK[4:6]
        oT_t = T(oT)

        k.dma("SP", cf.ap, cst, writes=[cf])
        k.op("DVE", lambda e: e.tensor_copy(out=trin.ap, in_=cf.ap[:, 0:128]), reads=[cf], writes=[trin])
        k.op("DVE", lambda e: e.tensor_copy(out=onen.ap, in_=cf.ap[:, 128:256]), reads=[cf], writes=[onen])
        k.op("DVE", lambda e: e.tensor_copy(out=m01.ap, in_=cf.ap[:, 256:2304]), reads=[cf], writes=[m01])
        k.op("DVE", lambda e: e.tensor_scalar(out=mneg.ap, in0=cf.ap[:, 256:2304], scalar1=-1.0, scalar2=30000.0, op0=ALU.add, op1=ALU.mult),
             reads=[cf], writes=[mneg])

        jobs = []
        for hd in range(NHD):
            for J in range(NQT):
                nb = 4 * J + 4
                for i, n in enumerate(range(nb - 1, -1, -1)):
                    jobs.append(dict(hd=hd, J=J, n=n, first=(i == 0), last=(n == 0), r=(n - 4 * J) if n >= 4 * J else -1))

        def load_head(hd):
            k.dma("SP", Qs.ap, qT[hd], writes=[Qs])
            k.dma("SP", Ks.ap, kT[hd], writes=[Ks])
            k.dma("SP", V3, vP[hd], writes=[Vs])

        def s1(j, i):
            if j["first"] and j["J"] == 0:
                load_head(j["hd"])
            A = PA[i % 2]
            qs = slice(j["J"] * 512, (j["J"] + 1) * 512)
            ks = slice(j["n"] * 128, (j["n"] + 1) * 128)
            k.op("PE", lambda e: e.matmul(A.ap, Ks.ap[:, ks], Qs.ap[:, qs], start=True, stop=False), reads=[Ks, Qs], writes=[A])
            k.op("ACT", lambda e: e.activation(out=E_[i % 2].ap, in_=A.ap, func=AF.Exp), reads=[A], writes=[E_[i % 2]])
            k.op("ACT", lambda e: e.activation(out=SPf[i % 2].ap, in_=E_[i % 2].ap, func=AF.Ln, bias=1.0, scale=1.0), reads=[E_[i % 2]], writes=[SPf[i % 2]])
            if j["r"] >= 0:
                r = j["r"]
                k.op("POOL", lambda e: e.tensor_tensor(out=SPm[i % 2].ap, in0=SPf[i % 2].ap, in1=m01.ap[:, r * 512:(r + 1) * 512], op=ALU.mult),
                     reads=[SPf[i % 2], m01], writes=[SPm[i % 2]])

        def s2(j, i):
            A, B = PA[i % 2], PBb[i % 2]
            sp = SPm[i % 2] if j["r"] >= 0 else SPf[i % 2]
            if j["first"]:
                k.op("DVE", lambda e: e.memset(OFF.ap, 0.0), writes=[OFF])
            k.op("PE", lambda e: e.matmul(A.ap, trin.ap, sp.ap, start=False, stop=True), reads=[trin, sp], writes=[A], pe_acc=True)
            k.op("PE", lambda e: e.matmul(B.ap, onen.ap, sp.ap, start=True, stop=True), reads=[onen, sp], writes=[B])
            tm = TMP[i % 2]
            k.op("DVE", lambda e: e.tensor_tensor(out=tm.ap, in0=A.ap, in1=OFF.ap, op=ALU.add), reads=[A, OFF], writes=[tm])
            if j["r"] >= 0:
                r = j["r"]
                k.op("DVE", lambda e: e.tensor_tensor(out=tm.ap, in0=tm.ap, in1=mneg.ap[:, r * 512:(r + 1) * 512], op=ALU.add), reads=[tm, mneg], writes=[tm])
            k.op("ACT", lambda e: e.activation(out=AT[i % 2].ap, in_=tm.ap, func=AF.Exp), reads=[tm], writes=[AT[i % 2]])
            if not j["last"]:
                k.op("DVE", lambda e: e.tensor_tensor(out=OFF.ap, in0=OFF.ap, in1=B.ap, op=ALU.add), reads=[B, OFF], writes=[OFF])

        grp = [0]

        def s3(j, i):
            O = PO[grp[0] % 2]
            k.op("PE", lambda e: e.matmul(O.ap, V3[:, j["n"], :], AT[i % 2].ap, start=j["first"], stop=j["last"]),
                 reads=[Vs, AT[i % 2]], writes=[O], pe_acc=(not j["first"]))
            if j["last"]:
                og = OST[grp[0] % 2]
                k.op("ACT", lambda e: e.activation(out=og.ap, in_=O.ap, func=AF.Copy), reads=[O], writes=[og])
                k.dma("SP", oT[j["hd"], :, j["J"] * 512:(j["J"] + 1) * 512], og.ap, reads=[og], writes=[oT_t], part=True)
                grp[0] += 1

        n = len(jobs)
        for i in range(n + 2):
            if i < n:
                if jobs[i]["first"] and jobs[i]["J"] == 0 and i > 0:
                    s2(jobs[i - 1], i - 1)
                    s3(jobs[i - 2], i - 2)
                    s3(jobs[i - 1], i - 1)
                    s1(jobs[i], i)
                    jobs[i - 1]["done"] = True
                    jobs[i - 2]["done3"] = True
                    jobs[i - 1]["done3"] = True
                    continue
                s1(jobs[i], i)
            if 0 <= i - 1 < n and not jobs[i - 1].get("done"):
                s2(jobs[i - 1], i - 1)
            if 0 <= i - 2 < n and not jobs[i - 2].get("done3"):
                s3(jobs[i - 2], i - 2)
        k.finish("SP", [oT_t])
        print("attn ninst", k.ninst, "jobs", n)
    return nc


def attn_consts():
    s = np.arange(128)
    tri_neg = -(s[:, None] >= s[None, :]).astype(np.float32)
    ones_neg = -np.ones((128, 128), np.float32)
    q = np.arange(512)
    masks = [(q[None, :] > (s[:, None] + 128 * r)).astype(np.float32) for r in range(4)]
    return np.ascontiguousarray(np.concatenate([tri_neg, ones_neg] + masks, axis=1))


def build_ssd(L):
    nc = bass.Bass("TRN2", target_bir_lowering=False)

    def din(name, shape, dt=F32):
        return nc.dram_tensor(name, list(shape), dt, kind="ExternalInput").ap()

    xT = din("xT", [1024, L])
    w_in = din("w_in", [1024, 1288])
    vecs = din("vecs", [128, 38])
    rowc = din("rowc", [1, 1040])
    cst = din("cst", [128, 640])
    ynT = nc.dram_tensor("ynT", [512, L], BF16, kind="ExternalOutput").ap()
    NTL = L // 512
    with ExitStack() as st:
        k = K(nc, st)
        W = k.sbt([128, 8 * 1288], BF16, "W")
        W3 = v3(W.ap, 8)
        XT = [k.sbt([128, 8 * 512], F32, "XT%d" % i) for i in range(2)]
        UT = [k.sbt([128, 8 * 512], BF16, "UT%d" % i) for i in range(2)]
        SQ = k.sbt([128, 8 * 512], BF16, "SQ")
        LNV = k.sbt([128, 512], F32, "LNV")
        RSTD = k.sbt([128, 512], F32, "RSTD")
        vec = k.sbt([128, 38], F32, "vec_sb")
        bc = k.sbt([128, 1040], F32, "bc")
        cf = k.sbt([128, 640], F32, "cf")
        identb = k.sbt([128, 128], BF16, "identb")
        ones_bf = k.sbt([128, 128], BF16, "ones_bf")
        A_bc = k.sbt([128, 8], F32, "A_bc")
        XR = [k.sbt([128, 515], F32, "XR%d" % i) for i in range(6)]
        ACC = [k.sbt([128, 512], F32, "ACC%d" % i) for i in range(2)]
        XC = [k.sbt([128, 6 * 512], BF16, "XC%d" % i) for i in range(2)]
        ZS_2 = [k.sbt([128, 512], F32, "ZS_%d" % i) for i in range(2)]
        DTt_2 = [k.sbt([128, 8], F32, "DTt_%d" % i) for i in range(2)]
        DT_2 = [k.sbt([128, 8], F32, "DT_%d" % i) for i in range(2)]
        AA_2 = [k.sbt([128, 8], F32, "AA_%d" % i) for i in range(2)]
        EXPS_2 = [k.sbt([128, 24], F32, "EXPS_%d" % i) for i in range(2)]
        LH = [k.sbt([128, 128], F32, "LH%d" % i) for i in range(2)]
        DEC_2 = [k.sbt([128, 1024], F32, "DEC_%d" % i) for i in range(2)]
        CBm_2 = [k.sbt([128, 128], F32, "CBm_%d" % i) for i in range(2)]
        WT_2 = [k.sbt([128, 1024], BF16, "WT_%d" % i) for i in range(2)]
        XTOK_2 = [k.sbt([128, 512], BF16, "XTOK_%d" % i) for i in range(2)]
        BTOK_2 = [k.sbt([128, 128], BF16, "BTOK_%d" % i) for i in range(2)]
        XDT_2 = [k.sbt([128, 512], BF16, "XDT_%d" % i) for i in range(2)]
        XW_2 = [k.sbt([128, 512], BF16, "XW_%d" % i) for i in range(2)]
        Y1_2 = [k.sbt([128, 512], F32, "Y1_%d" % i) for i in range(2)]
        Y2_2 = [k.sbt([128, 512], F32, "Y2_%d" % i) for i in range(2)]
        YZ_2 = [k.sbt([128, 512], F32, "YZ_%d" % i) for i in range(2)]
        YSQ_2 = [k.sbt([128, 512], F32, "YSQ_%d" % i) for i in range(2)]
        YN_2 = [k.sbt([128, 512], BF16, "YN_%d" % i) for i in range(2)]
        SF = k.sbt([128, 512], F32, "SF")
        SBF = k.sbt([128, 512], BF16, "SBF")
        sc_2 = [[k.sbt([128, 1], F32, "sc%d_%d" % (j, i)) for i in range(3)] for j in range(2)]
        LH4 = [k.sbt([128, 128], F32, "LHx%d" % i) for i in range(2)]
        YNT = [k.sbt([128, 4 * 512], BF16, "YNT%d" % i) for i in range(2)]
        BK = [T(k.ps([128, 512], F32, "bk%d" % i)[:]) for i in range(8)]
        P_proj, P_st, P_sm, P_sg0, P_sg1, P_tr, P_yd, P_yo = BK
        ynT_t = T(ynT)

        k.dma("SP", vec.ap, vecs, writes=[vec])
        k.dma("SP", cf.ap, cst, writes=[cf])
        k.dma("SP", bc.ap, rowc.partition_broadcast(128).rearrange("p o n -> p (o n)"), writes=[bc])
        k.dma("POOL", W3, w_in.rearrange("(a p) n -> p a n", p=128), writes=[W])
        ident = cf.ap[:, 0:128]
        tri = cf.ap[:, 128:256]
        Um = cf.ap[:, 256:384]
        maskU = cf.ap[:, 384:512]
        onesf = cf.ap[:, 512:640]
        k.op("DVE", lambda e: e.tensor_copy(out=identb.ap, in_=ident), reads=[cf], writes=[identb])
        k.op("DVE", lambda e: e.memset(ones_bf.ap, 1.0), writes=[ones_bf])
        k.op("ACT", lambda e: e.activation(out=A_bc.ap, in_=bc.ap[:, 8:16], func=AF.Exp), reads=[bc], writes=[A_bc])
        k.op("DVE", lambda e: e.tensor_scalar(out=A_bc.ap, in0=A_bc.ap, scalar1=-1.0, scalar2=None, op0=ALU.mult), reads=[A_bc], writes=[A_bc])
        for cc in range(6):
            k.op("DVE", lambda e: e.memset(XR[cc].ap[:, 0:3], 0.0), writes=[XR[cc]])
        k.op("DVE", lambda e: e.memset(SF.ap, 0.0), writes=[SF])
        k.op("DVE", lambda e: e.memset(SBF.ap, 0.0), writes=[SBF])
        dtb, Dx, gn = bc.ap[:, 0:8], bc.ap[:, 16:528], bc.ap[:, 528:1040]

        def load_x(t):
            k.dma("SP", v3(XT[t % 2].ap, 8), xT[:, t * 512:(t + 1) * 512].rearrange("(a p) n -> p a n", p=128), writes=[XT[t % 2]])

        load_x(0)
        for t in range(NTL):
            if t + 1 < NTL:
                load_x(t + 1)
            xt, ut, xc = XT[t % 2], UT[t % 2], XC[t % 2]
            x3, u3, xc3 = v3(xt.ap, 8), v3(ut.ap, 8), v3(xc.ap, 6)
            k.op("ACT", lambda e: e.activation(out=v3(SQ.ap, 8), in_=x3, func=AF.Square), reads=[xt], writes=[SQ])
            for kc in range(8):
                k.op("PE", lambda e: e.matmul(P_proj.ap, ones_bf.ap, v3(SQ.ap, 8)[:, kc, :], start=(kc == 0), stop=(kc == 7)),
                     reads=[SQ, ones_bf], writes=[P_proj], pe_acc=(kc > 0))
            k.op("ACT", lambda e: e.activation(out=LNV.ap, in_=P_proj.ap, func=AF.Ln, scale=1.0 / 1024, bias=EPS), reads=[P_proj], writes=[LNV])
            k.op("ACT", lambda e: e.activation(out=RSTD.ap, in_=LNV.ap, func=AF.Exp, scale=-0.5), reads=[LNV], writes=[RSTD])
            for kc in range(8):
                k.op("DVE", lambda e: e.scalar_tensor_tensor(out=u3[:, kc, :], in0=x3[:, kc, :], scalar=vec.ap[:, kc:kc + 1], in1=RSTD.ap,
                                                             op0=ALU.mult, op1=ALU.mult), reads=[xt, RSTD, vec], writes=[ut])
            for cc in range(6):
                for kc in range(8):
                    k.op("PE", lambda e: e.matmul(P_proj.ap, W3[:, kc, 512 + cc * 128:512 + (cc + 1) * 128], u3[:, kc, :], start=(kc == 0), stop=(kc == 7)),
                         reads=[W, ut], writes=[P_proj], pe_acc=(kc > 0))
                xr, acc = XR[cc], ACC[cc % 2]
                k.op("ACT", lambda e: e.activation(out=xr.ap[:, 3:515], in_=P_proj.ap, func=AF.Copy), reads=[P_proj], writes=[xr])
                k.op("DVE", lambda e: e.tensor_scalar(out=acc.ap, in0=xr.ap[:, 0:512], scalar1=vec.ap[:, 8 + cc * 4:9 + cc * 4], scalar2=None, op0=ALU.mult),
                     reads=[xr, vec], writes=[acc])
                for kk in range(1, 4):
                    k.op("DVE", lambda e: e.scalar_tensor_tensor(out=acc.ap, in0=xr.ap[:, kk:kk + 512], scalar=vec.ap[:, 8 + cc * 4 + kk:9 + cc * 4 + kk],
                                                                 in1=acc.ap, op0=ALU.mult, op1=ALU.add), reads=[xr, vec, acc], writes=[acc])
                k.op("ACT", lambda e: e.activation(out=xc3[:, cc, :], in_=acc.ap, func=AF.Silu, bias=vec.ap[:, 32 + cc:33 + cc], scale=1.0),
                     reads=[acc, vec], writes=[xc])
                k.op("DVE", lambda e: e.tensor_copy(out=xr.ap[:, 0:3], in_=xr.ap[:, 512:515]), reads=[xr], writes=[xr])
            ynt = YNT[t % 2]
            for q in range(4):
                cs = slice(q * 128, (q + 1) * 128)
                ci = t * 4 + q
                ZS = ZS_2[ci % 2]
                DTt = DTt_2[ci % 2]
                DT = DT_2[ci % 2]
                AA = AA_2[ci % 2]
                EXPS = EXPS_2[ci % 2]
                DEC = DEC_2[ci % 2]
                CBm = CBm_2[ci % 2]
                WT = WT_2[ci % 2]
                XTOK = XTOK_2[ci % 2]
                BTOK = BTOK_2[ci % 2]
                XDT = XDT_2[ci % 2]
                XW = XW_2[ci % 2]
                Y1 = Y1_2[ci % 2]
                Y2 = Y2_2[ci % 2]
                YZ = YZ_2[ci % 2]
                YSQ = YSQ_2[ci % 2]
                YN = YN_2[ci % 2]
                sc = sc_2[ci % 2]
                for kc in range(8):
                    k.op("PE", lambda e: e.matmul(P_proj.ap, u3[:, kc, cs], W3[:, kc, 0:512], start=(kc == 0), stop=(kc == 7)),
                         reads=[W, ut], writes=[P_proj], pe_acc=(kc > 0))
                k.op("ACT", lambda e: e.activation(out=ZS.ap, in_=P_proj.ap, func=AF.Silu), reads=[P_proj], writes=[ZS])
                for kc in range(8):
                    k.op("PE", lambda e: e.matmul(P_sm.ap[:, 0:8], u3[:, kc, cs], W3[:, kc, 1280:1288], start=(kc == 0), stop=(kc == 7)),
                         reads=[W, ut], writes=[P_sm], pe_acc=(kc > 0))
                k.op("DVE", lambda e: e.tensor_tensor(out=DTt.ap, in0=P_sm.ap[:, 0:8], in1=dtb, op=ALU.add), reads=[P_sm, bc], writes=[DTt])
                k.op("ACT", lambda e: e.activation(out=DTt.ap, in_=DTt.ap, func=AF.Exp), reads=[DTt], writes=[DTt])
                k.op("ACT", lambda e: e.activation(out=DT.ap, in_=DTt.ap, func=AF.Ln, bias=1.0, scale=1.0), reads=[DTt], writes=[DT])
                k.op("DVE", lambda e: e.tensor_tensor(out=AA.ap, in0=DT.ap, in1=A_bc.ap, op=ALU.mult), reads=[DT, A_bc], writes=[AA])
                trb = P_tr.ap.bitcast(BF16)
                for cc in range(4):
                    k.op("PE", lambda e: e.transpose(trb[:, cc * 128:(cc + 1) * 128], xc3[:, cc, cs], identb.ap), reads=[xc, identb], writes=[P_tr])
                k.op("PE", lambda e: e.transpose(trb[:, 512:640], xc3[:, 4, cs], identb.ap), reads=[xc, identb], writes=[P_tr])
                k.op("ACT", lambda e: e.activation(out=XTOK.ap, in_=trb[:, 0:512], func=AF.Copy), reads=[P_tr], writes=[XTOK])
                k.op("ACT", lambda e: e.activation(out=BTOK.ap, in_=trb[:, 512:640], func=AF.Copy), reads=[P_tr], writes=[BTOK])
                k.op("PE", lambda e: e.matmul(P_sm.ap[:, 32:40], tri, AA.ap, start=True, stop=True), reads=[cf, AA], writes=[P_sm])
                k.op("PE", lambda e: e.matmul(P_sm.ap[:, 40:48], Um, AA.ap, start=True, stop=True), reads=[cf, AA], writes=[P_sm])
                k.op("PE", lambda e: e.matmul(P_sm.ap[:, 48:56], onesf, AA.ap, start=True, stop=True), reads=[cf, AA], writes=[P_sm])
                k.op("ACT", lambda e: e.activation(out=EXPS.ap, in_=P_sm.ap[:, 32:56], func=AF.Exp), reads=[P_sm], writes=[EXPS])
                e_l, dte, cd = EXPS.ap[:, 0:8], EXPS.ap[:, 8:16], EXPS.ap[:, 16:24]
                k.op("PE", lambda e: e.matmul(P_sm.ap[:, 128:256], xc3[:, 4, cs], xc3[:, 5, cs], start=True, stop=True), reads=[xc], writes=[P_sm])
                k.op("DVE", lambda e: e.tensor_tensor(out=CBm.ap, in0=P_sm.ap[:, 128:256], in1=maskU, op=ALU.mult), reads=[P_sm, cf], writes=[CBm])
                for hh in range(8):
                    lh = (LH + LH4)[hh % 4]
                    k.op("DVE", lambda e: e.tensor_scalar(out=lh.ap, in0=Um, scalar1=AA.ap[:, hh:hh + 1], scalar2=None, op0=ALU.mult), reads=[cf, AA], writes=[lh])
                    bank = P_sg0 if hh < 4 else P_sg1
                    k.op("PE", lambda e: e.matmul(bank.ap[:, (hh % 4) * 128:(hh % 4 + 1) * 128], lh.ap, tri, start=True, stop=True), reads=[lh, cf], writes=[bank])
                k.op("ACT", lambda e: e.activation(out=DEC.ap[:, 0:512], in_=P_sg0.ap, func=AF.Exp), reads=[P_sg0], writes=[DEC])
                k.op("ACT", lambda e: e.activation(out=DEC.ap[:, 512:1024], in_=P_sg1.ap, func=AF.Exp), reads=[P_sg1], writes=[DEC])
                k.op("DVE", lambda e: e.tensor_tensor(out=v3(WT.ap, 8), in0=v3(DEC.ap, 8), in1=CBm.ap.unsqueeze(1).to_broadcast([128, 8, 128]), op=ALU.mult),
                     reads=[DEC, CBm], writes=[WT])
                k.op("DVE", lambda e: e.tensor_tensor(out=v3(XDT.ap, 8), in0=v3(XTOK.ap, 8), in1=DT.ap.unsqueeze(2).to_broadcast([128, 8, 64]), op=ALU.mult),
                     reads=[XTOK, DT], writes=[XDT])
                k.op("DVE", lambda e: e.tensor_tensor(out=v3(XW.ap, 8), in0=v3(XDT.ap, 8), in1=dte.unsqueeze(2).to_broadcast([128, 8, 64]), op=ALU.mult),
                     reads=[XDT, EXPS], writes=[XW])
                for hh in range(8):
                    k.op("PE", lambda e: e.matmul(P_yd.ap[:, hh * 64:(hh + 1) * 64], v3(WT.ap, 8)[:, hh, :], v3(XDT.ap, 8)[:, hh, :], start=True, stop=True),
                         reads=[WT, XDT], writes=[P_yd])
                k.op("PE", lambda e: e.matmul(P_yo.ap, xc3[:, 5, cs], SBF.ap, start=True, stop=True), reads=[xc, SBF], writes=[P_yo])
                k.op("DVE", lambda e: e.tensor_tensor(out=v3(Y1.ap, 8), in0=v3(P_yo.ap, 8), in1=e_l.unsqueeze(2).to_broadcast([128, 8, 64]), op=ALU.mult),
                     reads=[P_yo, EXPS], writes=[Y1])
                k.op("DVE", lambda e: e.tensor_tensor(out=Y1.ap, in0=Y1.ap, in1=P_yd.ap, op=ALU.add), reads=[Y1, P_yd], writes=[Y1])
                k.op("PE", lambda e: e.matmul(P_st.ap, BTOK.ap, XW.ap, start=True, stop=True), reads=[BTOK, XW], writes=[P_st])
                k.op("DVE", lambda e: e.tensor_tensor(out=v3(SF.ap, 8), in0=v3(SF.ap, 8), in1=cd.unsqueeze(2).to_broadcast([128, 8, 64]), op=ALU.mult),
                     reads=[SF, EXPS], writes=[SF])
                k.op("DVE", lambda e: e.tensor_tensor(out=SF.ap, in0=SF.ap, in1=P_st.ap, op=ALU.add), reads=[SF, P_st], writes=[SF])
                k.op("ACT", lambda e: e.activation(out=SBF.ap, in_=SF.ap, func=AF.Copy), reads=[SF], writes=[SBF])
                k.op("DVE", lambda e: e.tensor_tensor(out=Y2.ap, in0=XTOK.ap, in1=Dx, op=ALU.mult), reads=[XTOK, bc], writes=[Y2])
                k.op("DVE", lambda e: e.tensor_tensor(out=Y2.ap, in0=Y2.ap, in1=Y1.ap, op=ALU.add), reads=[Y2, Y1], writes=[Y2])
                k.op("DVE", lambda e: e.tensor_tensor(out=YZ.ap, in0=Y2.ap, in1=ZS.ap, op=ALU.mult), reads=[Y2, ZS], writes=[YZ])
                k.op("ACT", lambda e: e.activation(out=YSQ.ap, in_=YZ.ap, func=AF.Square, accum_out=sc[0].ap), reads=[YZ], writes=[YSQ, sc[0]])
                k.op("ACT", lambda e: e.activation(out=sc[1].ap, in_=sc[0].ap, func=AF.Ln, scale=1.0 / 512, bias=EPS), reads=[sc[0]], writes=[sc[1]])
                k.op("ACT", lambda e: e.activation(out=sc[2].ap, in_=sc[1].ap, func=AF.Exp, scale=-0.5), reads=[sc[1]], writes=[sc[2]])
                k.op("DVE", lambda e: e.scalar_tensor_tensor(out=YN.ap, in0=YZ.ap, scalar=sc[2].ap, in1=gn, op0=ALU.mult, op1=ALU.mult),
                     reads=[YZ, sc[2], bc], writes=[YN])
                for cc in range(4):
                    k.op("PE", lambda e: e.transpose(trb[:, cc * 128:(cc + 1) * 128], YN.ap[:, cc * 128:(cc + 1) * 128], identb.ap), reads=[YN, identb], writes=[P_tr])
                k.op("ACT", lambda e: e.activation(out=v3(ynt.ap, 4)[:, :, cs], in_=trb[:, 0:512].rearrange("p (a n) -> p a n", a=4), func=AF.Copy),
                     reads=[P_tr], writes=[ynt])
            k.dma("SP", ynT[:, t * 512:(t + 1) * 512].rearrange("(a p) n -> p a n", p=128), v3(ynt.ap, 4), reads=[ynt], writes=[ynT_t], part=True)
        k.finish("SP", [ynT_t])
        print("ssd ninst", k.ninst)
    return nc


def ssd_consts():
    s = np.arange(128)
    ident = np.eye(128, dtype=np.float32)
    tri = (s[:, None] <= s[None, :]).astype(np.float32)
    U = (s[:, None] > s[None, :]).astype(np.float32)
    maskU = (s[None, :] >= s[:, None]).astype(np.float32)
    ones = np.ones((128, 128), np.float32)
    return np.ascontiguousarray(np.concatenate([ident, tri, U, maskU, ones], axis=1))


def ssd_host_inputs(g, ssd_norm, w_in, conv_w, conv_b, dt_bias, a_log, d_skip, gnorm):
    cols = np.concatenate([np.arange(g * 512, (g + 1) * 512), 2048 + np.arange(g * 512, (g + 1) * 512),
                           4096 + np.arange(g * 128, (g + 1) * 128), 4096 + 512 + np.arange(g * 128, (g + 1) * 128),
                           5120 + np.arange(g * 8, (g + 1) * 8)])
    w = np.ascontiguousarray(w_in[:, cols])
    ch = np.concatenate([np.arange(g * 512, (g + 1) * 512), 2048 + np.arange(g * 128, (g + 1) * 128), 2048 + 512 + np.arange(g * 128, (g + 1) * 128)])
    cw = conv_w[:, ch]
    cb = conv_b[ch]
    vecs = np.zeros((128, 38), np.float32)
    vecs[:, 0:8] = ssd_norm.reshape(8, 128).T
    for cc in range(6):
        vecs[:, 8 + cc * 4:12 + cc * 4] = cw[:, cc * 128:(cc + 1) * 128].T
        vecs[:, 32 + cc] = cb[cc * 128:(cc + 1) * 128]
    rowc = np.concatenate([dt_bias[g * 8:(g + 1) * 8], a_log[g * 8:(g + 1) * 8], np.repeat(d_skip[g * 8:(g + 1) * 8], 64),
                           gnorm[g * 512:(g + 1) * 512]])[None, :].astype(np.float32)
    return w, vecs, np.ascontiguousarray(rowc)


_CACHE = {}


def _prog(name, fn, *a):
    key = (name,) + a
    if key not in _CACHE:
        _CACHE[key] = fn(*a)
    return _CACHE[key]


def _vcol(v):
    return np.asarray(v, np.float32).reshape(8, 128).T


def kernel(x, p, ssd_norm, ssd_w_in, ssd_conv_w, ssd_conv_b, ssd_dt_bias, ssd_a_log, ssd_d, ssd_gnorm, ssd_w_out,
           sb_norm, sb_w_qkv, sb_w_o, moe_norm, moe_w_rg, moe_b_rg, moe_w_re, moe_b_re, moe_w_gate, moe_w_up, moe_w_down,
           ple_norm, ple_w_gate, ple_b_gate, ple_w_proj, final_norm):
    f32 = lambda a: np.ascontiguousarray(np.asarray(a, dtype=np.float32))
    x, p = f32(x), f32(p)
    Bsz, L, D = x.shape
    NC = 8
    NT = (Bsz * L) // NC
    PB = NC // Bsz
    cores = list(range(NC))
    ident = np.eye(128, dtype=np.float32)

    nc1 = _prog("ssd", build_ssd, L)
    cst1 = ssd_consts()
    xTb = [np.ascontiguousarray(x[b].T) for b in range(Bsz)]
    maps = []
    for c in cores:
        b, g = c // PB, c % PB
        w, vecs, rowc = ssd_host_inputs(g, f32(ssd_norm[0]), f32(ssd_w_in[0]), f32(ssd_conv_w[0]), f32(ssd_conv_b[0]),
                                        f32(ssd_dt_bias[0]), f32(ssd_a_log[0]), f32(ssd_d[0]), f32(ssd_gnorm[0]))
        maps.append(dict(xT=xTb[b], w_in=w, vecs=vecs, rowc=rowc, cst=cst1))
    r1 = run_bass_kernel_spmd(nc1, maps, core_ids=cores).results
    ynT = [np.concatenate([r1[b * PB + g]["ynT"] for g in range(PB)], axis=0) for b in range(Bsz)]

    mcst = moe_consts(NT)

    def tok_maps(i, hT_list, aT_list, w_a, tail_norm, extra):
        vecs = np.ascontiguousarray(np.concatenate([_vcol(moe_norm[i]), _vcol(ple_norm[i]), _vcol(ple_b_gate[i]), _vcol(tail_norm)], axis=1))
        wr = np.ascontiguousarray(np.concatenate([f32(moe_w_rg[i]), f32(moe_w_re[i])], axis=1))
        br = np.ascontiguousarray(np.concatenate([f32(moe_b_rg[i]), f32(moe_b_re[i])])[None, :])
        wg_, wu_, wd_ = f32(moe_w_gate[i]), f32(moe_w_up[i]), f32(moe_w_down[i])
        plg_, plp_ = f32(ple_w_gate[i]), f32(ple_w_proj[i])
        out = []
        for c in cores:
            b, j = c // PB, c % PB
            sl = slice(j * NT, (j + 1) * NT)
            m = dict(hT=hT_list[c], aT=np.ascontiguousarray(aT_list[b][:, sl]), w_a=w_a, vecs=vecs, wr=wr, br=br, wg=wg_, wu=wu_, wd=wd_,
                     plg=plg_, plp=plp_, pT=np.ascontiguousarray(p[i, b, sl].T), mcst=mcst)
            m.update(extra)
            out.append(m)
        return out

    nc2 = _prog("tokA", build_tok2, NT, 2048, "A")
    hT0 = [np.ascontiguousarray(x[c // PB, (c % PB) * NT:(c % PB + 1) * NT].T) for c in cores]
    r2 = run_bass_kernel_spmd(nc2, tok_maps(0, hT0, ynT, f32(ssd_w_out[0]), f32(sb_norm[0]), dict(wqkv=f32(sb_w_qkv[0]))), core_ids=cores).results
    hT1 = [r2[c]["hT_o"] for c in cores]
    qTb = [np.concatenate([r2[b * PB + j]["qT_o"] for j in range(PB)], axis=1) for b in range(Bsz)]
    kTb = [np.concatenate([r2[b * PB + j]["kT_o"] for j in range(PB)], axis=1) for b in range(Bsz)]
    vb = [np.concatenate([r2[b * PB + j]["v_o"] for j in range(PB)], axis=0) for b in range(Bsz)]

    nc3 = _prog("attn", build_attn, L)
    cst3 = attn_consts()
    maps = []
    for c in cores:
        b, hp = c // PB, c % PB
        rows = slice(hp * 256, (hp + 1) * 256)
        vP = np.ascontiguousarray(vb[b][:, rows].reshape(L // 128, 128, 2, 128).transpose(2, 1, 0, 3))
        maps.append(dict(qT=np.ascontiguousarray(qTb[b][rows].reshape(2, 128, L)), kT=np.ascontiguousarray(kTb[b][rows].reshape(2, 128, L)),
                         vP=vP, cst=cst3))
    r3 = run_bass_kernel_spmd(nc3, maps, core_ids=cores).results
    oT = [np.concatenate([r3[b * PB + hp]["oT"].reshape(256, L) for hp in range(PB)], axis=0) for b in range(Bsz)]

    nc4 = _prog("tokB", build_tok2, NT, 1024, "B")
    r4 = run_bass_kernel_spmd(nc4, tok_maps(1, hT1, oT, f32(sb_w_o[0]), f32(final_norm), {}), core_ids=cores).results
    out = np.empty((Bsz, L, D), np.float32)
    for c in cores:
        b, j = c // PB, c % PB
        out[b, j * NT:(j + 1) * NT, :] = r4[c]["out_o"].T
    return out


I32 = mybir.dt.int32
MB = 128
NBLK_OF = lambda NT: (2 * NT) // MB + 64


def moe_consts(NT):
    nblk = NBLK_OF(NT)
    s = np.arange(128)
    SL = (s[:, None] < s[None, :]).astype(np.float32)
    THR = np.tile((np.arange(64) * MB).astype(np.float32)[None, :], (128, 1))
    JB = np.tile((np.arange(nblk) * MB).astype(np.float32)[None, :], (128, 1))
    KP = (np.arange(8)[None, :] * 128 + s[:, None]).astype(np.float32)
    return np.ascontiguousarray(np.concatenate([np.eye(128, dtype=np.float32), SL, THR, JB, KP], axis=1))


def idma(self, out_ap, in_ap, idx_t, idx_ap, gather, reads=(), writes=(), part=False, bound=None):
    t = writes[0]
    if t.dsem is None:
        key = "d%d" % self.ndsem
        t.dsem = self.stack.enter_context(self.nc.semaphore(key))
        self.semobj[key] = t.dsem
        t.name = key
        self.ndsem += 1
    key = t.name
    for r in list(reads) + [idx_t]:
        if r.w is not None:
            self._need("POOL", *r.w)
    if t.w is not None and not (part and t.w[0] == key):
        self._need("POOL", *t.w)
    for kk, v in t.r.items():
        self._need("POOL", kk, v)
    off = bass.IndirectOffsetOnAxis(ap=idx_ap, axis=0)
    if gather and bound is not None:
        ins = self.nc.gpsimd.indirect_dma_start(out=out_ap, out_offset=None, in_=in_ap, in_offset=off, bounds_check=bound, oob_is_err=False)
    elif gather:
        ins = self.nc.gpsimd.indirect_dma_start(out=out_ap, out_offset=None, in_=in_ap, in_offset=off)
    else:
        ins = self.nc.gpsimd.indirect_dma_start(out=out_ap, out_offset=off, in_=in_ap, in_offset=None)
    t.dcnt += 16
    ins.then_inc(t.dsem, 16)
    for r in list(reads) + [idx_t]:
        r.r[key] = max(r.r.get(key, 0), t.dcnt)
    t.w = (key, t.dcnt)
    if not part:
        t.r = {}
    self.ninst += 1
    return ins


K.idma = idma


def build_tok2(NT, KA, mode):
    nc = bass.Bass("TRN2", target_bir_lowering=False)

    def din(name, shape, dt=F32):
        return nc.dram_tensor(name, list(shape), dt, kind="ExternalInput").ap()

    def dout(name, shape, dt=F32):
        return nc.dram_tensor(name, list(shape), dt, kind="ExternalOutput").ap()

    NBLK = NBLK_OF(NT)
    NROWS = NBLK * MB
    NTB = NT // 128
    NTILE = NT // 512
    hT = din("hT", [1024, NT])
    aT = din("aT", [KA, NT], BF16)
    w_a = din("w_a", [KA, 1024])
    vecs = din("vecs", [128, 32])
    wr = din("wr", [1024, 72])
    br = din("br", [1, 72])
    wg = din("wg", [64, 1024, 512])
    wu = din("wu", [64, 1024, 512])
    wd = din("wd", [64, 512, 1024])
    plg = din("plg", [1024, 1024])
    plp = din("plp", [256, 1024])
    pT = din("pT", [256, NT])
    NCST = 256 + 64 + NBLK + 8
    mcst = din("mcst", [128, NCST])
    if mode == "A":
        wqkv = din("wqkv", [1024, 3072])
        hT_o = dout("hT_o", [1024, NT])
        qT_o = dout("qT_o", [1024, NT], BF16)
        kT_o = dout("kT_o", [1024, NT], BF16)
        v_o = dout("v_o", [NT, 1024], BF16)
    else:
        out_o = dout("out_o", [1024, NT])
    H1 = nc.dram_tensor("H1s", [1024, NT], F32, kind="Internal").ap()
    Xs = nc.dram_tensor("Xs", [NROWS, 1024], BF16, kind="Internal").ap()
    Ys = nc.dram_tensor("Ys", [NROWS, 1024], F32, kind="Internal").ap()
    wg_r = wg.rearrange("e k n -> (e k) n")
    wu_r = wu.rearrange("e k n -> (e k) n")
    wd_r = wd.rearrange("e k n -> (e k) n")
    KAC = KA // 128

    with ExitStack() as st:
        k = K(nc, st)
        HTL = [k.sbt([128, 8 * 512], F32, "htl%d" % i) for i in range(1)] * 2
        UTL = [k.sbt([128, 8 * 512], BF16, "utl%d" % i) for i in range(1)] * 2
        WB = [k.sbt([128, 12288], BF16, "wb%d" % i) for i in range(2)]
        RA = k.sb([128, 8192], BF16, "regA")
        RAf = RA[:].bitcast(F32)
        ABS = [T(RA[:, 0:KAC * 512])]
        BIG = T(RAf[:, 0:2048])
        IGF = T(RAf[:, 2048:3072])
        XB = [T(RA[:, i * 1024:(i + 1) * 1024]) for i in range(2)]
        XTB = [T(RA[:, 2048 + i * 1024:2048 + (i + 1) * 1024]) for i in range(2)]
        YB = [T(RAf[:, 2048 + i * 1024:2048 + (i + 1) * 1024]) for i in range(2)]
        NRX = max(NTB * 1024, 32768)
        RX = k.sb([128, NRX], BF16, "regX")
        RXf = RX[:].bitcast(F32)
        XROWS = T(RX[:, 0:NTB * 1024])
        XR3 = v3(XROWS.ap, NTB)
        WQ = [T(RX[:, i * 12288:(i + 1) * 12288]) for i in range(2)]
        Y0 = [T(RXf[:, 12288 + i * 1024:12288 + (i + 1) * 1024]) for i in range(2)]
        Y1 = [T(RXf[:, 14336 + i * 1024:14336 + (i + 1) * 1024]) for i in range(2)]
        PBt = k.sbt([128, 2 * 512], BF16, "pbt")
        LNV = k.sbt([128, 512], F32, "lnv")
        RSTD = k.sbt([128, 512], F32, "rstd")
        vec = k.sbt([128, 32], F32, "vec_sb")
        cf = k.sbt([128, NCST], F32, "cf")
        ident = cf.ap[:, 0:128]
        identb = k.sbt([128, 128], BF16, "identb")
        SLb = k.sbt([128, 128], BF16, "SLb")
        ones_bf = k.sbt([128, 128], BF16, "ones_bf")
        wr_sb = k.sbt([128, 8 * 72], F32, "wr_sb")
        br_bc = k.sbt([128, 72], F32, "brbc")
        STG = [k.sbt([128, 512], BF16, "stg%d" % i) for i in range(2)]
        STF = [k.sbt([128, 512], F32, "stf%d" % i) for i in range(2)]
        r_lg = k.sbt([128, 72], F32, "r_lg")
        r_a = k.sbt([128, 8], F32, "r_a")
        r_goh = k.sbt([128, 8], F32, "r_goh")
        r_t64 = k.sbt([128, 64], F32, "r_t64")
        r_es = k.sbt([128, 8], F32, "r_es")
        r_es2 = k.sbt([128, 8], F32, "r_es2")
        r_oh1 = k.sbt([128, 8], F32, "r_oh1")
        r_oh2 = k.sbt([128, 8], F32, "r_oh2")
        r_s = [k.sbt([128, 1], F32, "r_s%d" % i) for i in range(12)]
        OHC = k.sbt([128, 128], BF16, "ohc")
        OHS = k.sbt([128, NTB * 128], BF16, "ohs")
        OHS3 = v3(OHS.ap, NTB)
        RUN = k.sbt([128, 128], F32, "run")
        PRS = k.sbt([128, 128], F32, "prs")
        JNK = k.sbt([128, 64], F32, "jnk")
        RK0 = k.sbt([128, NTB], F32, "rk0")
        RK1 = k.sbt([128, NTB], F32, "rk1")
        GA = k.sbt([128, NTB], F32, "ga")
        GB = k.sbt([128, NTB], F32, "gb")
        DI0 = k.sbt([128, NTB], I32, "di0")
        DI1 = k.sbt([128, NTB], I32, "di1")
        CNT = k.sbt([128, 64], F32, "cnt")
        NB_ = k.sbt([128, 64], F32, "nb_")
        PE_ = [k.sbt([128, 64], F32, "pe%d" % i) for i in range(2)]
        BASE1 = k.sbt([128, 64], F32, "base1")
        BASE2 = k.sbt([128, 64], F32, "base2")
        DF = k.sbt([128, NTB], F32, "df")
        EJ = k.sbt([128, NBLK], F32, "ej")
        SAME = k.sbt([128, NBLK], F32, "same")
        IGI = k.sbt([128, NBLK * 8], I32, "igi")
        IDI = k.sbt([128, NBLK * 4], I32, "idi")
        SS = [k.sbt([128, 512], BF16, "ss%d" % i) for i in range(2)]
        HH = [k.sbt([128, 512], BF16, "hh%d" % i) for i in range(2)]
        BK = [T(k.ps([128, 512], F32, "bk%d" % i)[:]) for i in range(8)]
        PG, PU, PD, PBK = BK[0:2], BK[2:4], BK[4:6], BK[6:8]
        H1_t, Xs_t, Ys_t = T(H1), T(Xs), T(Ys)
        if mode == "A":
            qT_t, kT_t, v_t, ho_t = T(qT_o), T(kT_o), T(v_o), T(hT_o)
        else:
            out_t = T(out_o)

        k.dma("SP", vec.ap, vecs, writes=[vec])
        k.dma("SP", cf.ap, mcst, writes=[cf])
        k.dma("SP", v3(wr_sb.ap, 8), wr.rearrange("(a p) n -> p a n", p=128), writes=[wr_sb])
        k.dma("SP", br_bc.ap, br.partition_broadcast(128).rearrange("p o n -> p (o n)"), writes=[br_bc])
        k.op("DVE", lambda e: e.memset(ones_bf.ap, 1.0), writes=[ones_bf])
        k.op("DVE", lambda e: e.memset(RUN.ap, 0.0), writes=[RUN])
        k.op("DVE", lambda e: e.tensor_copy(out=identb.ap, in_=ident), reads=[cf], writes=[identb])
        k.op("DVE", lambda e: e.tensor_copy(out=SLb.ap, in_=cf.ap[:, 128:256]), reads=[cf], writes=[SLb])
        THR = cf.ap[:, 256:320]
        JB = cf.ap[:, 320:320 + NBLK]
        KP = cf.ap[:, 320 + NBLK:328 + NBLK]
        for kc in range(8):
            k.op("DVE", lambda e: e.tensor_scalar(out=wr_sb.ap[:, kc * 72:(kc + 1) * 72], in0=wr_sb.ap[:, kc * 72:(kc + 1) * 72],
                                                  scalar1=vec.ap[:, kc:kc + 1], scalar2=None, op0=ALU.mult), reads=[vec, wr_sb], writes=[wr_sb])

        def rmsnorm(src_t, src_ap, gcol, dst_t, dst_ap, bank):
            k.op("ACT", lambda e: e.activation(out=dst_ap, in_=src_ap, func=AF.Square), reads=[src_t], writes=[dst_t])
            for kc in range(8):
                k.op("PE", lambda e: e.matmul(bank.ap, ones_bf.ap, dst_ap[:, kc, :], start=(kc == 0), stop=(kc == 7)),
                     reads=[dst_t, ones_bf], writes=[bank], pe_acc=(kc > 0))
            k.op("ACT", lambda e: e.activation(out=LNV.ap, in_=bank.ap, func=AF.Ln, scale=1.0 / 1024, bias=EPS), reads=[bank], writes=[LNV])
            k.op("ACT", lambda e: e.activation(out=RSTD.ap, in_=LNV.ap, func=AF.Exp, scale=-0.5), reads=[LNV], writes=[RSTD])
            for kc in range(8):
                k.op("DVE", lambda e: e.scalar_tensor_tensor(out=dst_ap[:, kc, :], in0=src_ap[:, kc, :], scalar=vec.ap[:, gcol + kc:gcol + kc + 1],
                                                             in1=RSTD.ap, op0=ALU.mult, op1=ALU.mult), reads=[src_t, RSTD, vec], writes=[dst_t])

        for kp in range(KAC // 8):
            k.dma("POOL", v3(WB[kp].ap[:, 0:8192], 8), w_a[kp * 1024:(kp + 1) * 1024, :].rearrange("(a p) n -> p a n", p=128), writes=[WB[kp]])
        for t in range(NTILE):
            cs = slice(t * 512, (t + 1) * 512)
            ht, ut, AB = HTL[t % 2], UTL[t % 2], ABS[0]
            h3, u3 = v3(ht.ap, 8), v3(ut.ap, 8)
            k.dma("SP", h3, hT[:, cs].rearrange("(a p) n -> p a n", p=128), writes=[ht])
            k.dma("SP", v3(AB.ap, KAC), aT[:, cs].rearrange("(a p) n -> p a n", p=128), writes=[AB])
            for d in range(8):
                bank = PD[d % 2]
                for kc in range(KAC):
                    wbt = WB[kc // 8]
                    k.op("PE", lambda e: e.matmul(bank.ap, v3(wbt.ap[:, 0:8192], 8)[:, kc % 8, d * 128:(d + 1) * 128], v3(AB.ap, KAC)[:, kc, :],
                                                  start=(kc == 0), stop=(kc == KAC - 1)), reads=[wbt, AB], writes=[bank], pe_acc=(kc > 0))
                k.op("DVE", lambda e: e.tensor_tensor(out=h3[:, d, :], in0=h3[:, d, :], in1=bank.ap, op=ALU.add), reads=[bank, ht], writes=[ht])
            k.dma("SP", H1[:, cs].rearrange("(a p) n -> p a n", p=128), h3, reads=[ht], writes=[H1_t], part=True)
            rmsnorm(ht, h3, 0, ut, u3, PBK[0])
            for b in range(4):
                i = t * 4 + b
                bs = slice(b * 128, (b + 1) * 128)
                plg_b, prs = PG[b % 2], PU[b % 2]
                for kc in range(8):
                    k.op("PE", lambda e: e.matmul(plg_b.ap[:, 0:72], h3[:, kc, bs], wr_sb.ap[:, kc * 72:(kc + 1) * 72], start=(kc == 0), stop=(kc == 7)),
                         reads=[ht, wr_sb], writes=[plg_b], pe_acc=(kc > 0))
                k.op("PE", lambda e: e.matmul(prs.ap[:, 0:2], RSTD.ap[:, bs], ident[:, 0:2], start=True, stop=True), reads=[RSTD, cf], writes=[prs])
                rt, gmax, ngmax, gsum, gw, m1, m2, dd, ex, w1, w2 = r_s[0:11]
                k.op("DVE", lambda e: e.tensor_copy(out=rt.ap, in_=prs.ap[:, 0:1]), reads=[prs], writes=[rt])
                k.op("DVE", lambda e: e.scalar_tensor_tensor(out=r_lg.ap, in0=plg_b.ap[:, 0:72], scalar=rt.ap, in1=br_bc.ap, op0=ALU.mult, op1=ALU.add),
                     reads=[plg_b, rt, br_bc], writes=[r_lg])
                gl = r_lg.ap[:, 0:8]
                el = r_lg.ap[:, 8:72]
                k.op("DVE", lambda e: e.tensor_reduce(out=gmax.ap, in_=gl, axis=AX.X, op=ALU.max), reads=[r_lg], writes=[gmax])
                k.op("DVE", lambda e: e.tensor_scalar(out=r_goh.ap, in0=gl, scalar1=gmax.ap, scalar2=None, op0=ALU.is_equal), reads=[r_lg, gmax], writes=[r_goh])
                k.op("DVE", lambda e: e.tensor_scalar(out=ngmax.ap, in0=gmax.ap, scalar1=-1.0, scalar2=None, op0=ALU.mult), reads=[gmax], writes=[ngmax])
                k.op("ACT", lambda e: e.activation(out=r_a.ap, in_=gl, func=AF.Exp, bias=ngmax.ap, scale=1.0, accum_out=gsum.ap), reads=[r_lg, ngmax], writes=[r_a, gsum])
                k.op("DVE", lambda e: e.reciprocal(out=gw.ap, in_=gsum.ap), reads=[gsum], writes=[gw])
                k.op("DVE", lambda e: e.tensor_tensor(out=v3(r_t64.ap, 8), in0=v3(el, 8), in1=r_goh.ap.unsqueeze(2).to_broadcast([128, 8, 8]), op=ALU.mult),
                     reads=[r_lg, r_goh], writes=[r_t64])
                k.op("DVE", lambda e: e.tensor_reduce(out=r_es.ap, in_=v3(r_t64.ap, 8).rearrange("p g j -> p j g"), axis=AX.X, op=ALU.add), reads=[r_t64], writes=[r_es])
                k.op("DVE", lambda e: e.tensor_reduce(out=m1.ap, in_=r_es.ap, axis=AX.X, op=ALU.max), reads=[r_es], writes=[m1])
                k.op("DVE", lambda e: e.tensor_scalar(out=r_oh1.ap, in0=r_es.ap, scalar1=m1.ap, scalar2=None, op0=ALU.is_equal), reads=[r_es, m1], writes=[r_oh1])
                k.op("DVE", lambda e: e.scalar_tensor_tensor(out=r_es2.ap, in0=r_oh1.ap, scalar=-1e30, in1=r_es.ap, op0=ALU.mult, op1=ALU.add), reads=[r_oh1, r_es], writes=[r_es2])
                k.op("DVE", lambda e: e.tensor_reduce(out=m2.ap, in_=r_es2.ap, axis=AX.X, op=ALU.max), reads=[r_es2], writes=[m2])
                k.op("DVE", lambda e: e.tensor_scalar(out=r_oh2.ap, in0=r_es2.ap, scalar1=m2.ap, scalar2=None, op0=ALU.is_equal), reads=[r_es2, m2], writes=[r_oh2])
                k.op("DVE", lambda e: e.tensor_tensor(out=dd.ap, in0=m2.ap, in1=m1.ap, op=ALU.subtract), reads=[m1, m2], writes=[dd])
                k.op("ACT", lambda e: e.activation(out=ex.ap, in_=dd.ap, func=AF.Exp), reads=[dd], writes=[ex])
                k.op("DVE", lambda e: e.tensor_scalar(out=w1.ap, in0=ex.ap, scalar1=1.0, scalar2=None, op0=ALU.add), reads=[ex], writes=[w1])
                k.op("DVE", lambda e: e.reciprocal(out=w1.ap, in_=w1.ap), reads=[w1], writes=[w1])
                k.op("DVE", lambda e: e.tensor_tensor(out=w2.ap, in0=ex.ap, in1=w1.ap, op=ALU.mult), reads=[ex, w1], writes=[w2])
                k.op("DVE", lambda e: e.tensor_tensor(out=GA.ap[:, i:i + 1], in0=w1.ap, in1=gw.ap, op=ALU.mult), reads=[w1, gw], writes=[GA])
                k.op("DVE", lambda e: e.tensor_tensor(out=GB.ap[:, i:i + 1], in0=w2.ap, in1=gw.ap, op=ALU.mult), reads=[w2, gw], writes=[GB])
                k.op("DVE", lambda e: e.tensor_tensor(out=v3(OHC.ap[:, 0:64], 8), in0=r_goh.ap.unsqueeze(2).to_broadcast([128, 8, 8]),
                                                      in1=r_oh1.ap.unsqueeze(1).to_broadcast([128, 8, 8]), op=ALU.mult), reads=[r_goh, r_oh1], writes=[OHC])
                k.op("DVE", lambda e: e.tensor_tensor(out=v3(OHC.ap[:, 64:128], 8), in0=r_goh.ap.unsqueeze(2).to_broadcast([128, 8, 8]),
                                                      in1=r_oh2.ap.unsqueeze(1).to_broadcast([128, 8, 8]), op=ALU.mult), reads=[r_goh, r_oh2, OHC], writes=[OHC])
                k.op("DVE", lambda e: e.tensor_copy(out=OHS3[:, i, :], in_=OHC.ap), reads=[OHC], writes=[OHS])
                ppr, pcs = PD[0], PD[1]
                k.op("PE", lambda e: e.matmul(ppr.ap[:, 0:128], SLb.ap, OHC.ap, start=True, stop=True), reads=[SLb, OHC], writes=[ppr])
                k.op("PE", lambda e: e.matmul(pcs.ap[:, 0:128], ones_bf.ap, OHC.ap, start=True, stop=True), reads=[ones_bf, OHC], writes=[pcs])
                k.op("DVE", lambda e: e.tensor_tensor(out=PRS.ap, in0=ppr.ap[:, 0:128], in1=RUN.ap, op=ALU.add), reads=[ppr, RUN], writes=[PRS])
                k.op("DVE", lambda e: e.tensor_tensor(out=PRS.ap, in0=PRS.ap, in1=OHC.ap, op=ALU.mult), reads=[PRS, OHC], writes=[PRS])
                k.op("DVE", lambda e: e.tensor_reduce(out=RK0.ap[:, i:i + 1], in_=PRS.ap[:, 0:64], axis=AX.X, op=ALU.add), reads=[PRS], writes=[RK0])
                k.op("DVE", lambda e: e.tensor_reduce(out=RK1.ap[:, i:i + 1], in_=PRS.ap[:, 64:128], axis=AX.X, op=ALU.add), reads=[PRS], writes=[RK1])
                k.op("DVE", lambda e: e.tensor_tensor(out=RUN.ap, in0=RUN.ap, in1=pcs.ap[:, 0:128], op=ALU.add), reads=[pcs, RUN], writes=[RUN])
                trb = PBK[1].ap.bitcast(BF16)
                for kc in range(8):
                    k.op("PE", lambda e: e.transpose(trb[:, kc * 128:(kc + 1) * 128], u3[:, kc, bs], identb.ap), reads=[ut, identb], writes=[PBK[1]])
                k.op("ACT", lambda e: e.activation(out=XR3[:, i, :], in_=trb, func=AF.Copy), reads=[PBK[1]], writes=[XROWS])

        k.alias([BIG, IGF], ABS)
        cnt1, cnt2 = RUN.ap[:, 0:64], RUN.ap[:, 64:128]
        k.op("DVE", lambda e: e.tensor_tensor(out=CNT.ap, in0=cnt1, in1=cnt2, op=ALU.add), reads=[RUN], writes=[CNT])
        big_cm = BIG.ap[:, 0:2048].rearrange("p (a m) -> p a m", a=64)
        for mh in range(2):
            k.op("DVE", lambda e: e.tensor_tensor(out=big_cm, in0=CNT.ap.unsqueeze(2).to_broadcast([128, 64, 32]),
                                                  in1=THR[:, mh * 32:(mh + 1) * 32].unsqueeze(1).to_broadcast([128, 64, 32]), op=ALU.is_gt), reads=[CNT, cf], writes=[BIG])
            dstn = NB_ if mh == 0 else BASE1
            k.op("DVE", lambda e: e.tensor_reduce(out=dstn.ap, in_=big_cm, axis=AX.X, op=ALU.add), reads=[BIG], writes=[dstn])
        k.op("DVE", lambda e: e.tensor_tensor(out=NB_.ap, in0=NB_.ap, in1=BASE1.ap, op=ALU.add), reads=[NB_, BASE1], writes=[NB_])
        k.op("DVE", lambda e: e.tensor_scalar(out=NB_.ap, in0=NB_.ap, scalar1=float(MB), scalar2=None, op0=ALU.mult), reads=[NB_], writes=[NB_])
        k.op("DVE", lambda e: e.tensor_copy(out=PE_[0].ap, in_=NB_.ap), reads=[NB_], writes=[PE_[0]])
        cur = 0
        for sft in (1, 2, 4, 8, 16, 32):
            a_, b_ = PE_[cur], PE_[1 - cur]
            k.op("DVE", lambda e: e.tensor_copy(out=b_.ap[:, 0:sft], in_=a_.ap[:, 0:sft]), reads=[a_], writes=[b_])
            k.op("DVE", lambda e: e.tensor_tensor(out=b_.ap[:, sft:64], in0=a_.ap[:, sft:64], in1=a_.ap[:, 0:64 - sft], op=ALU.add), reads=[a_, b_], writes=[b_])
            cur = 1 - cur
        PEND = PE_[cur]
        k.op("DVE", lambda e: e.tensor_tensor(out=BASE1.ap, in0=PEND.ap, in1=NB_.ap, op=ALU.subtract), reads=[PEND, NB_], writes=[BASE1])
        k.op("DVE", lambda e: e.tensor_tensor(out=BASE2.ap, in0=BASE1.ap, in1=cnt1, op=ALU.add), reads=[BASE1, RUN], writes=[BASE2])
        TBC = 2048 // 64
        for (base, rk, di, lo) in ((BASE1, RK0, DI0, 0), (BASE2, RK1, DI1, 64)):
            for c0 in range(0, NTB, TBC):
                nb = min(TBC, NTB - c0)
                big_t = BIG.ap[:, 0:nb * 64].rearrange("p (a m) -> p a m", a=nb)
                k.op("DVE", lambda e: e.tensor_tensor(out=big_t, in0=OHS3[:, c0:c0 + nb, lo:lo + 64], in1=base.ap.unsqueeze(1).to_broadcast([128, nb, 64]), op=ALU.mult),
                     reads=[OHS, base], writes=[BIG])
                k.op("DVE", lambda e: e.tensor_reduce(out=DF.ap[:, c0:c0 + nb], in_=big_t, axis=AX.X, op=ALU.add), reads=[BIG], writes=[DF])
            k.op("DVE", lambda e: e.tensor_tensor(out=DF.ap, in0=DF.ap, in1=rk.ap, op=ALU.add), reads=[DF, rk], writes=[DF])
            k.op("DVE", lambda e: e.tensor_copy(out=di.ap, in_=DF.ap), reads=[DF], writes=[di])
        for c0 in range(0, NBLK, 32):
            nb = min(32, NBLK - c0)
            big_j = BIG.ap[:, 0:nb * 64].rearrange("p (a m) -> p a m", a=nb)
            k.op("DVE", lambda e: e.tensor_tensor(out=big_j, in0=PEND.ap.unsqueeze(1).to_broadcast([128, nb, 64]), in1=JB[:, c0:c0 + nb].unsqueeze(2).to_broadcast([128, nb, 64]), op=ALU.is_le),
                 reads=[PEND, cf], writes=[BIG])
            k.op("DVE", lambda e: e.tensor_reduce(out=EJ.ap[:, c0:c0 + nb], in_=big_j, axis=AX.X, op=ALU.add), reads=[BIG], writes=[EJ])
        k.op("DVE", lambda e: e.tensor_scalar(out=EJ.ap, in0=EJ.ap, scalar1=63.0, scalar2=None, op0=ALU.min), reads=[EJ], writes=[EJ])
        HB = NBLK // 2
        k.op("DVE", lambda e: e.memset(SAME.ap, 0.0), writes=[SAME])
        for s0 in (0, HB):
            k.op("DVE", lambda e: e.tensor_tensor(out=SAME.ap[:, s0 + 1:s0 + HB], in0=EJ.ap[:, s0 + 1:s0 + HB], in1=EJ.ap[:, s0:s0 + HB - 1], op=ALU.is_equal),
                 reads=[EJ, SAME], writes=[SAME])
        k.op("DVE", lambda e: e.tensor_scalar(out=SAME.ap, in0=SAME.ap, scalar1=float(1 << 22), scalar2=None, op0=ALU.mult), reads=[SAME], writes=[SAME])
        igf3 = v3(IGF.ap[:, 0:NBLK * 8], NBLK)
        k.op("DVE", lambda e: e.scalar_tensor_tensor(out=BIG.ap[:, 0:NBLK], in0=EJ.ap, scalar=1024.0, in1=SAME.ap, op0=ALU.mult, op1=ALU.add), reads=[EJ, SAME], writes=[BIG])
        k.op("DVE", lambda e: e.tensor_tensor(out=igf3, in0=BIG.ap[:, 0:NBLK].unsqueeze(2).to_broadcast([128, NBLK, 8]), in1=KP.unsqueeze(1).to_broadcast([128, NBLK, 8]), op=ALU.add),
             reads=[BIG, cf], writes=[IGF])
        k.op("DVE", lambda e: e.tensor_copy(out=IGI.ap, in_=IGF.ap[:, 0:NBLK * 8]), reads=[IGF], writes=[IGI])
        k.op("DVE", lambda e: e.scalar_tensor_tensor(out=BIG.ap[:, 0:NBLK], in0=EJ.ap, scalar=512.0, in1=SAME.ap, op0=ALU.mult, op1=ALU.add), reads=[EJ, SAME], writes=[BIG])
        idf3 = IGF.ap[:, 0:NBLK * 4].rearrange("p (a m) -> p a m", a=NBLK)
        k.op("DVE", lambda e: e.tensor_tensor(out=idf3, in0=BIG.ap[:, 0:NBLK].unsqueeze(2).to_broadcast([128, NBLK, 4]), in1=KP[:, 0:4].unsqueeze(1).to_broadcast([128, NBLK, 4]), op=ALU.add),
             reads=[BIG, cf], writes=[IGF])
        k.op("DVE", lambda e: e.tensor_copy(out=IDI.ap, in_=IGF.ap[:, 0:NBLK * 4]), reads=[IGF], writes=[IDI])

        for i in range(NTB):
            k.idma(Xs, XR3[:, i, :], DI0, DI0.ap[:, i:i + 1], gather=False, reads=[XROWS], writes=[Xs_t], part=True)
            k.idma(Xs, XR3[:, i, :], DI1, DI1.ap[:, i:i + 1], gather=False, reads=[XROWS], writes=[Xs_t], part=True)

        k.alias(XB + XTB + YB, [BIG, IGF] + ABS)
        igi3 = v3(IGI.ap, NBLK)
        idi3 = v3(IDI.ap, NBLK)

        bnd_gu = nc.gpsimd.to_reg(64 * 1024 - 1)
        bnd_d = nc.gpsimd.to_reg(64 * 512 - 1)

        def load_blk(j, n_):
            wb = WB[n_ % 2]
            for kc in range(8):
                k.idma(wb.ap[:, kc * 512:(kc + 1) * 512], wg_r, IGI, igi3[:, j, kc:kc + 1], gather=True, writes=[wb], part=(kc > 0), bound=bnd_gu)
            for kc in range(8):
                k.idma(wb.ap[:, 4096 + kc * 512:4096 + (kc + 1) * 512], wu_r, IGI, igi3[:, j, kc:kc + 1], gather=True, writes=[wb], part=True, bound=bnd_gu)
            for f in range(4):
                k.idma(wb.ap[:, 8192 + f * 1024:8192 + (f + 1) * 1024], wd_r, IDI, idi3[:, j, f:f + 1], gather=True, writes=[wb], part=True, bound=bnd_d)
            k.dma("SP", XB[n_ % 2].ap, Xs[j * MB:(j + 1) * MB, :], reads=[Xs_t], writes=[XB[n_ % 2]])

        def front(j, n_):
            wb, xb, xt = WB[n_ % 2], XB[n_ % 2], XTB[n_ % 2]
            trb0, trb1 = PBK[0].ap.bitcast(BF16), PBK[1].ap.bitcast(BF16)
            for kc in range(8):
                dst = (trb0 if kc < 4 else trb1)[:, (kc % 4) * 128:(kc % 4 + 1) * 128]
                k.op("PE", lambda e: e.transpose(dst, xb.ap[:, kc * 128:(kc + 1) * 128], identb.ap), reads=[xb, identb], writes=[PBK[0] if kc < 4 else PBK[1]])
            k.op("ACT", lambda e: e.activation(out=xt.ap[:, 0:512], in_=trb0[:, 0:512], func=AF.Copy), reads=[PBK[0]], writes=[xt])
            k.op("ACT", lambda e: e.activation(out=xt.ap[:, 512:1024], in_=trb1[:, 0:512], func=AF.Copy), reads=[PBK[1], xt], writes=[xt])
            Wg3, Wu3 = v3(wb.ap[:, 0:4096], 8), v3(wb.ap[:, 4096:8192], 8)
            pg, pu = PG[n_ % 2], PU[n_ % 2]
            for f in range(4):
                for kc in range(8):
                    k.op("PE", lambda e: e.matmul(pg.ap[:, f * 128:(f + 1) * 128], Wg3[:, kc, f * 128:(f + 1) * 128], xt.ap[:, kc * 128:(kc + 1) * 128],
                                                  start=(kc == 0), stop=(kc == 7)), reads=[wb, xt], writes=[pg], pe_acc=(kc > 0 or f > 0))
            for f in range(4):
                for kc in range(8):
                    k.op("PE", lambda e: e.matmul(pu.ap[:, f * 128:(f + 1) * 128], Wu3[:, kc, f * 128:(f + 1) * 128], xt.ap[:, kc * 128:(kc + 1) * 128],
                                                  start=(kc == 0), stop=(kc == 7)), reads=[wb, xt], writes=[pu], pe_acc=(kc > 0 or f > 0))
            k.op("ACT", lambda e: e.activation(out=SS[n_ % 2].ap, in_=pg.ap, func=AF.Silu), reads=[pg], writes=[SS[n_ % 2]])
            k.op("DVE", lambda e: e.tensor_tensor(out=HH[n_ % 2].ap, in0=SS[n_ % 2].ap, in1=pu.ap, op=ALU.mult), reads=[SS[n_ % 2], pu], writes=[HH[n_ % 2]])

        def back(j, n_):
            wb, hh, yb = WB[n_ % 2], HH[n_ % 2], YB[n_ % 2]
            Wd3 = v3(wb.ap[:, 8192:12288], 4)
            for dh in range(2):
                pd = PD[dh]
                for f in range(4):
                    k.op("PE", lambda e: e.matmul(pd.ap, hh.ap[:, f * 128:(f + 1) * 128], Wd3[:, f, dh * 512:(dh + 1) * 512], start=(f == 0), stop=(f == 3)),
                         reads=[wb, hh], writes=[pd], pe_acc=(f > 0))
                if dh == 0:
                    k.op("ACT", lambda e: e.activation(out=yb.ap[:, 0:512], in_=pd.ap, func=AF.Copy), reads=[pd], writes=[yb])
                else:
                    k.op("DVE", lambda e: e.tensor_copy(out=yb.ap[:, 512:1024], in_=pd.ap), reads=[pd, yb], writes=[yb])
            k.dma("SP", Ys[j * MB:(j + 1) * MB, :], yb.ap, reads=[yb], writes=[Ys_t], part=True)

        order = []
        for q in range(HB):
            order += [q, HB + q]
        load_blk(order[0], 0)
        for n_, j in enumerate(order):
            front(j, n_)
            if n_ > 0:
                back(order[n_ - 1], n_ - 1)
            if n_ + 1 < NBLK:
                load_blk(order[n_ + 1], n_ + 1)
        back(order[-1], NBLK - 1)

        k.dma("POOL", v3(WB[0].ap[:, 0:8192], 8), plg.rearrange("(a p) n -> p a n", p=128), writes=[WB[0]])
        k.dma("POOL", v3(WB[1].ap[:, 0:2048], 2), plp.rearrange("(a p) n -> p a n", p=128), writes=[WB[1]])
        k.alias(WQ + Y0 + Y1, [XROWS])
        if mode == "A":
            k.dma("POOL", v3(WQ[0].ap, 8), wqkv[:, 0:1536].rearrange("(a p) n -> p a n", p=128), writes=[WQ[0]])
            k.dma("POOL", v3(WQ[1].ap, 8), wqkv[:, 1536:3072].rearrange("(a p) n -> p a n", p=128), writes=[WQ[1]])
        for t in range(NTILE):
            cs = slice(t * 512, (t + 1) * 512)
            ht, ut = HTL[t % 2], UTL[t % 2]
            h3, u3 = v3(ht.ap, 8), v3(ut.ap, 8)
            k.dma("SP", h3, H1[:, cs].rearrange("(a p) n -> p a n", p=128), reads=[H1_t], writes=[ht])
            k.dma("POOL", v3(PBt.ap, 2), pT[:, cs].rearrange("(a p) n -> p a n", p=128), writes=[PBt])
            for b in range(4):
                i = t * 4 + b
                bs = slice(b * 128, (b + 1) * 128)
                y0, y1 = Y0[i % 2], Y1[i % 2]
                k.idma(y0.ap, Ys, DI0, DI0.ap[:, i:i + 1], gather=True, reads=[Ys_t], writes=[y0])
                k.idma(y1.ap, Ys, DI1, DI1.ap[:, i:i + 1], gather=True, reads=[Ys_t], writes=[y1])
                k.op("DVE", lambda e: e.tensor_scalar(out=y0.ap, in0=y0.ap, scalar1=GA.ap[:, i:i + 1], scalar2=None, op0=ALU.mult), reads=[y0, GA], writes=[y0])
                k.op("DVE", lambda e: e.scalar_tensor_tensor(out=y0.ap, in0=y1.ap, scalar=GB.ap[:, i:i + 1], in1=y0.ap, op0=ALU.mult, op1=ALU.add),
                     reads=[y1, GB, y0], writes=[y0])
                for half in range(2):
                    bank = PD[half]
                    for dq in range(4):
                        d = half * 4 + dq
                        k.op("PE", lambda e: e.transpose(bank.ap[:, dq * 128:(dq + 1) * 128], y0.ap[:, d * 128:(d + 1) * 128], ident), reads=[y0, cf], writes=[bank])
                    k.op("DVE", lambda e: e.tensor_tensor(out=h3[:, half * 4:(half + 1) * 4, bs], in0=h3[:, half * 4:(half + 1) * 4, bs],
                                                          in1=bank.ap.rearrange("p (a n) -> p a n", a=4), op=ALU.add), reads=[bank, ht], writes=[ht])
            rmsnorm(ht, h3, 8, ut, u3, PBK[0])
            for d in range(8):
                pg, pu = PG[d % 2], PU[d % 2]
                for kc in range(8):
                    k.op("PE", lambda e: e.matmul(pg.ap, v3(WB[0].ap[:, 0:8192], 8)[:, kc, d * 128:(d + 1) * 128], u3[:, kc, :], start=(kc == 0), stop=(kc == 7)),
                         reads=[WB[0], ut], writes=[pg], pe_acc=(kc > 0))
                for kc in range(2):
                    k.op("PE", lambda e: e.matmul(pu.ap, v3(WB[1].ap[:, 0:2048], 2)[:, kc, d * 128:(d + 1) * 128], v3(PBt.ap, 2)[:, kc, :], start=(kc == 0), stop=(kc == 1)),
                         reads=[WB[1], PBt], writes=[pu], pe_acc=(kc > 0))
                sf = STF[d % 2]
                k.op("ACT", lambda e: e.activation(out=sf.ap, in_=pg.ap, func=AF.Sigmoid, bias=vec.ap[:, 16 + d:17 + d], scale=1.0), reads=[pg, vec], writes=[sf])
                k.op("DVE", lambda e: e.tensor_tensor(out=sf.ap, in0=sf.ap, in1=pu.ap, op=ALU.mult), reads=[sf, pu], writes=[sf])
                k.op("DVE", lambda e: e.tensor_tensor(out=h3[:, d, :], in0=h3[:, d, :], in1=sf.ap, op=ALU.add), reads=[sf, ht], writes=[ht])
            if mode == "A":
                k.dma("SP", hT_o[:, cs].rearrange("(a p) n -> p a n", p=128), h3, reads=[ht], writes=[ho_t], part=True)
                rmsnorm(ht, h3, 24, ut, u3, PBK[0])
                for n in range(16):
                    col = n * 128
                    wbt = WQ[0] if col < 1536 else WQ[1]
                    lc = col if col < 1536 else col - 1536
                    bank = PG[n % 2]
                    for kc in range(8):
                        k.op("PE", lambda e: e.matmul(bank.ap, v3(wbt.ap, 8)[:, kc, lc:lc + 128], u3[:, kc, :], start=(kc == 0), stop=(kc == 7)),
                             reads=[wbt, ut], writes=[bank], pe_acc=(kc > 0))
                    sg = STG[n % 2]
                    sc_ = (1.0 / np.sqrt(128.0)) if n < 8 else 1.0
                    k.op("ACT", lambda e: e.activation(out=sg.ap, in_=bank.ap, func=AF.Identity, scale=float(sc_)), reads=[bank], writes=[sg])
                    if n < 8:
                        k.dma("SP", qT_o[n * 128:(n + 1) * 128, cs], sg.ap, reads=[sg], writes=[qT_t], part=True)
                    else:
                        k.dma("SP", kT_o[(n - 8) * 128:(n - 7) * 128, cs], sg.ap, reads=[sg], writes=[kT_t], part=True)
                for b in range(4):
                    for hh_ in range(2):
                        bank = PU[hh_]
                        for kc in range(8):
                            k.op("PE", lambda e: e.matmul(bank.ap, u3[:, kc, b * 128:(b + 1) * 128], v3(WQ[1].ap, 8)[:, kc, 512 + hh_ * 512:1024 + hh_ * 512],
                                                          start=(kc == 0), stop=(kc == 7)), reads=[WQ[1], ut], writes=[bank], pe_acc=(kc > 0))
                        sg = STG[hh_]
                        k.op("ACT", lambda e: e.activation(out=sg.ap, in_=bank.ap, func=AF.Copy), reads=[bank], writes=[sg])
                        r0 = t * 512 + b * 128
                        k.dma("SP", v_o[r0:r0 + 128, hh_ * 512:(hh_ + 1) * 512], sg.ap, reads=[sg], writes=[v_t], part=True)
            else:
                k.op("ACT", lambda e: e.activation(out=u3, in_=h3, func=AF.Square), reads=[ht], writes=[ut])
                bank = PBK[0]
                for kc in range(8):
                    k.op("PE", lambda e: e.matmul(bank.ap, ones_bf.ap, u3[:, kc, :], start=(kc == 0), stop=(kc == 7)),
                         reads=[ut, ones_bf], writes=[bank], pe_acc=(kc > 0))
                k.op("ACT", lambda e: e.activation(out=LNV.ap, in_=bank.ap, func=AF.Ln, scale=1.0 / 1024, bias=EPS), reads=[bank], writes=[LNV])
                k.op("ACT", lambda e: e.activation(out=RSTD.ap, in_=LNV.ap, func=AF.Exp, scale=-0.5), reads=[LNV], writes=[RSTD])
                for d in range(8):
                    k.op("DVE", lambda e: e.scalar_tensor_tensor(out=h3[:, d, :], in0=h3[:, d, :], scalar=vec.ap[:, 24 + d:25 + d], in1=RSTD.ap,
                                                                 op0=ALU.mult, op1=ALU.mult), reads=[ht, RSTD, vec], writes=[ht])
                k.dma("SP", out_o[:, cs].rearrange("(a p) n -> p a n", p=128), h3, reads=[ht], writes=[out_t], part=True)
        k.finish("SP", [qT_t, kT_t, v_t, ho_t] if mode == "A" else [out_t])
        print("tok2 ninst", k.ninst)
    return nc
```

```python
import numpy as np
import ml_dtypes
from contextlib import ExitStack
import concourse.bass as bass
import concourse.mybir as mybir
from concourse.bass_utils import run_bass_kernel_spmd

F32 = mybir.dt.float32
BF16 = mybir.dt.bfloat16
AF = mybir.ActivationFunctionType
ALU = mybir.AluOpType
AX = mybir.AxisListType
EPS = 1e-6
NPBF = ml_dtypes.bfloat16


class T:
    __slots__ = ("ap", "w", "r", "dsem", "dcnt", "name")

    def __init__(self, ap, name=""):
        self.ap = ap
        self.w = None
        self.r = {}
        self.dsem = None
        self.dcnt = 0
        self.name = name


class K:
    def __init__(self, nc, stack):
        self.nc = nc
        self.stack = stack
        self.eng = {"PE": nc.tensor, "ACT": nc.scalar, "DVE": nc.vector, "POOL": nc.gpsimd, "SP": nc.sync}
        self.sem = {}
        self.cnt = {}
        for e in ("PE", "ACT", "DVE", "POOL"):
            self.sem[e] = stack.enter_context(nc.semaphore("s_" + e))
            self.cnt[e] = 0
        self.seen = {e: {} for e in self.eng}
        self.semobj = dict(self.sem)
        self.ndsem = 0
        self.ninst = 0
        self.nalloc = 0

    def sb(self, shape, dt, name=None):
        self.nalloc += 1
        return self.stack.enter_context(self.nc.sbuf_tensor(name or ("sb%d" % self.nalloc), list(shape), dt))

    def ps(self, shape, dt, name=None):
        self.nalloc += 1
        return self.stack.enter_context(self.nc.psum_tensor(name or ("ps%d" % self.nalloc), list(shape), dt))

    def sbt(self, shape, dt, name=None):
        return T(self.sb(shape, dt, name)[:])

    def _need(self, e, key, val):
        if val <= 0 or self.seen[e].get(key, 0) >= val:
            return
        self.eng[e].wait_ge(self.semobj[key], val)
        self.seen[e][key] = val

    def op(self, e, fn, reads=(), writes=(), pe_acc=False):
        for t in reads:
            if t.w is not None:
                self._need(e, *t.w)
        for t in writes:
            if t.w is not None and not (pe_acc and t.w[0] == "PE"):
                self._need(e, *t.w)
            for kk, v in t.r.items():
                self._need(e, kk, v)
        ins = fn(self.eng[e])
        self.cnt[e] += 1
        c = self.cnt[e]
        ins.then_inc(self.sem[e], 1)
        for t in reads:
            t.r[e] = c
        for t in writes:
            t.w = (e, c)
            t.r = {}
        self.ninst += 1
        return ins

    def dma(self, q, out_ap, in_ap, reads=(), writes=(), part=False, **kw):
        assert len(writes) == 1
        t = writes[0]
        if t.dsem is None:
            key = "d%d" % self.ndsem
            t.dsem = self.stack.enter_context(self.nc.semaphore(key))
            self.semobj[key] = t.dsem
            t.name = key
            self.ndsem += 1
        key = t.name
        for r in reads:
            if r.w is not None:
                self._need(q, *r.w)
        if t.w is not None and not (part and t.w[0] == key):
            self._need(q, *t.w)
        for kk, v in t.r.items():
            self._need(q, kk, v)
        ins = self.eng[q].dma_start(out=out_ap, in_=in_ap, **kw)
        t.dcnt += 16
        ins.then_inc(t.dsem, 16)
        for r in reads:
            r.r[key] = max(r.r.get(key, 0), t.dcnt)
        t.w = (key, t.dcnt)
        if not part:
            t.r = {}
        self.ninst += 1
        return ins

    def alias(self, new, old):
        for n in new:
            for o in old:
                for kk, v in o.r.items():
                    n.r[kk] = max(n.r.get(kk, 0), v)
                if o.w is not None:
                    n.r[o.w[0]] = max(n.r.get(o.w[0], 0), o.w[1])

    def finish(self, e, tiles):
        for t in tiles:
            if t.w is not None:
                self._need(e, *t.w)


def v3(ap, a):
    return ap.rearrange("p (a n) -> p a n", a=a)


def build_tok(NT, TH, KA, mode, NE=64):
    nc = bass.Bass("TRN2", target_bir_lowering=False)

    def din(name, shape, dt=F32):
        return nc.dram_tensor(name, list(shape), dt, kind="ExternalInput").ap()

    def dout(name, shape, dt=F32):
        return nc.dram_tensor(name, list(shape), dt, kind="ExternalOutput").ap()

    hT = din("hT", [1024, NT])
    aT = din("aT", [KA, NT], BF16)
    w_a = din("w_a", [KA, 1024])
    vecs = din("vecs", [128, 32])
    wr = din("wr", [1024, 72])
    br = din("br", [1, 72])
    wg = din("wg", [64, 1024, 512])
    wu = din("wu", [64, 1024, 512])
    wd = din("wd", [64, 512, 1024])
    plg = din("plg", [1024, 1024])
    plp = din("plp", [256, 1024])
    pT = din("pT", [256, NT])
    ident_d = din("ident", [128, 128])
    if mode == "A":
        wqkv = din("wqkv", [1024, 3072])
        hT_o = dout("hT_o", [1024, NT])
        qT_o = dout("qT_o", [1024, NT], BF16)
        kT_o = dout("kT_o", [1024, NT], BF16)
        v_o = dout("v_o", [NT, 1024], BF16)
    else:
        out_o = dout("out_o", [1024, NT])
    KAC = KA // 128
    NTL = TH // 512
    NH = NT // TH

    with ExitStack() as st:
        k = K(nc, st)
        h_sb = k.sb([128, 8 * TH], F32, "h_sb")
        h3 = v3(h_sb[:], 8)
        HT = [[T(h3[:, d, t * 512:(t + 1) * 512]) for d in range(8)] for t in range(NTL)]
        u_sb = k.sb([128, 8 * TH], BF16, "u_sb")
        u3 = v3(u_sb[:], 8)
        UT = [T(u3[:, :, t * 512:(t + 1) * 512]) for t in range(NTL)]
        WB = [k.sbt([128, 12288], BF16, "wb%d" % i) for i in range(2)]
        GT = k.sbt([64, TH], F32, "gt")
        NAB = max(1, min(2, (8 * TH) // (KAC * 512)))
        ABS = [T(u_sb[:, i * KAC * 512:(i + 1) * KAC * 512]) for i in range(NAB)]
        PB = k.sbt([128, 2 * 512], BF16, "pb")
        SQ = k.sbt([128, 8 * 512], BF16, "sq")
        LNV = k.sbt([128, 512], F32, "lnv")
        RSTD = k.sbt([128, 512], F32, "rstd")
        vec = k.sbt([128, 32], F32, "vec_sb")
        ident = k.sbt([128, 128], F32, "ident_sb")
        ones_bf = k.sbt([128, 128], BF16, "ones")
        wr_sb = k.sbt([128, 8 * 72], F32, "wr_sb")
        br_bc = k.sbt([128, 72], F32, "brbc")
        S_ = [k.sbt([128, 512], BF16, "s%d" % i) for i in range(2)]
        TT = [k.sbt([128, 512], BF16, "tt%d" % i) for i in range(2)]
        HS = [[k.sbt([128, 512], BF16, "hs%d_%d" % (i, f)) for f in range(4)] for i in range(2)]
        EB = [k.sbt([64, 128], F32, "eb%d" % i) for i in range(2)]
        STG = [k.sbt([128, 512], BF16, "stg%d" % i) for i in range(2)]
        STF = [k.sbt([128, 512], F32, "stf%d" % i) for i in range(2)]
        r_lg = k.sbt([128, 72], F32, "r_lg")
        r_a = k.sbt([128, 8], F32, "r_a")
        r_goh = k.sbt([128, 8], F32, "r_goh")
        r_t64 = k.sbt([128, 64], F32, "r_t64")
        r_es = k.sbt([128, 8], F32, "r_es")
        r_es2 = k.sbt([128, 8], F32, "r_es2")
        r_oh1 = k.sbt([128, 8], F32, "r_oh1")
        r_oh2 = k.sbt([128, 8], F32, "r_oh2")
        r_ew = k.sbt([128, 8], F32, "r_ew")
        r_G = k.sbt([128, 64], F32, "r_G")
        r_s = [k.sbt([128, 1], F32, "r_s%d" % i) for i in range(12)]
        BK = [T(k.ps([128, 512], F32, "bk%d" % i)[:]) for i in range(8)]
        PG, PU, PD, PBK = BK[0:2], BK[2:4], BK[4:6], BK[6:8]

        k.dma("SP", vec.ap, vecs, writes=[vec])
        k.dma("SP", ident.ap, ident_d, writes=[ident])
        k.dma("SP", v3(wr_sb.ap, 8), wr.rearrange("(a p) n -> p a n", p=128), writes=[wr_sb])
        k.dma("SP", br_bc.ap, br.partition_broadcast(128).rearrange("p o n -> p (o n)"), writes=[br_bc])
        k.op("DVE", lambda e: e.memset(ones_bf.ap, 1.0), writes=[ones_bf])
        for kc in range(8):
            k.op("DVE", lambda e: e.tensor_scalar(out=wr_sb.ap[:, kc * 72:(kc + 1) * 72], in0=wr_sb.ap[:, kc * 72:(kc + 1) * 72],
                                                  scalar1=vec.ap[:, kc:kc + 1], scalar2=None, op0=ALU.mult),
                 reads=[vec, wr_sb], writes=[wr_sb])

        def rmsnorm(src_tiles, src_ap, gcol, dst_t, dst_ap, bank):
            k.op("ACT", lambda e: e.activation(out=v3(SQ.ap, 8), in_=src_ap, func=AF.Square), reads=src_tiles, writes=[SQ])
            for kc in range(8):
                k.op("PE", lambda e: e.matmul(bank.ap, ones_bf.ap, v3(SQ.ap, 8)[:, kc, :], start=(kc == 0), stop=(kc == 7)),
                     reads=[SQ, ones_bf], writes=[bank], pe_acc=(kc > 0))
            k.op("ACT", lambda e: e.activation(out=LNV.ap, in_=bank.ap, func=AF.Ln, scale=1.0 / 1024, bias=EPS), reads=[bank], writes=[LNV])
            k.op("ACT", lambda e: e.activation(out=RSTD.ap, in_=LNV.ap, func=AF.Exp, scale=-0.5), reads=[LNV], writes=[RSTD])
            for kc in range(8):
                k.op("DVE", lambda e: e.scalar_tensor_tensor(out=dst_ap[:, kc, :], in0=src_ap[:, kc, :], scalar=vec.ap[:, gcol + kc:gcol + kc + 1],
                                                             in1=RSTD.ap, op0=ALU.mult, op1=ALU.mult),
                     reads=list(src_tiles) + [RSTD, vec], writes=[dst_t])

        if mode == "A":
            qT_t, kT_t, v_t, ho_t = T(qT_o), T(kT_o), T(v_o), T(hT_o)
        else:
            out_t = T(out_o)
        for hf in range(NH):
            c0 = hf * TH
            k.alias(ABS, UT)
            for kp in range(KAC // 8):
                k.dma("POOL", v3(WB[kp].ap[:, 0:8192], 8), w_a[kp * 1024:(kp + 1) * 1024, :].rearrange("(a p) n -> p a n", p=128), writes=[WB[kp]])
            for t in range(NTL):
                cs = slice(c0 + t * 512, c0 + (t + 1) * 512)
                for d in range(8):
                    k.dma("SP", HT[t][d].ap, hT[d * 128:(d + 1) * 128, cs], writes=[HT[t][d]])
                AB = ABS[t % NAB]
                k.dma("SP", v3(AB.ap, KAC), aT[:, cs].rearrange("(a p) n -> p a n", p=128), writes=[AB])
                for d in range(8):
                    bank = PD[d % 2]
                    for kc in range(KAC):
                        wbt = WB[kc // 8]
                        k.op("PE", lambda e: e.matmul(bank.ap, v3(wbt.ap[:, 0:8192], 8)[:, kc % 8, d * 128:(d + 1) * 128], v3(AB.ap, KAC)[:, kc, :],
                                                      start=(kc == 0), stop=(kc == KAC - 1)),
                             reads=[wbt, AB], writes=[bank], pe_acc=(kc > 0))
                    k.op("DVE", lambda e: e.tensor_tensor(out=HT[t][d].ap, in0=HT[t][d].ap, in1=bank.ap, op=ALU.add),
                         reads=[bank, HT[t][d]], writes=[HT[t][d]])
            k.alias(UT, ABS)
            for t in range(NTL):
                hap = h3[:, :, t * 512:(t + 1) * 512]
                rmsnorm(HT[t], hap, 0, UT[t], UT[t].ap, PBK[0])
                for b in range(4):
                    bs = slice(t * 512 + b * 128, t * 512 + (b + 1) * 128)
                    plg_b, prs = PG[b % 2], PU[b % 2]
                    for kc in range(8):
                        k.op("PE", lambda e: e.matmul(plg_b.ap[:, 0:72], h3[:, kc, bs], wr_sb.ap[:, kc * 72:(kc + 1) * 72], start=(kc == 0), stop=(kc == 7)),
                             reads=[HT[t][kc], wr_sb], writes=[plg_b], pe_acc=(kc > 0))
                    k.op("PE", lambda e: e.matmul(prs.ap[:, 0:2], RSTD.ap[:, b * 128:(b + 1) * 128], ident.ap[:, 0:2], start=True, stop=True),
                         reads=[RSTD, ident], writes=[prs])
                    rt, gmax, ngmax, gsum, gw, m1, m2, dd, ex, w1, w2 = r_s[0:11]
                    k.op("DVE", lambda e: e.tensor_copy(out=rt.ap, in_=prs.ap[:, 0:1]), reads=[prs], writes=[rt])
                    k.op("DVE", lambda e: e.scalar_tensor_tensor(out=r_lg.ap, in0=plg_b.ap[:, 0:72], scalar=rt.ap, in1=br_bc.ap, op0=ALU.mult, op1=ALU.add),
                         reads=[plg_b, rt, br_bc], writes=[r_lg])
                    gl = r_lg.ap[:, 0:8]
                    el = r_lg.ap[:, 8:72]
                    k.op("DVE", lambda e: e.tensor_reduce(out=gmax.ap, in_=gl, axis=AX.X, op=ALU.max), reads=[r_lg], writes=[gmax])
                    k.op("DVE", lambda e: e.tensor_scalar(out=r_goh.ap, in0=gl, scalar1=gmax.ap, scalar2=None, op0=ALU.is_equal), reads=[r_lg, gmax], writes=[r_goh])
                    k.op("DVE", lambda e: e.tensor_scalar(out=ngmax.ap, in0=gmax.ap, scalar1=-1.0, scalar2=None, op0=ALU.mult), reads=[gmax], writes=[ngmax])
                    k.op("ACT", lambda e: e.activation(out=r_a.ap, in_=gl, func=AF.Exp, bias=ngmax.ap, scale=1.0, accum_out=gsum.ap), reads=[r_lg, ngmax], writes=[r_a, gsum])
                    k.op("DVE", lambda e: e.reciprocal(out=gw.ap, in_=gsum.ap), reads=[gsum], writes=[gw])
                    k.op("DVE", lambda e: e.tensor_tensor(out=v3(r_t64.ap, 8), in0=v3(el, 8), in1=r_goh.ap.unsqueeze(2).to_broadcast([128, 8, 8]), op=ALU.mult),
                         reads=[r_lg, r_goh], writes=[r_t64])
                    k.op("DVE", lambda e: e.tensor_reduce(out=r_es.ap, in_=v3(r_t64.ap, 8).rearrange("p g j -> p j g"), axis=AX.X, op=ALU.add), reads=[r_t64], writes=[r_es])
                    k.op("DVE", lambda e: e.tensor_reduce(out=m1.ap, in_=r_es.ap, axis=AX.X, op=ALU.max), reads=[r_es], writes=[m1])
                    k.op("DVE", lambda e: e.tensor_scalar(out=r_oh1.ap, in0=r_es.ap, scalar1=m1.ap, scalar2=None, op0=ALU.is_equal), reads=[r_es, m1], writes=[r_oh1])
                    k.op("DVE", lambda e: e.scalar_tensor_tensor(out=r_es2.ap, in0=r_oh1.ap, scalar=-1e30, in1=r_es.ap, op0=ALU.mult, op1=ALU.add), reads=[r_oh1, r_es], writes=[r_es2])
                    k.op("DVE", lambda e: e.tensor_reduce(out=m2.ap, in_=r_es2.ap, axis=AX.X, op=ALU.max), reads=[r_es2], writes=[m2])
                    k.op("DVE", lambda e: e.tensor_scalar(out=r_oh2.ap, in0=r_es2.ap, scalar1=m2.ap, scalar2=None, op0=ALU.is_equal), reads=[r_es2, m2], writes=[r_oh2])
                    k.op("DVE", lambda e: e.tensor_tensor(out=dd.ap, in0=m2.ap, in1=m1.ap, op=ALU.subtract), reads=[m1, m2], writes=[dd])
                    k.op("ACT", lambda e: e.activation(out=ex.ap, in_=dd.ap, func=AF.Exp), reads=[dd], writes=[ex])
                    k.op("DVE", lambda e: e.tensor_scalar(out=w1.ap, in0=ex.ap, scalar1=1.0, scalar2=None, op0=ALU.add), reads=[ex], writes=[w1])
                    k.op("DVE", lambda e: e.reciprocal(out=w1.ap, in_=w1.ap), reads=[w1], writes=[w1])
                    k.op("DVE", lambda e: e.tensor_tensor(out=w2.ap, in0=ex.ap, in1=w1.ap, op=ALU.mult), reads=[ex, w1], writes=[w2])
                    k.op("DVE", lambda e: e.tensor_tensor(out=w1.ap, in0=w1.ap, in1=gw.ap, op=ALU.mult), reads=[w1, gw], writes=[w1])
                    k.op("DVE", lambda e: e.tensor_tensor(out=w2.ap, in0=w2.ap, in1=gw.ap, op=ALU.mult), reads=[w2, gw], writes=[w2])
                    k.op("DVE", lambda e: e.tensor_scalar(out=r_ew.ap, in0=r_oh1.ap, scalar1=w1.ap, scalar2=None, op0=ALU.mult), reads=[r_oh1, w1], writes=[r_ew])
                    k.op("DVE", lambda e: e.scalar_tensor_tensor(out=r_ew.ap, in0=r_oh2.ap, scalar=w2.ap, in1=r_ew.ap, op0=ALU.mult, op1=ALU.add), reads=[r_oh2, w2, r_ew], writes=[r_ew])
                    k.op("DVE", lambda e: e.tensor_tensor(out=v3(r_G.ap, 8), in0=r_goh.ap.unsqueeze(2).to_broadcast([128, 8, 8]),
                                                          in1=r_ew.ap.unsqueeze(1).to_broadcast([128, 8, 8]), op=ALU.mult), reads=[r_goh, r_ew], writes=[r_G])
                    ptr = PD[b % 2]
                    k.op("PE", lambda e: e.transpose(ptr.ap[0:64, 0:128], r_G.ap, ident.ap), reads=[r_G, ident], writes=[ptr])
                    k.op("DVE", lambda e: e.tensor_copy(out=GT.ap[:, bs], in_=ptr.ap[0:64, 0:128]), reads=[ptr], writes=[GT])
            def load_expert(e_):
                wb = WB[e_ % 2]
                k.dma("POOL", v3(wb.ap[:, 0:4096], 8), wg[e_].rearrange("(a p) n -> p a n", p=128), writes=[wb])
                k.dma("POOL", v3(wb.ap[:, 4096:8192], 8), wu[e_].rearrange("(a p) n -> p a n", p=128), writes=[wb], part=True)
                k.dma("POOL", v3(wb.ap[:, 8192:12288], 4), wd[e_].rearrange("(a p) n -> p a n", p=128), writes=[wb], part=True)

            def emit_gu(e_, t, i):
                wb = WB[e_ % 2]
                Wg3 = v3(wb.ap[:, 0:4096], 8)
                Wu3 = v3(wb.ap[:, 4096:8192], 8)
                pb = PBK[i % 2]
                eb = EB[i % 2]
                k.op("DVE", lambda e: e.tensor_copy(out=eb.ap, in_=ident.ap[0:64, e_:e_ + 1].to_broadcast([64, 128])), reads=[ident], writes=[eb])
                k.op("PE", lambda e: e.matmul(pb.ap, eb.ap, GT.ap[:, t * 512:(t + 1) * 512], start=True, stop=True), reads=[eb, GT], writes=[pb])
                for f in range(4):
                    pg, pu = PG[f % 2], PU[f % 2]
                    for kc in range(8):
                        k.op("PE", lambda e: e.matmul(pg.ap, Wg3[:, kc, f * 128:(f + 1) * 128], UT[t].ap[:, kc, :], start=(kc == 0), stop=(kc == 7)),
                             reads=[wb, UT[t]], writes=[pg], pe_acc=(kc > 0))
                    for kc in range(8):
                        k.op("PE", lambda e: e.matmul(pu.ap, Wu3[:, kc, f * 128:(f + 1) * 128], UT[t].ap[:, kc, :], start=(kc == 0), stop=(kc == 7)),
                             reads=[wb, UT[t]], writes=[pu], pe_acc=(kc > 0))
                    s_, tt = S_[f % 2], TT[f % 2]
                    k.op("ACT", lambda e: e.activation(out=s_.ap, in_=pg.ap, func=AF.Silu), reads=[pg], writes=[s_])
                    k.op("DVE", lambda e: e.tensor_tensor(out=tt.ap, in0=s_.ap, in1=pu.ap, op=ALU.mult), reads=[s_, pu], writes=[tt])
                    k.op("DVE", lambda e: e.tensor_tensor(out=HS[i % 2][f].ap, in0=tt.ap, in1=pb.ap, op=ALU.mult), reads=[tt, pb], writes=[HS[i % 2][f]])

            def emit_dn(e_, t, i):
                wb = WB[e_ % 2]
                Wd3 = v3(wb.ap[:, 8192:12288], 4)
                for d in range(8):
                    pd = PD[d % 2]
                    for f in range(4):
                        k.op("PE", lambda e: e.matmul(pd.ap, Wd3[:, f, d * 128:(d + 1) * 128], HS[i % 2][f].ap, start=(f == 0), stop=(f == 3)),
                             reads=[wb, HS[i % 2][f]], writes=[pd], pe_acc=(f > 0))
                    k.op("DVE", lambda e: e.tensor_tensor(out=HT[t][d].ap, in0=HT[t][d].ap, in1=pd.ap, op=ALU.add), reads=[pd, HT[t][d]], writes=[HT[t][d]])

            seq = [(e_, t) for e_ in range(NE) for t in range(NTL)]
            load_expert(0)
            prev = None
            for i, (e_, t) in enumerate(seq):
                emit_gu(e_, t, i)
                if prev is not None:
                    emit_dn(*prev)
                if t == 0 and e_ + 1 < NE:
                    load_expert(e_ + 1)
                prev = (e_, t, i)
            emit_dn(*prev)
            k.dma("POOL", v3(WB[0].ap[:, 0:8192], 8), plg.rearrange("(a p) n -> p a n", p=128), writes=[WB[0]])
            k.dma("POOL", v3(WB[1].ap[:, 0:2048], 2), plp.rearrange("(a p) n -> p a n", p=128), writes=[WB[1]])
            for t in range(NTL):
                cs = slice(c0 + t * 512, c0 + (t + 1) * 512)
                hap = h3[:, :, t * 512:(t + 1) * 512]
                k.dma("POOL", v3(PB.ap, 2), pT[:, cs].rearrange("(a p) n -> p a n", p=128), writes=[PB])
                rmsnorm(HT[t], hap, 8, UT[t], UT[t].ap, PBK[0])
                for d in range(8):
                    pg, pu = PG[d % 2], PU[d % 2]
                    for kc in range(8):
                        k.op("PE", lambda e: e.matmul(pg.ap, v3(WB[0].ap[:, 0:8192], 8)[:, kc, d * 128:(d + 1) * 128], UT[t].ap[:, kc, :],
                                                      start=(kc == 0), stop=(kc == 7)), reads=[WB[0], UT[t]], writes=[pg], pe_acc=(kc > 0))
                    for kc in range(2):
                        k.op("PE", lambda e: e.matmul(pu.ap, v3(WB[1].ap[:, 0:2048], 2)[:, kc, d * 128:(d + 1) * 128], v3(PB.ap, 2)[:, kc, :],
                                                      start=(kc == 0), stop=(kc == 1)), reads=[WB[1], PB], writes=[pu], pe_acc=(kc > 0))
                    sf = STF[d % 2]
                    k.op("ACT", lambda e: e.activation(out=sf.ap, in_=pg.ap, func=AF.Sigmoid, bias=vec.ap[:, 16 + d:17 + d], scale=1.0), reads=[pg, vec], writes=[sf])
                    k.op("DVE", lambda e: e.tensor_tensor(out=sf.ap, in0=sf.ap, in1=pu.ap, op=ALU.mult), reads=[sf, pu], writes=[sf])
                    k.op("DVE", lambda e: e.tensor_tensor(out=HT[t][d].ap, in0=HT[t][d].ap, in1=sf.ap, op=ALU.add), reads=[sf, HT[t][d]], writes=[HT[t][d]])
            if mode == "A":
                k.dma("POOL", v3(WB[0].ap, 8), wqkv[:, 0:1536].rearrange("(a p) n -> p a n", p=128), writes=[WB[0]])
                k.dma("POOL", v3(WB[1].ap, 8), wqkv[:, 1536:3072].rearrange("(a p) n -> p a n", p=128), writes=[WB[1]])
                for t in range(NTL):
                    cs = slice(c0 + t * 512, c0 + (t + 1) * 512)
                    hap = h3[:, :, t * 512:(t + 1) * 512]
                    for d in range(8):
                        k.dma("SP", hT_o[d * 128:(d + 1) * 128, cs], HT[t][d].ap, reads=[HT[t][d]], writes=[ho_t], part=True)
                    rmsnorm(HT[t], hap, 24, UT[t], UT[t].ap, PBK[0])
                    U3 = UT[t].ap
                    for n in range(16):
                        col = n * 128
                        wbt = WB[0] if col < 1536 else WB[1]
                        lc = col if col < 1536 else col - 1536
                        bank = PG[n % 2]
                        for kc in range(8):
                            k.op("PE", lambda e: e.matmul(bank.ap, v3(wbt.ap, 8)[:, kc, lc:lc + 128], U3[:, kc, :], start=(kc == 0), stop=(kc == 7)),
                                 reads=[wbt, UT[t]], writes=[bank], pe_acc=(kc > 0))
                        sg = STG[n % 2]
                        sc = (1.0 / np.sqrt(128.0)) if n < 8 else 1.0
                        k.op("ACT", lambda e: e.activation(out=sg.ap, in_=bank.ap, func=AF.Identity, scale=float(sc)), reads=[bank], writes=[sg])
                        if n < 8:
                            k.dma("SP", qT_o[n * 128:(n + 1) * 128, cs], sg.ap, reads=[sg], writes=[qT_t], part=True)
                        else:
                            k.dma("SP", kT_o[(n - 8) * 128:(n - 7) * 128, cs], sg.ap, reads=[sg], writes=[kT_t], part=True)
                    for b in range(4):
                        for hh in range(2):
                            bank = PU[hh]
                            for kc in range(8):
                                k.op("PE", lambda e: e.matmul(bank.ap, U3[:, kc, b * 128:(b + 1) * 128], v3(WB[1].ap, 8)[:, kc, 512 + hh * 512:1024 + hh * 512],
                                                              start=(kc == 0), stop=(kc == 7)), reads=[WB[1], UT[t]], writes=[bank], pe_acc=(kc > 0))
                            sg = STG[hh]
                            k.op("ACT", lambda e: e.activation(out=sg.ap, in_=bank.ap, func=AF.Copy), reads=[bank], writes=[sg])
                            r0 = c0 + t * 512 + b * 128
                            k.dma("SP", v_o[r0:r0 + 128, hh * 512:(hh + 1) * 512], sg.ap, reads=[sg], writes=[v_t], part=True)
                fin = [qT_t, kT_t, v_t, ho_t]
            else:
                for t in range(NTL):
                    cs = slice(c0 + t * 512, c0 + (t + 1) * 512)
                    hap = h3[:, :, t * 512:(t + 1) * 512]
                    k.op("ACT", lambda e: e.activation(out=v3(SQ.ap, 8), in_=hap, func=AF.Square), reads=HT[t], writes=[SQ])
                    bank = PBK[0]
                    for kc in range(8):
                        k.op("PE", lambda e: e.matmul(bank.ap, ones_bf.ap, v3(SQ.ap, 8)[:, kc, :], start=(kc == 0), stop=(kc == 7)),
                             reads=[SQ, ones_bf], writes=[bank], pe_acc=(kc > 0))
                    k.op("ACT", lambda e: e.activation(out=LNV.ap, in_=bank.ap, func=AF.Ln, scale=1.0 / 1024, bias=EPS), reads=[bank], writes=[LNV])
                    k.op("ACT", lambda e: e.activation(out=RSTD.ap, in_=LNV.ap, func=AF.Exp, scale=-0.5), reads=[LNV], writes=[RSTD])
                    for d in range(8):
                        k.op("DVE", lambda e: e.scalar_tensor_tensor(out=HT[t][d].ap, in0=HT[t][d].ap, scalar=vec.ap[:, 24 + d:25 + d], in1=RSTD.ap,
                                                                     op0=ALU.mult, op1=ALU.mult), reads=[HT[t][d], RSTD, vec], writes=[HT[t][d]])
                        k.dma("SP", out_o[d * 128:(d + 1) * 128, cs], HT[t][d].ap, reads=[HT[t][d]], writes=[out_t], part=True)
                fin = [out_t]
            if hf == NH - 1:
                k.finish("SP", fin)
        print("tok ninst", k.ninst)
    return nc


def build_attn(L, NHD=2):
    nc = bass.Bass("TRN2", target_bir_lowering=False)

    def din(name, shape, dt=F32):
        return nc.dram_tensor(name, list(shape), dt, kind="ExternalInput").ap()

    qT = din("qT", [NHD, 128, L], BF16)
    kT = din("kT", [NHD, 128, L], BF16)
    vP = din("vP", [NHD, 128, L // 128, 128], BF16)
    cst = din("cst", [128, 256 + 4 * 512])
    oT = nc.dram_tensor("oT", [NHD, 128, L], BF16, kind="ExternalOutput").ap()
    NQT = L // 512
    NB = L // 128
    with ExitStack() as st:
        k = K(nc, st)
        Qs = k.sbt([128, L], BF16, "Qs")
        Ks = k.sbt([128, L], BF16, "Ks")
        Vs = k.sbt([128, L], BF16, "Vs")
        V3 = v3(Vs.ap, NB)
        cf = k.sbt([128, 256 + 2048], F32, "cf")
        trin = k.sbt([128, 128], BF16, "trin")
        onen = k.sbt([128, 128], BF16, "onen")
        m01 = k.sbt([128, 2048], BF16, "m01")
        mneg = k.sbt([128, 2048], F32, "mneg")
        E_ = [k.sbt([128, 512], F32, "E%d" % i) for i in range(2)]
        SPf = [k.sbt([128, 512], BF16, "SPf%d" % i) for i in range(2)]
        SPm = [k.sbt([128, 512], BF16, "SPm%d" % i) for i in range(2)]
        TMP = [k.sbt([128, 512], F32, "TMP%d" % i) for i in range(2)]
        AT = [k.sbt([128, 512], BF16, "AT%d" % i) for i in range(2)]
        OFF = k.sbt([128, 512], F32, "OFF")
        OST = [k.sbt([128, 512], BF16, "OST%d" % i) for i in range(2)]
        BK = [T(k.ps([128, 512], F32, "bk%d" % i)[:]) for i in range(8)]
        PA, PBb, PO = BK[0:2], BK[2:4], BK[4:6]
        oT_t = T(oT)

        k.dma("SP", cf.ap, cst, writes=[cf])
        k.op("DVE", lambda e: e.tensor_copy(out=trin.ap, in_=cf.ap[:, 0:128]), reads=[cf], writes=[trin])
        k.op("DVE", lambda e: e.tensor_copy(out=onen.ap, in_=cf.ap[:, 128:256]), reads=[cf], writes=[onen])
        k.op("DVE", lambda e: e.tensor_copy(out=m01.ap, in_=cf.ap[:, 256:2304]), reads=[cf], writes=[m01])
        k.op("DVE", lambda e: e.tensor_scalar(out=mneg.ap, in0=cf.ap[:, 256:2304], scalar1=-1.0, scalar2=30000.0, op0=ALU.add, op1=ALU.mult),
             reads=[cf], writes=[mneg])

        jobs = []
        for hd in range(NHD):
            for J in range(NQT):
                nb = 4 * J + 4
                for i, n in enumerate(range(nb - 1, -1, -1)):
                    jobs.append(dict(hd=hd, J=J, n=n, first=(i == 0), last=(n == 0), r=(n - 4 * J) if n >= 4 * J else -1))

        def load_head(hd):
            k.dma("SP", Qs.ap, qT[hd], writes=[Qs])
            k.dma("SP", Ks.ap, kT[hd], writes=[Ks])
            k.dma("SP", V3, vP[hd], writes=[Vs])

        def s1(j, i):
            if j["first"] and j["J"] == 0:
                load_head(j["hd"])
            A = PA[i % 2]
            qs = slice(j["J"] * 512, (j["J"] + 1) * 512)
            ks = slice(j["n"] * 128, (j["n"] + 1) * 128)
            k.op("PE", lambda e: e.matmul(A.ap, Ks.ap[:, ks], Qs.ap[:, qs], start=True, stop=False), reads=[Ks, Qs], writes=[A])
            k.op("ACT", lambda e: e.activation(out=E_[i % 2].ap, in_=A.ap, func=AF.Exp), reads=[A], writes=[E_[i % 2]])
            k.op("ACT", lambda e: e.activation(out=SPf[i % 2].ap, in_=E_[i % 2].ap, func=AF.Ln, bias=1.0, scale=1.0), reads=[E_[i % 2]], writes=[SPf[i % 2]])
            if j["r"] >= 0:
                r = j["r"]
                k.op("POOL", lambda e: e.tensor_tensor(out=SPm[i % 2].ap, in0=SPf[i % 2].ap, in1=m01.ap[:, r * 512:(r + 1) * 512], op=ALU.mult),
                     reads=[SPf[i % 2], m01], writes=[SPm[i % 2]])

        def s2(j, i):
            A, B = PA[i % 2], PBb[i % 2]
            sp = SPm[i % 2] if j["r"] >= 0 else SPf[i % 2]
            if j["first"]:
                k.op("DVE", lambda e: e.memset(OFF.ap, 0.0), writes=[OFF])
            k.op("PE", lambda e: e.matmul(A.ap, trin.ap, sp.ap, start=False, stop=True), reads=[trin, sp], writes=[A], pe_acc=True)
            k.op("PE", lambda e: e.matmul(B.ap, onen.ap, sp.ap, start=True, stop=True), reads=[onen, sp], writes=[B])
            tm = TMP[i % 2]
            k.op("DVE", lambda e: e.tensor_tensor(out=tm.ap, in0=A.ap, in1=OFF.ap, op=ALU.add), reads=[A, OFF], writes=[tm])
            if j["r"] >= 0:
                r = j["r"]
                k.op("DVE", lambda e: e.tensor_tensor(out=tm.ap, in0=tm.ap, in1=mneg.ap[:, r * 512:(r + 1) * 512], op=ALU.add), reads=[tm, mneg], writes=[tm])
            k.op("ACT", lambda e: e.activation(out=AT[i % 2].ap, in_=tm.ap, func=AF.Exp), reads=[tm], writes=[AT[i % 2]])
            if not j["last"]:
                k.op("DVE", lambda e: e.tensor_tensor(out=OFF.ap, in0=OFF.ap, in1=B.ap, op=ALU.add), reads=[B, OFF], writes=[OFF])

        grp = [0]

        def s3(j, i):
            O = PO[grp[0] % 2]
            k.op("PE", lambda e: e.matmul(O.ap, V3[:, j["n"], :], AT[i % 2].ap, start=j["first"], stop=j["last"]),
                 reads=[Vs, AT[i % 2]], writes=[O], pe_acc=(not j["first"]))
            if j["last"]:
                og = OST[grp[0] % 2]
                k.op("ACT", lambda e: e.activation(out=og.ap, in_=O.ap, func=AF.Copy), reads=[O], writes=[og])
                k.dma("SP", oT[j["hd"], :, j["J"] * 512:(j["J"] + 1) * 512], og.ap, reads=[og], writes=[oT_t], part=True)
                grp[0] += 1

        n = len(jobs)
        for i in range(n + 2):
            if i < n:
                if jobs[i]["first"] and jobs[i]["J"] == 0 and i > 0:
                    s2(jobs[i - 1], i - 1)
                    s3(jobs[i - 2], i - 2)
                    s3(jobs[i - 1], i - 1)
                    s1(jobs[i], i)
                    jobs[i - 1]["done"] = True
                    jobs[i - 2]["done3"] = True
                    jobs[i - 1]["done3"] = True
                    continue
                s1(jobs[i], i)
            if 0 <= i - 1 < n and not jobs[i - 1].get("done"):
                s2(jobs[i - 1], i - 1)
            if 0 <= i - 2 < n and not jobs[i - 2].get("done3"):
                s3(jobs[i - 2], i - 2)
        k.finish("SP", [oT_t])
        print("attn ninst", k.ninst, "jobs", n)
    return nc


def attn_consts():
    s = np.arange(128)
    tri_neg = -(s[:, None] >= s[None, :]).astype(np.float32)
    ones_neg = -np.ones((128, 128), np.float32)
    q = np.arange(512)
    masks = [(q[None, :] > (s[:, None] + 128 * r)).astype(np.float32) for r in range(4)]
    return np.ascontiguousarray(np.concatenate([tri_neg, ones_neg] + masks, axis=1))


def build_ssd(L):
    nc = bass.Bass("TRN2", target_bir_lowering=False)

    def din(name, shape, dt=F32):
        return nc.dram_tensor(name, list(shape), dt, kind="ExternalInput").ap()

    xT = din("xT", [1024, L])
    w_in = din("w_in", [1024, 1288])
    vecs = din("vecs", [128, 38])
    rowc = din("rowc", [1, 1040])
    cst = din("cst", [128, 640])
    ynT = nc.dram_tensor("ynT", [512, L], BF16, kind="ExternalOutput").ap()
    NTL = L // 512
    with ExitStack() as st:
        k = K(nc, st)
        W = k.sbt([128, 8 * 1288], BF16, "W")
        W3 = v3(W.ap, 8)
        XT = [k.sbt([128, 8 * 512], F32, "XT%d" % i) for i in range(2)]
        UT = [k.sbt([128, 8 * 512], BF16, "UT%d" % i) for i in range(2)]
        SQ = k.sbt([128, 8 * 512], BF16, "SQ")
        LNV = k.sbt([128, 512], F32, "LNV")
        RSTD = k.sbt([128, 512], F32, "RSTD")
        vec = k.sbt([128, 38], F32, "vec_sb")
        bc = k.sbt([128, 1040], F32, "bc")
        cf = k.sbt([128, 640], F32, "cf")
        identb = k.sbt([128, 128], BF16, "identb")
        ones_bf = k.sbt([128, 128], BF16, "ones_bf")
        A_bc = k.sbt([128, 8], F32, "A_bc")
        XR = [k.sbt([128, 515], F32, "XR%d" % i) for i in range(6)]
        ACC = [k.sbt([128, 512], F32, "ACC%d" % i) for i in range(2)]
        XC = [k.sbt([128, 6 * 512], BF16, "XC%d" % i) for i in range(2)]
        ZS_2 = [k.sbt([128, 512], F32, "ZS_%d" % i) for i in range(2)]
        DTt_2 = [k.sbt([128, 8], F32, "DTt_%d" % i) for i in range(2)]
        DT_2 = [k.sbt([128, 8], F32, "DT_%d" % i) for i in range(2)]
        AA_2 = [k.sbt([128, 8], F32, "AA_%d" % i) for i in range(2)]
        EXPS_2 = [k.sbt([128, 24], F32, "EXPS_%d" % i) for i in range(2)]
        LH = [k.sbt([128, 128], F32, "LH%d" % i) for i in range(2)]
        DEC_2 = [k.sbt([128, 1024], F32, "DEC_%d" % i) for i in range(2)]
        CBm_2 = [k.sbt([128, 128], F32, "CBm_%d" % i) for i in range(2)]
        WT_2 = [k.sbt([128, 1024], BF16, "WT_%d" % i) for i in range(2)]
        XTOK_2 = [k.sbt([128, 512], BF16, "XTOK_%d" % i) for i in range(2)]
        BTOK_2 = [k.sbt([128, 128], BF16, "BTOK_%d" % i) for i in range(2)]
        XDT_2 = [k.sbt([128, 512], BF16, "XDT_%d" % i) for i in range(2)]
        XW_2 = [k.sbt([128, 512], BF16, "XW_%d" % i) for i in range(2)]
        Y1_2 = [k.sbt([128, 512], F32, "Y1_%d" % i) for i in range(2)]
        Y2_2 = [k.sbt([128, 512], F32, "Y2_%d" % i) for i in range(2)]
        YZ_2 = [k.sbt([128, 512], F32, "YZ_%d" % i) for i in range(2)]
        YSQ_2 = [k.sbt([128, 512], F32, "YSQ_%d" % i) for i in range(2)]
        YN_2 = [k.sbt([128, 512], BF16, "YN_%d" % i) for i in range(2)]
        SF = k.sbt([128, 512], F32, "SF")
        SBF = k.sbt([128, 512], BF16, "SBF")
        sc_2 = [[k.sbt([128, 1], F32, "sc%d_%d" % (j, i)) for i in range(3)] for j in range(2)]
        LH4 = [k.sbt([128, 128], F32, "LHx%d" % i) for i in range(2)]
        YNT = [k.sbt([128, 4 * 512], BF16, "YNT%d" % i) for i in range(2)]
        BK = [T(k.ps([128, 512], F32, "bk%d" % i)[:]) for i in range(8)]
        P_proj, P_st, P_sm, P_sg0, P_sg1, P_tr, P_yd, P_yo = BK
        ynT_t = T(ynT)

        k.dma("SP", vec.ap, vecs, writes=[vec])
        k.dma("SP", cf.ap, cst, writes=[cf])
        k.dma("SP", bc.ap, rowc.partition_broadcast(128).rearrange("p o n -> p (o n)"), writes=[bc])
        k.dma("POOL", W3, w_in.rearrange("(a p) n -> p a n", p=128), writes=[W])
        ident = cf.ap[:, 0:128]
        tri = cf.ap[:, 128:256]
        Um = cf.ap[:, 256:384]
        maskU = cf.ap[:, 384:512]
        onesf = cf.ap[:, 512:640]
        k.op("DVE", lambda e: e.tensor_copy(out=identb.ap, in_=ident), reads=[cf], writes=[identb])
        k.op("DVE", lambda e: e.memset(ones_bf.ap, 1.0), writes=[ones_bf])
        k.op("ACT", lambda e: e.activation(out=A_bc.ap, in_=bc.ap[:, 8:16], func=AF.Exp), reads=[bc], writes=[A_bc])
        k.op("DVE", lambda e: e.tensor_scalar(out=A_bc.ap, in0=A_bc.ap, scalar1=-1.0, scalar2=None, op0=ALU.mult), reads=[A_bc], writes=[A_bc])
        for cc in range(6):
            k.op("DVE", lambda e: e.memset(XR[cc].ap[:, 0:3], 0.0), writes=[XR[cc]])
        k.op("DVE", lambda e: e.memset(SF.ap, 0.0), writes=[SF])
        k.op("DVE", lambda e: e.memset(SBF.ap, 0.0), writes=[SBF])
        dtb, Dx, gn = bc.ap[:, 0:8], bc.ap[:, 16:528], bc.ap[:, 528:1040]

        def load_x(t):
            k.dma("SP", v3(XT[t % 2].ap, 8), xT[:, t * 512:(t + 1) * 512].rearrange("(a p) n -> p a n", p=128), writes=[XT[t % 2]])

        load_x(0)
        for t in range(NTL):
            if t + 1 < NTL:
                load_x(t + 1)
            xt, ut, xc = XT[t % 2], UT[t % 2], XC[t % 2]
            x3, u3, xc3 = v3(xt.ap, 8), v3(ut.ap, 8), v3(xc.ap, 6)
            k.op("ACT", lambda e: e.activation(out=v3(SQ.ap, 8), in_=x3, func=AF.Square), reads=[xt], writes=[SQ])
            for kc in range(8):
                k.op("PE", lambda e: e.matmul(P_proj.ap, ones_bf.ap, v3(SQ.ap, 8)[:, kc, :], start=(kc == 0), stop=(kc == 7)),
                     reads=[SQ, ones_bf], writes=[P_proj], pe_acc=(kc > 0))
            k.op("ACT", lambda e: e.activation(out=LNV.ap, in_=P_proj.ap, func=AF.Ln, scale=1.0 / 1024, bias=EPS), reads=[P_proj], writes=[LNV])
            k.op("ACT", lambda e: e.activation(out=RSTD.ap, in_=LNV.ap, func=AF.Exp, scale=-0.5), reads=[LNV], writes=[RSTD])
            for kc in range(8):
                k.op("DVE", lambda e: e.scalar_tensor_tensor(out=u3[:, kc, :], in0=x3[:, kc, :], scalar=vec.ap[:, kc:kc + 1], in1=RSTD.ap,
                                                             op0=ALU.mult, op1=ALU.mult), reads=[xt, RSTD, vec], writes=[ut])
            for cc in range(6):
                for kc in range(8):
                    k.op("PE", lambda e: e.matmul(P_proj.ap, W3[:, kc, 512 + cc * 128:512 + (cc + 1) * 128], u3[:, kc, :], start=(kc == 0), stop=(kc == 7)),
                         reads=[W, ut], writes=[P_proj], pe_acc=(kc > 0))
                xr, acc = XR[cc], ACC[cc % 2]
                k.op("ACT", lambda e: e.activation(out=xr.ap[:, 3:515], in_=P_proj.ap, func=AF.Copy), reads=[P_proj], writes=[xr])
                k.op("DVE", lambda e: e.tensor_scalar(out=acc.ap, in0=xr.ap[:, 0:512], scalar1=vec.ap[:, 8 + cc * 4:9 + cc * 4], scalar2=None, op0=ALU.mult),
                     reads=[xr, vec], writes=[acc])
                for kk in range(1, 4):
                    k.op("DVE", lambda e: e.scalar_tensor_tensor(out=acc.ap, in0=xr.ap[:, kk:kk + 512], scalar=vec.ap[:, 8 + cc * 4 + kk:9 + cc * 4 + kk],
                                                                 in1=acc.ap, op0=ALU.mult, op1=ALU.add), reads=[xr, vec, acc], writes=[acc])
                k.op("ACT", lambda e: e.activation(out=xc3[:, cc, :], in_=acc.ap, func=AF.Silu, bias=vec.ap[:, 32 + cc:33 + cc], scale=1.0),
                     reads=[acc, vec], writes=[xc])
                k.op("DVE", lambda e: e.tensor_copy(out=xr.ap[:, 0:3], in_=xr.ap[:, 512:515]), reads=[xr], writes=[xr])
            ynt = YNT[t % 2]
            for q in range(4):
                cs = slice(q * 128, (q + 1) * 128)
                ci = t * 4 + q
                ZS = ZS_2[ci % 2]
                DTt = DTt_2[ci % 2]
                DT = DT_2[ci % 2]
                AA = AA_2[ci % 2]
                EXPS = EXPS_2[ci % 2]
                DEC = DEC_2[ci % 2]
                CBm = CBm_2[ci % 2]
                WT = WT_2[ci % 2]
                XTOK = XTOK_2[ci % 2]
                BTOK = BTOK_2[ci % 2]
                XDT = XDT_2[ci % 2]
                XW = XW_2[ci % 2]
                Y1 = Y1_2[ci % 2]
                Y2 = Y2_2[ci % 2]
                YZ = YZ_2[ci % 2]
                YSQ = YSQ_2[ci % 2]
                YN = YN_2[ci % 2]
                sc = sc_2[ci % 2]
                for kc in range(8):
                    k.op("PE", lambda e: e.matmul(P_proj.ap, u3[:, kc, cs], W3[:, kc, 0:512], start=(kc == 0), stop=(kc == 7)),
                         reads=[W, ut], writes=[P_proj], pe_acc=(kc > 0))
                k.op("ACT", lambda e: e.activation(out=ZS.ap, in_=P_proj.ap, func=AF.Silu), reads=[P_proj], writes=[ZS])
                for kc in range(8):
                    k.op("PE", lambda e: e.matmul(P_sm.ap[:, 0:8], u3[:, kc, cs], W3[:, kc, 1280:1288], start=(kc == 0), stop=(kc == 7)),
                         reads=[W, ut], writes=[P_sm], pe_acc=(kc > 0))
                k.op("DVE", lambda e: e.tensor_tensor(out=DTt.ap, in0=P_sm.ap[:, 0:8], in1=dtb, op=ALU.add), reads=[P_sm, bc], writes=[DTt])
                k.op("ACT", lambda e: e.activation(out=DTt.ap, in_=DTt.ap, func=AF.Exp), reads=[DTt], writes=[DTt])
                k.op("ACT", lambda e: e.activation(out=DT.ap, in_=DTt.ap, func=AF.Ln, bias=1.0, scale=1.0), reads=[DTt], writes=[DT])
                k.op("DVE", lambda e: e.tensor_tensor(out=AA.ap, in0=DT.ap, in1=A_bc.ap, op=ALU.mult), reads=[DT, A_bc], writes=[AA])
                trb = P_tr.ap.bitcast(BF16)
                for cc in range(4):
                    k.op("PE", lambda e: e.transpose(trb[:, cc * 128:(cc + 1) * 128], xc3[:, cc, cs], identb.ap), reads=[xc, identb], writes=[P_tr])
                k.op("PE", lambda e: e.transpose(trb[:, 512:640], xc3[:, 4, cs], identb.ap), reads=[xc, identb], writes=[P_tr])
                k.op("ACT", lambda e: e.activation(out=XTOK.ap, in_=trb[:, 0:512], func=AF.Copy), reads=[P_tr], writes=[XTOK])
                k.op("ACT", lambda e: e.activation(out=BTOK.ap, in_=trb[:, 512:640], func=AF.Copy), reads=[P_tr], writes=[BTOK])
                k.op("PE", lambda e: e.matmul(P_sm.ap[:, 32:40], tri, AA.ap, start=True, stop=True), reads=[cf, AA], writes=[P_sm])
                k.op("PE", lambda e: e.matmul(P_sm.ap[:, 40:48], Um, AA.ap, start=True, stop=True), reads=[cf, AA], writes=[P_sm])
                k.op("PE", lambda e: e.matmul(P_sm.ap[:, 48:56], onesf, AA.ap, start=True, stop=True), reads=[cf, AA], writes=[P_sm])
                k.op("ACT", lambda e: e.activation(out=EXPS.ap, in_=P_sm.ap[:, 32:56], func=AF.Exp), reads=[P_sm], writes=[EXPS])
                e_l, dte, cd = EXPS.ap[:, 0:8], EXPS.ap[:, 8:16], EXPS.ap[:, 16:24]
                k.op("PE", lambda e: e.matmul(P_sm.ap[:, 128:256], xc3[:, 4, cs], xc3[:, 5, cs], start=True, stop=True), reads=[xc], writes=[P_sm])
                k.op("DVE", lambda e: e.tensor_tensor(out=CBm.ap, in0=P_sm.ap[:, 128:256], in1=maskU, op=ALU.mult), reads=[P_sm, cf], writes=[CBm])
                for hh in range(8):
                    lh = (LH + LH4)[hh % 4]
                    k.op("DVE", lambda e: e.tensor_scalar(out=lh.ap, in0=Um, scalar1=AA.ap[:, hh:hh + 1], scalar2=None, op0=ALU.mult), reads=[cf, AA], writes=[lh])
                    bank = P_sg0 if hh < 4 else P_sg1
                    k.op("PE", lambda e: e.matmul(bank.ap[:, (hh % 4) * 128:(hh % 4 + 1) * 128], lh.ap, tri, start=True, stop=True), reads=[lh, cf], writes=[bank])
                k.op("ACT", lambda e: e.activation(out=DEC.ap[:, 0:512], in_=P_sg0.ap, func=AF.Exp), reads=[P_sg0], writes=[DEC])
                k.op("ACT", lambda e: e.activation(out=DEC.ap[:, 512:1024], in_=P_sg1.ap, func=AF.Exp), reads=[P_sg1], writes=[DEC])
                k.op("DVE", lambda e: e.tensor_tensor(out=v3(WT.ap, 8), in0=v3(DEC.ap, 8), in1=CBm.ap.unsqueeze(1).to_broadcast([128, 8, 128]), op=ALU.mult),
                     reads=[DEC, CBm], writes=[WT])
                k.op("DVE", lambda e: e.tensor_tensor(out=v3(XDT.ap, 8), in0=v3(XTOK.ap, 8), in1=DT.ap.unsqueeze(2).to_broadcast([128, 8, 64]), op=ALU.mult),
                     reads=[XTOK, DT], writes=[XDT])
                k.op("DVE", lambda e: e.tensor_tensor(out=v3(XW.ap, 8), in0=v3(XDT.ap, 8), in1=dte.unsqueeze(2).to_broadcast([128, 8, 64]), op=ALU.mult),
                     reads=[XDT, EXPS], writes=[XW])
                for hh in range(8):
                    k.op("PE", lambda e: e.matmul(P_yd.ap[:, hh * 64:(hh + 1) * 64], v3(WT.ap, 8)[:, hh, :], v3(XDT.ap, 8)[:, hh, :], start=True, stop=True),
                         reads=[WT, XDT], writes=[P_yd])
                k.op("PE", lambda e: e.matmul(P_yo.ap, xc3[:, 5, cs], SBF.ap, start=True, stop=True), reads=[xc, SBF], writes=[P_yo])
                k.op("DVE", lambda e: e.tensor_tensor(out=v3(Y1.ap, 8), in0=v3(P_yo.ap, 8), in1=e_l.unsqueeze(2).to_broadcast([128, 8, 64]), op=ALU.mult),
                     reads=[P_yo, EXPS], writes=[Y1])
                k.op("DVE", lambda e: e.tensor_tensor(out=Y1.ap, in0=Y1.ap, in1=P_yd.ap, op=ALU.add), reads=[Y1, P_yd], writes=[Y1])
                k.op("PE", lambda e: e.matmul(P_st.ap, BTOK.ap, XW.ap, start=True, stop=True), reads=[BTOK, XW], writes=[P_st])
                k.op("DVE", lambda e: e.tensor_tensor(out=v3(SF.ap, 8), in0=v3(SF.ap, 8), in1=cd.unsqueeze(2).to_broadcast([128, 8, 64]), op=ALU.mult),
                     reads=[SF, EXPS], writes=[SF])
                k.op("DVE", lambda e: e.tensor_tensor(out=SF.ap, in0=SF.ap, in1=P_st.ap, op=ALU.add), reads=[SF, P_st], writes=[SF])
                k.op("ACT", lambda e: e.activation(out=SBF.ap, in_=SF.ap, func=AF.Copy), reads=[SF], writes=[SBF])
                k.op("DVE", lambda e: e.tensor_tensor(out=Y2.ap, in0=XTOK.ap, in1=Dx, op=ALU.mult), reads=[XTOK, bc], writes=[Y2])
                k.op("DVE", lambda e: e.tensor_tensor(out=Y2.ap, in0=Y2.ap, in1=Y1.ap, op=ALU.add), reads=[Y2, Y1], writes=[Y2])
                k.op("DVE", lambda e: e.tensor_tensor(out=YZ.ap, in0=Y2.ap, in1=ZS.ap, op=ALU.mult), reads=[Y2, ZS], writes=[YZ])
                k.op("ACT", lambda e: e.activation(out=YSQ.ap, in_=YZ.ap, func=AF.Square, accum_out=sc[0].ap), reads=[YZ], writes=[YSQ, sc[0]])
                k.op("ACT", lambda e: e.activation(out=sc[1].ap, in_=sc[0].ap, func=AF.Ln, scale=1.0 / 512, bias=EPS), reads=[sc[0]], writes=[sc[1]])
                k.op("ACT", lambda e: e.activation(out=sc[2].ap, in_=sc[1].ap, func=AF.Exp, scale=-0.5), reads=[sc[1]], writes=[sc[2]])
                k.op("DVE", lambda e: e.scalar_tensor_tensor(out=YN.ap, in0=YZ.ap, scalar=sc[2].ap, in1=gn, op0=ALU.mult, op1=ALU.mult),
                     reads=[YZ, sc[2], bc], writes=[YN])
                for cc in range(4):
                    k.op("PE", lambda e: e.transpose(trb[:, cc * 128:(cc + 1) * 128], YN.ap[:, cc * 128:(cc + 1) * 128], identb.ap), reads=[YN, identb], writes=[P_tr])
                k.op("ACT", lambda e: e.activation(out=v3(ynt.ap, 4)[:, :, cs], in_=trb[:, 0:512].rearrange("p (a n) -> p a n", a=4), func=AF.Copy),
                     reads=[P_tr], writes=[ynt])
            k.dma("SP", ynT[:, t * 512:(t + 1) * 512].rearrange("(a p) n -> p a n", p=128), v3(ynt.ap, 4), reads=[ynt], writes=[ynT_t], part=True)
        k.finish("SP", [ynT_t])
        print("ssd ninst", k.ninst)
    return nc


def ssd_consts():
    s = np.arange(128)
    ident = np.eye(128, dtype=np.float32)
    tri = (s[:, None] <= s[None, :]).astype(np.float32)
    U = (s[:, None] > s[None, :]).astype(np.float32)
    maskU = (s[None, :] >= s[:, None]).astype(np.float32)
    ones = np.ones((128, 128), np.float32)
    return np.ascontiguousarray(np.concatenate([ident, tri, U, maskU, ones], axis=1))


def ssd_host_inputs(g, ssd_norm, w_in, conv_w, conv_b, dt_bias, a_log, d_skip, gnorm):
    cols = np.concatenate([np.arange(g * 512, (g + 1) * 512), 2048 + np.arange(g * 512, (g + 1) * 512),
                           4096 + np.arange(g * 128, (g + 1) * 128), 4096 + 512 + np.arange(g * 128, (g + 1) * 128),
                           5120 + np.arange(g * 8, (g + 1) * 8)])
    w = np.ascontiguousarray(w_in[:, cols])
    ch = np.concatenate([np.arange(g * 512, (g + 1) * 512), 2048 + np.arange(g * 128, (g + 1) * 128), 2048 + 512 + np.arange(g * 128, (g + 1) * 128)])
    cw = conv_w[:, ch]
    cb = conv_b[ch]
    vecs = np.zeros((128, 38), np.float32)
    vecs[:, 0:8] = ssd_norm.reshape(8, 128).T
    for cc in range(6):
        vecs[:, 8 + cc * 4:12 + cc * 4] = cw[:, cc * 128:(cc + 1) * 128].T
        vecs[:, 32 + cc] = cb[cc * 128:(cc + 1) * 128]
    rowc = np.concatenate([dt_bias[g * 8:(g + 1) * 8], a_log[g * 8:(g + 1) * 8], np.repeat(d_skip[g * 8:(g + 1) * 8], 64),
                           gnorm[g * 512:(g + 1) * 512]])[None, :].astype(np.float32)
    return w, vecs, np.ascontiguousarray(rowc)


_CACHE = {}


def _prog(name, fn, *a):
    key = (name,) + a
    if key not in _CACHE:
        _CACHE[key] = fn(*a)
    return _CACHE[key]


def _vcol(v):
    return np.asarray(v, np.float32).reshape(8, 128).T


def kernel(x, p, ssd_norm, ssd_w_in, ssd_conv_w, ssd_conv_b, ssd_dt_bias, ssd_a_log, ssd_d, ssd_gnorm, ssd_w_out,
           sb_norm, sb_w_qkv, sb_w_o, moe_norm, moe_w_rg, moe_b_rg, moe_w_re, moe_b_re, moe_w_gate, moe_w_up, moe_w_down,
           ple_norm, ple_w_gate, ple_b_gate, ple_w_proj, final_norm):
    f32 = lambda a: np.ascontiguousarray(np.asarray(a, dtype=np.float32))
    x, p = f32(x), f32(p)
    Bsz, L, D = x.shape
    NC = 8
    NT = (Bsz * L) // NC
    PB = NC // Bsz
    cores = list(range(NC))
    ident = np.eye(128, dtype=np.float32)

    nc1 = _prog("ssd", build_ssd, L)
    cst1 = ssd_consts()
    xTb = [np.ascontiguousarray(x[b].T) for b in range(Bsz)]
    maps = []
    for c in cores:
        b, g = c // PB, c % PB
        w, vecs, rowc = ssd_host_inputs(g, f32(ssd_norm[0]), f32(ssd_w_in[0]), f32(ssd_conv_w[0]), f32(ssd_conv_b[0]),
                                        f32(ssd_dt_bias[0]), f32(ssd_a_log[0]), f32(ssd_d[0]), f32(ssd_gnorm[0]))
        maps.append(dict(xT=xTb[b], w_in=w, vecs=vecs, rowc=rowc, cst=cst1))
    r1 = run_bass_kernel_spmd(nc1, maps, core_ids=cores).results
    ynT = [np.concatenate([r1[b * PB + g]["ynT"] for g in range(PB)], axis=0) for b in range(Bsz)]

    mcst = moe_consts(NT)

    def tok_maps(i, hT_list, aT_list, w_a, tail_norm, extra):
        vecs = np.ascontiguousarray(np.concatenate([_vcol(moe_norm[i]), _vcol(ple_norm[i]), _vcol(ple_b_gate[i]), _vcol(tail_norm)], axis=1))
        wr = np.ascontiguousarray(np.concatenate([f32(moe_w_rg[i]), f32(moe_w_re[i])], axis=1))
        br = np.ascontiguousarray(np.concatenate([f32(moe_b_rg[i]), f32(moe_b_re[i])])[None, :])
        wg_, wu_, wd_ = f32(moe_w_gate[i]), f32(moe_w_up[i]), f32(moe_w_down[i])
        plg_, plp_ = f32(ple_w_gate[i]), f32(ple_w_proj[i])
        out = []
        for c in cores:
            b, j = c // PB, c % PB
            sl = slice(j * NT, (j + 1) * NT)
            m = dict(hT=hT_list[c], aT=np.ascontiguousarray(aT_list[b][:, sl]), w_a=w_a, vecs=vecs, wr=wr, br=br, wg=wg_, wu=wu_, wd=wd_,
                     plg=plg_, plp=plp_, pT=np.ascontiguousarray(p[i, b, sl].T), mcst=mcst)
            m.update(extra)
            out.append(m)
        return out

    nc2 = _prog("tokA", build_tok2, NT, 2048, "A")
    hT0 = [np.ascontiguousarray(x[c // PB, (c % PB) * NT:(c % PB + 1) * NT].T) for c in cores]
    r2 = run_bass_kernel_spmd(nc2, tok_maps(0, hT0, ynT, f32(ssd_w_out[0]), f32(sb_norm[0]), dict(wqkv=f32(sb_w_qkv[0]))), core_ids=cores).results
    hT1 = [r2[c]["hT_o"] for c in cores]
    qTb = [np.concatenate([r2[b * PB + j]["qT_o"] for j in range(PB)], axis=1) for b in range(Bsz)]
    kTb = [np.concatenate([r2[b * PB + j]["kT_o"] for j in range(PB)], axis=1) for b in range(Bsz)]
    vb = [np.concatenate([r2[b * PB + j]["v_o"] for j in range(PB)], axis=0) for b in range(Bsz)]

    nc3 = _prog("attn", build_attn, L)
    cst3 = attn_consts()
    maps = []
    for c in cores:
        b, hp = c // PB, c % PB
        rows = slice(hp * 256, (hp + 1) * 256)
        vP = np.ascontiguousarray(vb[b][:, rows].reshape(L // 128, 128, 2, 128).transpose(2, 1, 0, 3))
        maps.append(dict(qT=np.ascontiguousarray(qTb[b][rows].reshape(2, 128, L)), kT=np.ascontiguousarray(kTb[b][rows].reshape(2, 128, L)),
                         vP=vP, cst=cst3))
    r3 = run_bass_kernel_spmd(nc3, maps, core_ids=cores).results
    oT = [np.concatenate([r3[b * PB + hp]["oT"].reshape(256, L) for hp in range(PB)], axis=0) for b in range(Bsz)]

    nc4 = _prog("tokB", build_tok2, NT, 1024, "B")
    r4 = run_bass_kernel_spmd(nc4, tok_maps(1, hT1, oT, f32(sb_w_o[0]), f32(final_norm), {}), core_ids=cores).results
    out = np.empty((Bsz, L, D), np.float32)
    for c in cores:
        b, j = c // PB, c % PB
        out[b, j * NT:(j + 1) * NT, :] = r4[c]["out_o"].T
    return out


I32 = mybir.dt.int32
MB = 128
NBLK_OF = lambda NT: (2 * NT) // MB + 64


def moe_consts(NT):
    nblk = NBLK_OF(NT)
    s = np.arange(128)
    SL = (s[:, None] < s[None, :]).astype(np.float32)
    THR = np.tile((np.arange(64) * MB).astype(np.float32)[None, :], (128, 1))
    JB = np.tile((np.arange(nblk) * MB).astype(np.float32)[None, :], (128, 1))
    KP = (np.arange(8)[None, :] + 2 * s[:, None]).astype(np.float32)
    return np.ascontiguousarray(np.concatenate([np.eye(128, dtype=np.float32), SL, THR, JB, KP], axis=1))


def idma(self, out_ap, in_ap, idx_t, idx_ap, gather, reads=(), writes=(), part=False, bound=None):
    t = writes[0]
    if t.dsem is None:
        key = "d%d" % self.ndsem
        t.dsem = self.stack.enter_context(self.nc.semaphore(key))
        self.semobj[key] = t.dsem
        t.name = key
        self.ndsem += 1
    key = t.name
    for r in list(reads) + [idx_t]:
        if r.w is not None:
            self._need("POOL", *r.w)
    if t.w is not None and not (part and t.w[0] == key):
        self._need("POOL", *t.w)
    for kk, v in t.r.items():
        self._need("POOL", kk, v)
    off = bass.IndirectOffsetOnAxis(ap=idx_ap, axis=0)
    if gather and bound is not None:
        ins = self.nc.gpsimd.indirect_dma_start(out=out_ap, out_offset=None, in_=in_ap, in_offset=off, bounds_check=bound, oob_is_err=False)
    elif gather:
        ins = self.nc.gpsimd.indirect_dma_start(out=out_ap, out_offset=None, in_=in_ap, in_offset=off)
    else:
        ins = self.nc.gpsimd.indirect_dma_start(out=out_ap, out_offset=off, in_=in_ap, in_offset=None)
    t.dcnt += 16
    ins.then_inc(t.dsem, 16)
    for r in list(reads) + [idx_t]:
        r.r[key] = max(r.r.get(key, 0), t.dcnt)
    t.w = (key, t.dcnt)
    if not part:
        t.r = {}
    self.ninst += 1
    return ins


K.idma = idma


def build_tok2(NT, KA, mode):
    nc = bass.Bass("TRN2", target_bir_lowering=False)

    def din(name, shape, dt=F32):
        return nc.dram_tensor(name, list(shape), dt, kind="ExternalInput").ap()

    def dout(name, shape, dt=F32):
        return nc.dram_tensor(name, list(shape), dt, kind="ExternalOutput").ap()

    NBLK = NBLK_OF(NT)
    NROWS = NBLK * MB
    NTB = NT // 128
    NTILE = NT // 512
    hT = din("hT", [1024, NT])
    aT = din("aT", [KA, NT], BF16)
    w_a = din("w_a", [KA, 1024])
    vecs = din("vecs", [128, 32])
    wr = din("wr", [1024, 72])
    br = din("br", [1, 72])
    wg = din("wg", [64, 1024, 512])
    wu = din("wu", [64, 1024, 512])
    wd = din("wd", [64, 512, 1024])
    plg = din("plg", [1024, 1024])
    plp = din("plp", [256, 1024])
    pT = din("pT", [256, NT])
    NCST = 256 + 64 + NBLK + 8
    mcst = din("mcst", [128, NCST])
    if mode == "A":
        wqkv = din("wqkv", [1024, 3072])
        hT_o = dout("hT_o", [1024, NT])
        qT_o = dout("qT_o", [1024, NT], BF16)
        kT_o = dout("kT_o", [1024, NT], BF16)
        v_o = dout("v_o", [NT, 1024], BF16)
    else:
        out_o = dout("out_o", [1024, NT])
    H1 = nc.dram_tensor("H1s", [1024, NT], F32, kind="Internal").ap()
    Xs = nc.dram_tensor("Xs", [NROWS, 1024], BF16, kind="Internal").ap()
    Ys = nc.dram_tensor("Ys", [NROWS, 1024], F32, kind="Internal").ap()
    wg_r = wg.rearrange("e (p h r) n -> (e p h) (r n)", p=128, h=2, r=4)
    wu_r = wu.rearrange("e (p h r) n -> (e p h) (r n)", p=128, h=2, r=4)
    wd_r = wd.rearrange("e (p h r) n -> (e p h) (r n)", p=128, h=2, r=2)
    KAC = KA // 128

    with ExitStack() as st:
        k = K(nc, st)
        HTL = [k.sbt([128, 8 * 512], F32, "htl%d" % i) for i in range(1)] * 2
        UTL = [k.sbt([128, 8 * 512], BF16, "utl%d" % i) for i in range(1)] * 2
        WB = [k.sbt([128, 12288], BF16, "wb%d" % i) for i in range(2)]
        RA = k.sb([128, 8192], BF16, "regA")
        RAf = RA[:].bitcast(F32)
        ABS = [T(RA[:, 0:KAC * 512])]
        BIG = T(RAf[:, 0:2048])
        IGF = T(RAf[:, 2048:3072])
        XB = [T(RA[:, i * 1024:(i + 1) * 1024]) for i in range(2)]
        XTB = [T(RA[:, 2048 + i * 1024:2048 + (i + 1) * 1024]) for i in range(2)]
        YB = [T(RAf[:, 2048 + i * 1024:2048 + (i + 1) * 1024]) for i in range(2)]
        NRX = max(NTB * 1024, 32768)
        RX = k.sb([128, NRX], BF16, "regX")
        RXf = RX[:].bitcast(F32)
        XROWS = T(RX[:, 0:NTB * 1024])
        XR3 = v3(XROWS.ap, NTB)
        WQ = [T(RX[:, i * 12288:(i + 1) * 12288]) for i in range(2)]
        Y0 = [T(RXf[:, 12288 + i * 1024:12288 + (i + 1) * 1024]) for i in range(2)]
        Y1 = [T(RXf[:, 14336 + i * 1024:14336 + (i + 1) * 1024]) for i in range(2)]
        PBt = k.sbt([128, 2 * 512], BF16, "pbt")
        LNV = k.sbt([128, 512], F32, "lnv")
        RSTD = k.sbt([128, 512], F32, "rstd")
        vec = k.sbt([128, 32], F32, "vec_sb")
        cf = k.sbt([128, NCST], F32, "cf")
        ident = cf.ap[:, 0:128]
        identb = k.sbt([128, 128], BF16, "identb")
        SLb = k.sbt([128, 128], BF16, "SLb")
        ones_bf = k.sbt([128, 128], BF16, "ones_bf")
        wr_sb = k.sbt([128, 8 * 72], F32, "wr_sb")
        br_bc = k.sbt([128, 72], F32, "brbc")
        STG = [k.sbt([128, 512], BF16, "stg%d" % i) for i in range(2)]
        STF = [k.sbt([128, 512], F32, "stf%d" % i) for i in range(2)]
        r_lg = k.sbt([128, 72], F32, "r_lg")
        r_a = k.sbt([128, 8], F32, "r_a")
        r_goh = k.sbt([128, 8], F32, "r_goh")
        r_t64 = k.sbt([128, 64], F32, "r_t64")
        r_es = k.sbt([128, 8], F32, "r_es")
        r_es2 = k.sbt([128, 8], F32, "r_es2")
        r_oh1 = k.sbt([128, 8], F32, "r_oh1")
        r_oh2 = k.sbt([128, 8], F32, "r_oh2")
        r_s = [k.sbt([128, 1], F32, "r_s%d" % i) for i in range(12)]
        OHC = k.sbt([128, 128], BF16, "ohc")
        OHS = k.sbt([128, NTB * 128], BF16, "ohs")
        OHS3 = v3(OHS.ap, NTB)
        RUN = k.sbt([128, 128], F32, "run")
        PRS = k.sbt([128, 128], F32, "prs")
        JNK = k.sbt([128, 64], F32, "jnk")
        RK0 = k.sbt([128, NTB], F32, "rk0")
        RK1 = k.sbt([128, NTB], F32, "rk1")
        GA = k.sbt([128, NTB], F32, "ga")
        GB = k.sbt([128, NTB], F32, "gb")
        DI0 = k.sbt([128, NTB], I32, "di0")
        DI1 = k.sbt([128, NTB], I32, "di1")
        CNT = k.sbt([128, 64], F32, "cnt")
        NB_ = k.sbt([128, 64], F32, "nb_")
        PE_ = [k.sbt([128, 64], F32, "pe%d" % i) for i in range(2)]
        BASE1 = k.sbt([128, 64], F32, "base1")
        BASE2 = k.sbt([128, 64], F32, "base2")
        DF = k.sbt([128, NTB], F32, "df")
        EJ = k.sbt([128, NBLK], F32, "ej")
        SAME = k.sbt([128, NBLK], F32, "same")
        IGI = k.sbt([128, NBLK * 8], I32, "igi")
        IDI = k.sbt([128, NBLK * 4], I32, "idi")
        SS = [k.sbt([128, 512], BF16, "ss%d" % i) for i in range(2)]
        HH = [k.sbt([128, 512], BF16, "hh%d" % i) for i in range(2)]
        BK = [T(k.ps([128, 512], F32, "bk%d" % i)[:]) for i in range(8)]
        PG, PU, PD, PBK = BK[0:2], BK[2:4], BK[4:6], BK[6:8]
        H1_t, Xs_t, Ys_t = T(H1), T(Xs), T(Ys)
        if mode == "A":
            qT_t, kT_t, v_t, ho_t = T(qT_o), T(kT_o), T(v_o), T(hT_o)
        else:
            out_t = T(out_o)

        k.dma("SP", vec.ap, vecs, writes=[vec])
        k.dma("SP", cf.ap, mcst, writes=[cf])
        k.dma("SP", v3(wr_sb.ap, 8), wr.rearrange("(a p) n -> p a n", p=128), writes=[wr_sb])
        k.dma("SP", br_bc.ap, br.partition_broadcast(128).rearrange("p o n -> p (o n)"), writes=[br_bc])
        k.op("DVE", lambda e: e.memset(ones_bf.ap, 1.0), writes=[ones_bf])
        k.op("DVE", lambda e: e.memset(RUN.ap, 0.0), writes=[RUN])
        k.op("DVE", lambda e: e.tensor_copy(out=identb.ap, in_=ident), reads=[cf], writes=[identb])
        k.op("DVE", lambda e: e.tensor_copy(out=SLb.ap, in_=cf.ap[:, 128:256]), reads=[cf], writes=[SLb])
        THR = cf.ap[:, 256:320]
        JB = cf.ap[:, 320:320 + NBLK]
        KP = cf.ap[:, 320 + NBLK:328 + NBLK]
        for kc in range(8):
            k.op("DVE", lambda e: e.tensor_scalar(out=wr_sb.ap[:, kc * 72:(kc + 1) * 72], in0=wr_sb.ap[:, kc * 72:(kc + 1) * 72],
                                                  scalar1=vec.ap[:, kc:kc + 1], scalar2=None, op0=ALU.mult), reads=[vec, wr_sb], writes=[wr_sb])

        def rmsnorm(src_t, src_ap, gcol, dst_t, dst_ap, bank):
            k.op("ACT", lambda e: e.activation(out=dst_ap, in_=src_ap, func=AF.Square), reads=[src_t], writes=[dst_t])
            for kc in range(8):
                k.op("PE", lambda e: e.matmul(bank.ap, ones_bf.ap, dst_ap[:, kc, :], start=(kc == 0), stop=(kc == 7)),
                     reads=[dst_t, ones_bf], writes=[bank], pe_acc=(kc > 0))
            k.op("ACT", lambda e: e.activation(out=LNV.ap, in_=bank.ap, func=AF.Ln, scale=1.0 / 1024, bias=EPS), reads=[bank], writes=[LNV])
            k.op("ACT", lambda e: e.activation(out=RSTD.ap, in_=LNV.ap, func=AF.Exp, scale=-0.5), reads=[LNV], writes=[RSTD])
            for kc in range(8):
                k.op("DVE", lambda e: e.scalar_tensor_tensor(out=dst_ap[:, kc, :], in0=src_ap[:, kc, :], scalar=vec.ap[:, gcol + kc:gcol + kc + 1],
                                                             in1=RSTD.ap, op0=ALU.mult, op1=ALU.mult), reads=[src_t, RSTD, vec], writes=[dst_t])

        for kp in range(KAC // 8):
            k.dma("POOL", v3(WB[kp].ap[:, 0:8192], 8), w_a[kp * 1024:(kp + 1) * 1024, :].rearrange("(a p) n -> p a n", p=128), writes=[WB[kp]])
        for t in range(NTILE):
            cs = slice(t * 512, (t + 1) * 512)
            ht, ut, AB = HTL[t % 2], UTL[t % 2], ABS[0]
            h3, u3 = v3(ht.ap, 8), v3(ut.ap, 8)
            k.dma("SP", h3, hT[:, cs].rearrange("(a p) n -> p a n", p=128), writes=[ht])
            k.dma("SP", v3(AB.ap, KAC), aT[:, cs].rearrange("(a p) n -> p a n", p=128), writes=[AB])
            for d in range(8):
                bank = PD[d % 2]
                for kc in range(KAC):
                    wbt = WB[kc // 8]
                    k.op("PE", lambda e: e.matmul(bank.ap, v3(wbt.ap[:, 0:8192], 8)[:, kc % 8, d * 128:(d + 1) * 128], v3(AB.ap, KAC)[:, kc, :],
                                                  start=(kc == 0), stop=(kc == KAC - 1)), reads=[wbt, AB], writes=[bank], pe_acc=(kc > 0))
                k.op("DVE", lambda e: e.tensor_tensor(out=h3[:, d, :], in0=h3[:, d, :], in1=bank.ap, op=ALU.add), reads=[bank, ht], writes=[ht])
            k.dma("SP", H1[:, cs].rearrange("(a p) n -> p a n", p=128), h3, reads=[ht], writes=[H1_t], part=True)
            rmsnorm(ht, h3, 0, ut, u3, PBK[0])
            for b in range(4):
                i = t * 4 + b
                bs = slice(b * 128, (b + 1) * 128)
                plg_b, prs = PG[b % 2], PU[b % 2]
                for kc in range(8):
                    k.op("PE", lambda e: e.matmul(plg_b.ap[:, 0:72], h3[:, kc, bs], wr_sb.ap[:, kc * 72:(kc + 1) * 72], start=(kc == 0), stop=(kc == 7)),
                         reads=[ht, wr_sb], writes=[plg_b], pe_acc=(kc > 0))
                k.op("PE", lambda e: e.matmul(prs.ap[:, 0:2], RSTD.ap[:, bs], ident[:, 0:2], start=True, stop=True), reads=[RSTD, cf], writes=[prs])
                rt, gmax, ngmax, gsum, gw, m1, m2, dd, ex, w1, w2 = r_s[0:11]
                k.op("DVE", lambda e: e.tensor_copy(out=rt.ap, in_=prs.ap[:, 0:1]), reads=[prs], writes=[rt])
                k.op("DVE", lambda e: e.scalar_tensor_tensor(out=r_lg.ap, in0=plg_b.ap[:, 0:72], scalar=rt.ap, in1=br_bc.ap, op0=ALU.mult, op1=ALU.add),
                     reads=[plg_b, rt, br_bc], writes=[r_lg])
                gl = r_lg.ap[:, 0:8]
                el = r_lg.ap[:, 8:72]
                k.op("DVE", lambda e: e.tensor_reduce(out=gmax.ap, in_=gl, axis=AX.X, op=ALU.max), reads=[r_lg], writes=[gmax])
                k.op("DVE", lambda e: e.tensor_scalar(out=r_goh.ap, in0=gl, scalar1=gmax.ap, scalar2=None, op0=ALU.is_equal), reads=[r_lg, gmax], writes=[r_goh])
                k.op("DVE", lambda e: e.tensor_scalar(out=ngmax.ap, in0=gmax.ap, scalar1=-1.0, scalar2=None, op0=ALU.mult), reads=[gmax], writes=[ngmax])
                k.op("ACT", lambda e: e.activation(out=r_a.ap, in_=gl, func=AF.Exp, bias=ngmax.ap, scale=1.0, accum_out=gsum.ap), reads=[r_lg, ngmax], writes=[r_a, gsum])
                k.op("DVE", lambda e: e.reciprocal(out=gw.ap, in_=gsum.ap), reads=[gsum], writes=[gw])
                k.op("DVE", lambda e: e.tensor_tensor(out=v3(r_t64.ap, 8), in0=v3(el, 8), in1=r_goh.ap.unsqueeze(2).to_broadcast([128, 8, 8]), op=ALU.mult),
                     reads=[r_lg, r_goh], writes=[r_t64])
                k.op("DVE", lambda e: e.tensor_reduce(out=r_es.ap, in_=v3(r_t64.ap, 8).rearrange("p g j -> p j g"), axis=AX.X, op=ALU.add), reads=[r_t64], writes=[r_es])
                k.op("DVE", lambda e: e.tensor_reduce(out=m1.ap, in_=r_es.ap, axis=AX.X, op=ALU.max), reads=[r_es], writes=[m1])
                k.op("DVE", lambda e: e.tensor_scalar(out=r_oh1.ap, in0=r_es.ap, scalar1=m1.ap, scalar2=None, op0=ALU.is_equal), reads=[r_es, m1], writes=[r_oh1])
                k.op("DVE", lambda e: e.scalar_tensor_tensor(out=r_es2.ap, in0=r_oh1.ap, scalar=-1e30, in1=r_es.ap, op0=ALU.mult, op1=ALU.add), reads=[r_oh1, r_es], writes=[r_es2])
                k.op("DVE", lambda e: e.tensor_reduce(out=m2.ap, in_=r_es2.ap, axis=AX.X, op=ALU.max), reads=[r_es2], writes=[m2])
                k.op("DVE", lambda e: e.tensor_scalar(out=r_oh2.ap, in0=r_es2.ap, scalar1=m2.ap, scalar2=None, op0=ALU.is_equal), reads=[r_es2, m2], writes=[r_oh2])
                k.op("DVE", lambda e: e.tensor_tensor(out=dd.ap, in0=m2.ap, in1=m1.ap, op=ALU.subtract), reads=[m1, m2], writes=[dd])
                k.op("ACT", lambda e: e.activation(out=ex.ap, in_=dd.ap, func=AF.Exp), reads=[dd], writes=[ex])
                k.op("DVE", lambda e: e.tensor_scalar(out=w1.ap, in0=ex.ap, scalar1=1.0, scalar2=None, op0=ALU.add), reads=[ex], writes=[w1])
                k.op("DVE", lambda e: e.reciprocal(out=w1.ap, in_=w1.ap), reads=[w1], writes=[w1])
                k.op("DVE", lambda e: e.tensor_tensor(out=w2.ap, in0=ex.ap, in1=w1.ap, op=ALU.mult), reads=[ex, w1], writes=[w2])
                k.op("DVE", lambda e: e.tensor_tensor(out=GA.ap[:, i:i + 1], in0=w1.ap, in1=gw.ap, op=ALU.mult), reads=[w1, gw], writes=[GA])
                k.op("DVE", lambda e: e.tensor_tensor(out=GB.ap[:, i:i + 1], in0=w2.ap, in1=gw.ap, op=ALU.mult), reads=[w2, gw], writes=[GB])
                k.op("DVE", lambda e: e.tensor_tensor(out=v3(OHC.ap[:, 0:64], 8), in0=r_goh.ap.unsqueeze(2).to_broadcast([128, 8, 8]),
                                                      in1=r_oh1.ap.unsqueeze(1).to_broadcast([128, 8, 8]), op=ALU.mult), reads=[r_goh, r_oh1], writes=[OHC])
                k.op("DVE", lambda e: e.tensor_tensor(out=v3(OHC.ap[:, 64:128], 8), in0=r_goh.ap.unsqueeze(2).to_broadcast([128, 8, 8]),
                                                      in1=r_oh2.ap.unsqueeze(1).to_broadcast([128, 8, 8]), op=ALU.mult), reads=[r_goh, r_oh2, OHC], writes=[OHC])
                k.op("DVE", lambda e: e.tensor_copy(out=OHS3[:, i, :], in_=OHC.ap), reads=[OHC], writes=[OHS])
                ppr, pcs = PD[0], PD[1]
                k.op("PE", lambda e: e.matmul(ppr.ap[:, 0:128], SLb.ap, OHC.ap, start=True, stop=True), reads=[SLb, OHC], writes=[ppr])
                k.op("PE", lambda e: e.matmul(pcs.ap[:, 0:128], ones_bf.ap, OHC.ap, start=True, stop=True), reads=[ones_bf, OHC], writes=[pcs])
                k.op("DVE", lambda e: e.tensor_tensor(out=PRS.ap, in0=ppr.ap[:, 0:128], in1=RUN.ap, op=ALU.add), reads=[ppr, RUN], writes=[PRS])
                k.op("DVE", lambda e: e.tensor_tensor(out=PRS.ap, in0=PRS.ap, in1=OHC.ap, op=ALU.mult), reads=[PRS, OHC], writes=[PRS])
                k.op("DVE", lambda e: e.tensor_reduce(out=RK0.ap[:, i:i + 1], in_=PRS.ap[:, 0:64], axis=AX.X, op=ALU.add), reads=[PRS], writes=[RK0])
                k.op("DVE", lambda e: e.tensor_reduce(out=RK1.ap[:, i:i + 1], in_=PRS.ap[:, 64:128], axis=AX.X, op=ALU.add), reads=[PRS], writes=[RK1])
                k.op("DVE", lambda e: e.tensor_tensor(out=RUN.ap, in0=RUN.ap, in1=pcs.ap[:, 0:128], op=ALU.add), reads=[pcs, RUN], writes=[RUN])
                trb = PBK[1].ap.bitcast(BF16)
                for kc in range(8):
                    k.op("PE", lambda e: e.transpose(trb[:, kc * 128:(kc + 1) * 128], u3[:, kc, bs], identb.ap), reads=[ut, identb], writes=[PBK[1]])
                k.op("ACT", lambda e: e.activation(out=XR3[:, i, :], in_=trb, func=AF.Copy), reads=[PBK[1]], writes=[XROWS])

        k.alias([BIG, IGF], ABS)
        cnt1, cnt2 = RUN.ap[:, 0:64], RUN.ap[:, 64:128]
        k.op("DVE", lambda e: e.tensor_tensor(out=CNT.ap, in0=cnt1, in1=cnt2, op=ALU.add), reads=[RUN], writes=[CNT])
        big_cm = BIG.ap[:, 0:2048].rearrange("p (a m) -> p a m", a=64)
        for mh in range(2):
            k.op("DVE", lambda e: e.tensor_tensor(out=big_cm, in0=CNT.ap.unsqueeze(2).to_broadcast([128, 64, 32]),
                                                  in1=THR[:, mh * 32:(mh + 1) * 32].unsqueeze(1).to_broadcast([128, 64, 32]), op=ALU.is_gt), reads=[CNT, cf], writes=[BIG])
            dstn = NB_ if mh == 0 else BASE1
            k.op("DVE", lambda e: e.tensor_reduce(out=dstn.ap, in_=big_cm, axis=AX.X, op=ALU.add), reads=[BIG], writes=[dstn])
        k.op("DVE", lambda e: e.tensor_tensor(out=NB_.ap, in0=NB_.ap, in1=BASE1.ap, op=ALU.add), reads=[NB_, BASE1], writes=[NB_])
        k.op("DVE", lambda e: e.tensor_scalar(out=NB_.ap, in0=NB_.ap, scalar1=float(MB), scalar2=None, op0=ALU.mult), reads=[NB_], writes=[NB_])
        k.op("DVE", lambda e: e.tensor_copy(out=PE_[0].ap, in_=NB_.ap), reads=[NB_], writes=[PE_[0]])
        cur = 0
        for sft in (1, 2, 4, 8, 16, 32):
            a_, b_ = PE_[cur], PE_[1 - cur]
            k.op("DVE", lambda e: e.tensor_copy(out=b_.ap[:, 0:sft], in_=a_.ap[:, 0:sft]), reads=[a_], writes=[b_])
            k.op("DVE", lambda e: e.tensor_tensor(out=b_.ap[:, sft:64], in0=a_.ap[:, sft:64], in1=a_.ap[:, 0:64 - sft], op=ALU.add), reads=[a_, b_], writes=[b_])
            cur = 1 - cur
        PEND = PE_[cur]
        k.op("DVE", lambda e: e.tensor_tensor(out=BASE1.ap, in0=PEND.ap, in1=NB_.ap, op=ALU.subtract), reads=[PEND, NB_], writes=[BASE1])
        k.op("DVE", lambda e: e.tensor_tensor(out=BASE2.ap, in0=BASE1.ap, in1=cnt1, op=ALU.add), reads=[BASE1, RUN], writes=[BASE2])
        TBC = 2048 // 64
        for (base, rk, di, lo) in ((BASE1, RK0, DI0, 0), (BASE2, RK1, DI1, 64)):
            for c0 in range(0, NTB, TBC):
                nb = min(TBC, NTB - c0)
                big_t = BIG.ap[:, 0:nb * 64].rearrange("p (a m) -> p a m", a=nb)
                k.op("DVE", lambda e: e.tensor_tensor(out=big_t, in0=OHS3[:, c0:c0 + nb, lo:lo + 64], in1=base.ap.unsqueeze(1).to_broadcast([128, nb, 64]), op=ALU.mult),
                     reads=[OHS, base], writes=[BIG])
                k.op("DVE", lambda e: e.tensor_reduce(out=DF.ap[:, c0:c0 + nb], in_=big_t, axis=AX.X, op=ALU.add), reads=[BIG], writes=[DF])
            k.op("DVE", lambda e: e.tensor_tensor(out=DF.ap, in0=DF.ap, in1=rk.ap, op=ALU.add), reads=[DF, rk], writes=[DF])
            k.op("DVE", lambda e: e.tensor_copy(out=di.ap, in_=DF.ap), reads=[DF], writes=[di])
        for c0 in range(0, NBLK, 32):
            nb = min(32, NBLK - c0)
            big_j = BIG.ap[:, 0:nb * 64].rearrange("p (a m) -> p a m", a=nb)
            k.op("DVE", lambda e: e.tensor_tensor(out=big_j, in0=PEND.ap.unsqueeze(1).to_broadcast([128, nb, 64]), in1=JB[:, c0:c0 + nb].unsqueeze(2).to_broadcast([128, nb, 64]), op=ALU.is_le),
                 reads=[PEND, cf], writes=[BIG])
            k.op("DVE", lambda e: e.tensor_reduce(out=EJ.ap[:, c0:c0 + nb], in_=big_j, axis=AX.X, op=ALU.add), reads=[BIG], writes=[EJ])
        k.op("DVE", lambda e: e.tensor_scalar(out=EJ.ap, in0=EJ.ap, scalar1=63.0, scalar2=None, op0=ALU.min), reads=[EJ], writes=[EJ])
        HB = NBLK // 2
        k.op("DVE", lambda e: e.memset(SAME.ap, 0.0), writes=[SAME])
        for s0 in (0, HB):
            k.op("DVE", lambda e: e.tensor_tensor(out=SAME.ap[:, s0 + 1:s0 + HB], in0=EJ.ap[:, s0 + 1:s0 + HB], in1=EJ.ap[:, s0:s0 + HB - 1], op=ALU.is_equal),
                 reads=[EJ, SAME], writes=[SAME])
        k.op("DVE", lambda e: e.tensor_scalar(out=SAME.ap, in0=SAME.ap, scalar1=float(1 << 22), scalar2=None, op0=ALU.mult), reads=[SAME], writes=[SAME])
        igf3 = v3(IGF.ap[:, 0:NBLK * 2], NBLK)
        k.op("DVE", lambda e: e.scalar_tensor_tensor(out=BIG.ap[:, 0:NBLK], in0=EJ.ap, scalar=256.0, in1=SAME.ap, op0=ALU.mult, op1=ALU.add), reads=[EJ, SAME], writes=[BIG])
        k.op("DVE", lambda e: e.tensor_tensor(out=igf3, in0=BIG.ap[:, 0:NBLK].unsqueeze(2).to_broadcast([128, NBLK, 2]), in1=KP[:, 0:2].unsqueeze(1).to_broadcast([128, NBLK, 2]), op=ALU.add),
             reads=[BIG, cf], writes=[IGF])
        k.op("DVE", lambda e: e.tensor_copy(out=IGI.ap[:, 0:NBLK * 2], in_=IGF.ap[:, 0:NBLK * 2]), reads=[IGF], writes=[IGI])

        for i in range(NTB):
            k.idma(Xs, XR3[:, i, :], DI0, DI0.ap[:, i:i + 1], gather=False, reads=[XROWS], writes=[Xs_t], part=True)
            k.idma(Xs, XR3[:, i, :], DI1, DI1.ap[:, i:i + 1], gather=False, reads=[XROWS], writes=[Xs_t], part=True)

        k.alias(XB + XTB + YB, [BIG, IGF] + ABS)

        bnd = nc.gpsimd.to_reg(64 * 256 - 1)
        igi2 = v3(IGI.ap[:, 0:NBLK * 2], NBLK)

        def load_blk(j, n_):
            wb = WB[n_ % 2]
            first = True
            for (src, base) in ((wg_r, 0), (wu_r, 4096), (wd_r, 8192)):
                for h in range(2):
                    k.idma(wb.ap[:, base + h * 2048:base + (h + 1) * 2048], src, IGI, igi2[:, j, h:h + 1], gather=True, writes=[wb], part=(not first), bound=bnd)
                    first = False
            k.dma("SP", XB[n_ % 2].ap, Xs[j * MB:(j + 1) * MB, :], reads=[Xs_t], writes=[XB[n_ % 2]])

        def front(j, n_):
            wb, xb, xt = WB[n_ % 2], XB[n_ % 2], XTB[n_ % 2]
            trb0, trb1 = PBK[0].ap.bitcast(BF16), PBK[1].ap.bitcast(BF16)
            for kc in range(8):
                dst = (trb0 if kc < 4 else trb1)[:, (kc % 4) * 128:(kc % 4 + 1) * 128]
                k.op("PE", lambda e: e.transpose(dst, xb.ap.rearrange("p (m c) -> p c m", c=8)[:, kc, :], identb.ap), reads=[xb, identb], writes=[PBK[0] if kc < 4 else PBK[1]])
            k.op("ACT", lambda e: e.activation(out=xt.ap[:, 0:512], in_=trb0[:, 0:512], func=AF.Copy), reads=[PBK[0]], writes=[xt])
            k.op("ACT", lambda e: e.activation(out=xt.ap[:, 512:1024], in_=trb1[:, 0:512], func=AF.Copy), reads=[PBK[1], xt], writes=[xt])
            Wg3, Wu3 = v3(wb.ap[:, 0:4096], 8), v3(wb.ap[:, 4096:8192], 8)
            pg, pu = PG[n_ % 2], PU[n_ % 2]
            for f in range(4):
                for kc in range(8):
                    k.op("PE", lambda e: e.matmul(pg.ap[:, f * 128:(f + 1) * 128], Wg3[:, kc, :].rearrange("p (m c) -> p c m", c=4)[:, f, :], xt.ap[:, kc * 128:(kc + 1) * 128],
                                                  start=(kc == 0), stop=(kc == 7)), reads=[wb, xt], writes=[pg], pe_acc=(kc > 0 or f > 0))
            for f in range(4):
                for kc in range(8):
                    k.op("PE", lambda e: e.matmul(pu.ap[:, f * 128:(f + 1) * 128], Wu3[:, kc, :].rearrange("p (m c) -> p c m", c=4)[:, f, :], xt.ap[:, kc * 128:(kc + 1) * 128],
                                                  start=(kc == 0), stop=(kc == 7)), reads=[wb, xt], writes=[pu], pe_acc=(kc > 0 or f > 0))
            k.op("ACT", lambda e: e.activation(out=SS[n_ % 2].ap, in_=pg.ap, func=AF.Silu), reads=[pg], writes=[SS[n_ % 2]])
            k.op("DVE", lambda e: e.tensor_tensor(out=HH[n_ % 2].ap, in0=SS[n_ % 2].ap, in1=pu.ap, op=ALU.mult), reads=[SS[n_ % 2], pu], writes=[HH[n_ % 2]])

        def back(j, n_):
            wb, hh, yb = WB[n_ % 2], HH[n_ % 2], YB[n_ % 2]
            Wd3 = v3(wb.ap[:, 8192:12288], 4)
            for dh in range(2):
                pd = PD[dh]
                for f in range(4):
                    k.op("PE", lambda e: e.matmul(pd.ap, hh.ap[:, f * 128:(f + 1) * 128], Wd3[:, f, dh * 512:(dh + 1) * 512], start=(f == 0), stop=(f == 3)),
                         reads=[wb, hh], writes=[pd], pe_acc=(f > 0))
                if dh == 0:
                    k.op("ACT", lambda e: e.activation(out=yb.ap[:, 0:512], in_=pd.ap, func=AF.Copy), reads=[pd], writes=[yb])
                else:
                    k.op("DVE", lambda e: e.tensor_copy(out=yb.ap[:, 512:1024], in_=pd.ap), reads=[pd, yb], writes=[yb])
            k.dma("SP", Ys[j * MB:(j + 1) * MB, :], yb.ap, reads=[yb], writes=[Ys_t], part=True)

        order = []
        for q in range(HB):
            order += [q, HB + q]
        load_blk(order[0], 0)
        for n_, j in enumerate(order):
            front(j, n_)
            if n_ > 0:
                back(order[n_ - 1], n_ - 1)
            if n_ + 1 < NBLK:
                load_blk(order[n_ + 1], n_ + 1)
        back(order[-1], NBLK - 1)

        k.dma("POOL", v3(WB[0].ap[:, 0:8192], 8), plg.rearrange("(a p) n -> p a n", p=128), writes=[WB[0]])
        k.dma("POOL", v3(WB[1].ap[:, 0:2048], 2), plp.rearrange("(a p) n -> p a n", p=128), writes=[WB[1]])
        k.alias(WQ + Y0 + Y1, [XROWS])
        if mode == "A":
            k.dma("POOL", v3(WQ[0].ap, 8), wqkv[:, 0:1536].rearrange("(a p) n -> p a n", p=128), writes=[WQ[0]])
            k.dma("POOL", v3(WQ[1].ap, 8), wqkv[:, 1536:3072].rearrange("(a p) n -> p a n", p=128), writes=[WQ[1]])
        for t in range(NTILE):
            cs = slice(t * 512, (t + 1) * 512)
            ht, ut = HTL[t % 2], UTL[t % 2]
            h3, u3 = v3(ht.ap, 8), v3(ut.ap, 8)
            k.dma("SP", h3, H1[:, cs].rearrange("(a p) n -> p a n", p=128), reads=[H1_t], writes=[ht])
            k.dma("POOL", v3(PBt.ap, 2), pT[:, cs].rearrange("(a p) n -> p a n", p=128), writes=[PBt])
            for b in range(4):
                i = t * 4 + b
                bs = slice(b * 128, (b + 1) * 128)
                y0, y1 = Y0[i % 2], Y1[i % 2]
                k.idma(y0.ap, Ys, DI0, DI0.ap[:, i:i + 1], gather=True, reads=[Ys_t], writes=[y0])
                k.idma(y1.ap, Ys, DI1, DI1.ap[:, i:i + 1], gather=True, reads=[Ys_t], writes=[y1])
                k.op("DVE", lambda e: e.tensor_scalar(out=y0.ap, in0=y0.ap, scalar1=GA.ap[:, i:i + 1], scalar2=None, op0=ALU.mult), reads=[y0, GA], writes=[y0])
                k.op("DVE", lambda e: e.scalar_tensor_tensor(out=y0.ap, in0=y1.ap, scalar=GB.ap[:, i:i + 1], in1=y0.ap, op0=ALU.mult, op1=ALU.add),
                     reads=[y1, GB, y0], writes=[y0])
                for half in range(2):
                    bank = PD[half]
                    for dq in range(4):
                        d = half * 4 + dq
                        k.op("PE", lambda e: e.transpose(bank.ap[:, dq * 128:(dq + 1) * 128], y0.ap[:, d * 128:(d + 1) * 128], ident), reads=[y0, cf], writes=[bank])
                    k.op("DVE", lambda e: e.tensor_tensor(out=h3[:, half * 4:(half + 1) * 4, bs], in0=h3[:, half * 4:(half + 1) * 4, bs],
                                                          in1=bank.ap.rearrange("p (a n) -> p a n", a=4), op=ALU.add), reads=[bank, ht], writes=[ht])
            rmsnorm(ht, h3, 8, ut, u3, PBK[0])
            for d in range(8):
                pg, pu = PG[d % 2], PU[d % 2]
                for kc in range(8):
                    k.op("PE", lambda e: e.matmul(pg.ap, v3(WB[0].ap[:, 0:8192], 8)[:, kc, d * 128:(d + 1) * 128], u3[:, kc, :], start=(kc == 0), stop=(kc == 7)),
                         reads=[WB[0], ut], writes=[pg], pe_acc=(kc > 0))
                for kc in range(2):
                    k.op("PE", lambda e: e.matmul(pu.ap, v3(WB[1].ap[:, 0:2048], 2)[:, kc, d * 128:(d + 1) * 128], v3(PBt.ap, 2)[:, kc, :], start=(kc == 0), stop=(kc == 1)),
                         reads=[WB[1], PBt], writes=[pu], pe_acc=(kc > 0))
                sf = STF[d % 2]
                k.op("ACT", lambda e: e.activation(out=sf.ap, in_=pg.ap, func=AF.Sigmoid, bias=vec.ap[:, 16 + d:17 + d], scale=1.0), reads=[pg, vec], writes=[sf])
                k.op("DVE", lambda e: e.tensor_tensor(out=sf.ap, in0=sf.ap, in1=pu.ap, op=ALU.mult), reads=[sf, pu], writes=[sf])
                k.op("DVE", lambda e: e.tensor_tensor(out=h3[:, d, :], in0=h3[:, d, :], in1=sf.ap, op=ALU.add), reads=[sf, ht], writes=[ht])
            if mode == "A":
                k.dma("SP", hT_o[:, cs].rearrange("(a p) n -> p a n", p=128), h3, reads=[ht], writes=[ho_t], part=True)
                rmsnorm(ht, h3, 24, ut, u3, PBK[0])
                for n in range(16):
                    col = n * 128
                    wbt = WQ[0] if col < 1536 else WQ[1]
                    lc = col if col < 1536 else col - 1536
                    bank = PG[n % 2]
                    for kc in range(8):
                        k.op("PE", lambda e: e.matmul(bank.ap, v3(wbt.ap, 8)[:, kc, lc:lc + 128], u3[:, kc, :], start=(kc == 0), stop=(kc == 7)),
                             reads=[wbt, ut], writes=[bank], pe_acc=(kc > 0))
                    sg = STG[n % 2]
                    sc_ = (1.0 / np.sqrt(128.0)) if n < 8 else 1.0
                    k.op("ACT", lambda e: e.activation(out=sg.ap, in_=bank.ap, func=AF.Identity, scale=float(sc_)), reads=[bank], writes=[sg])
                    if n < 8:
                        k.dma("SP", qT_o[n * 128:(n + 1) * 128, cs], sg.ap, reads=[sg], writes=[qT_t], part=True)
                    else:
                        k.dma("SP", kT_o[(n - 8) * 128:(n - 7) * 128, cs], sg.ap, reads=[sg], writes=[kT_t], part=True)
                for b in range(4):
                    for hh_ in range(2):
                        bank = PU[hh_]
                        for kc in range(8):
                            k.op("PE", lambda e: e.matmul(bank.ap, u3[:, kc, b * 128:(b + 1) * 128], v3(WQ[1].ap, 8)[:, kc, 512 + hh_ * 512:1024 + hh_ * 512],
                                                          start=(kc == 0), stop=(kc == 7)), reads=[WQ[1], ut], writes=[bank], pe_acc=(kc > 0))
                        sg = STG[hh_]
                        k.op("ACT", lambda e: e.activation(out=sg.ap, in_=bank.ap, func=AF.Copy), reads=[bank], writes=[sg])
                        r0 = t * 512 + b * 128
                        k.dma("SP", v_o[r0:r0 + 128, hh_ * 512:(hh_ + 1) * 512], sg.ap, reads=[sg], writes=[v_t], part=True)
            else:
                k.op("ACT", lambda e: e.activation(out=u3, in_=h3, func=AF.Square), reads=[ht], writes=[ut])
                bank = PBK[0]
                for kc in range(8):
                    k.op("PE", lambda e: e.matmul(bank.ap, ones_bf.ap, u3[:, kc, :], start=(kc == 0), stop=(kc == 7)),
                         reads=[ut, ones_bf], writes=[bank], pe_acc=(kc > 0))
                k.op("ACT", lambda e: e.activation(out=LNV.ap, in_=bank.ap, func=AF.Ln, scale=1.0 / 1024, bias=EPS), reads=[bank], writes=[LNV])
                k.op("ACT", lambda e: e.activation(out=RSTD.ap, in_=LNV.ap, func=AF.Exp, scale=-0.5), reads=[LNV], writes=[RSTD])
                for d in range(8):
                    k.op("DVE", lambda e: e.scalar_tensor_tensor(out=h3[:, d, :], in0=h3[:, d, :], scalar=vec.ap[:, 24 + d:25 + d], in1=RSTD.ap,
                                                                 op0=ALU.mult, op1=ALU.mult), reads=[ht, RSTD, vec], writes=[ht])
                k.dma("SP", out_o[:, cs].rearrange("(a p) n -> p a n", p=128), h3, reads=[ht], writes=[out_t], part=True)
        k.finish("SP", [qT_t, kT_t, v_t, ho_t] if mode == "A" else [out_t])
        print("tok2 ninst", k.ninst)
    return nc
```

```python
import numpy as np
import ml_dtypes
from contextlib import ExitStack
import concourse.bass as bass
import concourse.mybir as mybir
from concourse.bass_utils import run_bass_kernel_spmd

F32 = mybir.dt.float32
BF16 = mybir.dt.bfloat16
AF = mybir.ActivationFunctionType
ALU = mybir.AluOpType
AX = mybir.AxisListType
EPS = 1e-6
NPBF = ml_dtypes.bfloat16


class T:
    __slots__ = ("ap", "w", "r", "dsem", "dcnt", "name")

    def __init__(self, ap, name=""):
        self.ap = ap
        self.w = None
        self.r = {}
        self.dsem = None
        self.dcnt = 0
        self.name = name


class K:
    def __init__(self, nc, stack):
        self.nc = nc
        self.stack = stack
        self.eng = {"PE": nc.tensor, "ACT": nc.scalar, "DVE": nc.vector, "POOL": nc.gpsimd, "SP": nc.sync}
        self.sem = {}
        self.cnt = {}
        for e in ("PE", "ACT", "DVE", "POOL"):
            self.sem[e] = stack.enter_context(nc.semaphore("s_" + e))
            self.cnt[e] = 0
        self.seen = {e: {} for e in self.eng}
        self.semobj = dict(self.sem)
        self.ndsem = 0
        self.ninst = 0
        self.nalloc = 0

    def sb(self, shape, dt, name=None):
        self.nalloc += 1
        return self.stack.enter_context(self.nc.sbuf_tensor(name or ("sb%d" % self.nalloc), list(shape), dt))

    def ps(self, shape, dt, name=None):
        self.nalloc += 1
        return self.stack.enter_context(self.nc.psum_tensor(name or ("ps%d" % self.nalloc), list(shape), dt))

    def sbt(self, shape, dt, name=None):
        return T(self.sb(shape, dt, name)[:])

    def _need(self, e, key, val):
        if val <= 0 or self.seen[e].get(key, 0) >= val:
            return
        self.eng[e].wait_ge(self.semobj[key], val)
        self.seen[e][key] = val

    def op(self, e, fn, reads=(), writes=(), pe_acc=False):
        for t in reads:
            if t.w is not None:
                self._need(e, *t.w)
        for t in writes:
            if t.w is not None and not (pe_acc and t.w[0] == "PE"):
                self._need(e, *t.w)
            for kk, v in t.r.items():
                self._need(e, kk, v)
        ins = fn(self.eng[e])
        self.cnt[e] += 1
        c = self.cnt[e]
        ins.then_inc(self.sem[e], 1)
        for t in reads:
            t.r[e] = c
        for t in writes:
            t.w = (e, c)
            t.r = {}
        self.ninst += 1
        return ins

    def dma(self, q, out_ap, in_ap, reads=(), writes=(), part=False, **kw):
        assert len(writes) == 1
        t = writes[0]
        if t.dsem is None:
            key = "d%d" % self.ndsem
            t.dsem = self.stack.enter_context(self.nc.semaphore(key))
            self.semobj[key] = t.dsem
            t.name = key
            self.ndsem += 1
        key = t.name
        for r in reads:
            if r.w is not None:
                self._need(q, *r.w)
        if t.w is not None and not (part and t.w[0] == key):
            self._need(q, *t.w)
        for kk, v in t.r.items():
            self._need(q, kk, v)
        ins = self.eng[q].dma_start(out=out_ap, in_=in_ap, **kw)
        t.dcnt += 16
        ins.then_inc(t.dsem, 16)
        for r in reads:
            r.r[key] = max(r.r.get(key, 0), t.dcnt)
        t.w = (key, t.dcnt)
        if not part:
            t.r = {}
        self.ninst += 1
        return ins

    def alias(self, new, old):
        for n in new:
            for o in old:
                for kk, v in o.r.items():
                    n.r[kk] = max(n.r.get(kk, 0), v)
                if o.w is not None:
                    n.r[o.w[0]] = max(n.r.get(o.w[0], 0), o.w[1])

    def finish(self, e, tiles):
        for t in tiles:
            if t.w is not None:
                self._need(e, *t.w)


def v3(ap, a):
    return ap.rearrange("p (a n) -> p a n", a=a)


def build_tok(NT, TH, KA, mode, NE=64):
    nc = bass.Bass("TRN2", target_bir_lowering=False)

    def din(name, shape, dt=F32):
        return nc.dram_tensor(name, list(shape), dt, kind="ExternalInput").ap()

    def dout(name, shape, dt=F32):
        return nc.dram_tensor(name, list(shape), dt, kind="ExternalOutput").ap()

    hT = din("hT", [1024, NT])
    aT = din("aT", [KA, NT], BF16)
    w_a = din("w_a", [KA, 1024])
    vecs = din("vecs", [128, 32])
    wr = din("wr", [1024, 72])
    br = din("br", [1, 72])
    wg = din("wg", [64, 1024, 512])
    wu = din("wu", [64, 1024, 512])
    wd = din("wd", [64, 512, 1024])
    plg = din("plg", [1024, 1024])
    plp = din("plp", [256, 1024])
    pT = din("pT", [256, NT])
    ident_d = din("ident", [128, 128])
    if mode == "A":
        wqkv = din("wqkv", [1024, 3072])
        hT_o = dout("hT_o", [1024, NT])
        qT_o = dout("qT_o", [1024, NT], BF16)
        kT_o = dout("kT_o", [1024, NT], BF16)
        v_o = dout("v_o", [NT, 1024], BF16)
    else:
        out_o = dout("out_o", [1024, NT])
    KAC = KA // 128
    NTL = TH // 512
    NH = NT // TH

    with ExitStack() as st:
        k = K(nc, st)
        h_sb = k.sb([128, 8 * TH], F32, "h_sb")
        h3 = v3(h_sb[:], 8)
        HT = [[T(h3[:, d, t * 512:(t + 1) * 512]) for d in range(8)] for t in range(NTL)]
        u_sb = k.sb([128, 8 * TH], BF16, "u_sb")
        u3 = v3(u_sb[:], 8)
        UT = [T(u3[:, :, t * 512:(t + 1) * 512]) for t in range(NTL)]
        WB = [k.sbt([128, 12288], BF16, "wb%d" % i) for i in range(2)]
        GT = k.sbt([64, TH], F32, "gt")
        NAB = max(1, min(2, (8 * TH) // (KAC * 512)))
        ABS = [T(u_sb[:, i * KAC * 512:(i + 1) * KAC * 512]) for i in range(NAB)]
        PB = k.sbt([128, 2 * 512], BF16, "pb")
        SQ = k.sbt([128, 8 * 512], BF16, "sq")
        LNV = k.sbt([128, 512], F32, "lnv")
        RSTD = k.sbt([128, 512], F32, "rstd")
        vec = k.sbt([128, 32], F32, "vec_sb")
        ident = k.sbt([128, 128], F32, "ident_sb")
        ones_bf = k.sbt([128, 128], BF16, "ones")
        wr_sb = k.sbt([128, 8 * 72], F32, "wr_sb")
        br_bc = k.sbt([128, 72], F32, "brbc")
        S_ = [k.sbt([128, 512], BF16, "s%d" % i) for i in range(2)]
        TT = [k.sbt([128, 512], BF16, "tt%d" % i) for i in range(2)]
        HS = [[k.sbt([128, 512], BF16, "hs%d_%d" % (i, f)) for f in range(4)] for i in range(2)]
        EB = [k.sbt([64, 128], F32, "eb%d" % i) for i in range(2)]
        STG = [k.sbt([128, 512], BF16, "stg%d" % i) for i in range(2)]
        STF = [k.sbt([128, 512], F32, "stf%d" % i) for i in range(2)]
        r_lg = k.sbt([128, 72], F32, "r_lg")
        r_a = k.sbt([128, 8], F32, "r_a")
        r_goh = k.sbt([128, 8], F32, "r_goh")
        r_t64 = k.sbt([128, 64], F32, "r_t64")
        r_es = k.sbt([128, 8], F32, "r_es")
        r_es2 = k.sbt([128, 8], F32, "r_es2")
        r_oh1 = k.sbt([128, 8], F32, "r_oh1")
        r_oh2 = k.sbt([128, 8], F32, "r_oh2")
        r_ew = k.sbt([128, 8], F32, "r_ew")
        r_G = k.sbt([128, 64], F32, "r_G")
        r_s = [k.sbt([128, 1], F32, "r_s%d" % i) for i in range(12)]
        BK = [T(k.ps([128, 512], F32, "bk%d" % i)[:]) for i in range(8)]
        PG, PU, PD, PBK = BK[0:2], BK[2:4], BK[4:6], BK[6:8]

        k.dma("SP", vec.ap, vecs, writes=[vec])
        k.dma("SP", ident.ap, ident_d, writes=[ident])
        k.dma("SP", v3(wr_sb.ap, 8), wr.rearrange("(a p) n -> p a n", p=128), writes=[wr_sb])
        k.dma("SP", br_bc.ap, br.partition_broadcast(128).rearrange("p o n -> p (o n)"), writes=[br_bc])
        k.op("DVE", lambda e: e.memset(ones_bf.ap, 1.0), writes=[ones_bf])
        for kc in range(8):
            k.op("DVE", lambda e: e.tensor_scalar(out=wr_sb.ap[:, kc * 72:(kc + 1) * 72], in0=wr_sb.ap[:, kc * 72:(kc + 1) * 72],
                                                  scalar1=vec.ap[:, kc:kc + 1], scalar2=None, op0=ALU.mult),
                 reads=[vec, wr_sb], writes=[wr_sb])

        def rmsnorm(src_tiles, src_ap, gcol, dst_t, dst_ap, bank):
            k.op("ACT", lambda e: e.activation(out=v3(SQ.ap, 8), in_=src_ap, func=AF.Square), reads=src_tiles, writes=[SQ])
            for kc in range(8):
                k.op("PE", lambda e: e.matmul(bank.ap, ones_bf.ap, v3(SQ.ap, 8)[:, kc, :], start=(kc == 0), stop=(kc == 7)),
                     reads=[SQ, ones_bf], writes=[bank], pe_acc=(kc > 0))
            k.op("ACT", lambda e: e.activation(out=LNV.ap, in_=bank.ap, func=AF.Ln, scale=1.0 / 1024, bias=EPS), reads=[bank], writes=[LNV])
            k.op("ACT", lambda e: e.activation(out=RSTD.ap, in_=LNV.ap, func=AF.Exp, scale=-0.5), reads=[LNV], writes=[RSTD])
            for kc in range(8):
                k.op("DVE", lambda e: e.scalar_tensor_tensor(out=dst_ap[:, kc, :], in0=src_ap[:, kc, :], scalar=vec.ap[:, gcol + kc:gcol + kc + 1],
                                                             in1=RSTD.ap, op0=ALU.mult, op1=ALU.mult),
                     reads=list(src_tiles) + [RSTD, vec], writes=[dst_t])

        if mode == "A":
            qT_t, kT_t, v_t, ho_t = T(qT_o), T(kT_o), T(v_o), T(hT_o)
        else:
            out_t = T(out_o)
        for hf in range(NH):
            c0 = hf * TH
            k.alias(ABS, UT)
            for kp in range(KAC // 8):
                k.dma("POOL", v3(WB[kp].ap[:, 0:8192], 8), w_a[kp * 1024:(kp + 1) * 1024, :].rearrange("(a p) n -> p a n", p=128), writes=[WB[kp]])
            for t in range(NTL):
                cs = slice(c0 + t * 512, c0 + (t + 1) * 512)
                for d in range(8):
                    k.dma("SP", HT[t][d].ap, hT[d * 128:(d + 1) * 128, cs], writes=[HT[t][d]])
                AB = ABS[t % NAB]
                k.dma("SP", v3(AB.ap, KAC), aT[:, cs].rearrange("(a p) n -> p a n", p=128), writes=[AB])
                for d in range(8):
                    bank = PD[d % 2]
                    for kc in range(KAC):
                        wbt = WB[kc // 8]
                        k.op("PE", lambda e: e.matmul(bank.ap, v3(wbt.ap[:, 0:8192], 8)[:, kc % 8, d * 128:(d + 1) * 128], v3(AB.ap, KAC)[:, kc, :],
                                                      start=(kc == 0), stop=(kc == KAC - 1)),
                             reads=[wbt, AB], writes=[bank], pe_acc=(kc > 0))
                    k.op("DVE", lambda e: e.tensor_tensor(out=HT[t][d].ap, in0=HT[t][d].ap, in1=bank.ap, op=ALU.add),
                         reads=[bank, HT[t][d]], writes=[HT[t][d]])
            k.alias(UT, ABS)
            for t in range(NTL):
                hap = h3[:, :, t * 512:(t + 1) * 512]
                rmsnorm(HT[t], hap, 0, UT[t], UT[t].ap, PBK[0])
                for b in range(4):
                    bs = slice(t * 512 + b * 128, t * 512 + (b + 1) * 128)
                    plg_b, prs = PG[b % 2], PU[b % 2]
                    for kc in range(8):
                        k.op("PE", lambda e: e.matmul(plg_b.ap[:, 0:72], h3[:, kc, bs], wr_sb.ap[:, kc * 72:(kc + 1) * 72], start=(kc == 0), stop=(kc == 7)),
                             reads=[HT[t][kc], wr_sb], writes=[plg_b], pe_acc=(kc > 0))
                    k.op("PE", lambda e: e.matmul(prs.ap[:, 0:2], RSTD.ap[:, b * 128:(b + 1) * 128], ident.ap[:, 0:2], start=True, stop=True),
                         reads=[RSTD, ident], writes=[prs])
                    rt, gmax, ngmax, gsum, gw, m1, m2, dd, ex, w1, w2 = r_s[0:11]
                    k.op("DVE", lambda e: e.tensor_copy(out=rt.ap, in_=prs.ap[:, 0:1]), reads=[prs], writes=[rt])
                    k.op("DVE", lambda e: e.scalar_tensor_tensor(out=r_lg.ap, in0=plg_b.ap[:, 0:72], scalar=rt.ap, in1=br_bc.ap, op0=ALU.mult, op1=ALU.add),
                         reads=[plg_b, rt, br_bc], writes=[r_lg])
                    gl = r_lg.ap[:, 0:8]
                    el = r_lg.ap[:, 8:72]
                    k.op("DVE", lambda e: e.tensor_reduce(out=gmax.ap, in_=gl, axis=AX.X, op=ALU.max), reads=[r_lg], writes=[gmax])
                    k.op("DVE", lambda e: e.tensor_scalar(out=r_goh.ap, in0=gl, scalar1=gmax.ap, scalar2=None, op0=ALU.is_equal), reads=[r_lg, gmax], writes=[r_goh])
                    k.op("DVE", lambda e: e.tensor_scalar(out=ngmax.ap, in0=gmax.ap, scalar1=-1.0, scalar2=None, op0=ALU.mult), reads=[gmax], writes=[ngmax])
                    k.op("ACT", lambda e: e.activation(out=r_a.ap, in_=gl, func=AF.Exp, bias=ngmax.ap, scale=1.0, accum_out=gsum.ap), reads=[r_lg, ngmax], writes=[r_a, gsum])
                    k.op("DVE", lambda e: e.reciprocal(out=gw.ap, in_=gsum.ap), reads=[gsum], writes=[gw])
                    k.op("DVE", lambda e: e.tensor_tensor(out=v3(r_t64.ap, 8), in0=v3(el, 8), in1=r_goh.ap.unsqueeze(2).to_broadcast([128, 8, 8]), op=ALU.mult),
                         reads=[r_lg, r_goh], writes=[r_t64])
                    k.op("DVE", lambda e: e.tensor_reduce(out=r_es.ap, in_=v3(r_t64.ap, 8).rearrange("p g j -> p j g"), axis=AX.X, op=ALU.add), reads=[r_t64], writes=[r_es])
                    k.op("DVE", lambda e: e.tensor_reduce(out=m1.ap, in_=r_es.ap, axis=AX.X, op=ALU.max), reads=[r_es], writes=[m1])
                    k.op("DVE", lambda e: e.tensor_scalar(out=r_oh1.ap, in0=r_es.ap, scalar1=m1.ap, scalar2=None, op0=ALU.is_equal), reads=[r_es, m1], writes=[r_oh1])
                    k.op("DVE", lambda e: e.scalar_tensor_tensor(out=r_es2.ap, in0=r_oh1.ap, scalar=-1e30, in1=r_es.ap, op0=ALU.mult, op1=ALU.add), reads=[r_oh1, r_es], writes=[r_es2])
                    k.op("DVE", lambda e: e.tensor_reduce(out=m2.ap, in_=r_es2.ap, axis=AX.X, op=ALU.max), reads=[r_es2], writes=[m2])
                    k.op("DVE", lambda e: e.tensor_scalar(out=r_oh2.ap, in0=r_es2.ap, scalar1=m2.ap, scalar2=None, op0=ALU.is_equal), reads=[r_es2, m2], writes=[r_oh2])
                    k.op("DVE", lambda e: e.tensor_tensor(out=dd.ap, in0=m2.ap, in1=m1.ap, op=ALU.subtract), reads=[m1, m2], writes=[dd])
                    k.op("ACT", lambda e: e.activation(out=ex.ap, in_=dd.ap, func=AF.Exp), reads=[dd], writes=[ex])
                    k.op("DVE", lambda e: e.tensor_scalar(out=w1.ap, in0=ex.ap, scalar1=1.0, scalar2=None, op0=ALU.add), reads=[ex], writes=[w1])
                    k.op("DVE", lambda e: e.reciprocal(out=w1.ap, in_=w1.ap), reads=[w1], writes=[w1])
                    k.op("DVE", lambda e: e.tensor_tensor(out=w2.ap, in0=ex.ap, in1=w1.ap, op=ALU.mult), reads=[ex, w1], writes=[w2])
                    k.op("DVE", lambda e: e.tensor_tensor(out=w1.ap, in0=w1.ap, in1=gw.ap, op=ALU.mult), reads=[w1, gw], writes=[w1])
                    k.op("DVE", lambda e: e.tensor_tensor(out=w2.ap, in0=w2.ap, in1=gw.ap, op=ALU.mult), reads=[w2, gw], writes=[w2])
                    k.op("DVE", lambda e: e.tensor_scalar(out=r_ew.ap, in0=r_oh1.ap, scalar1=w1.ap, scalar2=None, op0=ALU.mult), reads=[r_oh1, w1], writes=[r_ew])
                    k.op("DVE", lambda e: e.scalar_tensor_tensor(out=r_ew.ap, in0=r_oh2.ap, scalar=w2.ap, in1=r_ew.ap, op0=ALU.mult, op1=ALU.add), reads=[r_oh2, w2, r_ew], writes=[r_ew])
                    k.op("DVE", lambda e: e.tensor_tensor(out=v3(r_G.ap, 8), in0=r_goh.ap.unsqueeze(2).to_broadcast([128, 8, 8]),
                                                          in1=r_ew.ap.unsqueeze(1).to_broadcast([128, 8, 8]), op=ALU.mult), reads=[r_goh, r_ew], writes=[r_G])
                    ptr = PD[b % 2]
                    k.op("PE", lambda e: e.transpose(ptr.ap[0:64, 0:128], r_G.ap, ident.ap), reads=[r_G, ident], writes=[ptr])
                    k.op("DVE", lambda e: e.tensor_copy(out=GT.ap[:, bs], in_=ptr.ap[0:64, 0:128]), reads=[ptr], writes=[GT])
            def load_expert(e_):
                wb = WB[e_ % 2]
                k.dma("POOL", v3(wb.ap[:, 0:4096], 8), wg[e_].rearrange("(a p) n -> p a n", p=128), writes=[wb])
                k.dma("POOL", v3(wb.ap[:, 4096:8192], 8), wu[e_].rearrange("(a p) n -> p a n", p=128), writes=[wb], part=True)
                k.dma("POOL", v3(wb.ap[:, 8192:12288], 4), wd[e_].rearrange("(a p) n -> p a n", p=128), writes=[wb], part=True)

            def emit_gu(e_, t, i):
                wb = WB[e_ % 2]
                Wg3 = v3(wb.ap[:, 0:4096], 8)
                Wu3 = v3(wb.ap[:, 4096:8192], 8)
                pb = PBK[i % 2]
                eb = EB[i % 2]
                k.op("DVE", lambda e: e.tensor_copy(out=eb.ap, in_=ident.ap[0:64, e_:e_ + 1].to_broadcast([64, 128])), reads=[ident], writes=[eb])
                k.op("PE", lambda e: e.matmul(pb.ap, eb.ap, GT.ap[:, t * 512:(t + 1) * 512], start=True, stop=True), reads=[eb, GT], writes=[pb])
                for f in range(4):
                    pg, pu = PG[f % 2], PU[f % 2]
                    for kc in range(8):
                        k.op("PE", lambda e: e.matmul(pg.ap, Wg3[:, kc, f * 128:(f + 1) * 128], UT[t].ap[:, kc, :], start=(kc == 0), stop=(kc == 7)),
                             reads=[wb, UT[t]], writes=[pg], pe_acc=(kc > 0))
                    for kc in range(8):
                        k.op("PE", lambda e: e.matmul(pu.ap, Wu3[:, kc, f * 128:(f + 1) * 128], UT[t].ap[:, kc, :], start=(kc == 0), stop=(kc == 7)),
                             reads=[wb, UT[t]], writes=[pu], pe_acc=(kc > 0))
                    s_, tt = S_[f % 2], TT[f % 2]
                    k.op("ACT", lambda e: e.activation(out=s_.ap, in_=pg.ap, func=AF.Silu), reads=[pg], writes=[s_])
                    k.op("DVE", lambda e: e.tensor_tensor(out=tt.ap, in0=s_.ap, in1=pu.ap, op=ALU.mult), reads=[s_, pu], writes=[tt])
                    k.op("DVE", lambda e: e.tensor_tensor(out=HS[i % 2][f].ap, in0=tt.ap, in1=pb.ap, op=ALU.mult), reads=[tt, pb], writes=[HS[i % 2][f]])

            def emit_dn(e_, t, i):
                wb = WB[e_ % 2]
                Wd3 = v3(wb.ap[:, 8192:12288], 4)
                for d in range(8):
                    pd = PD[d % 2]
                    for f in range(4):
                        k.op("PE", lambda e: e.matmul(pd.ap, Wd3[:, f, d * 128:(d + 1) * 128], HS[i % 2][f].ap, start=(f == 0), stop=(f == 3)),
                             reads=[wb, HS[i % 2][f]], writes=[pd], pe_acc=(f > 0))
                    k.op("DVE", lambda e: e.tensor_tensor(out=HT[t][d].ap, in0=HT[t][d].ap, in1=pd.ap, op=ALU.add), reads=[pd, HT[t][d]], writes=[HT[t][d]])

            seq = [(e_, t) for e_ in range(NE) for t in range(NTL)]
            load_expert(0)
            prev = None
            for i, (e_, t) in enumerate(seq):
                emit_gu(e_, t, i)
                if prev is not None:
                    emit_dn(*prev)
                if t == 0 and e_ + 1 < NE:
                    load_expert(e_ + 1)
                prev = (e_, t, i)
            emit_dn(*prev)
            k.dma("POOL", v3(WB[0].ap[:, 0:8192], 8), plg.rearrange("(a p) n -> p a n", p=128), writes=[WB[0]])
            k.dma("POOL", v3(WB[1].ap[:, 0:2048], 2), plp.rearrange("(a p) n -> p a n", p=128), writes=[WB[1]])
            for t in range(NTL):
                cs = slice(c0 + t * 512, c0 + (t + 1) * 512)
                hap = h3[:, :, t * 512:(t + 1) * 512]
                k.dma("POOL", v3(PB.ap, 2), pT[:, cs].rearrange("(a p) n -> p a n", p=128), writes=[PB])
                rmsnorm(HT[t], hap, 8, UT[t], UT[t].ap, PBK[0])
                for d in range(8):
                    pg, pu = PG[d % 2], PU[d % 2]
                    for kc in range(8):
                        k.op("PE", lambda e: e.matmul(pg.ap, v3(WB[0].ap[:, 0:8192], 8)[:, kc, d * 128:(d + 1) * 128], UT[t].ap[:, kc, :],
                                                      start=(kc == 0), stop=(kc == 7)), reads=[WB[0], UT[t]], writes=[pg], pe_acc=(kc > 0))
                    for kc in range(2):
                        k.op("PE", lambda e: e.matmul(pu.ap, v3(WB[1].ap[:, 0:2048], 2)[:, kc, d * 128:(d + 1) * 128], v3(PB.ap, 2)[:, kc, :],
                                                      start=(kc == 0), stop=(kc == 1)), reads=[WB[1], PB], writes=[pu], pe_acc=(kc > 0))
                    sf = STF[d % 2]
                    k.op("ACT", lambda e: e.activation(out=sf.ap, in_=pg.ap, func=AF.Sigmoid, bias=vec.ap[:, 16 + d:17 + d], scale=1.0), reads=[pg, vec], writes=[sf])
                    k.op("DVE", lambda e: e.tensor_tensor(out=sf.ap, in0=sf.ap, in1=pu.ap, op=ALU.mult), reads=[sf, pu], writes=[sf])
                    k.op("DVE", lambda e: e.tensor_tensor(out=HT[t][d].ap, in0=HT[t][d].ap, in1=sf.ap, op=ALU.add), reads=[sf, HT[t][d]], writes=[HT[t][d]])
            if mode == "A":
                k.dma("POOL", v3(WB[0].ap, 8), wqkv[:, 0:1536].rearrange("(a p) n -> p a n", p=128), writes=[WB[0]])
                k.dma("POOL", v3(WB[1].ap, 8), wqkv[:, 1536:3072].rearrange("(a p) n -> p a n", p=128), writes=[WB[1]])
                for t in range(NTL):
                    cs = slice(c0 + t * 512, c0 + (t + 1) * 512)
                    hap = h3[:, :, t * 512:(t + 1) * 512]
                    for d in range(8):
                        k.dma("SP", hT_o[d * 128:(d + 1) * 128, cs], HT[t][d].ap, reads=[HT[t][d]], writes=[ho_t], part=True)
                    rmsnorm(HT[t], hap, 24, UT[t], UT[t].ap, PBK[0])
                    U3 = UT[t].ap
                    for n in range(16):
                        col = n * 128
                        wbt = WB[0] if col < 1536 else WB[1]
                        lc = col if col < 1536 else col - 1536
                        bank = PG[n % 2]
                        for kc in range(8):
                            k.op("PE", lambda e: e.matmul(bank.ap, v3(wbt.ap, 8)[:, kc, lc:lc + 128], U3[:, kc, :], start=(kc == 0), stop=(kc == 7)),
                                 reads=[wbt, UT[t]], writes=[bank], pe_acc=(kc > 0))
                        sg = STG[n % 2]
                        sc = (1.0 / np.sqrt(128.0)) if n < 8 else 1.0
                        k.op("ACT", lambda e: e.activation(out=sg.ap, in_=bank.ap, func=AF.Identity, scale=float(sc)), reads=[bank], writes=[sg])
                        if n < 8:
                            k.dma("SP", qT_o[n * 128:(n + 1) * 128, cs], sg.ap, reads=[sg], writes=[qT_t], part=True)
                        else:
                            k.dma("SP", kT_o[(n - 8) * 128:(n - 7) * 128, cs], sg.ap, reads=[sg], writes=[kT_t], part=True)
                    for b in range(4):
                        for hh in range(2):
                            bank = PU[hh]
                            for kc in range(8):
                                k.op("PE", lambda e: e.matmul(bank.ap, U3[:, kc, b * 128:(b + 1) * 128], v3(WB[1].ap, 8)[:, kc, 512 + hh * 512:1024 + hh * 512],
                                                              start=(kc == 0), stop=(kc == 7)), reads=[WB[1], UT[t]], writes=[bank], pe_acc=(kc > 0))
                            sg = STG[hh]
                            k.op("ACT", lambda e: e.activation(out=sg.ap, in_=bank.ap, func=AF.Copy), reads=[bank], writes=[sg])
                            r0 = c0 + t * 512 + b * 128
                            k.dma("SP", v_o[r0:r0 + 128, hh * 512:(hh + 1) * 512], sg.ap, reads=[sg], writes=[v_t], part=True)
                fin = [qT_t, kT_t, v_t, ho_t]
            else:
                for t in range(NTL):
                    cs = slice(c0 + t * 512, c0 + (t + 1) * 512)
                    hap = h3[:, :, t * 512:(t + 1) * 512]
                    k.op("ACT", lambda e: e.activation(out=v3(SQ.ap, 8), in_=hap, func=AF.Square), reads=HT[t], writes=[SQ])
                    bank = PBK[0]
                    for kc in range(8):
                        k.op("PE", lambda e: e.matmul(bank.ap, ones_bf.ap, v3(SQ.ap, 8)[:, kc, :], start=(kc == 0), stop=(kc == 7)),
                             reads=[SQ, ones_bf], writes=[bank], pe_acc=(kc > 0))
                    k.op("ACT", lambda e: e.activation(out=LNV.ap, in_=bank.ap, func=AF.Ln, scale=1.0 / 1024, bias=EPS), reads=[bank], writes=[LNV])
                    k.op("ACT", lambda e: e.activation(out=RSTD.ap, in_=LNV.ap, func=AF.Exp, scale=-0.5), reads=[LNV], writes=[RSTD])
                    for d in range(8):
                        k.op("DVE", lambda e: e.scalar_tensor_tensor(out=HT[t][d].ap, in0=HT[t][d].ap, scalar=vec.ap[:, 24 + d:25 + d], in1=RSTD.ap,
                                                                     op0=ALU.mult, op1=ALU.mult), reads=[HT[t][d], RSTD, vec], writes=[HT[t][d]])
                        k.dma("SP", out_o[d * 128:(d + 1) * 128, cs], HT[t][d].ap, reads=[HT[t][d]], writes=[out_t], part=True)
                fin = [out_t]
            if hf == NH - 1:
                k.finish("SP", fin)
        print("tok ninst", k.ninst)
    return nc


def build_attn(L, NHD=2):
    nc = bass.Bass("TRN2", target_bir_lowering=False)

    def din(name, shape, dt=F32):
        return nc.dram_tensor(name, list(shape), dt, kind="ExternalInput").ap()

    qT = din("qT", [NHD, 128, L], BF16)
    kT = din("kT", [NHD, 128, L], BF16)
    vP = din("vP", [NHD, 128, L // 128, 128], BF16)
    cst = din("cst", [128, 256 + 4 * 512])
    oT = nc.dram_tensor("oT", [NHD, 128, L], BF16, kind="ExternalOutput").ap()
    NQT = L // 512
    NB = L // 128
    with ExitStack() as st:
        k = K(nc, st)
        Qs = k.sbt([128, L], BF16, "Qs")
        Ks = k.sbt([128, L], BF16, "Ks")
        Vs = k.sbt([128, L], BF16, "Vs")
        V3 = v3(Vs.ap, NB)
        cf = k.sbt([128, 256 + 2048], F32, "cf")
        trin = k.sbt([128, 128], BF16, "trin")
        onen = k.sbt([128, 128], BF16, "onen")
        m01 = k.sbt([128, 2048], BF16, "m01")
        mneg = k.sbt([128, 2048], F32, "mneg")
        E_ = [k.sbt([128, 512], F32, "E%d" % i) for i in range(3)]
        SPf = [k.sbt([128, 512], BF16, "SPf%d" % i) for i in range(3)]
        SPm = [k.sbt([128, 512], BF16, "SPm%d" % i) for i in range(3)]
        TMP = [k.sbt([128, 512], F32, "TMP%d" % i) for i in range(3)]
        AT = [k.sbt([128, 512], BF16, "AT%d" % i) for i in range(3)]
        OFFS = [k.sbt([128, 512], F32, "OFF%d" % i) for i in range(2)]
        offi = [0]
        OST = [k.sbt([128, 512], BF16, "OST%d" % i) for i in range(2)]
        BK = [T(k.ps([128, 512], F32, "bk%d" % i)[:]) for i in range(8)]
        PA, PBb, PO = BK[0:3], BK[3:6], BK[6:8]
        oT_t = T(oT)

        k.dma("SP", cf.ap, cst, writes=[cf])
        k.op("DVE", lambda e: e.tensor_copy(out=trin.ap, in_=cf.ap[:, 0:128]), reads=[cf], writes=[trin])
        k.op("DVE", lambda e: e.tensor_copy(out=onen.ap, in_=cf.ap[:, 128:256]), reads=[cf], writes=[onen])
        k.op("DVE", lambda e: e.tensor_copy(out=m01.ap, in_=cf.ap[:, 256:2304]), reads=[cf], writes=[m01])
        k.op("DVE", lambda e: e.tensor_scalar(out=mneg.ap, in0=cf.ap[:, 256:2304], scalar1=-1.0, scalar2=30000.0, op0=ALU.add, op1=ALU.mult),
             reads=[cf], writes=[mneg])

        jobs = []
        for hd in range(NHD):
            for J in range(NQT):
                nb = 4 * J + 4
                for i, n in enumerate(range(nb - 1, -1, -1)):
                    jobs.append(dict(hd=hd, J=J, n=n, first=(i == 0), last=(n == 0), r=(n - 4 * J) if n >= 4 * J else -1))

        def load_head(hd):
            k.dma("SP", Qs.ap, qT[hd], writes=[Qs])
            k.dma("SP", Ks.ap, kT[hd], writes=[Ks])
            k.dma("SP", V3, vP[hd], writes=[Vs])

        def s1(j, i):
            if j["first"] and j["J"] == 0:
                load_head(j["hd"])
            A = PA[i % 3]
            qs = slice(j["J"] * 512, (j["J"] + 1) * 512)
            ks = slice(j["n"] * 128, (j["n"] + 1) * 128)
            k.op("PE", lambda e: e.matmul(A.ap, Ks.ap[:, ks], Qs.ap[:, qs], start=True, stop=False), reads=[Ks, Qs], writes=[A])
            k.op("ACT", lambda e: e.activation(out=E_[i % 3].ap, in_=A.ap, func=AF.Exp), reads=[A], writes=[E_[i % 3]])

        def s1b(j, i):
            k.op("ACT", lambda e: e.activation(out=SPf[i % 3].ap, in_=E_[i % 3].ap, func=AF.Ln, bias=1.0, scale=1.0), reads=[E_[i % 3]], writes=[SPf[i % 3]])
            if j["r"] >= 0:
                r = j["r"]
                k.op("POOL", lambda e: e.tensor_tensor(out=SPm[i % 3].ap, in0=SPf[i % 3].ap, in1=m01.ap[:, r * 512:(r + 1) * 512], op=ALU.mult),
                     reads=[SPf[i % 3], m01], writes=[SPm[i % 3]])

        def s2(j, i):
            A, B = PA[i % 3], PBb[i % 3]
            sp = SPm[i % 3] if j["r"] >= 0 else SPf[i % 3]
            OFF = OFFS[offi[0] % 2]
            if j["first"]:
                k.op("DVE", lambda e: e.memset(OFF.ap, 0.0), writes=[OFF])
            k.op("PE", lambda e: e.matmul(A.ap, trin.ap, sp.ap, start=False, stop=True), reads=[trin, sp], writes=[A], pe_acc=True)
            k.op("PE", lambda e: e.matmul(B.ap, onen.ap, sp.ap, start=True, stop=True), reads=[onen, sp], writes=[B])
            tm = TMP[i % 3]
            k.op("DVE", lambda e: e.tensor_tensor(out=tm.ap, in0=A.ap, in1=OFF.ap, op=ALU.add), reads=[A, OFF], writes=[tm])
            if j["r"] >= 0:
                r = j["r"]
                k.op("DVE", lambda e: e.tensor_tensor(out=tm.ap, in0=tm.ap, in1=mneg.ap[:, r * 512:(r + 1) * 512], op=ALU.add), reads=[tm, mneg], writes=[tm])
            k.op("ACT", lambda e: e.activation(out=AT[i % 3].ap, in_=tm.ap, func=AF.Exp), reads=[tm], writes=[AT[i % 3]])
            if not j["last"]:
                OFN = OFFS[(offi[0] + 1) % 2]
                k.op("DVE", lambda e: e.tensor_tensor(out=OFN.ap, in0=OFF.ap, in1=B.ap, op=ALU.add), reads=[B, OFF], writes=[OFN])
            offi[0] += 1

        grp = [0]

        def s3(j, i):
            O = PO[grp[0] % 2]
            k.op("PE", lambda e: e.matmul(O.ap, V3[:, j["n"], :], AT[i % 3].ap, start=j["first"], stop=j["last"]),
                 reads=[Vs, AT[i % 3]], writes=[O], pe_acc=(not j["first"]))
            if j["last"]:
                og = OST[grp[0] % 2]
                k.op("ACT", lambda e: e.activation(out=og.ap, in_=O.ap, func=AF.Copy), reads=[O], writes=[og])
                k.dma("SP", oT[j["hd"], :, j["J"] * 512:(j["J"] + 1) * 512], og.ap, reads=[og], writes=[oT_t], part=True)
                grp[0] += 1

        n = len(jobs)
        stages = [s1, s1b, s2, s3]
        done = [0] * n

        def run_next(jj):
            stages[done[jj]](jobs[jj], jj)
            done[jj] += 1

        for i in range(n + 3):
            if i < n and jobs[i]["first"] and jobs[i]["J"] == 0 and i > 0:
                for jj in range(max(0, i - 3), i):
                    while done[jj] < 4:
                        run_next(jj)
            for d_ in range(4):
                jj = i - d_
                if 0 <= jj < n and done[jj] == d_:
                    run_next(jj)
        assert all(x == 4 for x in done)
        k.finish("SP", [oT_t])
        print("attn ninst", k.ninst, "jobs", n)
    return nc


def attn_consts():
    s = np.arange(128)
    tri_neg = -(s[:, None] >= s[None, :]).astype(np.float32)
    ones_neg = -np.ones((128, 128), np.float32)
    q = np.arange(512)
    masks = [(q[None, :] > (s[:, None] + 128 * r)).astype(np.float32) for r in range(4)]
    return np.ascontiguousarray(np.concatenate([tri_neg, ones_neg] + masks, axis=1))


def build_ssd(L):
    nc = bass.Bass("TRN2", target_bir_lowering=False)

    def din(name, shape, dt=F32):
        return nc.dram_tensor(name, list(shape), dt, kind="ExternalInput").ap()

    xT = din("xT", [1024, L])
    w_in = din("w_in", [1024, 1288])
    vecs = din("vecs", [128, 38])
    rowc = din("rowc", [1, 1040])
    cst = din("cst", [128, 640])
    ynT = nc.dram_tensor("ynT", [512, L], BF16, kind="ExternalOutput").ap()
    NTL = L // 512
    with ExitStack() as st:
        k = K(nc, st)
        W = k.sbt([128, 8 * 1288], BF16, "W")
        W3 = v3(W.ap, 8)
        XT = [k.sbt([128, 8 * 512], F32, "XT%d" % i) for i in range(2)]
        UT = [k.sbt([128, 8 * 512], BF16, "UT%d" % i) for i in range(2)]
        SQ = k.sbt([128, 8 * 512], BF16, "SQ")
        LNV = k.sbt([128, 512], F32, "LNV")
        RSTD = k.sbt([128, 512], F32, "RSTD")
        vec = k.sbt([128, 38], F32, "vec_sb")
        bc = k.sbt([128, 1040], F32, "bc")
        cf = k.sbt([128, 640], F32, "cf")
        identb = k.sbt([128, 128], BF16, "identb")
        ones_bf = k.sbt([128, 128], BF16, "ones_bf")
        A_bc = k.sbt([128, 8], F32, "A_bc")
        XR = [k.sbt([128, 515], F32, "XR%d" % i) for i in range(6)]
        ACC = [k.sbt([128, 512], F32, "ACC%d" % i) for i in range(2)]
        XC = [k.sbt([128, 6 * 512], BF16, "XC%d" % i) for i in range(2)]
        ZS_2 = [k.sbt([128, 512], F32, "ZS_%d" % i) for i in range(3)]
        DTt_2 = [k.sbt([128, 8], F32, "DTt_%d" % i) for i in range(3)]
        DT_2 = [k.sbt([128, 8], F32, "DT_%d" % i) for i in range(3)]
        AA_2 = [k.sbt([128, 8], F32, "AA_%d" % i) for i in range(3)]
        EXPS_2 = [k.sbt([128, 24], F32, "EXPS_%d" % i) for i in range(3)]
        LH = [k.sbt([128, 128], F32, "LH%d" % i) for i in range(2)]
        DEC_2 = [k.sbt([128, 1024], F32, "DEC_%d" % i) for i in range(3)]
        CBm_2 = [k.sbt([128, 128], F32, "CBm_%d" % i) for i in range(3)]
        WT_2 = [k.sbt([128, 1024], BF16, "WT_%d" % i) for i in range(3)]
        XTOK_2 = [k.sbt([128, 512], BF16, "XTOK_%d" % i) for i in range(3)]
        BTOK_2 = [k.sbt([128, 128], BF16, "BTOK_%d" % i) for i in range(3)]
        XDT_2 = [k.sbt([128, 512], BF16, "XDT_%d" % i) for i in range(3)]
        XW_2 = [k.sbt([128, 512], BF16, "XW_%d" % i) for i in range(3)]
        Y1_2 = [k.sbt([128, 512], F32, "Y1_%d" % i) for i in range(3)]
        Y2_2 = [k.sbt([128, 512], F32, "Y2_%d" % i) for i in range(3)]
        YZ_2 = [k.sbt([128, 512], F32, "YZ_%d" % i) for i in range(3)]
        YSQ_2 = [k.sbt([128, 512], F32, "YSQ_%d" % i) for i in range(3)]
        YN_2 = [k.sbt([128, 512], BF16, "YN_%d" % i) for i in range(3)]
        SF = k.sbt([128, 512], F32, "SF")
        SBF = k.sbt([128, 512], BF16, "SBF")
        sc_2 = [[k.sbt([128, 1], F32, "sc%d_%d" % (j, i)) for i in range(3)] for j in range(3)]
        LH4 = [k.sbt([128, 128], F32, "LHx%d" % i) for i in range(2)]
        YNT = [k.sbt([128, 4 * 512], BF16, "YNT%d" % i) for i in range(2)]
        BK = [T(k.ps([128, 512], F32, "bk%d" % i)[:]) for i in range(8)]
        P_proj, P_st, P_sm, P_sg0, P_sg1, P_tr, P_yd, P_yo = BK
        ynT_t = T(ynT)

        k.dma("SP", vec.ap, vecs, writes=[vec])
        k.dma("SP", cf.ap, cst, writes=[cf])
        k.dma("SP", bc.ap, rowc.partition_broadcast(128).rearrange("p o n -> p (o n)"), writes=[bc])
        k.dma("POOL", W3, w_in.rearrange("(a p) n -> p a n", p=128), writes=[W])
        ident = cf.ap[:, 0:128]
        tri = cf.ap[:, 128:256]
        Um = cf.ap[:, 256:384]
        maskU = cf.ap[:, 384:512]
        onesf = cf.ap[:, 512:640]
        k.op("DVE", lambda e: e.tensor_copy(out=identb.ap, in_=ident), reads=[cf], writes=[identb])
        k.op("DVE", lambda e: e.memset(ones_bf.ap, 1.0), writes=[ones_bf])
        k.op("ACT", lambda e: e.activation(out=A_bc.ap, in_=bc.ap[:, 8:16], func=AF.Exp), reads=[bc], writes=[A_bc])
        k.op("DVE", lambda e: e.tensor_scalar(out=A_bc.ap, in0=A_bc.ap, scalar1=-1.0, scalar2=None, op0=ALU.mult), reads=[A_bc], writes=[A_bc])
        for cc in range(6):
            k.op("DVE", lambda e: e.memset(XR[cc].ap[:, 0:3], 0.0), writes=[XR[cc]])
        k.op("DVE", lambda e: e.memset(SF.ap, 0.0), writes=[SF])
        k.op("DVE", lambda e: e.memset(SBF.ap, 0.0), writes=[SBF])
        dtb, Dx, gn = bc.ap[:, 0:8], bc.ap[:, 16:528], bc.ap[:, 528:1040]

        def load_x(t):
            k.dma("SP", v3(XT[t % 2].ap, 8), xT[:, t * 512:(t + 1) * 512].rearrange("(a p) n -> p a n", p=128), writes=[XT[t % 2]])

        def stage_a(t):
            if t + 1 < NTL:
                load_x(t + 1)
            xt, ut, xc = XT[t % 2], UT[t % 2], XC[t % 2]
            x3, u3, xc3 = v3(xt.ap, 8), v3(ut.ap, 8), v3(xc.ap, 6)
            k.op("ACT", lambda e: e.activation(out=v3(SQ.ap, 8), in_=x3, func=AF.Square), reads=[xt], writes=[SQ])
            for kc in range(8):
                k.op("PE", lambda e: e.matmul(P_proj.ap, ones_bf.ap, v3(SQ.ap, 8)[:, kc, :], start=(kc == 0), stop=(kc == 7)),
                     reads=[SQ, ones_bf], writes=[P_proj], pe_acc=(kc > 0))
            k.op("ACT", lambda e: e.activation(out=LNV.ap, in_=P_proj.ap, func=AF.Ln, scale=1.0 / 1024, bias=EPS), reads=[P_proj], writes=[LNV])
            k.op("ACT", lambda e: e.activation(out=RSTD.ap, in_=LNV.ap, func=AF.Exp, scale=-0.5), reads=[LNV], writes=[RSTD])
            for kc in range(8):
                k.op("DVE", lambda e: e.scalar_tensor_tensor(out=u3[:, kc, :], in0=x3[:, kc, :], scalar=vec.ap[:, kc:kc + 1], in1=RSTD.ap,
                                                             op0=ALU.mult, op1=ALU.mult), reads=[xt, RSTD, vec], writes=[ut])
            for cc in range(6):
                for kc in range(8):
                    k.op("PE", lambda e: e.matmul(P_proj.ap, W3[:, kc, 512 + cc * 128:512 + (cc + 1) * 128], u3[:, kc, :], start=(kc == 0), stop=(kc == 7)),
                         reads=[W, ut], writes=[P_proj], pe_acc=(kc > 0))
                xr, acc = XR[cc], ACC[cc % 2]
                k.op("ACT", lambda e: e.activation(out=xr.ap[:, 3:515], in_=P_proj.ap, func=AF.Copy), reads=[P_proj], writes=[xr])
                k.op("DVE", lambda e: e.tensor_scalar(out=acc.ap, in0=xr.ap[:, 0:512], scalar1=vec.ap[:, 8 + cc * 4:9 + cc * 4], scalar2=None, op0=ALU.mult),
                     reads=[xr, vec], writes=[acc])
                for kk in range(1, 4):
                    k.op("DVE", lambda e: e.scalar_tensor_tensor(out=acc.ap, in0=xr.ap[:, kk:kk + 512], scalar=vec.ap[:, 8 + cc * 4 + kk:9 + cc * 4 + kk],
                                                                 in1=acc.ap, op0=ALU.mult, op1=ALU.add), reads=[xr, vec, acc], writes=[acc])
                k.op("ACT", lambda e: e.activation(out=xc3[:, cc, :], in_=acc.ap, func=AF.Silu, bias=vec.ap[:, 32 + cc:33 + cc], scale=1.0),
                     reads=[acc, vec], writes=[xc])
                k.op("DVE", lambda e: e.tensor_copy(out=xr.ap[:, 0:3], in_=xr.ap[:, 512:515]), reads=[xr], writes=[xr])

        def chunk_gen(t, q):
            ut, xc = UT[t % 2], XC[t % 2]
            u3, xc3 = v3(ut.ap, 8), v3(xc.ap, 6)
            ynt = YNT[t % 2]
            cs = slice(q * 128, (q + 1) * 128)
            ci = t * 4 + q
            ZS = ZS_2[ci % 3]
            DTt = DTt_2[ci % 3]
            DT = DT_2[ci % 3]
            AA = AA_2[ci % 3]
            EXPS = EXPS_2[ci % 3]
            DEC = DEC_2[ci % 3]
            CBm = CBm_2[ci % 3]
            WT = WT_2[ci % 3]
            XTOK = XTOK_2[ci % 3]
            BTOK = BTOK_2[ci % 3]
            XDT = XDT_2[ci % 3]
            XW = XW_2[ci % 3]
            Y1 = Y1_2[ci % 3]
            Y2 = Y2_2[ci % 3]
            YZ = YZ_2[ci % 3]
            YSQ = YSQ_2[ci % 3]
            YN = YN_2[ci % 3]
            sc = sc_2[ci % 3]
            for kc in range(8):
                k.op("PE", lambda e: e.matmul(P_proj.ap, u3[:, kc, cs], W3[:, kc, 0:512], start=(kc == 0), stop=(kc == 7)),
                     reads=[W, ut], writes=[P_proj], pe_acc=(kc > 0))
            k.op("ACT", lambda e: e.activation(out=ZS.ap, in_=P_proj.ap, func=AF.Silu), reads=[P_proj], writes=[ZS])
            yield
            for kc in range(8):
                k.op("PE", lambda e: e.matmul(P_sm.ap[:, 0:8], u3[:, kc, cs], W3[:, kc, 1280:1288], start=(kc == 0), stop=(kc == 7)),
                     reads=[W, ut], writes=[P_sm], pe_acc=(kc > 0))
            k.op("DVE", lambda e: e.tensor_tensor(out=DTt.ap, in0=P_sm.ap[:, 0:8], in1=dtb, op=ALU.add), reads=[P_sm, bc], writes=[DTt])
            k.op("ACT", lambda e: e.activation(out=DTt.ap, in_=DTt.ap, func=AF.Exp), reads=[DTt], writes=[DTt])
            k.op("ACT", lambda e: e.activation(out=DT.ap, in_=DTt.ap, func=AF.Ln, bias=1.0, scale=1.0), reads=[DTt], writes=[DT])
            k.op("DVE", lambda e: e.tensor_tensor(out=AA.ap, in0=DT.ap, in1=A_bc.ap, op=ALU.mult), reads=[DT, A_bc], writes=[AA])
            yield
            trb = P_tr.ap.bitcast(BF16)
            for cc in range(4):
                k.op("PE", lambda e: e.transpose(trb[:, cc * 128:(cc + 1) * 128], xc3[:, cc, cs], identb.ap), reads=[xc, identb], writes=[P_tr])
            k.op("PE", lambda e: e.transpose(trb[:, 512:640], xc3[:, 4, cs], identb.ap), reads=[xc, identb], writes=[P_tr])
            k.op("ACT", lambda e: e.activation(out=XTOK.ap, in_=trb[:, 0:512], func=AF.Copy), reads=[P_tr], writes=[XTOK])
            k.op("ACT", lambda e: e.activation(out=BTOK.ap, in_=trb[:, 512:640], func=AF.Copy), reads=[P_tr], writes=[BTOK])
            yield
            k.op("PE", lambda e: e.matmul(P_sm.ap[:, 32:40], tri, AA.ap, start=True, stop=True), reads=[cf, AA], writes=[P_sm])
            k.op("PE", lambda e: e.matmul(P_sm.ap[:, 40:48], Um, AA.ap, start=True, stop=True), reads=[cf, AA], writes=[P_sm])
            k.op("PE", lambda e: e.matmul(P_sm.ap[:, 48:56], onesf, AA.ap, start=True, stop=True), reads=[cf, AA], writes=[P_sm])
            k.op("ACT", lambda e: e.activation(out=EXPS.ap, in_=P_sm.ap[:, 32:56], func=AF.Exp), reads=[P_sm], writes=[EXPS])
            e_l, dte, cd = EXPS.ap[:, 0:8], EXPS.ap[:, 8:16], EXPS.ap[:, 16:24]
            yield
            k.op("PE", lambda e: e.matmul(P_sm.ap[:, 128:256], xc3[:, 4, cs], xc3[:, 5, cs], start=True, stop=True), reads=[xc], writes=[P_sm])
            k.op("DVE", lambda e: e.tensor_tensor(out=CBm.ap, in0=P_sm.ap[:, 128:256], in1=maskU, op=ALU.mult), reads=[P_sm, cf], writes=[CBm])
            yield
            for hh in range(8):
                lh = (LH + LH4)[hh % 4]
                k.op("DVE", lambda e: e.tensor_scalar(out=lh.ap, in0=Um, scalar1=AA.ap[:, hh:hh + 1], scalar2=None, op0=ALU.mult), reads=[cf, AA], writes=[lh])
                bank = P_sg0 if hh < 4 else P_sg1
                k.op("PE", lambda e: e.matmul(bank.ap[:, (hh % 4) * 128:(hh % 4 + 1) * 128], lh.ap, tri, start=True, stop=True), reads=[lh, cf], writes=[bank])
            k.op("ACT", lambda e: e.activation(out=DEC.ap[:, 0:512], in_=P_sg0.ap, func=AF.Exp), reads=[P_sg0], writes=[DEC])
            yield
            k.op("ACT", lambda e: e.activation(out=DEC.ap[:, 512:1024], in_=P_sg1.ap, func=AF.Exp), reads=[P_sg1], writes=[DEC])
            k.op("DVE", lambda e: e.tensor_tensor(out=v3(WT.ap, 8), in0=v3(DEC.ap, 8), in1=CBm.ap.unsqueeze(1).to_broadcast([128, 8, 128]), op=ALU.mult),
                 reads=[DEC, CBm], writes=[WT])
            k.op("DVE", lambda e: e.tensor_tensor(out=v3(XDT.ap, 8), in0=v3(XTOK.ap, 8), in1=DT.ap.unsqueeze(2).to_broadcast([128, 8, 64]), op=ALU.mult),
                 reads=[XTOK, DT], writes=[XDT])
            k.op("DVE", lambda e: e.tensor_tensor(out=v3(XW.ap, 8), in0=v3(XDT.ap, 8), in1=dte.unsqueeze(2).to_broadcast([128, 8, 64]), op=ALU.mult),
                 reads=[XDT, EXPS], writes=[XW])
            yield
            for hh in range(8):
                k.op("PE", lambda e: e.matmul(P_yd.ap[:, hh * 64:(hh + 1) * 64], v3(WT.ap, 8)[:, hh, :], v3(XDT.ap, 8)[:, hh, :], start=True, stop=True),
                     reads=[WT, XDT], writes=[P_yd])
            k.op("PE", lambda e: e.matmul(P_yo.ap, xc3[:, 5, cs], SBF.ap, start=True, stop=True), reads=[xc, SBF], writes=[P_yo])
            k.op("DVE", lambda e: e.tensor_tensor(out=v3(Y1.ap, 8), in0=v3(P_yo.ap, 8), in1=e_l.unsqueeze(2).to_broadcast([128, 8, 64]), op=ALU.mult),
                 reads=[P_yo, EXPS], writes=[Y1])
            k.op("DVE", lambda e: e.tensor_tensor(out=Y1.ap, in0=Y1.ap, in1=P_yd.ap, op=ALU.add), reads=[Y1, P_yd], writes=[Y1])
            yield
            k.op("PE", lambda e: e.matmul(P_st.ap, BTOK.ap, XW.ap, start=True, stop=True), reads=[BTOK, XW], writes=[P_st])
            k.op("DVE", lambda e: e.tensor_tensor(out=v3(SF.ap, 8), in0=v3(SF.ap, 8), in1=cd.unsqueeze(2).to_broadcast([128, 8, 64]), op=ALU.mult),
                 reads=[SF, EXPS], writes=[SF])
            k.op("DVE", lambda e: e.tensor_tensor(out=SF.ap, in0=SF.ap, in1=P_st.ap, op=ALU.add), reads=[SF, P_st], writes=[SF])
            k.op("ACT", lambda e: e.activation(out=SBF.ap, in_=SF.ap, func=AF.Copy), reads=[SF], writes=[SBF])
            yield
            k.op("DVE", lambda e: e.tensor_tensor(out=Y2.ap, in0=XTOK.ap, in1=Dx, op=ALU.mult), reads=[XTOK, bc], writes=[Y2])
            k.op("DVE", lambda e: e.tensor_tensor(out=Y2.ap, in0=Y2.ap, in1=Y1.ap, op=ALU.add), reads=[Y2, Y1], writes=[Y2])
            k.op("DVE", lambda e: e.tensor_tensor(out=YZ.ap, in0=Y2.ap, in1=ZS.ap, op=ALU.mult), reads=[Y2, ZS], writes=[YZ])
            k.op("ACT", lambda e: e.activation(out=YSQ.ap, in_=YZ.ap, func=AF.Square, accum_out=sc[0].ap), reads=[YZ], writes=[YSQ, sc[0]])
            k.op("ACT", lambda e: e.activation(out=sc[1].ap, in_=sc[0].ap, func=AF.Ln, scale=1.0 / 512, bias=EPS), reads=[sc[0]], writes=[sc[1]])
            k.op("ACT", lambda e: e.activation(out=sc[2].ap, in_=sc[1].ap, func=AF.Exp, scale=-0.5), reads=[sc[1]], writes=[sc[2]])
            k.op("DVE", lambda e: e.scalar_tensor_tensor(out=YN.ap, in0=YZ.ap, scalar=sc[2].ap, in1=gn, op0=ALU.mult, op1=ALU.mult),
                 reads=[YZ, sc[2], bc], writes=[YN])
            yield
            for cc in range(4):
                k.op("PE", lambda e: e.transpose(trb[:, cc * 128:(cc + 1) * 128], YN.ap[:, cc * 128:(cc + 1) * 128], identb.ap), reads=[YN, identb], writes=[P_tr])
            k.op("ACT", lambda e: e.activation(out=v3(ynt.ap, 4)[:, :, cs], in_=trb[:, 0:512].rearrange("p (a n) -> p a n", a=4), func=AF.Copy),
                 reads=[P_tr], writes=[ynt])
            if q == 3:
                k.dma("SP", ynT[:, t * 512:(t + 1) * 512].rearrange("(a p) n -> p a n", p=128), v3(ynt.ap, 4), reads=[ynt], writes=[ynT_t], part=True)


        load_x(0)
        NS, PER = 12, 6
        chunks = [(t, q) for t in range(NTL) for q in range(4)]
        gens = {}
        nslot = (len(chunks) - 1) * PER + NS + 2
        for tau in range(nslot):
            for ci, (t, q) in enumerate(chunks):
                st_ = tau - ci * PER
                if st_ < 0:
                    break
                if ci in gens and gens[ci] is None:
                    continue
                if ci not in gens:
                    if q == 0:
                        stage_a(t)
                    gens[ci] = chunk_gen(t, q)
                try:
                    next(gens[ci])
                except StopIteration:
                    gens[ci] = None
        k.finish("SP", [ynT_t])
        print("ssd ninst", k.ninst)
    return nc


def ssd_consts():
    s = np.arange(128)
    ident = np.eye(128, dtype=np.float32)
    tri = (s[:, None] <= s[None, :]).astype(np.float32)
    U = (s[:, None] > s[None, :]).astype(np.float32)
    maskU = (s[None, :] >= s[:, None]).astype(np.float32)
    ones = np.ones((128, 128), np.float32)
    return np.ascontiguousarray(np.concatenate([ident, tri, U, maskU, ones], axis=1))


def ssd_host_inputs(g, ssd_norm, w_in, conv_w, conv_b, dt_bias, a_log, d_skip, gnorm):
    cols = np.concatenate([np.arange(g * 512, (g + 1) * 512), 2048 + np.arange(g * 512, (g + 1) * 512),
                           4096 + np.arange(g * 128, (g + 1) * 128), 4096 + 512 + np.arange(g * 128, (g + 1) * 128),
                           5120 + np.arange(g * 8, (g + 1) * 8)])
    w = np.ascontiguousarray(w_in[:, cols])
    ch = np.concatenate([np.arange(g * 512, (g + 1) * 512), 2048 + np.arange(g * 128, (g + 1) * 128), 2048 + 512 + np.arange(g * 128, (g + 1) * 128)])
    cw = conv_w[:, ch]
    cb = conv_b[ch]
    vecs = np.zeros((128, 38), np.float32)
    vecs[:, 0:8] = ssd_norm.reshape(8, 128).T
    for cc in range(6):
        vecs[:, 8 + cc * 4:12 + cc * 4] = cw[:, cc * 128:(cc + 1) * 128].T
        vecs[:, 32 + cc] = cb[cc * 128:(cc + 1) * 128]
    rowc = np.concatenate([dt_bias[g * 8:(g + 1) * 8], a_log[g * 8:(g + 1) * 8], np.repeat(d_skip[g * 8:(g + 1) * 8], 64),
                           gnorm[g * 512:(g + 1) * 512]])[None, :].astype(np.float32)
    return w, vecs, np.ascontiguousarray(rowc)


_CACHE = {}


def _prog(name, fn, *a):
    key = (name,) + a
    if key not in _CACHE:
        _CACHE[key] = fn(*a)
    return _CACHE[key]


def _vcol(v):
    return np.asarray(v, np.float32).reshape(8, 128).T


def kernel(x, p, ssd_norm, ssd_w_in, ssd_conv_w, ssd_conv_b, ssd_dt_bias, ssd_a_log, ssd_d, ssd_gnorm, ssd_w_out,
           sb_norm, sb_w_qkv, sb_w_o, moe_norm, moe_w_rg, moe_b_rg, moe_w_re, moe_b_re, moe_w_gate, moe_w_up, moe_w_down,
           ple_norm, ple_w_gate, ple_b_gate, ple_w_proj, final_norm):
    f32 = lambda a: np.ascontiguousarray(np.asarray(a, dtype=np.float32))
    x, p = f32(x), f32(p)
    Bsz, L, D = x.shape
    NC = 8
    NT = (Bsz * L) // NC
    PB = NC // Bsz
    cores = list(range(NC))
    ident = np.eye(128, dtype=np.float32)

    nc1 = _prog("ssd", build_ssd, L)
    cst1 = ssd_consts()
    xTb = [np.ascontiguousarray(x[b].T) for b in range(Bsz)]
    maps = []
    for c in cores:
        b, g = c // PB, c % PB
        w, vecs, rowc = ssd_host_inputs(g, f32(ssd_norm[0]), f32(ssd_w_in[0]), f32(ssd_conv_w[0]), f32(ssd_conv_b[0]),
                                        f32(ssd_dt_bias[0]), f32(ssd_a_log[0]), f32(ssd_d[0]), f32(ssd_gnorm[0]))
        maps.append(dict(xT=xTb[b], w_in=w, vecs=vecs, rowc=rowc, cst=cst1))
    r1 = run_bass_kernel_spmd(nc1, maps, core_ids=cores).results
    ynT = [np.concatenate([r1[b * PB + g]["ynT"] for g in range(PB)], axis=0) for b in range(Bsz)]

    mcst = moe_consts(NT)

    def tok_maps(i, hT_list, aT_list, w_a, tail_norm, extra):
        vecs = np.ascontiguousarray(np.concatenate([_vcol(moe_norm[i]), _vcol(ple_norm[i]), _vcol(ple_b_gate[i]), _vcol(tail_norm)], axis=1))
        wr = np.ascontiguousarray(np.concatenate([f32(moe_w_rg[i]), f32(moe_w_re[i])], axis=1))
        br = np.ascontiguousarray(np.concatenate([f32(moe_b_rg[i]), f32(moe_b_re[i])])[None, :])
        wg_, wu_, wd_ = f32(moe_w_gate[i]), f32(moe_w_up[i]), f32(moe_w_down[i])
        plg_, plp_ = f32(ple_w_gate[i]), f32(ple_w_proj[i])
        out = []
        for c in cores:
            b, j = c // PB, c % PB
            sl = slice(j * NT, (j + 1) * NT)
            m = dict(hT=hT_list[c], aT=np.ascontiguousarray(aT_list[b][:, sl]), w_a=w_a, vecs=vecs, wr=wr, br=br, wg=wg_, wu=wu_, wd=wd_,
                     plg=plg_, plp=plp_, pT=np.ascontiguousarray(p[i, b, sl].T), mcst=mcst)
            m.update(extra)
            out.append(m)
        return out

    nc2 = _prog("tokA", build_tok2, NT, 2048, "A")
    hT0 = [np.ascontiguousarray(x[c // PB, (c % PB) * NT:(c % PB + 1) * NT].T) for c in cores]
    r2 = run_bass_kernel_spmd(nc2, tok_maps(0, hT0, ynT, f32(ssd_w_out[0]), f32(sb_norm[0]), dict(wqkv=f32(sb_w_qkv[0]))), core_ids=cores).results
    hT1 = [r2[c]["hT_o"] for c in cores]
    qTb = [np.concatenate([r2[b * PB + j]["qT_o"] for j in range(PB)], axis=1) for b in range(Bsz)]
    kTb = [np.concatenate([r2[b * PB + j]["kT_o"] for j in range(PB)], axis=1) for b in range(Bsz)]
    vb = [np.concatenate([r2[b * PB + j]["v_o"] for j in range(PB)], axis=0) for b in range(Bsz)]

    nc3 = _prog("attn", build_attn, L)
    cst3 = attn_consts()
    maps = []
    for c in cores:
        b, hp = c // PB, c % PB
        rows = slice(hp * 256, (hp + 1) * 256)
        vP = np.ascontiguousarray(vb[b][:, rows].reshape(L // 128, 128, 2, 128).transpose(2, 1, 0, 3))
        maps.append(dict(qT=np.ascontiguousarray(qTb[b][rows].reshape(2, 128, L)), kT=np.ascontiguousarray(kTb[b][rows].reshape(2, 128, L)),
                         vP=vP, cst=cst3))
    r3 = run_bass_kernel_spmd(nc3, maps, core_ids=cores).results
    oT = [np.concatenate([r3[b * PB + hp]["oT"].reshape(256, L) for hp in range(PB)], axis=0) for b in range(Bsz)]

    nc4 = _prog("tokB", build_tok2, NT, 1024, "B")
    r4 = run_bass_kernel_spmd(nc4, tok_maps(1, hT1, oT, f32(sb_w_o[0]), f32(final_norm), {}), core_ids=cores).results
    out = np.empty((Bsz, L, D), np.float32)
    for c in cores:
        b, j = c // PB, c % PB
        out[b, j * NT:(j + 1) * NT, :] = r4[c]["out_o"].T
    return out


I32 = mybir.dt.int32
MB = 128
NBLK_OF = lambda NT: (2 * NT) // MB + 64


def moe_consts(NT):
    nblk = NBLK_OF(NT)
    s = np.arange(128)
    SL = (s[:, None] < s[None, :]).astype(np.float32)
    THR = np.tile((np.arange(64) * MB).astype(np.float32)[None, :], (128, 1))
    JB = np.tile((np.arange(nblk) * MB).astype(np.float32)[None, :], (128, 1))
    KP = (np.arange(8)[None, :] + 2 * s[:, None]).astype(np.float32)
    return np.ascontiguousarray(np.concatenate([np.eye(128, dtype=np.float32), SL, THR, JB, KP], axis=1))


def idma(self, out_ap, in_ap, idx_t, idx_ap, gather, reads=(), writes=(), part=False, bound=None):
    t = writes[0]
    if t.dsem is None:
        key = "d%d" % self.ndsem
        t.dsem = self.stack.enter_context(self.nc.semaphore(key))
        self.semobj[key] = t.dsem
        t.name = key
        self.ndsem += 1
    key = t.name
    for r in list(reads) + [idx_t]:
        if r.w is not None:
            self._need("POOL", *r.w)
    if t.w is not None and not (part and t.w[0] == key):
        self._need("POOL", *t.w)
    for kk, v in t.r.items():
        self._need("POOL", kk, v)
    off = bass.IndirectOffsetOnAxis(ap=idx_ap, axis=0)
    if gather and bound is not None:
        ins = self.nc.gpsimd.indirect_dma_start(out=out_ap, out_offset=None, in_=in_ap, in_offset=off, bounds_check=bound, oob_is_err=False)
    elif gather:
        ins = self.nc.gpsimd.indirect_dma_start(out=out_ap, out_offset=None, in_=in_ap, in_offset=off)
    else:
        ins = self.nc.gpsimd.indirect_dma_start(out=out_ap, out_offset=off, in_=in_ap, in_offset=None)
    t.dcnt += 16
    ins.then_inc(t.dsem, 16)
    for r in list(reads) + [idx_t]:
        r.r[key] = max(r.r.get(key, 0), t.dcnt)
    t.w = (key, t.dcnt)
    if not part:
        t.r = {}
    self.ninst += 1
    return ins


K.idma = idma


def build_tok2(NT, KA, mode):
    nc = bass.Bass("TRN2", target_bir_lowering=False)

    def din(name, shape, dt=F32):
        return nc.dram_tensor(name, list(shape), dt, kind="ExternalInput").ap()

    def dout(name, shape, dt=F32):
        return nc.dram_tensor(name, list(shape), dt, kind="ExternalOutput").ap()

    NBLK = NBLK_OF(NT)
    NROWS = NBLK * MB
    NTB = NT // 128
    NTILE = NT // 512
    hT = din("hT", [1024, NT])
    aT = din("aT", [KA, NT], BF16)
    w_a = din("w_a", [KA, 1024])
    vecs = din("vecs", [128, 32])
    wr = din("wr", [1024, 72])
    br = din("br", [1, 72])
    wg = din("wg", [64, 1024, 512])
    wu = din("wu", [64, 1024, 512])
    wd = din("wd", [64, 512, 1024])
    plg = din("plg", [1024, 1024])
    plp = din("plp", [256, 1024])
    pT = din("pT", [256, NT])
    NCST = 256 + 64 + NBLK + 8
    mcst = din("mcst", [128, NCST])
    if mode == "A":
        wqkv = din("wqkv", [1024, 3072])
        hT_o = dout("hT_o", [1024, NT])
        qT_o = dout("qT_o", [1024, NT], BF16)
        kT_o = dout("kT_o", [1024, NT], BF16)
        v_o = dout("v_o", [NT, 1024], BF16)
    else:
        out_o = dout("out_o", [1024, NT])
    H1 = nc.dram_tensor("H1s", [1024, NT], F32, kind="Internal").ap()
    Xs = nc.dram_tensor("Xs", [NROWS, 1024], BF16, kind="Internal").ap()
    Ys = nc.dram_tensor("Ys", [NROWS, 1024], F32, kind="Internal").ap()
    wg_r = wg.rearrange("e (p h r) n -> (e p h) (r n)", p=128, h=2, r=4)
    wu_r = wu.rearrange("e (p h r) n -> (e p h) (r n)", p=128, h=2, r=4)
    wd_r = wd.rearrange("e (p h r) n -> (e p h) (r n)", p=128, h=2, r=2)
    KAC = KA // 128

    with ExitStack() as st:
        k = K(nc, st)
        HTL = [k.sbt([128, 8 * 512], F32, "htl%d" % i) for i in range(1)] * 2
        UTL = [k.sbt([128, 8 * 512], BF16, "utl%d" % i) for i in range(1)] * 2
        WB = [k.sbt([128, 12288], BF16, "wb%d" % i) for i in range(2)]
        RA = k.sb([128, 8192], BF16, "regA")
        RAf = RA[:].bitcast(F32)
        ABS = [T(RA[:, 0:KAC * 512])]
        BIG = T(RAf[:, 0:2048])
        IGF = T(RAf[:, 2048:3072])
        XB = [T(RA[:, i * 1024:(i + 1) * 1024]) for i in range(2)]
        XTB = [T(RA[:, 2048 + i * 1024:2048 + (i + 1) * 1024]) for i in range(2)]
        YB = [T(RAf[:, 2048 + i * 1024:2048 + (i + 1) * 1024]) for i in range(2)]
        NRX = max(NTB * 1024, 32768)
        RX = k.sb([128, NRX], BF16, "regX")
        RXf = RX[:].bitcast(F32)
        XROWS = T(RX[:, 0:NTB * 1024])
        XR3 = v3(XROWS.ap, NTB)
        WQ = [T(RX[:, i * 12288:(i + 1) * 12288]) for i in range(2)]
        Y0 = [T(RXf[:, 12288 + i * 1024:12288 + (i + 1) * 1024]) for i in range(2)]
        Y1 = [T(RXf[:, 14336 + i * 1024:14336 + (i + 1) * 1024]) for i in range(2)]
        PBt = k.sbt([128, 2 * 512], BF16, "pbt")
        LNV = k.sbt([128, 512], F32, "lnv")
        RSTD = k.sbt([128, 512], F32, "rstd")
        vec = k.sbt([128, 32], F32, "vec_sb")
        cf = k.sbt([128, NCST], F32, "cf")
        ident = cf.ap[:, 0:128]
        identb = k.sbt([128, 128], BF16, "identb")
        SLb = k.sbt([128, 128], BF16, "SLb")
        ones_bf = k.sbt([128, 128], BF16, "ones_bf")
        wr_sb = k.sbt([128, 8 * 72], F32, "wr_sb")
        br_bc = k.sbt([128, 72], F32, "brbc")
        STG = [k.sbt([128, 512], BF16, "stg%d" % i) for i in range(2)]
        STF = [k.sbt([128, 512], F32, "stf%d" % i) for i in range(2)]
        r_lg = k.sbt([128, 72], F32, "r_lg")
        r_a = k.sbt([128, 8], F32, "r_a")
        r_goh = k.sbt([128, 8], F32, "r_goh")
        r_t64 = k.sbt([128, 64], F32, "r_t64")
        r_es = k.sbt([128, 8], F32, "r_es")
        r_es2 = k.sbt([128, 8], F32, "r_es2")
        r_oh1 = k.sbt([128, 8], F32, "r_oh1")
        r_oh2 = k.sbt([128, 8], F32, "r_oh2")
        r_s = [k.sbt([128, 1], F32, "r_s%d" % i) for i in range(12)]
        OHC = k.sbt([128, 128], BF16, "ohc")
        OHS = k.sbt([128, NTB * 128], BF16, "ohs")
        OHS3 = v3(OHS.ap, NTB)
        RUN = k.sbt([128, 128], F32, "run")
        PRS = k.sbt([128, 128], F32, "prs")
        JNK = k.sbt([128, 64], F32, "jnk")
        RK0 = k.sbt([128, NTB], F32, "rk0")
        RK1 = k.sbt([128, NTB], F32, "rk1")
        GA = k.sbt([128, NTB], F32, "ga")
        GB = k.sbt([128, NTB], F32, "gb")
        DI0 = k.sbt([128, NTB], I32, "di0")
        DI1 = k.sbt([128, NTB], I32, "di1")
        CNT = k.sbt([128, 64], F32, "cnt")
        NB_ = k.sbt([128, 64], F32, "nb_")
        PE_ = [k.sbt([128, 64], F32, "pe%d" % i) for i in range(2)]
        BASE1 = k.sbt([128, 64], F32, "base1")
        BASE2 = k.sbt([128, 64], F32, "base2")
        DF = k.sbt([128, NTB], F32, "df")
        EJ = k.sbt([128, NBLK], F32, "ej")
        SAME = k.sbt([128, NBLK], F32, "same")
        IGI = k.sbt([128, NBLK * 8], I32, "igi")
        IDI = k.sbt([128, NBLK * 4], I32, "idi")
        SS = [k.sbt([128, 512], BF16, "ss%d" % i) for i in range(2)]
        HH = [k.sbt([128, 512], BF16, "hh%d" % i) for i in range(2)]
        BK = [T(k.ps([128, 512], F32, "bk%d" % i)[:]) for i in range(8)]
        PG, PU, PD, PBK = BK[0:2], BK[2:4], BK[4:6], BK[6:8]
        H1_t, Xs_t, Ys_t = T(H1), T(Xs), T(Ys)
        if mode == "A":
            qT_t, kT_t, v_t, ho_t = T(qT_o), T(kT_o), T(v_o), T(hT_o)
        else:
            out_t = T(out_o)

        k.dma("SP", vec.ap, vecs, writes=[vec])
        k.dma("SP", cf.ap, mcst, writes=[cf])
        k.dma("SP", v3(wr_sb.ap, 8), wr.rearrange("(a p) n -> p a n", p=128), writes=[wr_sb])
        k.dma("SP", br_bc.ap, br.partition_broadcast(128).rearrange("p o n -> p (o n)"), writes=[br_bc])
        k.op("DVE", lambda e: e.memset(ones_bf.ap, 1.0), writes=[ones_bf])
        k.op("DVE", lambda e: e.memset(RUN.ap, 0.0), writes=[RUN])
        k.op("DVE", lambda e: e.tensor_copy(out=identb.ap, in_=ident), reads=[cf], writes=[identb])
        k.op("DVE", lambda e: e.tensor_copy(out=SLb.ap, in_=cf.ap[:, 128:256]), reads=[cf], writes=[SLb])
        THR = cf.ap[:, 256:320]
        JB = cf.ap[:, 320:320 + NBLK]
        KP = cf.ap[:, 320 + NBLK:328 + NBLK]
        for kc in range(8):
            k.op("DVE", lambda e: e.tensor_scalar(out=wr_sb.ap[:, kc * 72:(kc + 1) * 72], in0=wr_sb.ap[:, kc * 72:(kc + 1) * 72],
                                                  scalar1=vec.ap[:, kc:kc + 1], scalar2=None, op0=ALU.mult), reads=[vec, wr_sb], writes=[wr_sb])

        def rmsnorm(src_t, src_ap, gcol, dst_t, dst_ap, bank):
            k.op("ACT", lambda e: e.activation(out=dst_ap, in_=src_ap, func=AF.Square), reads=[src_t], writes=[dst_t])
            for kc in range(8):
                k.op("PE", lambda e: e.matmul(bank.ap, ones_bf.ap, dst_ap[:, kc, :], start=(kc == 0), stop=(kc == 7)),
                     reads=[dst_t, ones_bf], writes=[bank], pe_acc=(kc > 0))
            k.op("ACT", lambda e: e.activation(out=LNV.ap, in_=bank.ap, func=AF.Ln, scale=1.0 / 1024, bias=EPS), reads=[bank], writes=[LNV])
            k.op("ACT", lambda e: e.activation(out=RSTD.ap, in_=LNV.ap, func=AF.Exp, scale=-0.5), reads=[LNV], writes=[RSTD])
            for kc in range(8):
                k.op("DVE", lambda e: e.scalar_tensor_tensor(out=dst_ap[:, kc, :], in0=src_ap[:, kc, :], scalar=vec.ap[:, gcol + kc:gcol + kc + 1],
                                                             in1=RSTD.ap, op0=ALU.mult, op1=ALU.mult), reads=[src_t, RSTD, vec], writes=[dst_t])

        for kp in range(KAC // 8):
            k.dma("POOL", v3(WB[kp].ap[:, 0:8192], 8), w_a[kp * 1024:(kp + 1) * 1024, :].rearrange("(a p) n -> p a n", p=128), writes=[WB[kp]])
        for t in range(NTILE):
            cs = slice(t * 512, (t + 1) * 512)
            ht, ut, AB = HTL[t % 2], UTL[t % 2], ABS[0]
            h3, u3 = v3(ht.ap, 8), v3(ut.ap, 8)
            k.dma("SP", h3, hT[:, cs].rearrange("(a p) n -> p a n", p=128), writes=[ht])
            k.dma("SP", v3(AB.ap, KAC), aT[:, cs].rearrange("(a p) n -> p a n", p=128), writes=[AB])
            for d in range(8):
                bank = PD[d % 2]
                for kc in range(KAC):
                    wbt = WB[kc // 8]
                    k.op("PE", lambda e: e.matmul(bank.ap, v3(wbt.ap[:, 0:8192], 8)[:, kc % 8, d * 128:(d + 1) * 128], v3(AB.ap, KAC)[:, kc, :],
                                                  start=(kc == 0), stop=(kc == KAC - 1)), reads=[wbt, AB], writes=[bank], pe_acc=(kc > 0))
                k.op("DVE", lambda e: e.tensor_tensor(out=h3[:, d, :], in0=h3[:, d, :], in1=bank.ap, op=ALU.add), reads=[bank, ht], writes=[ht])
            k.dma("SP", H1[:, cs].rearrange("(a p) n -> p a n", p=128), h3, reads=[ht], writes=[H1_t], part=True)
            rmsnorm(ht, h3, 0, ut, u3, PBK[0])
            for b in range(4):
                i = t * 4 + b
                bs = slice(b * 128, (b + 1) * 128)
                plg_b, prs = PG[b % 2], PU[b % 2]
                for kc in range(8):
                    k.op("PE", lambda e: e.matmul(plg_b.ap[:, 0:72], h3[:, kc, bs], wr_sb.ap[:, kc * 72:(kc + 1) * 72], start=(kc == 0), stop=(kc == 7)),
                         reads=[ht, wr_sb], writes=[plg_b], pe_acc=(kc > 0))
                k.op("PE", lambda e: e.matmul(prs.ap[:, 0:2], RSTD.ap[:, bs], ident[:, 0:2], start=True, stop=True), reads=[RSTD, cf], writes=[prs])
                rt, gmax, ngmax, gsum, gw, m1, m2, dd, ex, w1, w2 = r_s[0:11]
                k.op("DVE", lambda e: e.tensor_copy(out=rt.ap, in_=prs.ap[:, 0:1]), reads=[prs], writes=[rt])
                k.op("DVE", lambda e: e.scalar_tensor_tensor(out=r_lg.ap, in0=plg_b.ap[:, 0:72], scalar=rt.ap, in1=br_bc.ap, op0=ALU.mult, op1=ALU.add),
                     reads=[plg_b, rt, br_bc], writes=[r_lg])
                gl = r_lg.ap[:, 0:8]
                el = r_lg.ap[:, 8:72]
                k.op("DVE", lambda e: e.tensor_reduce(out=gmax.ap, in_=gl, axis=AX.X, op=ALU.max), reads=[r_lg], writes=[gmax])
                k.op("DVE", lambda e: e.tensor_scalar(out=r_goh.ap, in0=gl, scalar1=gmax.ap, scalar2=None, op0=ALU.is_equal), reads=[r_lg, gmax], writes=[r_goh])
                k.op("DVE", lambda e: e.tensor_scalar(out=ngmax.ap, in0=gmax.ap, scalar1=-1.0, scalar2=None, op0=ALU.mult), reads=[gmax], writes=[ngmax])
                k.op("ACT", lambda e: e.activation(out=r_a.ap, in_=gl, func=AF.Exp, bias=ngmax.ap, scale=1.0, accum_out=gsum.ap), reads=[r_lg, ngmax], writes=[r_a, gsum])
                k.op("DVE", lambda e: e.reciprocal(out=gw.ap, in_=gsum.ap), reads=[gsum], writes=[gw])
                k.op("DVE", lambda e: e.tensor_tensor(out=v3(r_t64.ap, 8), in0=v3(el, 8), in1=r_goh.ap.unsqueeze(2).to_broadcast([128, 8, 8]), op=ALU.mult),
                     reads=[r_lg, r_goh], writes=[r_t64])
                k.op("DVE", lambda e: e.tensor_reduce(out=r_es.ap, in_=v3(r_t64.ap, 8).rearrange("p g j -> p j g"), axis=AX.X, op=ALU.add), reads=[r_t64], writes=[r_es])
                k.op("DVE", lambda e: e.tensor_reduce(out=m1.ap, in_=r_es.ap, axis=AX.X, op=ALU.max), reads=[r_es], writes=[m1])
                k.op("DVE", lambda e: e.tensor_scalar(out=r_oh1.ap, in0=r_es.ap, scalar1=m1.ap, scalar2=None, op0=ALU.is_equal), reads=[r_es, m1], writes=[r_oh1])
                k.op("DVE", lambda e: e.scalar_tensor_tensor(out=r_es2.ap, in0=r_oh1.ap, scalar=-1e30, in1=r_es.ap, op0=ALU.mult, op1=ALU.add), reads=[r_oh1, r_es], writes=[r_es2])
                k.op("DVE", lambda e: e.tensor_reduce(out=m2.ap, in_=r_es2.ap, axis=AX.X, op=ALU.max), reads=[r_es2], writes=[m2])
                k.op("DVE", lambda e: e.tensor_scalar(out=r_oh2.ap, in0=r_es2.ap, scalar1=m2.ap, scalar2=None, op0=ALU.is_equal), reads=[r_es2, m2], writes=[r_oh2])
                k.op("DVE", lambda e: e.tensor_tensor(out=dd.ap, in0=m2.ap, in1=m1.ap, op=ALU.subtract), reads=[m1, m2], writes=[dd])
                k.op("ACT", lambda e: e.activation(out=ex.ap, in_=dd.ap, func=AF.Exp), reads=[dd], writes=[ex])
                k.op("DVE", lambda e: e.tensor_scalar(out=w1.ap, in0=ex.ap, scalar1=1.0, scalar2=None, op0=ALU.add), reads=[ex], writes=[w1])
                k.op("DVE", lambda e: e.reciprocal(out=w1.ap, in_=w1.ap), reads=[w1], writes=[w1])
                k.op("DVE", lambda e: e.tensor_tensor(out=w2.ap, in0=ex.ap, in1=w1.ap, op=ALU.mult), reads=[ex, w1], writes=[w2])
                k.op("DVE", lambda e: e.tensor_tensor(out=GA.ap[:, i:i + 1], in0=w1.ap, in1=gw.ap, op=ALU.mult), reads=[w1, gw], writes=[GA])
                k.op("DVE", lambda e: e.tensor_tensor(out=GB.ap[:, i:i + 1], in0=w2.ap, in1=gw.ap, op=ALU.mult), reads=[w2, gw], writes=[GB])
                k.op("DVE", lambda e: e.tensor_tensor(out=v3(OHC.ap[:, 0:64], 8), in0=r_goh.ap.unsqueeze(2).to_broadcast([128, 8, 8]),
                                                      in1=r_oh1.ap.unsqueeze(1).to_broadcast([128, 8, 8]), op=ALU.mult), reads=[r_goh, r_oh1], writes=[OHC])
                k.op("DVE", lambda e: e.tensor_tensor(out=v3(OHC.ap[:, 64:128], 8), in0=r_goh.ap.unsqueeze(2).to_broadcast([128, 8, 8]),
                                                      in1=r_oh2.ap.unsqueeze(1).to_broadcast([128, 8, 8]), op=ALU.mult), reads=[r_goh, r_oh2, OHC], writes=[OHC])
                k.op("DVE", lambda e: e.tensor_copy(out=OHS3[:, i, :], in_=OHC.ap), reads=[OHC], writes=[OHS])
                ppr, pcs = PD[0], PD[1]
                k.op("PE", lambda e: e.matmul(ppr.ap[:, 0:128], SLb.ap, OHC.ap, start=True, stop=True), reads=[SLb, OHC], writes=[ppr])
                k.op("PE", lambda e: e.matmul(pcs.ap[:, 0:128], ones_bf.ap, OHC.ap, start=True, stop=True), reads=[ones_bf, OHC], writes=[pcs])
                k.op("DVE", lambda e: e.tensor_tensor(out=PRS.ap, in0=ppr.ap[:, 0:128], in1=RUN.ap, op=ALU.add), reads=[ppr, RUN], writes=[PRS])
                k.op("DVE", lambda e: e.tensor_tensor(out=PRS.ap, in0=PRS.ap, in1=OHC.ap, op=ALU.mult), reads=[PRS, OHC], writes=[PRS])
                k.op("DVE", lambda e: e.tensor_reduce(out=RK0.ap[:, i:i + 1], in_=PRS.ap[:, 0:64], axis=AX.X, op=ALU.add), reads=[PRS], writes=[RK0])
                k.op("DVE", lambda e: e.tensor_reduce(out=RK1.ap[:, i:i + 1], in_=PRS.ap[:, 64:128], axis=AX.X, op=ALU.add), reads=[PRS], writes=[RK1])
                k.op("DVE", lambda e: e.tensor_tensor(out=RUN.ap, in0=RUN.ap, in1=pcs.ap[:, 0:128], op=ALU.add), reads=[pcs, RUN], writes=[RUN])
                trb = PBK[1].ap.bitcast(BF16)
                for kc in range(8):
                    k.op("PE", lambda e: e.transpose(trb[:, kc * 128:(kc + 1) * 128], u3[:, kc, bs], identb.ap), reads=[ut, identb], writes=[PBK[1]])
                k.op("ACT", lambda e: e.activation(out=XR3[:, i, :], in_=trb, func=AF.Copy), reads=[PBK[1]], writes=[XROWS])

        k.alias([BIG, IGF], ABS)
        cnt1, cnt2 = RUN.ap[:, 0:64], RUN.ap[:, 64:128]
        k.op("DVE", lambda e: e.tensor_tensor(out=CNT.ap, in0=cnt1, in1=cnt2, op=ALU.add), reads=[RUN], writes=[CNT])
        big_cm = BIG.ap[:, 0:2048].rearrange("p (a m) -> p a m", a=64)
        for mh in range(2):
            k.op("DVE", lambda e: e.tensor_tensor(out=big_cm, in0=CNT.ap.unsqueeze(2).to_broadcast([128, 64, 32]),
                                                  in1=THR[:, mh * 32:(mh + 1) * 32].unsqueeze(1).to_broadcast([128, 64, 32]), op=ALU.is_gt), reads=[CNT, cf], writes=[BIG])
            dstn = NB_ if mh == 0 else BASE1
            k.op("DVE", lambda e: e.tensor_reduce(out=dstn.ap, in_=big_cm, axis=AX.X, op=ALU.add), reads=[BIG], writes=[dstn])
        k.op("DVE", lambda e: e.tensor_tensor(out=NB_.ap, in0=NB_.ap, in1=BASE1.ap, op=ALU.add), reads=[NB_, BASE1], writes=[NB_])
        k.op("DVE", lambda e: e.tensor_scalar(out=NB_.ap, in0=NB_.ap, scalar1=float(MB), scalar2=None, op0=ALU.mult), reads=[NB_], writes=[NB_])
        k.op("DVE", lambda e: e.tensor_copy(out=PE_[0].ap, in_=NB_.ap), reads=[NB_], writes=[PE_[0]])
        cur = 0
        for sft in (1, 2, 4, 8, 16, 32):
            a_, b_ = PE_[cur], PE_[1 - cur]
            k.op("DVE", lambda e: e.tensor_copy(out=b_.ap[:, 0:sft], in_=a_.ap[:, 0:sft]), reads=[a_], writes=[b_])
            k.op("DVE", lambda e: e.tensor_tensor(out=b_.ap[:, sft:64], in0=a_.ap[:, sft:64], in1=a_.ap[:, 0:64 - sft], op=ALU.add), reads=[a_, b_], writes=[b_])
            cur = 1 - cur
        PEND = PE_[cur]
        k.op("DVE", lambda e: e.tensor_tensor(out=BASE1.ap, in0=PEND.ap, in1=NB_.ap, op=ALU.subtract), reads=[PEND, NB_], writes=[BASE1])
        k.op("DVE", lambda e: e.tensor_tensor(out=BASE2.ap, in0=BASE1.ap, in1=cnt1, op=ALU.add), reads=[BASE1, RUN], writes=[BASE2])
        TBC = 2048 // 64
        for (base, rk, di, lo) in ((BASE1, RK0, DI0, 0), (BASE2, RK1, DI1, 64)):
            for c0 in range(0, NTB, TBC):
                nb = min(TBC, NTB - c0)
                big_t = BIG.ap[:, 0:nb * 64].rearrange("p (a m) -> p a m", a=nb)
                k.op("DVE", lambda e: e.tensor_tensor(out=big_t, in0=OHS3[:, c0:c0 + nb, lo:lo + 64], in1=base.ap.unsqueeze(1).to_broadcast([128, nb, 64]), op=ALU.mult),
                     reads=[OHS, base], writes=[BIG])
                k.op("DVE", lambda e: e.tensor_reduce(out=DF.ap[:, c0:c0 + nb], in_=big_t, axis=AX.X, op=ALU.add), reads=[BIG], writes=[DF])
            k.op("DVE", lambda e: e.tensor_tensor(out=DF.ap, in0=DF.ap, in1=rk.ap, op=ALU.add), reads=[DF, rk], writes=[DF])
            k.op("DVE", lambda e: e.tensor_copy(out=di.ap, in_=DF.ap), reads=[DF], writes=[di])
        for c0 in range(0, NBLK, 32):
            nb = min(32, NBLK - c0)
            big_j = BIG.ap[:, 0:nb * 64].rearrange("p (a m) -> p a m", a=nb)
            k.op("DVE", lambda e: e.tensor_tensor(out=big_j, in0=PEND.ap.unsqueeze(1).to_broadcast([128, nb, 64]), in1=JB[:, c0:c0 + nb].unsqueeze(2).to_broadcast([128, nb, 64]), op=ALU.is_le),
                 reads=[PEND, cf], writes=[BIG])
            k.op("DVE", lambda e: e.tensor_reduce(out=EJ.ap[:, c0:c0 + nb], in_=big_j, axis=AX.X, op=ALU.add), reads=[BIG], writes=[EJ])
        k.op("DVE", lambda e: e.tensor_scalar(out=EJ.ap, in0=EJ.ap, scalar1=63.0, scalar2=None, op0=ALU.min), reads=[EJ], writes=[EJ])
        HB = NBLK // 2
        k.op("DVE", lambda e: e.memset(SAME.ap, 0.0), writes=[SAME])
        for s0 in (0, HB):
            k.op("DVE", lambda e: e.tensor_tensor(out=SAME.ap[:, s0 + 1:s0 + HB], in0=EJ.ap[:, s0 + 1:s0 + HB], in1=EJ.ap[:, s0:s0 + HB - 1], op=ALU.is_equal),
                 reads=[EJ, SAME], writes=[SAME])
        k.op("DVE", lambda e: e.tensor_scalar(out=SAME.ap, in0=SAME.ap, scalar1=float(1 << 22), scalar2=None, op0=ALU.mult), reads=[SAME], writes=[SAME])
        igf3 = v3(IGF.ap[:, 0:NBLK * 2], NBLK)
        k.op("DVE", lambda e: e.scalar_tensor_tensor(out=BIG.ap[:, 0:NBLK], in0=EJ.ap, scalar=256.0, in1=SAME.ap, op0=ALU.mult, op1=ALU.add), reads=[EJ, SAME], writes=[BIG])
        k.op("DVE", lambda e: e.tensor_tensor(out=igf3, in0=BIG.ap[:, 0:NBLK].unsqueeze(2).to_broadcast([128, NBLK, 2]), in1=KP[:, 0:2].unsqueeze(1).to_broadcast([128, NBLK, 2]), op=ALU.add),
             reads=[BIG, cf], writes=[IGF])
        k.op("DVE", lambda e: e.tensor_copy(out=IGI.ap[:, 0:NBLK * 2], in_=IGF.ap[:, 0:NBLK * 2]), reads=[IGF], writes=[IGI])

        for i in range(NTB):
            k.idma(Xs, XR3[:, i, :], DI0, DI0.ap[:, i:i + 1], gather=False, reads=[XROWS], writes=[Xs_t], part=True)
            k.idma(Xs, XR3[:, i, :], DI1, DI1.ap[:, i:i + 1], gather=False, reads=[XROWS], writes=[Xs_t], part=True)

        k.alias(XB + XTB + YB, [BIG, IGF] + ABS)

        bnd = nc.gpsimd.to_reg(64 * 256 - 1)
        igi2 = v3(IGI.ap[:, 0:NBLK * 2], NBLK)

        def load_blk(j, n_):
            wb = WB[n_ % 2]
            first = True
            for (src, base) in ((wg_r, 0), (wu_r, 4096), (wd_r, 8192)):
                for h in range(2):
                    k.idma(wb.ap[:, base + h * 2048:base + (h + 1) * 2048], src, IGI, igi2[:, j, h:h + 1], gather=True, writes=[wb], part=(not first), bound=bnd)
                    first = False
            k.dma("SP", XB[n_ % 2].ap, Xs[j * MB:(j + 1) * MB, :], reads=[Xs_t], writes=[XB[n_ % 2]])

        def front(j, n_):
            wb, xb, xt = WB[n_ % 2], XB[n_ % 2], XTB[n_ % 2]
            trb0, trb1 = PBK[0].ap.bitcast(BF16), PBK[1].ap.bitcast(BF16)
            for kc in range(8):
                dst = (trb0 if kc < 4 else trb1)[:, (kc % 4) * 128:(kc % 4 + 1) * 128]
                k.op("PE", lambda e: e.transpose(dst, xb.ap.rearrange("p (m c) -> p c m", c=8)[:, kc, :], identb.ap), reads=[xb, identb], writes=[PBK[0] if kc < 4 else PBK[1]])
            k.op("ACT", lambda e: e.activation(out=xt.ap[:, 0:512], in_=trb0[:, 0:512], func=AF.Copy), reads=[PBK[0]], writes=[xt])
            k.op("ACT", lambda e: e.activation(out=xt.ap[:, 512:1024], in_=trb1[:, 0:512], func=AF.Copy), reads=[PBK[1], xt], writes=[xt])
            Wg3, Wu3 = v3(wb.ap[:, 0:4096], 8), v3(wb.ap[:, 4096:8192], 8)
            pg, pu = PG[n_ % 2], PU[n_ % 2]
            for f in range(4):
                for kc in range(8):
                    k.op("PE", lambda e: e.matmul(pg.ap[:, f * 128:(f + 1) * 128], Wg3[:, kc, :].rearrange("p (m c) -> p c m", c=4)[:, f, :], xt.ap[:, kc * 128:(kc + 1) * 128],
                                                  start=(kc == 0), stop=(kc == 7)), reads=[wb, xt], writes=[pg], pe_acc=(kc > 0 or f > 0))
            for f in range(4):
                for kc in range(8):
                    k.op("PE", lambda e: e.matmul(pu.ap[:, f * 128:(f + 1) * 128], Wu3[:, kc, :].rearrange("p (m c) -> p c m", c=4)[:, f, :], xt.ap[:, kc * 128:(kc + 1) * 128],
                                                  start=(kc == 0), stop=(kc == 7)), reads=[wb, xt], writes=[pu], pe_acc=(kc > 0 or f > 0))
            k.op("ACT", lambda e: e.activation(out=SS[n_ % 2].ap, in_=pg.ap, func=AF.Silu), reads=[pg], writes=[SS[n_ % 2]])
            k.op("DVE", lambda e: e.tensor_tensor(out=HH[n_ % 2].ap, in0=SS[n_ % 2].ap, in1=pu.ap, op=ALU.mult), reads=[SS[n_ % 2], pu], writes=[HH[n_ % 2]])

        def back(j, n_):
            wb, hh, yb = WB[n_ % 2], HH[n_ % 2], YB[n_ % 2]
            Wd3 = v3(wb.ap[:, 8192:12288], 4)
            for dh in range(2):
                pd = PD[dh]
                for f in range(4):
                    k.op("PE", lambda e: e.matmul(pd.ap, hh.ap[:, f * 128:(f + 1) * 128], Wd3[:, f, dh * 512:(dh + 1) * 512], start=(f == 0), stop=(f == 3)),
                         reads=[wb, hh], writes=[pd], pe_acc=(f > 0))
                if dh == 0:
                    k.op("ACT", lambda e: e.activation(out=yb.ap[:, 0:512], in_=pd.ap, func=AF.Copy), reads=[pd], writes=[yb])
                else:
                    k.op("DVE", lambda e: e.tensor_copy(out=yb.ap[:, 512:1024], in_=pd.ap), reads=[pd, yb], writes=[yb])
            k.dma("SP", Ys[j * MB:(j + 1) * MB, :], yb.ap, reads=[yb], writes=[Ys_t], part=True)

        order = []
        for q in range(HB):
            order += [q, HB + q]
        load_blk(order[0], 0)
        for n_, j in enumerate(order):
            front(j, n_)
            if n_ > 0:
                back(order[n_ - 1], n_ - 1)
            if n_ + 1 < NBLK:
                load_blk(order[n_ + 1], n_ + 1)
        back(order[-1], NBLK - 1)

        k.dma("POOL", v3(WB[0].ap[:, 0:8192], 8), plg.rearrange("(a p) n -> p a n", p=128), writes=[WB[0]])
        k.dma("POOL", v3(WB[1].ap[:, 0:2048], 2), plp.rearrange("(a p) n -> p a n", p=128), writes=[WB[1]])
        k.alias(WQ + Y0 + Y1, [XROWS])
        if mode == "A":
            k.dma("POOL", v3(WQ[0].ap, 8), wqkv[:, 0:1536].rearrange("(a p) n -> p a n", p=128), writes=[WQ[0]])
            k.dma("POOL", v3(WQ[1].ap, 8), wqkv[:, 1536:3072].rearrange("(a p) n -> p a n", p=128), writes=[WQ[1]])
        for t in range(NTILE):
            cs = slice(t * 512, (t + 1) * 512)
            ht, ut = HTL[t % 2], UTL[t % 2]
            h3, u3 = v3(ht.ap, 8), v3(ut.ap, 8)
            k.dma("SP", h3, H1[:, cs].rearrange("(a p) n -> p a n", p=128), reads=[H1_t], writes=[ht])
            k.dma("POOL", v3(PBt.ap, 2), pT[:, cs].rearrange("(a p) n -> p a n", p=128), writes=[PBt])
            for b in range(4):
                i = t * 4 + b
                bs = slice(b * 128, (b + 1) * 128)
                y0, y1 = Y0[i % 2], Y1[i % 2]
                k.idma(y0.ap, Ys, DI0, DI0.ap[:, i:i + 1], gather=True, reads=[Ys_t], writes=[y0])
                k.idma(y1.ap, Ys, DI1, DI1.ap[:, i:i + 1], gather=True, reads=[Ys_t], writes=[y1])
                k.op("DVE", lambda e: e.tensor_scalar(out=y0.ap, in0=y0.ap, scalar1=GA.ap[:, i:i + 1], scalar2=None, op0=ALU.mult), reads=[y0, GA], writes=[y0])
                k.op("DVE", lambda e: e.scalar_tensor_tensor(out=y0.ap, in0=y1.ap, scalar=GB.ap[:, i:i + 1], in1=y0.ap, op0=ALU.mult, op1=ALU.add),
                     reads=[y1, GB, y0], writes=[y0])
                for half in range(2):
                    bank = PD[half]
                    for dq in range(4):
                        d = half * 4 + dq
                        k.op("PE", lambda e: e.transpose(bank.ap[:, dq * 128:(dq + 1) * 128], y0.ap[:, d * 128:(d + 1) * 128], ident), reads=[y0, cf], writes=[bank])
                    k.op("DVE", lambda e: e.tensor_tensor(out=h3[:, half * 4:(half + 1) * 4, bs], in0=h3[:, half * 4:(half + 1) * 4, bs],
                                                          in1=bank.ap.rearrange("p (a n) -> p a n", a=4), op=ALU.add), reads=[bank, ht], writes=[ht])
            rmsnorm(ht, h3, 8, ut, u3, PBK[0])
            for d in range(8):
                pg, pu = PG[d % 2], PU[d % 2]
                for kc in range(8):
                    k.op("PE", lambda e: e.matmul(pg.ap, v3(WB[0].ap[:, 0:8192], 8)[:, kc, d * 128:(d + 1) * 128], u3[:, kc, :], start=(kc == 0), stop=(kc == 7)),
                         reads=[WB[0], ut], writes=[pg], pe_acc=(kc > 0))
                for kc in range(2):
                    k.op("PE", lambda e: e.matmul(pu.ap, v3(WB[1].ap[:, 0:2048], 2)[:, kc, d * 128:(d + 1) * 128], v3(PBt.ap, 2)[:, kc, :], start=(kc == 0), stop=(kc == 1)),
                         reads=[WB[1], PBt], writes=[pu], pe_acc=(kc > 0))
                sf = STF[d % 2]
                k.op("ACT", lambda e: e.activation(out=sf.ap, in_=pg.ap, func=AF.Sigmoid, bias=vec.ap[:, 16 + d:17 + d], scale=1.0), reads=[pg, vec], writes=[sf])
                k.op("DVE", lambda e: e.tensor_tensor(out=sf.ap, in0=sf.ap, in1=pu.ap, op=ALU.mult), reads=[sf, pu], writes=[sf])
                k.op("DVE", lambda e: e.tensor_tensor(out=h3[:, d, :], in0=h3[:, d, :], in1=sf.ap, op=ALU.add), reads=[sf, ht], writes=[ht])
            if mode == "A":
                k.dma("SP", hT_o[:, cs].rearrange("(a p) n -> p a n", p=128), h3, reads=[ht], writes=[ho_t], part=True)
                rmsnorm(ht, h3, 24, ut, u3, PBK[0])
                for n in range(16):
                    col = n * 128
                    wbt = WQ[0] if col < 1536 else WQ[1]
                    lc = col if col < 1536 else col - 1536
                    bank = PG[n % 2]
                    for kc in range(8):
                        k.op("PE", lambda e: e.matmul(bank.ap, v3(wbt.ap, 8)[:, kc, lc:lc + 128], u3[:, kc, :], start=(kc == 0), stop=(kc == 7)),
                             reads=[wbt, ut], writes=[bank], pe_acc=(kc > 0))
                    sg = STG[n % 2]
                    sc_ = (1.0 / np.sqrt(128.0)) if n < 8 else 1.0
                    k.op("ACT", lambda e: e.activation(out=sg.ap, in_=bank.ap, func=AF.Identity, scale=float(sc_)), reads=[bank], writes=[sg])
                    if n < 8:
                        k.dma("SP", qT_o[n * 128:(n + 1) * 128, cs], sg.ap, reads=[sg], writes=[qT_t], part=True)
                    else:
                        k.dma("SP", kT_o[(n - 8) * 128:(n - 7) * 128, cs], sg.ap, reads=[sg], writes=[kT_t], part=True)
                for b in range(4):
                    for hh_ in range(2):
                        bank = PU[hh_]
                        for kc in range(8):
                            k.op("PE", lambda e: e.matmul(bank.ap, u3[:, kc, b * 128:(b + 1) * 128], v3(WQ[1].ap, 8)[:, kc, 512 + hh_ * 512:1024 + hh_ * 512],
                                                          start=(kc == 0), stop=(kc == 7)), reads=[WQ[1], ut], writes=[bank], pe_acc=(kc > 0))
                        sg = STG[hh_]
                        k.op("ACT", lambda e: e.activation(out=sg.ap, in_=bank.ap, func=AF.Copy), reads=[bank], writes=[sg])
                        r0 = t * 512 + b * 128
                        k.dma("SP", v_o[r0:r0 + 128, hh_ * 512:(hh_ + 1) * 512], sg.ap, reads=[sg], writes=[v_t], part=True)
            else:
                k.op("ACT", lambda e: e.activation(out=u3, in_=h3, func=AF.Square), reads=[ht], writes=[ut])
                bank = PBK[0]
                for kc in range(8):
                    k.op("PE", lambda e: e.matmul(bank.ap, ones_bf.ap, u3[:, kc, :], start=(kc == 0), stop=(kc == 7)),
                         reads=[ut, ones_bf], writes=[bank], pe_acc=(kc > 0))
                k.op("ACT", lambda e: e.activation(out=LNV.ap, in_=bank.ap, func=AF.Ln, scale=1.0 / 1024, bias=EPS), reads=[bank], writes=[LNV])
                k.op("ACT", lambda e: e.activation(out=RSTD.ap, in_=LNV.ap, func=AF.Exp, scale=-0.5), reads=[LNV], writes=[RSTD])
                for d in range(8):
                    k.op("DVE", lambda e: e.scalar_tensor_tensor(out=h3[:, d, :], in0=h3[:, d, :], scalar=vec.ap[:, 24 + d:25 + d], in1=RSTD.ap,
                                                                 op0=ALU.mult, op1=ALU.mult), reads=[ht, RSTD, vec], writes=[ht])
                k.dma("SP", out_o[:, cs].rearrange("(a p) n -> p a n", p=128), h3, reads=[ht], writes=[out_t], part=True)
        k.finish("SP", [qT_t, kT_t, v_t, ho_t] if mode == "A" else [out_t])
        print("tok2 ninst", k.ninst)
    return nc
```

```python
import numpy as np
import ml_dtypes
from contextlib import ExitStack
import concourse.bass as bass
import concourse.mybir as mybir
from concourse.bass_utils import run_bass_kernel_spmd

F32 = mybir.dt.float32
BF16 = mybir.dt.bfloat16
AF = mybir.ActivationFunctionType
ALU = mybir.AluOpType
AX = mybir.AxisListType
EPS = 1e-6
NPBF = ml_dtypes.bfloat16


class T:
    __slots__ = ("ap", "w", "r", "dsem", "dcnt", "name")

    def __init__(self, ap, name=""):
        self.ap = ap
        self.w = None
        self.r = {}
        self.dsem = None
        self.dcnt = 0
        self.name = name


class K:
    def __init__(self, nc, stack):
        self.nc = nc
        self.stack = stack
        self.eng = {"PE": nc.tensor, "ACT": nc.scalar, "DVE": nc.vector, "POOL": nc.gpsimd, "SP": nc.sync}
        self.sem = {}
        self.cnt = {}
        for e in ("PE", "ACT", "DVE", "POOL"):
            self.sem[e] = stack.enter_context(nc.semaphore("s_" + e))
            self.cnt[e] = 0
        self.seen = {e: {} for e in self.eng}
        self.semobj = dict(self.sem)
        self.ndsem = 0
        self.ninst = 0
        self.nalloc = 0

    def sb(self, shape, dt, name=None):
        self.nalloc += 1
        return self.stack.enter_context(self.nc.sbuf_tensor(name or ("sb%d" % self.nalloc), list(shape), dt))

    def ps(self, shape, dt, name=None):
        self.nalloc += 1
        return self.stack.enter_context(self.nc.psum_tensor(name or ("ps%d" % self.nalloc), list(shape), dt))

    def sbt(self, shape, dt, name=None):
        return T(self.sb(shape, dt, name)[:])

    def _need(self, e, key, val):
        if val <= 0 or self.seen[e].get(key, 0) >= val:
            return
        self.eng[e].wait_ge(self.semobj[key], val)
        self.seen[e][key] = val

    def op(self, e, fn, reads=(), writes=(), pe_acc=False):
        for t in reads:
            if t.w is not None:
                self._need(e, *t.w)
        for t in writes:
            if t.w is not None and not (pe_acc and t.w[0] == "PE"):
                self._need(e, *t.w)
            for kk, v in t.r.items():
                self._need(e, kk, v)
        ins = fn(self.eng[e])
        self.cnt[e] += 1
        c = self.cnt[e]
        ins.then_inc(self.sem[e], 1)
        for t in reads:
            t.r[e] = c
        for t in writes:
            t.w = (e, c)
            t.r = {}
        self.ninst += 1
        return ins

    def dma(self, q, out_ap, in_ap, reads=(), writes=(), part=False, **kw):
        assert len(writes) == 1
        t = writes[0]
        if t.dsem is None:
            key = "d%d" % self.ndsem
            t.dsem = self.stack.enter_context(self.nc.semaphore(key))
            self.semobj[key] = t.dsem
            t.name = key
            self.ndsem += 1
        key = t.name
        for r in reads:
            if r.w is not None:
                self._need(q, *r.w)
        if t.w is not None and not (part and t.w[0] == key):
            self._need(q, *t.w)
        for kk, v in t.r.items():
            self._need(q, kk, v)
        ins = self.eng[q].dma_start(out=out_ap, in_=in_ap, **kw)
        t.dcnt += 16
        ins.then_inc(t.dsem, 16)
        for r in reads:
            r.r[key] = max(r.r.get(key, 0), t.dcnt)
        t.w = (key, t.dcnt)
        if not part:
            t.r = {}
        self.ninst += 1
        return ins

    def alias(self, new, old):
        for n in new:
            for o in old:
                for kk, v in o.r.items():
                    n.r[kk] = max(n.r.get(kk, 0), v)
                if o.w is not None:
                    n.r[o.w[0]] = max(n.r.get(o.w[0], 0), o.w[1])

    def finish(self, e, tiles):
        for t in tiles:
            if t.w is not None:
                self._need(e, *t.w)


def v3(ap, a):
    return ap.rearrange("p (a n) -> p a n", a=a)


def build_tok(NT, TH, KA, mode, NE=64):
    nc = bass.Bass("TRN2", target_bir_lowering=False)

    def din(name, shape, dt=F32):
        return nc.dram_tensor(name, list(shape), dt, kind="ExternalInput").ap()

    def dout(name, shape, dt=F32):
        return nc.dram_tensor(name, list(shape), dt, kind="ExternalOutput").ap()

    hT = din("hT", [1024, NT])
    aT = din("aT", [KA, NT], BF16)
    w_a = din("w_a", [KA, 1024])
    vecs = din("vecs", [128, 32])
    wr = din("wr", [1024, 72])
    br = din("br", [1, 72])
    wg = din("wg", [64, 1024, 512])
    wu = din("wu", [64, 1024, 512])
    wd = din("wd", [64, 512, 1024])
    plg = din("plg", [1024, 1024])
    plp = din("plp", [256, 1024])
    pT = din("pT", [256, NT])
    ident_d = din("ident", [128, 128])
    if mode == "A":
        wqkv = din("wqkv", [1024, 3072])
        hT_o = dout("hT_o", [1024, NT])
        qT_o = dout("qT_o", [1024, NT], BF16)
        kT_o = dout("kT_o", [1024, NT], BF16)
        v_o = dout("v_o", [NT, 1024], BF16)
    else:
        out_o = dout("out_o", [1024, NT])
    KAC = KA // 128
    NTL = TH // 512
    NH = NT // TH

    with ExitStack() as st:
        k = K(nc, st)
        h_sb = k.sb([128, 8 * TH], F32, "h_sb")
        h3 = v3(h_sb[:], 8)
        HT = [[T(h3[:, d, t * 512:(t + 1) * 512]) for d in range(8)] for t in range(NTL)]
        u_sb = k.sb([128, 8 * TH], BF16, "u_sb")
        u3 = v3(u_sb[:], 8)
        UT = [T(u3[:, :, t * 512:(t + 1) * 512]) for t in range(NTL)]
        WB = [k.sbt([128, 12288], BF16, "wb%d" % i) for i in range(2)]
        GT = k.sbt([64, TH], F32, "gt")
        NAB = max(1, min(2, (8 * TH) // (KAC * 512)))
        ABS = [T(u_sb[:, i * KAC * 512:(i + 1) * KAC * 512]) for i in range(NAB)]
        PB = k.sbt([128, 2 * 512], BF16, "pb")
        SQ = k.sbt([128, 8 * 512], BF16, "sq")
        LNV = k.sbt([128, 512], F32, "lnv")
        RSTD = k.sbt([128, 512], F32, "rstd")
        vec = k.sbt([128, 32], F32, "vec_sb")
        ident = k.sbt([128, 128], F32, "ident_sb")
        ones_bf = k.sbt([128, 128], BF16, "ones")
        wr_sb = k.sbt([128, 8 * 72], F32, "wr_sb")
        br_bc = k.sbt([128, 72], F32, "brbc")
        S_ = [k.sbt([128, 512], BF16, "s%d" % i) for i in range(2)]
        TT = [k.sbt([128, 512], BF16, "tt%d" % i) for i in range(2)]
        HS = [[k.sbt([128, 512], BF16, "hs%d_%d" % (i, f)) for f in range(4)] for i in range(2)]
        EB = [k.sbt([64, 128], F32, "eb%d" % i) for i in range(2)]
        STG = [k.sbt([128, 512], BF16, "stg%d" % i) for i in range(2)]
        STF = [k.sbt([128, 512], F32, "stf%d" % i) for i in range(2)]
        r_lg = k.sbt([128, 72], F32, "r_lg")
        r_a = k.sbt([128, 8], F32, "r_a")
        r_goh = k.sbt([128, 8], F32, "r_goh")
        r_t64 = k.sbt([128, 64], F32, "r_t64")
        r_es = k.sbt([128, 8], F32, "r_es")
        r_es2 = k.sbt([128, 8], F32, "r_es2")
        r_oh1 = k.sbt([128, 8], F32, "r_oh1")
        r_oh2 = k.sbt([128, 8], F32, "r_oh2")
        r_ew = k.sbt([128, 8], F32, "r_ew")
        r_G = k.sbt([128, 64], F32, "r_G")
        r_s = [k.sbt([128, 1], F32, "r_s%d" % i) for i in range(12)]
        BK = [T(k.ps([128, 512], F32, "bk%d" % i)[:]) for i in range(8)]
        PG, PU, PD, PBK = BK[0:2], BK[2:4], BK[4:6], BK[6:8]

        k.dma("SP", vec.ap, vecs, writes=[vec])
        k.dma("SP", ident.ap, ident_d, writes=[ident])
        k.dma("SP", v3(wr_sb.ap, 8), wr.rearrange("(a p) n -> p a n", p=128), writes=[wr_sb])
        k.dma("SP", br_bc.ap, br.partition_broadcast(128).rearrange("p o n -> p (o n)"), writes=[br_bc])
        k.op("DVE", lambda e: e.memset(ones_bf.ap, 1.0), writes=[ones_bf])
        for kc in range(8):
            k.op("DVE", lambda e: e.tensor_scalar(out=wr_sb.ap[:, kc * 72:(kc + 1) * 72], in0=wr_sb.ap[:, kc * 72:(kc + 1) * 72],
                                                  scalar1=vec.ap[:, kc:kc + 1], scalar2=None, op0=ALU.mult),
                 reads=[vec, wr_sb], writes=[wr_sb])

        def rmsnorm(src_tiles, src_ap, gcol, dst_t, dst_ap, bank):
            k.op("ACT", lambda e: e.activation(out=v3(SQ.ap, 8), in_=src_ap, func=AF.Square), reads=src_tiles, writes=[SQ])
            for kc in range(8):
                k.op("PE", lambda e: e.matmul(bank.ap, ones_bf.ap, v3(SQ.ap, 8)[:, kc, :], start=(kc == 0), stop=(kc == 7)),
                     reads=[SQ, ones_bf], writes=[bank], pe_acc=(kc > 0))
            k.op("ACT", lambda e: e.activation(out=LNV.ap, in_=bank.ap, func=AF.Ln, scale=1.0 / 1024, bias=EPS), reads=[bank], writes=[LNV])
            k.op("ACT", lambda e: e.activation(out=RSTD.ap, in_=LNV.ap, func=AF.Exp, scale=-0.5), reads=[LNV], writes=[RSTD])
            for kc in range(8):
                k.op("DVE", lambda e: e.scalar_tensor_tensor(out=dst_ap[:, kc, :], in0=src_ap[:, kc, :], scalar=vec.ap[:, gcol + kc:gcol + kc + 1],
                                                             in1=RSTD.ap, op0=ALU.mult, op1=ALU.mult),
                     reads=list(src_tiles) + [RSTD, vec], writes=[dst_t])

        if mode == "A":
            qT_t, kT_t, v_t, ho_t = T(qT_o), T(kT_o), T(v_o), T(hT_o)
        else:
            out_t = T(out_o)
        for hf in range(NH):
            c0 = hf * TH
            k.alias(ABS, UT)
            for kp in range(KAC // 8):
                k.dma("POOL", v3(WB[kp].ap[:, 0:8192], 8), w_a[kp * 1024:(kp + 1) * 1024, :].rearrange("(a p) n -> p a n", p=128), writes=[WB[kp]])
            for t in range(NTL):
                cs = slice(c0 + t * 512, c0 + (t + 1) * 512)
                for d in range(8):
                    k.dma("SP", HT[t][d].ap, hT[d * 128:(d + 1) * 128, cs], writes=[HT[t][d]])
                AB = ABS[t % NAB]
                k.dma("SP", v3(AB.ap, KAC), aT[:, cs].rearrange("(a p) n -> p a n", p=128), writes=[AB])
                for d in range(8):
                    bank = PD[d % 2]
                    for kc in range(KAC):
                        wbt = WB[kc // 8]
                        k.op("PE", lambda e: e.matmul(bank.ap, v3(wbt.ap[:, 0:8192], 8)[:, kc % 8, d * 128:(d + 1) * 128], v3(AB.ap, KAC)[:, kc, :],
                                                      start=(kc == 0), stop=(kc == KAC - 1)),
                             reads=[wbt, AB], writes=[bank], pe_acc=(kc > 0))
                    k.op("DVE", lambda e: e.tensor_tensor(out=HT[t][d].ap, in0=HT[t][d].ap, in1=bank.ap, op=ALU.add),
                         reads=[bank, HT[t][d]], writes=[HT[t][d]])
            k.alias(UT, ABS)
            for t in range(NTL):
                hap = h3[:, :, t * 512:(t + 1) * 512]
                rmsnorm(HT[t], hap, 0, UT[t], UT[t].ap, PBK[0])
                for b in range(4):
                    bs = slice(t * 512 + b * 128, t * 512 + (b + 1) * 128)
                    plg_b, prs = PG[b % 2], PU[b % 2]
                    for kc in range(8):
                        k.op("PE", lambda e: e.matmul(plg_b.ap[:, 0:72], h3[:, kc, bs], wr_sb.ap[:, kc * 72:(kc + 1) * 72], start=(kc == 0), stop=(kc == 7)),
                             reads=[HT[t][kc], wr_sb], writes=[plg_b], pe_acc=(kc > 0))
                    k.op("PE", lambda e: e.matmul(prs.ap[:, 0:2], RSTD.ap[:, b * 128:(b + 1) * 128], ident.ap[:, 0:2], start=True, stop=True),
                         reads=[RSTD, ident], writes=[prs])
                    rt, gmax, ngmax, gsum, gw, m1, m2, dd, ex, w1, w2 = r_s[0:11]
                    k.op("DVE", lambda e: e.tensor_copy(out=rt.ap, in_=prs.ap[:, 0:1]), reads=[prs], writes=[rt])
                    k.op("DVE", lambda e: e.scalar_tensor_tensor(out=r_lg.ap, in0=plg_b.ap[:, 0:72], scalar=rt.ap, in1=br_bc.ap, op0=ALU.mult, op1=ALU.add),
                         reads=[plg_b, rt, br_bc], writes=[r_lg])
                    gl = r_lg.ap[:, 0:8]
                    el = r_lg.ap[:, 8:72]
                    k.op("DVE", lambda e: e.tensor_reduce(out=gmax.ap, in_=gl, axis=AX.X, op=ALU.max), reads=[r_lg], writes=[gmax])
                    k.op("DVE", lambda e: e.tensor_scalar(out=r_goh.ap, in0=gl, scalar1=gmax.ap, scalar2=None, op0=ALU.is_equal), reads=[r_lg, gmax], writes=[r_goh])
                    k.op("DVE", lambda e: e.tensor_scalar(out=ngmax.ap, in0=gmax.ap, scalar1=-1.0, scalar2=None, op0=ALU.mult), reads=[gmax], writes=[ngmax])
                    k.op("ACT", lambda e: e.activation(out=r_a.ap, in_=gl, func=AF.Exp, bias=ngmax.ap, scale=1.0, accum_out=gsum.ap), reads=[r_lg, ngmax], writes=[r_a, gsum])
                    k.op("DVE", lambda e: e.reciprocal(out=gw.ap, in_=gsum.ap), reads=[gsum], writes=[gw])
                    k.op("DVE", lambda e: e.tensor_tensor(out=v3(r_t64.ap, 8), in0=v3(el, 8), in1=r_goh.ap.unsqueeze(2).to_broadcast([128, 8, 8]), op=ALU.mult),
                         reads=[r_lg, r_goh], writes=[r_t64])
                    k.op("DVE", lambda e: e.tensor_reduce(out=r_es.ap, in_=v3(r_t64.ap, 8).rearrange("p g j -> p j g"), axis=AX.X, op=ALU.add), reads=[r_t64], writes=[r_es])
                    k.op("DVE", lambda e: e.tensor_reduce(out=m1.ap, in_=r_es.ap, axis=AX.X, op=ALU.max), reads=[r_es], writes=[m1])
                    k.op("DVE", lambda e: e.tensor_scalar(out=r_oh1.ap, in0=r_es.ap, scalar1=m1.ap, scalar2=None, op0=ALU.is_equal), reads=[r_es, m1], writes=[r_oh1])
                    k.op("DVE", lambda e: e.scalar_tensor_tensor(out=r_es2.ap, in0=r_oh1.ap, scalar=-1e30, in1=r_es.ap, op0=ALU.mult, op1=ALU.add), reads=[r_oh1, r_es], writes=[r_es2])
                    k.op("DVE", lambda e: e.tensor_reduce(out=m2.ap, in_=r_es2.ap, axis=AX.X, op=ALU.max), reads=[r_es2], writes=[m2])
                    k.op("DVE", lambda e: e.tensor_scalar(out=r_oh2.ap, in0=r_es2.ap, scalar1=m2.ap, scalar2=None, op0=ALU.is_equal), reads=[r_es2, m2], writes=[r_oh2])
                    k.op("DVE", lambda e: e.tensor_tensor(out=dd.ap, in0=m2.ap, in1=m1.ap, op=ALU.subtract), reads=[m1, m2], writes=[dd])
                    k.op("ACT", lambda e: e.activation(out=ex.ap, in_=dd.ap, func=AF.Exp), reads=[dd], writes=[ex])
                    k.op("DVE", lambda e: e.tensor_scalar(out=w1.ap, in0=ex.ap, scalar1=1.0, scalar2=None, op0=ALU.add), reads=[ex], writes=[w1])
                    k.op("DVE", lambda e: e.reciprocal(out=w1.ap, in_=w1.ap), reads=[w1], writes=[w1])
                    k.op("DVE", lambda e: e.tensor_tensor(out=w2.ap, in0=ex.ap, in1=w1.ap, op=ALU.mult), reads=[ex, w1], writes=[w2])
                    k.op("DVE", lambda e: e.tensor_tensor(out=w1.ap, in0=w1.ap, in1=gw.ap, op=ALU.mult), reads=[w1, gw], writes=[w1])
                    k.op("DVE", lambda e: e.tensor_tensor(out=w2.ap, in0=w2.ap, in1=gw.ap, op=ALU.mult), reads=[w2, gw], writes=[w2])
                    k.op("DVE", lambda e: e.tensor_scalar(out=r_ew.ap, in0=r_oh1.ap, scalar1=w1.ap, scalar2=None, op0=ALU.mult), reads=[r_oh1, w1], writes=[r_ew])
                    k.op("DVE", lambda e: e.scalar_tensor_tensor(out=r_ew.ap, in0=r_oh2.ap, scalar=w2.ap, in1=r_ew.ap, op0=ALU.mult, op1=ALU.add), reads=[r_oh2, w2, r_ew], writes=[r_ew])
                    k.op("DVE", lambda e: e.tensor_tensor(out=v3(r_G.ap, 8), in0=r_goh.ap.unsqueeze(2).to_broadcast([128, 8, 8]),
                                                          in1=r_ew.ap.unsqueeze(1).to_broadcast([128, 8, 8]), op=ALU.mult), reads=[r_goh, r_ew], writes=[r_G])
                    ptr = PD[b % 2]
                    k.op("PE", lambda e: e.transpose(ptr.ap[0:64, 0:128], r_G.ap, ident.ap), reads=[r_G, ident], writes=[ptr])
                    k.op("DVE", lambda e: e.tensor_copy(out=GT.ap[:, bs], in_=ptr.ap[0:64, 0:128]), reads=[ptr], writes=[GT])
            def load_expert(e_):
                wb = WB[e_ % 2]
                k.dma("POOL", v3(wb.ap[:, 0:4096], 8), wg[e_].rearrange("(a p) n -> p a n", p=128), writes=[wb])
                k.dma("POOL", v3(wb.ap[:, 4096:8192], 8), wu[e_].rearrange("(a p) n -> p a n", p=128), writes=[wb], part=True)
                k.dma("POOL", v3(wb.ap[:, 8192:12288], 4), wd[e_].rearrange("(a p) n -> p a n", p=128), writes=[wb], part=True)

            def emit_gu(e_, t, i):
                wb = WB[e_ % 2]
                Wg3 = v3(wb.ap[:, 0:4096], 8)
                Wu3 = v3(wb.ap[:, 4096:8192], 8)
                pb = PBK[i % 2]
                eb = EB[i % 2]
                k.op("DVE", lambda e: e.tensor_copy(out=eb.ap, in_=ident.ap[0:64, e_:e_ + 1].to_broadcast([64, 128])), reads=[ident], writes=[eb])
                k.op("PE", lambda e: e.matmul(pb.ap, eb.ap, GT.ap[:, t * 512:(t + 1) * 512], start=True, stop=True), reads=[eb, GT], writes=[pb])
                for f in range(4):
                    pg, pu = PG[f % 2], PU[f % 2]
                    for kc in range(8):
                        k.op("PE", lambda e: e.matmul(pg.ap, Wg3[:, kc, f * 128:(f + 1) * 128], UT[t].ap[:, kc, :], start=(kc == 0), stop=(kc == 7)),
                             reads=[wb, UT[t]], writes=[pg], pe_acc=(kc > 0))
                    for kc in range(8):
                        k.op("PE", lambda e: e.matmul(pu.ap, Wu3[:, kc, f * 128:(f + 1) * 128], UT[t].ap[:, kc, :], start=(kc == 0), stop=(kc == 7)),
                             reads=[wb, UT[t]], writes=[pu], pe_acc=(kc > 0))
                    s_, tt = S_[f % 2], TT[f % 2]
                    k.op("ACT", lambda e: e.activation(out=s_.ap, in_=pg.ap, func=AF.Silu), reads=[pg], writes=[s_])
                    k.op("DVE", lambda e: e.tensor_tensor(out=tt.ap, in0=s_.ap, in1=pu.ap, op=ALU.mult), reads=[s_, pu], writes=[tt])
                    k.op("DVE", lambda e: e.tensor_tensor(out=HS[i % 2][f].ap, in0=tt.ap, in1=pb.ap, op=ALU.mult), reads=[tt, pb], writes=[HS[i % 2][f]])

            def emit_dn(e_, t, i):
                wb = WB[e_ % 2]
                Wd3 = v3(wb.ap[:, 8192:12288], 4)
                for d in range(8):
                    pd = PD[d % 2]
                    for f in range(4):
                        k.op("PE", lambda e: e.matmul(pd.ap, Wd3[:, f, d * 128:(d + 1) * 128], HS[i % 2][f].ap, start=(f == 0), stop=(f == 3)),
                             reads=[wb, HS[i % 2][f]], writes=[pd], pe_acc=(f > 0))
                    k.op("DVE", lambda e: e.tensor_tensor(out=HT[t][d].ap, in0=HT[t][d].ap, in1=pd.ap, op=ALU.add), reads=[pd, HT[t][d]], writes=[HT[t][d]])

            seq = [(e_, t) for e_ in range(NE) for t in range(NTL)]
            load_expert(0)
            prev = None
            for i, (e_, t) in enumerate(seq):
                emit_gu(e_, t, i)
                if prev is not None:
                    emit_dn(*prev)
                if t == 0 and e_ + 1 < NE:
                    load_expert(e_ + 1)
                prev = (e_, t, i)
            emit_dn(*prev)
            k.dma("POOL", v3(WB[0].ap[:, 0:8192], 8), plg.rearrange("(a p) n -> p a n", p=128), writes=[WB[0]])
            k.dma("POOL", v3(WB[1].ap[:, 0:2048], 2), plp.rearrange("(a p) n -> p a n", p=128), writes=[WB[1]])
            for t in range(NTL):
                cs = slice(c0 + t * 512, c0 + (t + 1) * 512)
                hap = h3[:, :, t * 512:(t + 1) * 512]
                k.dma("POOL", v3(PB.ap, 2), pT[:, cs].rearrange("(a p) n -> p a n", p=128), writes=[PB])
                rmsnorm(HT[t], hap, 8, UT[t], UT[t].ap, PBK[0])
                for d in range(8):
                    pg, pu = PG[d % 2], PU[d % 2]
                    for kc in range(8):
                        k.op("PE", lambda e: e.matmul(pg.ap, v3(WB[0].ap[:, 0:8192], 8)[:, kc, d * 128:(d + 1) * 128], UT[t].ap[:, kc, :],
                                                      start=(kc == 0), stop=(kc == 7)), reads=[WB[0], UT[t]], writes=[pg], pe_acc=(kc > 0))
                    for kc in range(2):
                        k.op("PE", lambda e: e.matmul(pu.ap, v3(WB[1].ap[:, 0:2048], 2)[:, kc, d * 128:(d + 1) * 128], v3(PB.ap, 2)[:, kc, :],
                                                      start=(kc == 0), stop=(kc == 1)), reads=[WB[1], PB], writes=[pu], pe_acc=(kc > 0))
                    sf = STF[d % 2]
                    k.op("ACT", lambda e: e.activation(out=sf.ap, in_=pg.ap, func=AF.Sigmoid, bias=vec.ap[:, 16 + d:17 + d], scale=1.0), reads=[pg, vec], writes=[sf])
                    k.op("DVE", lambda e: e.tensor_tensor(out=sf.ap, in0=sf.ap, in1=pu.ap, op=ALU.mult), reads=[sf, pu], writes=[sf])
                    k.op("DVE", lambda e: e.tensor_tensor(out=HT[t][d].ap, in0=HT[t][d].ap, in1=sf.ap, op=ALU.add), reads=[sf, HT[t][d]], writes=[HT[t][d]])
            if mode == "A":
                k.dma("POOL", v3(WB[0].ap, 8), wqkv[:, 0:1536].rearrange("(a p) n -> p a n", p=128), writes=[WB[0]])
                k.dma("POOL", v3(WB[1].ap, 8), wqkv[:, 1536:3072].rearrange("(a p) n -> p a n", p=128), writes=[WB[1]])
                for t in range(NTL):
                    cs = slice(c0 + t * 512, c0 + (t + 1) * 512)
                    hap = h3[:, :, t * 512:(t + 1) * 512]
                    for d in range(8):
                        k.dma("SP", hT_o[d * 128:(d + 1) * 128, cs], HT[t][d].ap, reads=[HT[t][d]], writes=[ho_t], part=True)
                    rmsnorm(HT[t], hap, 24, UT[t], UT[t].ap, PBK[0])
                    U3 = UT[t].ap
                    for n in range(16):
                        col = n * 128
                        wbt = WB[0] if col < 1536 else WB[1]
                        lc = col if col < 1536 else col - 1536
                        bank = PG[n % 2]
                        for kc in range(8):
                            k.op("PE", lambda e: e.matmul(bank.ap, v3(wbt.ap, 8)[:, kc, lc:lc + 128], U3[:, kc, :], start=(kc == 0), stop=(kc == 7)),
                                 reads=[wbt, UT[t]], writes=[bank], pe_acc=(kc > 0))
                        sg = STG[n % 2]
                        sc = (1.0 / np.sqrt(128.0)) if n < 8 else 1.0
                        k.op("ACT", lambda e: e.activation(out=sg.ap, in_=bank.ap, func=AF.Identity, scale=float(sc)), reads=[bank], writes=[sg])
                        if n < 8:
                            k.dma("SP", qT_o[n * 128:(n + 1) * 128, cs], sg.ap, reads=[sg], writes=[qT_t], part=True)
                        else:
                            k.dma("SP", kT_o[(n - 8) * 128:(n - 7) * 128, cs], sg.ap, reads=[sg], writes=[kT_t], part=True)
                    for b in range(4):
                        for hh in range(2):
                            bank = PU[hh]
                            for kc in range(8):
                                k.op("PE", lambda e: e.matmul(bank.ap, U3[:, kc, b * 128:(b + 1) * 128], v3(WB[1].ap, 8)[:, kc, 512 + hh * 512:1024 + hh * 512],
                                                              start=(kc == 0), stop=(kc == 7)), reads=[WB[1], UT[t]], writes=[bank], pe_acc=(kc > 0))
                            sg = STG[hh]
                            k.op("ACT", lambda e: e.activation(out=sg.ap, in_=bank.ap, func=AF.Copy), reads=[bank], writes=[sg])
                            r0 = c0 + t * 512 + b * 128
                            k.dma("SP", v_o[r0:r0 + 128, hh * 512:(hh + 1) * 512], sg.ap, reads=[sg], writes=[v_t], part=True)
                fin = [qT_t, kT_t, v_t, ho_t]
            else:
                for t in range(NTL):
                    cs = slice(c0 + t * 512, c0 + (t + 1) * 512)
                    hap = h3[:, :, t * 512:(t + 1) * 512]
                    k.op("ACT", lambda e: e.activation(out=v3(SQ.ap, 8), in_=hap, func=AF.Square), reads=HT[t], writes=[SQ])
                    bank = PBK[0]
                    for kc in range(8):
                        k.op("PE", lambda e: e.matmul(bank.ap, ones_bf.ap, v3(SQ.ap, 8)[:, kc, :], start=(kc == 0), stop=(kc == 7)),
                             reads=[SQ, ones_bf], writes=[bank], pe_acc=(kc > 0))
                    k.op("ACT", lambda e: e.activation(out=LNV.ap, in_=bank.ap, func=AF.Ln, scale=1.0 / 1024, bias=EPS), reads=[bank], writes=[LNV])
                    k.op("ACT", lambda e: e.activation(out=RSTD.ap, in_=LNV.ap, func=AF.Exp, scale=-0.5), reads=[LNV], writes=[RSTD])
                    for d in range(8):
                        k.op("DVE", lambda e: e.scalar_tensor_tensor(out=HT[t][d].ap, in0=HT[t][d].ap, scalar=vec.ap[:, 24 + d:25 + d], in1=RSTD.ap,
                                                                     op0=ALU.mult, op1=ALU.mult), reads=[HT[t][d], RSTD, vec], writes=[HT[t][d]])
                        k.dma("SP", out_o[d * 128:(d + 1) * 128, cs], HT[t][d].ap, reads=[HT[t][d]], writes=[out_t], part=True)
                fin = [out_t]
            if hf == NH - 1:
                k.finish("SP", fin)
        print("tok ninst", k.ninst)
    return nc


def build_attn(L, NHD=2):
    nc = bass.Bass("TRN2", target_bir_lowering=False)

    def din(name, shape, dt=F32):
        return nc.dram_tensor(name, list(shape), dt, kind="ExternalInput").ap()

    qT = din("qT", [NHD, 128, L], BF16)
    kT = din("kT", [NHD, 128, L], BF16)
    vP = din("vP", [NHD, 128, L // 128, 128], BF16)
    cst = din("cst", [128, 256 + 4 * 512])
    oT = nc.dram_tensor("oT", [NHD, 128, L], BF16, kind="ExternalOutput").ap()
    NQT = L // 512
    NB = L // 128
    with ExitStack() as st:
        k = K(nc, st)
        Qs = k.sbt([128, L], BF16, "Qs")
        Ks = k.sbt([128, L], BF16, "Ks")
        Vs = k.sbt([128, L], BF16, "Vs")
        V3 = v3(Vs.ap, NB)
        cf = k.sbt([128, 256 + 2048], F32, "cf")
        trin = k.sbt([128, 128], BF16, "trin")
        onen = k.sbt([128, 128], BF16, "onen")
        m01 = k.sbt([128, 2048], BF16, "m01")
        mneg = k.sbt([128, 2048], F32, "mneg")
        E_ = [k.sbt([128, 512], F32, "E%d" % i) for i in range(3)]
        SPf = [k.sbt([128, 512], BF16, "SPf%d" % i) for i in range(3)]
        SPm = [k.sbt([128, 512], BF16, "SPm%d" % i) for i in range(3)]
        TMP = [k.sbt([128, 512], F32, "TMP%d" % i) for i in range(3)]
        AT = [k.sbt([128, 512], BF16, "AT%d" % i) for i in range(3)]
        OFFS = [k.sbt([128, 512], F32, "OFF%d" % i) for i in range(2)]
        offi = [0]
        OST = [k.sbt([128, 512], BF16, "OST%d" % i) for i in range(2)]
        BK = [T(k.ps([128, 512], F32, "bk%d" % i)[:]) for i in range(8)]
        PA, PBb, PO = BK[0:3], BK[3:6], BK[6:8]
        oT_t = T(oT)

        k.dma("SP", cf.ap, cst, writes=[cf])
        k.op("DVE", lambda e: e.tensor_copy(out=trin.ap, in_=cf.ap[:, 0:128]), reads=[cf], writes=[trin])
        k.op("DVE", lambda e: e.tensor_copy(out=onen.ap, in_=cf.ap[:, 128:256]), reads=[cf], writes=[onen])
        k.op("DVE", lambda e: e.tensor_copy(out=m01.ap, in_=cf.ap[:, 256:2304]), reads=[cf], writes=[m01])
        k.op("DVE", lambda e: e.tensor_scalar(out=mneg.ap, in0=cf.ap[:, 256:2304], scalar1=-1.0, scalar2=30000.0, op0=ALU.add, op1=ALU.mult),
             reads=[cf], writes=[mneg])

        jobs = []
        for hd in range(NHD):
            for J in range(NQT):
                nb = 4 * J + 4
                for i, n in enumerate(range(nb - 1, -1, -1)):
                    jobs.append(dict(hd=hd, J=J, n=n, first=(i == 0), last=(n == 0), r=(n - 4 * J) if n >= 4 * J else -1))

        def load_head(hd):
            k.dma("SP", Qs.ap, qT[hd], writes=[Qs])
            k.dma("SP", Ks.ap, kT[hd], writes=[Ks])
            k.dma("SP", V3, vP[hd], writes=[Vs])

        def s1(j, i):
            if j["first"] and j["J"] == 0:
                load_head(j["hd"])
            A = PA[i % 3]
            qs = slice(j["J"] * 512, (j["J"] + 1) * 512)
            ks = slice(j["n"] * 128, (j["n"] + 1) * 128)
            k.op("PE", lambda e: e.matmul(A.ap, Ks.ap[:, ks], Qs.ap[:, qs], start=True, stop=False), reads=[Ks, Qs], writes=[A])
            k.op("ACT", lambda e: e.activation(out=E_[i % 3].ap, in_=A.ap, func=AF.Exp), reads=[A], writes=[E_[i % 3]])

        def s1b(j, i):
            k.op("ACT", lambda e: e.activation(out=SPf[i % 3].ap, in_=E_[i % 3].ap, func=AF.Ln, bias=1.0, scale=1.0), reads=[E_[i % 3]], writes=[SPf[i % 3]])
            if j["r"] >= 0:
                r = j["r"]
                k.op("POOL", lambda e: e.tensor_tensor(out=SPm[i % 3].ap, in0=SPf[i % 3].ap, in1=m01.ap[:, r * 512:(r + 1) * 512], op=ALU.mult),
                     reads=[SPf[i % 3], m01], writes=[SPm[i % 3]])

        def s2(j, i):
            A, B = PA[i % 3], PBb[i % 3]
            sp = SPm[i % 3] if j["r"] >= 0 else SPf[i % 3]
            OFF = OFFS[offi[0] % 2]
            if j["first"]:
                k.op("DVE", lambda e: e.memset(OFF.ap, 0.0), writes=[OFF])
            k.op("PE", lambda e: e.matmul(A.ap, trin.ap, sp.ap, start=False, stop=True), reads=[trin, sp], writes=[A], pe_acc=True)
            k.op("PE", lambda e: e.matmul(B.ap, onen.ap, sp.ap, start=True, stop=True), reads=[onen, sp], writes=[B])
            tm = TMP[i % 3]
            k.op("DVE", lambda e: e.tensor_tensor(out=tm.ap, in0=A.ap, in1=OFF.ap, op=ALU.add), reads=[A, OFF], writes=[tm])
            if j["r"] >= 0:
                r = j["r"]
                k.op("DVE", lambda e: e.tensor_tensor(out=tm.ap, in0=tm.ap, in1=mneg.ap[:, r * 512:(r + 1) * 512], op=ALU.add), reads=[tm, mneg], writes=[tm])
            k.op("ACT", lambda e: e.activation(out=AT[i % 3].ap, in_=tm.ap, func=AF.Exp), reads=[tm], writes=[AT[i % 3]])
            if not j["last"]:
                OFN = OFFS[(offi[0] + 1) % 2]
                k.op("DVE", lambda e: e.tensor_tensor(out=OFN.ap, in0=OFF.ap, in1=B.ap, op=ALU.add), reads=[B, OFF], writes=[OFN])
            offi[0] += 1

        grp = [0]

        def s3(j, i):
            O = PO[grp[0] % 2]
            k.op("PE", lambda e: e.matmul(O.ap, V3[:, j["n"], :], AT[i % 3].ap, start=j["first"], stop=j["last"]),
                 reads=[Vs, AT[i % 3]], writes=[O], pe_acc=(not j["first"]))
            if j["last"]:
                og = OST[grp[0] % 2]
                k.op("ACT", lambda e: e.activation(out=og.ap, in_=O.ap, func=AF.Copy), reads=[O], writes=[og])
                k.dma("SP", oT[j["hd"], :, j["J"] * 512:(j["J"] + 1) * 512], og.ap, reads=[og], writes=[oT_t], part=True)
                grp[0] += 1

        n = len(jobs)
        stages = [s1, s1b, s2, s3]
        done = [0] * n

        def run_next(jj):
            stages[done[jj]](jobs[jj], jj)
            done[jj] += 1

        for i in range(n + 3):
            if i < n and jobs[i]["first"] and jobs[i]["J"] == 0 and i > 0:
                for jj in range(max(0, i - 3), i):
                    while done[jj] < 4:
                        run_next(jj)
            for d_ in range(4):
                jj = i - d_
                if 0 <= jj < n and done[jj] == d_:
                    run_next(jj)
        assert all(x == 4 for x in done)
        k.finish("SP", [oT_t])
        print("attn ninst", k.ninst, "jobs", n)
    return nc


def attn_consts():
    s = np.arange(128)
    tri_neg = -(s[:, None] >= s[None, :]).astype(np.float32)
    ones_neg = -np.ones((128, 128), np.float32)
    q = np.arange(512)
    masks = [(q[None, :] > (s[:, None] + 128 * r)).astype(np.float32) for r in range(4)]
    return np.ascontiguousarray(np.concatenate([tri_neg, ones_neg] + masks, axis=1))


def build_ssd(L):
    nc = bass.Bass("TRN2", target_bir_lowering=False)

    def din(name, shape, dt=F32):
        return nc.dram_tensor(name, list(shape), dt, kind="ExternalInput").ap()

    xT = din("xT", [1024, L])
    w_in = din("w_in", [1024, 1288])
    vecs = din("vecs", [128, 38])
    rowc = din("rowc", [1, 1040])
    cst = din("cst", [128, 640])
    ynT = nc.dram_tensor("ynT", [512, L], BF16, kind="ExternalOutput").ap()
    NTL = L // 512
    with ExitStack() as st:
        k = K(nc, st)
        W = k.sbt([128, 8 * 1288], BF16, "W")
        W3 = v3(W.ap, 8)
        XT = [k.sbt([128, 8 * 512], F32, "XT%d" % i) for i in range(2)]
        UT = [k.sbt([128, 8 * 512], BF16, "UT%d" % i) for i in range(2)]
        SQ = k.sbt([128, 8 * 512], BF16, "SQ")
        LNV = k.sbt([128, 512], F32, "LNV")
        RSTD = k.sbt([128, 512], F32, "RSTD")
        vec = k.sbt([128, 38], F32, "vec_sb")
        bc = k.sbt([128, 1040], F32, "bc")
        cf = k.sbt([128, 640], F32, "cf")
        identb = k.sbt([128, 128], BF16, "identb")
        ones_bf = k.sbt([128, 128], BF16, "ones_bf")
        A_bc = k.sbt([128, 8], F32, "A_bc")
        XR = [k.sbt([128, 515], F32, "XR%d" % i) for i in range(6)]
        ACC = [k.sbt([128, 512], F32, "ACC%d" % i) for i in range(2)]
        XC = [k.sbt([128, 6 * 512], BF16, "XC%d" % i) for i in range(2)]
        ZS_2 = [k.sbt([128, 512], F32, "ZS_%d" % i) for i in range(3)]
        DTt_2 = [k.sbt([128, 8], F32, "DTt_%d" % i) for i in range(3)]
        DT_2 = [k.sbt([128, 8], F32, "DT_%d" % i) for i in range(3)]
        AA_2 = [k.sbt([128, 8], F32, "AA_%d" % i) for i in range(3)]
        EXPS_2 = [k.sbt([128, 24], F32, "EXPS_%d" % i) for i in range(3)]
        LH = [k.sbt([128, 128], F32, "LH%d" % i) for i in range(2)]
        DEC_2 = [k.sbt([128, 1024], F32, "DEC_%d" % i) for i in range(3)]
        CBm_2 = [k.sbt([128, 128], F32, "CBm_%d" % i) for i in range(3)]
        WT_2 = [k.sbt([128, 1024], BF16, "WT_%d" % i) for i in range(3)]
        XTOK_2 = [k.sbt([128, 512], BF16, "XTOK_%d" % i) for i in range(3)]
        BTOK_2 = [k.sbt([128, 128], BF16, "BTOK_%d" % i) for i in range(3)]
        XDT_2 = [k.sbt([128, 512], BF16, "XDT_%d" % i) for i in range(3)]
        XW_2 = [k.sbt([128, 512], BF16, "XW_%d" % i) for i in range(3)]
        Y1_2 = [k.sbt([128, 512], F32, "Y1_%d" % i) for i in range(3)]
        Y2_2 = [k.sbt([128, 512], F32, "Y2_%d" % i) for i in range(3)]
        YZ_2 = [k.sbt([128, 512], F32, "YZ_%d" % i) for i in range(3)]
        YSQ_2 = [k.sbt([128, 512], F32, "YSQ_%d" % i) for i in range(3)]
        YN_2 = [k.sbt([128, 512], BF16, "YN_%d" % i) for i in range(3)]
        SF = k.sbt([128, 512], F32, "SF")
        SBF = k.sbt([128, 512], BF16, "SBF")
        sc_2 = [[k.sbt([128, 1], F32, "sc%d_%d" % (j, i)) for i in range(3)] for j in range(3)]
        LH4 = [k.sbt([128, 128], F32, "LHx%d" % i) for i in range(2)]
        YNT = [k.sbt([128, 4 * 512], BF16, "YNT%d" % i) for i in range(2)]
        BK = [T(k.ps([128, 512], F32, "bk%d" % i)[:]) for i in range(8)]
        P_proj, P_st, P_sm, P_sg0, P_sg1, P_tr, P_yd, P_yo = BK
        ynT_t = T(ynT)

        k.dma("SP", vec.ap, vecs, writes=[vec])
        k.dma("SP", cf.ap, cst, writes=[cf])
        k.dma("SP", bc.ap, rowc.partition_broadcast(128).rearrange("p o n -> p (o n)"), writes=[bc])
        k.dma("POOL", W3, w_in.rearrange("(a p) n -> p a n", p=128), writes=[W])
        ident = cf.ap[:, 0:128]
        tri = cf.ap[:, 128:256]
        Um = cf.ap[:, 256:384]
        maskU = cf.ap[:, 384:512]
        onesf = cf.ap[:, 512:640]
        k.op("DVE", lambda e: e.tensor_copy(out=identb.ap, in_=ident), reads=[cf], writes=[identb])
        k.op("DVE", lambda e: e.memset(ones_bf.ap, 1.0), writes=[ones_bf])
        k.op("ACT", lambda e: e.activation(out=A_bc.ap, in_=bc.ap[:, 8:16], func=AF.Exp), reads=[bc], writes=[A_bc])
        k.op("DVE", lambda e: e.tensor_scalar(out=A_bc.ap, in0=A_bc.ap, scalar1=-1.0, scalar2=None, op0=ALU.mult), reads=[A_bc], writes=[A_bc])
        for cc in range(6):
            k.op("DVE", lambda e: e.memset(XR[cc].ap[:, 0:3], 0.0), writes=[XR[cc]])
        k.op("DVE", lambda e: e.memset(SF.ap, 0.0), writes=[SF])
        k.op("DVE", lambda e: e.memset(SBF.ap, 0.0), writes=[SBF])
        dtb, Dx, gn = bc.ap[:, 0:8], bc.ap[:, 16:528], bc.ap[:, 528:1040]

        def load_x(t):
            k.dma("SP", v3(XT[t % 2].ap, 8), xT[:, t * 512:(t + 1) * 512].rearrange("(a p) n -> p a n", p=128), writes=[XT[t % 2]])

        def stage_a(t):
            if t + 1 < NTL:
                load_x(t + 1)
            xt, ut, xc = XT[t % 2], UT[t % 2], XC[t % 2]
            x3, u3, xc3 = v3(xt.ap, 8), v3(ut.ap, 8), v3(xc.ap, 6)
            k.op("ACT", lambda e: e.activation(out=v3(SQ.ap, 8), in_=x3, func=AF.Square), reads=[xt], writes=[SQ])
            for kc in range(8):
                k.op("PE", lambda e: e.matmul(P_proj.ap, ones_bf.ap, v3(SQ.ap, 8)[:, kc, :], start=(kc == 0), stop=(kc == 7)),
                     reads=[SQ, ones_bf], writes=[P_proj], pe_acc=(kc > 0))
            k.op("ACT", lambda e: e.activation(out=LNV.ap, in_=P_proj.ap, func=AF.Ln, scale=1.0 / 1024, bias=EPS), reads=[P_proj], writes=[LNV])
            k.op("ACT", lambda e: e.activation(out=RSTD.ap, in_=LNV.ap, func=AF.Exp, scale=-0.5), reads=[LNV], writes=[RSTD])
            for kc in range(8):
                k.op("DVE", lambda e: e.scalar_tensor_tensor(out=u3[:, kc, :], in0=x3[:, kc, :], scalar=vec.ap[:, kc:kc + 1], in1=RSTD.ap,
                                                             op0=ALU.mult, op1=ALU.mult), reads=[xt, RSTD, vec], writes=[ut])
            for cc in range(6):
                for kc in range(8):
                    k.op("PE", lambda e: e.matmul(P_proj.ap, W3[:, kc, 512 + cc * 128:512 + (cc + 1) * 128], u3[:, kc, :], start=(kc == 0), stop=(kc == 7)),
                         reads=[W, ut], writes=[P_proj], pe_acc=(kc > 0))
                xr, acc = XR[cc], ACC[cc % 2]
                k.op("ACT", lambda e: e.activation(out=xr.ap[:, 3:515], in_=P_proj.ap, func=AF.Copy), reads=[P_proj], writes=[xr])
                k.op("DVE", lambda e: e.tensor_scalar(out=acc.ap, in0=xr.ap[:, 0:512], scalar1=vec.ap[:, 8 + cc * 4:9 + cc * 4], scalar2=None, op0=ALU.mult),
                     reads=[xr, vec], writes=[acc])
                for kk in range(1, 4):
                    k.op("DVE", lambda e: e.scalar_tensor_tensor(out=acc.ap, in0=xr.ap[:, kk:kk + 512], scalar=vec.ap[:, 8 + cc * 4 + kk:9 + cc * 4 + kk],
                                                                 in1=acc.ap, op0=ALU.mult, op1=ALU.add), reads=[xr, vec, acc], writes=[acc])
                k.op("ACT", lambda e: e.activation(out=xc3[:, cc, :], in_=acc.ap, func=AF.Silu, bias=vec.ap[:, 32 + cc:33 + cc], scale=1.0),
                     reads=[acc, vec], writes=[xc])
                k.op("DVE", lambda e: e.tensor_copy(out=xr.ap[:, 0:3], in_=xr.ap[:, 512:515]), reads=[xr], writes=[xr])

        def chunk_gen(t, q):
            ut, xc = UT[t % 2], XC[t % 2]
            u3, xc3 = v3(ut.ap, 8), v3(xc.ap, 6)
            ynt = YNT[t % 2]
            cs = slice(q * 128, (q + 1) * 128)
            ci = t * 4 + q
            ZS = ZS_2[ci % 3]
            DTt = DTt_2[ci % 3]
            DT = DT_2[ci % 3]
            AA = AA_2[ci % 3]
            EXPS = EXPS_2[ci % 3]
            DEC = DEC_2[ci % 3]
            CBm = CBm_2[ci % 3]
            WT = WT_2[ci % 3]
            XTOK = XTOK_2[ci % 3]
            BTOK = BTOK_2[ci % 3]
            XDT = XDT_2[ci % 3]
            XW = XW_2[ci % 3]
            Y1 = Y1_2[ci % 3]
            Y2 = Y2_2[ci % 3]
            YZ = YZ_2[ci % 3]
            YSQ = YSQ_2[ci % 3]
            YN = YN_2[ci % 3]
            sc = sc_2[ci % 3]
            for kc in range(8):
                k.op("PE", lambda e: e.matmul(P_proj.ap, u3[:, kc, cs], W3[:, kc, 0:512], start=(kc == 0), stop=(kc == 7)),
                     reads=[W, ut], writes=[P_proj], pe_acc=(kc > 0))
            k.op("ACT", lambda e: e.activation(out=ZS.ap, in_=P_proj.ap, func=AF.Silu), reads=[P_proj], writes=[ZS])
            yield
            for kc in range(8):
                k.op("PE", lambda e: e.matmul(P_sm.ap[:, 0:8], u3[:, kc, cs], W3[:, kc, 1280:1288], start=(kc == 0), stop=(kc == 7)),
                     reads=[W, ut], writes=[P_sm], pe_acc=(kc > 0))
            k.op("DVE", lambda e: e.tensor_tensor(out=DTt.ap, in0=P_sm.ap[:, 0:8], in1=dtb, op=ALU.add), reads=[P_sm, bc], writes=[DTt])
            k.op("ACT", lambda e: e.activation(out=DTt.ap, in_=DTt.ap, func=AF.Exp), reads=[DTt], writes=[DTt])
            k.op("ACT", lambda e: e.activation(out=DT.ap, in_=DTt.ap, func=AF.Ln, bias=1.0, scale=1.0), reads=[DTt], writes=[DT])
            k.op("DVE", lambda e: e.tensor_tensor(out=AA.ap, in0=DT.ap, in1=A_bc.ap, op=ALU.mult), reads=[DT, A_bc], writes=[AA])
            yield
            trb = P_tr.ap.bitcast(BF16)
            for cc in range(4):
                k.op("PE", lambda e: e.transpose(trb[:, cc * 128:(cc + 1) * 128], xc3[:, cc, cs], identb.ap), reads=[xc, identb], writes=[P_tr])
            k.op("PE", lambda e: e.transpose(trb[:, 512:640], xc3[:, 4, cs], identb.ap), reads=[xc, identb], writes=[P_tr])
            k.op("ACT", lambda e: e.activation(out=XTOK.ap, in_=trb[:, 0:512], func=AF.Copy), reads=[P_tr], writes=[XTOK])
            k.op("ACT", lambda e: e.activation(out=BTOK.ap, in_=trb[:, 512:640], func=AF.Copy), reads=[P_tr], writes=[BTOK])
            yield
            k.op("PE", lambda e: e.matmul(P_sm.ap[:, 32:40], tri, AA.ap, start=True, stop=True), reads=[cf, AA], writes=[P_sm])
            k.op("PE", lambda e: e.matmul(P_sm.ap[:, 40:48], Um, AA.ap, start=True, stop=True), reads=[cf, AA], writes=[P_sm])
            k.op("PE", lambda e: e.matmul(P_sm.ap[:, 48:56], onesf, AA.ap, start=True, stop=True), reads=[cf, AA], writes=[P_sm])
            k.op("ACT", lambda e: e.activation(out=EXPS.ap, in_=P_sm.ap[:, 32:56], func=AF.Exp), reads=[P_sm], writes=[EXPS])
            e_l, dte, cd = EXPS.ap[:, 0:8], EXPS.ap[:, 8:16], EXPS.ap[:, 16:24]
            yield
            k.op("PE", lambda e: e.matmul(P_sm.ap[:, 128:256], xc3[:, 4, cs], xc3[:, 5, cs], start=True, stop=True), reads=[xc], writes=[P_sm])
            k.op("DVE", lambda e: e.tensor_tensor(out=CBm.ap, in0=P_sm.ap[:, 128:256], in1=maskU, op=ALU.mult), reads=[P_sm, cf], writes=[CBm])
            yield
            for hh in range(8):
                lh = (LH + LH4)[hh % 4]
                k.op("DVE", lambda e: e.tensor_scalar(out=lh.ap, in0=Um, scalar1=AA.ap[:, hh:hh + 1], scalar2=None, op0=ALU.mult), reads=[cf, AA], writes=[lh])
                bank = P_sg0 if hh < 4 else P_sg1
                k.op("PE", lambda e: e.matmul(bank.ap[:, (hh % 4) * 128:(hh % 4 + 1) * 128], lh.ap, tri, start=True, stop=True), reads=[lh, cf], writes=[bank])
            k.op("ACT", lambda e: e.activation(out=DEC.ap[:, 0:512], in_=P_sg0.ap, func=AF.Exp), reads=[P_sg0], writes=[DEC])
            yield
            k.op("ACT", lambda e: e.activation(out=DEC.ap[:, 512:1024], in_=P_sg1.ap, func=AF.Exp), reads=[P_sg1], writes=[DEC])
            k.op("DVE", lambda e: e.tensor_tensor(out=v3(WT.ap, 8), in0=v3(DEC.ap, 8), in1=CBm.ap.unsqueeze(1).to_broadcast([128, 8, 128]), op=ALU.mult),
                 reads=[DEC, CBm], writes=[WT])
            k.op("DVE", lambda e: e.tensor_tensor(out=v3(XDT.ap, 8), in0=v3(XTOK.ap, 8), in1=DT.ap.unsqueeze(2).to_broadcast([128, 8, 64]), op=ALU.mult),
                 reads=[XTOK, DT], writes=[XDT])
            k.op("DVE", lambda e: e.tensor_tensor(out=v3(XW.ap, 8), in0=v3(XDT.ap, 8), in1=dte.unsqueeze(2).to_broadcast([128, 8, 64]), op=ALU.mult),
                 reads=[XDT, EXPS], writes=[XW])
            yield
            for hh in range(8):
                k.op("PE", lambda e: e.matmul(P_yd.ap[:, hh * 64:(hh + 1) * 64], v3(WT.ap, 8)[:, hh, :], v3(XDT.ap, 8)[:, hh, :], start=True, stop=True),
                     reads=[WT, XDT], writes=[P_yd])
            k.op("PE", lambda e: e.matmul(P_yo.ap, xc3[:, 5, cs], SBF.ap, start=True, stop=True), reads=[xc, SBF], writes=[P_yo])
            k.op("DVE", lambda e: e.tensor_tensor(out=v3(Y1.ap, 8), in0=v3(P_yo.ap, 8), in1=e_l.unsqueeze(2).to_broadcast([128, 8, 64]), op=ALU.mult),
                 reads=[P_yo, EXPS], writes=[Y1])
            k.op("DVE", lambda e: e.tensor_tensor(out=Y1.ap, in0=Y1.ap, in1=P_yd.ap, op=ALU.add), reads=[Y1, P_yd], writes=[Y1])
            yield
            k.op("PE", lambda e: e.matmul(P_st.ap, BTOK.ap, XW.ap, start=True, stop=True), reads=[BTOK, XW], writes=[P_st])
            k.op("DVE", lambda e: e.tensor_tensor(out=v3(SF.ap, 8), in0=v3(SF.ap, 8), in1=cd.unsqueeze(2).to_broadcast([128, 8, 64]), op=ALU.mult),
                 reads=[SF, EXPS], writes=[SF])
            k.op("DVE", lambda e: e.tensor_tensor(out=SF.ap, in0=SF.ap, in1=P_st.ap, op=ALU.add), reads=[SF, P_st], writes=[SF])
            k.op("ACT", lambda e: e.activation(out=SBF.ap, in_=SF.ap, func=AF.Copy), reads=[SF], writes=[SBF])
            yield
            k.op("DVE", lambda e: e.tensor_tensor(out=Y2.ap, in0=XTOK.ap, in1=Dx, op=ALU.mult), reads=[XTOK, bc], writes=[Y2])
            k.op("DVE", lambda e: e.tensor_tensor(out=Y2.ap, in0=Y2.ap, in1=Y1.ap, op=ALU.add), reads=[Y2, Y1], writes=[Y2])
            k.op("DVE", lambda e: e.tensor_tensor(out=YZ.ap, in0=Y2.ap, in1=ZS.ap, op=ALU.mult), reads=[Y2, ZS], writes=[YZ])
            k.op("ACT", lambda e: e.activation(out=YSQ.ap, in_=YZ.ap, func=AF.Square, accum_out=sc[0].ap), reads=[YZ], writes=[YSQ, sc[0]])
            k.op("ACT", lambda e: e.activation(out=sc[1].ap, in_=sc[0].ap, func=AF.Ln, scale=1.0 / 512, bias=EPS), reads=[sc[0]], writes=[sc[1]])
            k.op("ACT", lambda e: e.activation(out=sc[2].ap, in_=sc[1].ap, func=AF.Exp, scale=-0.5), reads=[sc[1]], writes=[sc[2]])
            k.op("DVE", lambda e: e.scalar_tensor_tensor(out=YN.ap, in0=YZ.ap, scalar=sc[2].ap, in1=gn, op0=ALU.mult, op1=ALU.mult),
                 reads=[YZ, sc[2], bc], writes=[YN])
            yield
            for cc in range(4):
                k.op("PE", lambda e: e.transpose(trb[:, cc * 128:(cc + 1) * 128], YN.ap[:, cc * 128:(cc + 1) * 128], identb.ap), reads=[YN, identb], writes=[P_tr])
            k.op("ACT", lambda e: e.activation(out=v3(ynt.ap, 4)[:, :, cs], in_=trb[:, 0:512].rearrange("p (a n) -> p a n", a=4), func=AF.Copy),
                 reads=[P_tr], writes=[ynt])
            if q == 3:
                k.dma("SP", ynT[:, t * 512:(t + 1) * 512].rearrange("(a p) n -> p a n", p=128), v3(ynt.ap, 4), reads=[ynt], writes=[ynT_t], part=True)


        load_x(0)
        NS, PER = 12, 6
        chunks = [(t, q) for t in range(NTL) for q in range(4)]
        gens = {}
        nslot = (len(chunks) - 1) * PER + NS + 2
        for tau in range(nslot):
            for ci, (t, q) in enumerate(chunks):
                st_ = tau - ci * PER
                if st_ < 0:
                    break
                if ci in gens and gens[ci] is None:
                    continue
                if ci not in gens:
                    if q == 0:
                        stage_a(t)
                    gens[ci] = chunk_gen(t, q)
                try:
                    next(gens[ci])
                except StopIteration:
                    gens[ci] = None
        k.finish("SP", [ynT_t])
        print("ssd ninst", k.ninst)
    return nc


def ssd_consts():
    s = np.arange(128)
    ident = np.eye(128, dtype=np.float32)
    tri = (s[:, None] <= s[None, :]).astype(np.float32)
    U = (s[:, None] > s[None, :]).astype(np.float32)
    maskU = (s[None, :] >= s[:, None]).astype(np.float32)
    ones = np.ones((128, 128), np.float32)
    return np.ascontiguousarray(np.concatenate([ident, tri, U, maskU, ones], axis=1))


def ssd_host_inputs(g, ssd_norm, w_in, conv_w, conv_b, dt_bias, a_log, d_skip, gnorm):
    cols = np.concatenate([np.arange(g * 512, (g + 1) * 512), 2048 + np.arange(g * 512, (g + 1) * 512),
                           4096 + np.arange(g * 128, (g + 1) * 128), 4096 + 512 + np.arange(g * 128, (g + 1) * 128),
                           5120 + np.arange(g * 8, (g + 1) * 8)])
    w = np.ascontiguousarray(w_in[:, cols])
    ch = np.concatenate([np.arange(g * 512, (g + 1) * 512), 2048 + np.arange(g * 128, (g + 1) * 128), 2048 + 512 + np.arange(g * 128, (g + 1) * 128)])
    cw = conv_w[:, ch]
    cb = conv_b[ch]
    vecs = np.zeros((128, 38), np.float32)
    vecs[:, 0:8] = ssd_norm.reshape(8, 128).T
    for cc in range(6):
        vecs[:, 8 + cc * 4:12 + cc * 4] = cw[:, cc * 128:(cc + 1) * 128].T
        vecs[:, 32 + cc] = cb[cc * 128:(cc + 1) * 128]
    rowc = np.concatenate([dt_bias[g * 8:(g + 1) * 8], a_log[g * 8:(g + 1) * 8], np.repeat(d_skip[g * 8:(g + 1) * 8], 64),
                           gnorm[g * 512:(g + 1) * 512]])[None, :].astype(np.float32)
    return w, vecs, np.ascontiguousarray(rowc)


_CACHE = {}


def _prog(name, fn, *a):
    key = (name,) + a
    if key not in _CACHE:
        _CACHE[key] = fn(*a)
    return _CACHE[key]


def _vcol(v):
    return np.asarray(v, np.float32).reshape(8, 128).T


def kernel(x, p, ssd_norm, ssd_w_in, ssd_conv_w, ssd_conv_b, ssd_dt_bias, ssd_a_log, ssd_d, ssd_gnorm, ssd_w_out,
           sb_norm, sb_w_qkv, sb_w_o, moe_norm, moe_w_rg, moe_b_rg, moe_w_re, moe_b_re, moe_w_gate, moe_w_up, moe_w_down,
           ple_norm, ple_w_gate, ple_b_gate, ple_w_proj, final_norm):
    f32 = lambda a: np.ascontiguousarray(np.asarray(a, dtype=np.float32))
    x, p = f32(x), f32(p)
    Bsz, L, D = x.shape
    NC = 8
    NT = (Bsz * L) // NC
    PB = NC // Bsz
    cores = list(range(NC))
    ident = np.eye(128, dtype=np.float32)

    nc1 = _prog("ssd", build_ssd, L)
    cst1 = ssd_consts()
    xTb = [np.ascontiguousarray(x[b].T) for b in range(Bsz)]
    maps = []
    for c in cores:
        b, g = c // PB, c % PB
        w, vecs, rowc = ssd_host_inputs(g, f32(ssd_norm[0]), f32(ssd_w_in[0]), f32(ssd_conv_w[0]), f32(ssd_conv_b[0]),
                                        f32(ssd_dt_bias[0]), f32(ssd_a_log[0]), f32(ssd_d[0]), f32(ssd_gnorm[0]))
        maps.append(dict(xT=xTb[b], w_in=w, vecs=vecs, rowc=rowc, cst=cst1))
    r1 = run_bass_kernel_spmd(nc1, maps, core_ids=cores).results
    ynT = [np.concatenate([r1[b * PB + g]["ynT"] for g in range(PB)], axis=0) for b in range(Bsz)]

    mcst = moe_consts(NT)

    def tok_maps(i, hT_list, aT_list, w_a, tail_norm, extra):
        vecs = np.ascontiguousarray(np.concatenate([_vcol(moe_norm[i]), _vcol(ple_norm[i]), _vcol(ple_b_gate[i]), _vcol(tail_norm)], axis=1))
        wr = np.ascontiguousarray(np.concatenate([f32(moe_w_rg[i]), f32(moe_w_re[i])], axis=1))
        br = np.ascontiguousarray(np.concatenate([f32(moe_b_rg[i]), f32(moe_b_re[i])])[None, :])
        wg_, wu_, wd_ = f32(moe_w_gate[i]), f32(moe_w_up[i]), f32(moe_w_down[i])
        plg_, plp_ = f32(ple_w_gate[i]), f32(ple_w_proj[i])
        out = []
        for c in cores:
            b, j = c // PB, c % PB
            sl = slice(j * NT, (j + 1) * NT)
            m = dict(hT=hT_list[c], aT=np.ascontiguousarray(aT_list[b][:, sl]), w_a=w_a, vecs=vecs, wr=wr, br=br, wg=wg_, wu=wu_, wd=wd_,
                     plg=plg_, plp=plp_, pT=np.ascontiguousarray(p[i, b, sl].T), mcst=mcst)
            m.update(extra)
            out.append(m)
        return out

    nc2 = _prog("tokA", build_tok2, NT, 2048, "A")
    hT0 = [np.ascontiguousarray(x[c // PB, (c % PB) * NT:(c % PB + 1) * NT].T) for c in cores]
    r2 = run_bass_kernel_spmd(nc2, tok_maps(0, hT0, ynT, f32(ssd_w_out[0]), f32(sb_norm[0]), dict(wqkv=f32(sb_w_qkv[0]))), core_ids=cores).results
    hT1 = [r2[c]["hT_o"] for c in cores]
    qTb = [np.concatenate([r2[b * PB + j]["qT_o"] for j in range(PB)], axis=1) for b in range(Bsz)]
    kTb = [np.concatenate([r2[b * PB + j]["kT_o"] for j in range(PB)], axis=1) for b in range(Bsz)]
    vb = [np.concatenate([r2[b * PB + j]["v_o"] for j in range(PB)], axis=0) for b in range(Bsz)]

    nc3 = _prog("attn", build_attn, L)
    cst3 = attn_consts()
    maps = []
    for c in cores:
        b, hp = c // PB, c % PB
        rows = slice(hp * 256, (hp + 1) * 256)
        vP = np.ascontiguousarray(vb[b][:, rows].reshape(L // 128, 128, 2, 128).transpose(2, 1, 0, 3))
        maps.append(dict(qT=np.ascontiguousarray(qTb[b][rows].reshape(2, 128, L)), kT=np.ascontiguousarray(kTb[b][rows].reshape(2, 128, L)),
                         vP=vP, cst=cst3))
    r3 = run_bass_kernel_spmd(nc3, maps, core_ids=cores).results
    oT = [np.concatenate([r3[b * PB + hp]["oT"].reshape(256, L) for hp in range(PB)], axis=0) for b in range(Bsz)]

    nc4 = _prog("tokB", build_tok2, NT, 1024, "B")
    r4 = run_bass_kernel_spmd(nc4, tok_maps(1, hT1, oT, f32(sb_w_o[0]), f32(final_norm), {}), core_ids=cores).results
    out = np.empty((Bsz, L, D), np.float32)
    for c in cores:
        b, j = c // PB, c % PB
        out[b, j * NT:(j + 1) * NT, :] = r4[c]["out_o"].T
    return out


I32 = mybir.dt.int32
MB = 128
NBLK_OF = lambda NT: (2 * NT) // MB + 64


def moe_consts(NT):
    nblk = NBLK_OF(NT)
    s = np.arange(128)
    SL = (s[:, None] < s[None, :]).astype(np.float32)
    THR = np.tile((np.arange(64) * MB).astype(np.float32)[None, :], (128, 1))
    JB = np.tile((np.arange(nblk) * MB).astype(np.float32)[None, :], (128, 1))
    KP = (np.arange(8)[None, :] + 2 * s[:, None]).astype(np.float32)
    return np.ascontiguousarray(np.concatenate([np.eye(128, dtype=np.float32), SL, THR, JB, KP], axis=1))


def idma(self, out_ap, in_ap, idx_t, idx_ap, gather, reads=(), writes=(), part=False, bound=None):
    t = writes[0]
    if t.dsem is None:
        key = "d%d" % self.ndsem
        t.dsem = self.stack.enter_context(self.nc.semaphore(key))
        self.semobj[key] = t.dsem
        t.name = key
        self.ndsem += 1
    key = t.name
    for r in list(reads) + [idx_t]:
        if r.w is not None:
            self._need("POOL", *r.w)
    if t.w is not None and not (part and t.w[0] == key):
        self._need("POOL", *t.w)
    for kk, v in t.r.items():
        self._need("POOL", kk, v)
    off = bass.IndirectOffsetOnAxis(ap=idx_ap, axis=0)
    if gather and bound is not None:
        ins = self.nc.gpsimd.indirect_dma_start(out=out_ap, out_offset=None, in_=in_ap, in_offset=off, bounds_check=bound, oob_is_err=False)
    elif gather:
        ins = self.nc.gpsimd.indirect_dma_start(out=out_ap, out_offset=None, in_=in_ap, in_offset=off)
    else:
        ins = self.nc.gpsimd.indirect_dma_start(out=out_ap, out_offset=off, in_=in_ap, in_offset=None)
    t.dcnt += 16
    ins.then_inc(t.dsem, 16)
    for r in list(reads) + [idx_t]:
        r.r[key] = max(r.r.get(key, 0), t.dcnt)
    t.w = (key, t.dcnt)
    if not part:
        t.r = {}
    self.ninst += 1
    return ins


K.idma = idma


def build_tok2(NT, KA, mode):
    nc = bass.Bass("TRN2", target_bir_lowering=False)

    def din(name, shape, dt=F32):
        return nc.dram_tensor(name, list(shape), dt, kind="ExternalInput").ap()

    def dout(name, shape, dt=F32):
        return nc.dram_tensor(name, list(shape), dt, kind="ExternalOutput").ap()

    NBLK = NBLK_OF(NT)
    NROWS = NBLK * MB
    NTB = NT // 128
    NTILE = NT // 512
    hT = din("hT", [1024, NT])
    aT = din("aT", [KA, NT], BF16)
    w_a = din("w_a", [KA, 1024])
    vecs = din("vecs", [128, 32])
    wr = din("wr", [1024, 72])
    br = din("br", [1, 72])
    wg = din("wg", [64, 1024, 512])
    wu = din("wu", [64, 1024, 512])
    wd = din("wd", [64, 512, 1024])
    plg = din("plg", [1024, 1024])
    plp = din("plp", [256, 1024])
    pT = din("pT", [256, NT])
    NCST = 256 + 64 + NBLK + 8
    mcst = din("mcst", [128, NCST])
    if mode == "A":
        wqkv = din("wqkv", [1024, 3072])
        hT_o = dout("hT_o", [1024, NT])
        qT_o = dout("qT_o", [1024, NT], BF16)
        kT_o = dout("kT_o", [1024, NT], BF16)
        v_o = dout("v_o", [NT, 1024], BF16)
    else:
        out_o = dout("out_o", [1024, NT])
    H1 = nc.dram_tensor("H1s", [1024, NT], F32, kind="Internal").ap()
    Xs = nc.dram_tensor("Xs", [NROWS, 1024], BF16, kind="Internal").ap()
    Ys = nc.dram_tensor("Ys", [NROWS, 1024], F32, kind="Internal").ap()
    wg_r = wg.rearrange("e (p h r) n -> (e p h) (r n)", p=128, h=2, r=4)
    wu_r = wu.rearrange("e (p h r) n -> (e p h) (r n)", p=128, h=2, r=4)
    wd_r = wd.rearrange("e (p h r) n -> (e p h) (r n)", p=128, h=2, r=2)
    KAC = KA // 128

    with ExitStack() as st:
        k = K(nc, st)
        HTL = [k.sbt([128, 8 * 512], F32, "htl%d" % i) for i in range(1)] * 2
        UTL = [k.sbt([128, 8 * 512], BF16, "utl%d" % i) for i in range(1)] * 2
        WB = [k.sbt([128, 12288], BF16, "wb%d" % i) for i in range(2)]
        RA = k.sb([128, 8192], BF16, "regA")
        RAf = RA[:].bitcast(F32)
        ABS = [T(RA[:, 0:KAC * 512])]
        BIG = T(RAf[:, 0:2048])
        IGF = T(RAf[:, 2048:3072])
        XB = [T(RA[:, i * 1024:(i + 1) * 1024]) for i in range(2)]
        XTB = [T(RA[:, 2048 + i * 1024:2048 + (i + 1) * 1024]) for i in range(2)]
        YB = [T(RAf[:, 2048 + i * 1024:2048 + (i + 1) * 1024]) for i in range(2)]
        NRX = max(NTB * 1024, 32768)
        RX = k.sb([128, NRX], BF16, "regX")
        RXf = RX[:].bitcast(F32)
        XROWS = T(RX[:, 0:NTB * 1024])
        XR3 = v3(XROWS.ap, NTB)
        WQ = [T(RX[:, i * 12288:(i + 1) * 12288]) for i in range(2)]
        WBX = [T(RX[:, i * 12288:(i + 1) * 12288]) for i in range(2)]
        Y0 = [T(RXf[:, 12288 + i * 1024:12288 + (i + 1) * 1024]) for i in range(2)]
        Y1 = [T(RXf[:, 14336 + i * 1024:14336 + (i + 1) * 1024]) for i in range(2)]
        PBt = k.sbt([128, 2 * 512], BF16, "pbt")
        LNV = k.sbt([128, 512], F32, "lnv")
        RSTD = k.sbt([128, 512], F32, "rstd")
        vec = k.sbt([128, 32], F32, "vec_sb")
        cf = k.sbt([128, NCST], F32, "cf")
        ident = cf.ap[:, 0:128]
        identb = k.sbt([128, 128], BF16, "identb")
        SLb = k.sbt([128, 128], BF16, "SLb")
        ones_bf = k.sbt([128, 128], BF16, "ones_bf")
        wr_sb = k.sbt([128, 8 * 72], F32, "wr_sb")
        br_bc = k.sbt([128, 72], F32, "brbc")
        STG = [k.sbt([128, 512], BF16, "stg%d" % i) for i in range(2)]
        STF = [k.sbt([128, 512], F32, "stf%d" % i) for i in range(2)]
        r_lg = k.sbt([128, 72], F32, "r_lg")
        r_a = k.sbt([128, 8], F32, "r_a")
        r_goh = k.sbt([128, 8], F32, "r_goh")
        r_t64 = k.sbt([128, 64], F32, "r_t64")
        r_es = k.sbt([128, 8], F32, "r_es")
        r_es2 = k.sbt([128, 8], F32, "r_es2")
        r_oh1 = k.sbt([128, 8], F32, "r_oh1")
        r_oh2 = k.sbt([128, 8], F32, "r_oh2")
        r_s = [k.sbt([128, 1], F32, "r_s%d" % i) for i in range(12)]
        OHC = k.sbt([128, 128], BF16, "ohc")
        OHS = k.sbt([128, NTB * 128], BF16, "ohs")
        OHS3 = v3(OHS.ap, NTB)
        RUN = k.sbt([128, 128], F32, "run")
        PRS = k.sbt([128, 128], F32, "prs")
        JNK = k.sbt([128, 64], F32, "jnk")
        RK0 = k.sbt([128, NTB], F32, "rk0")
        RK1 = k.sbt([128, NTB], F32, "rk1")
        GA = k.sbt([128, NTB], F32, "ga")
        GB = k.sbt([128, NTB], F32, "gb")
        DI0 = k.sbt([128, NTB], I32, "di0")
        DI1 = k.sbt([128, NTB], I32, "di1")
        CNT = k.sbt([128, 64], F32, "cnt")
        NB_ = k.sbt([128, 64], F32, "nb_")
        PE_ = [k.sbt([128, 64], F32, "pe%d" % i) for i in range(2)]
        BASE1 = k.sbt([128, 64], F32, "base1")
        BASE2 = k.sbt([128, 64], F32, "base2")
        DF = k.sbt([128, NTB], F32, "df")
        EJ = k.sbt([128, NBLK], F32, "ej")
        SAME = k.sbt([128, NBLK], F32, "same")
        IGI = k.sbt([128, NBLK * 8], I32, "igi")
        IDI = k.sbt([128, NBLK * 4], I32, "idi")
        SS = [k.sbt([128, 512], BF16, "ss%d" % i) for i in range(2)]
        HH = [k.sbt([128, 512], BF16, "hh%d" % i) for i in range(2)]
        BK = [T(k.ps([128, 512], F32, "bk%d" % i)[:]) for i in range(8)]
        PG, PU, PD, PBK = BK[0:2], BK[2:4], BK[4:6], BK[6:8]
        H1_t, Xs_t, Ys_t = T(H1), T(Xs), T(Ys)
        if mode == "A":
            qT_t, kT_t, v_t, ho_t = T(qT_o), T(kT_o), T(v_o), T(hT_o)
        else:
            out_t = T(out_o)

        k.dma("SP", vec.ap, vecs, writes=[vec])
        k.dma("SP", cf.ap, mcst, writes=[cf])
        k.dma("SP", v3(wr_sb.ap, 8), wr.rearrange("(a p) n -> p a n", p=128), writes=[wr_sb])
        k.dma("SP", br_bc.ap, br.partition_broadcast(128).rearrange("p o n -> p (o n)"), writes=[br_bc])
        k.op("DVE", lambda e: e.memset(ones_bf.ap, 1.0), writes=[ones_bf])
        k.op("DVE", lambda e: e.memset(RUN.ap, 0.0), writes=[RUN])
        k.op("DVE", lambda e: e.tensor_copy(out=identb.ap, in_=ident), reads=[cf], writes=[identb])
        k.op("DVE", lambda e: e.tensor_copy(out=SLb.ap, in_=cf.ap[:, 128:256]), reads=[cf], writes=[SLb])
        THR = cf.ap[:, 256:320]
        JB = cf.ap[:, 320:320 + NBLK]
        KP = cf.ap[:, 320 + NBLK:328 + NBLK]
        for kc in range(8):
            k.op("DVE", lambda e: e.tensor_scalar(out=wr_sb.ap[:, kc * 72:(kc + 1) * 72], in0=wr_sb.ap[:, kc * 72:(kc + 1) * 72],
                                                  scalar1=vec.ap[:, kc:kc + 1], scalar2=None, op0=ALU.mult), reads=[vec, wr_sb], writes=[wr_sb])

        def rmsnorm(src_t, src_ap, gcol, dst_t, dst_ap, bank):
            k.op("ACT", lambda e: e.activation(out=dst_ap, in_=src_ap, func=AF.Square), reads=[src_t], writes=[dst_t])
            for kc in range(8):
                k.op("PE", lambda e: e.matmul(bank.ap, ones_bf.ap, dst_ap[:, kc, :], start=(kc == 0), stop=(kc == 7)),
                     reads=[dst_t, ones_bf], writes=[bank], pe_acc=(kc > 0))
            k.op("ACT", lambda e: e.activation(out=LNV.ap, in_=bank.ap, func=AF.Ln, scale=1.0 / 1024, bias=EPS), reads=[bank], writes=[LNV])
            k.op("ACT", lambda e: e.activation(out=RSTD.ap, in_=LNV.ap, func=AF.Exp, scale=-0.5), reads=[LNV], writes=[RSTD])
            for kc in range(8):
                k.op("DVE", lambda e: e.scalar_tensor_tensor(out=dst_ap[:, kc, :], in0=src_ap[:, kc, :], scalar=vec.ap[:, gcol + kc:gcol + kc + 1],
                                                             in1=RSTD.ap, op0=ALU.mult, op1=ALU.mult), reads=[src_t, RSTD, vec], writes=[dst_t])

        for kp in range(KAC // 8):
            k.dma("POOL", v3(WB[kp].ap[:, 0:8192], 8), w_a[kp * 1024:(kp + 1) * 1024, :].rearrange("(a p) n -> p a n", p=128), writes=[WB[kp]])
        for t in range(NTILE):
            cs = slice(t * 512, (t + 1) * 512)
            ht, ut, AB = HTL[t % 2], UTL[t % 2], ABS[0]
            h3, u3 = v3(ht.ap, 8), v3(ut.ap, 8)
            k.dma("SP", h3, hT[:, cs].rearrange("(a p) n -> p a n", p=128), writes=[ht])
            k.dma("SP", v3(AB.ap, KAC), aT[:, cs].rearrange("(a p) n -> p a n", p=128), writes=[AB])
            for d in range(8):
                bank = PD[d % 2]
                for kc in range(KAC):
                    wbt = WB[kc // 8]
                    k.op("PE", lambda e: e.matmul(bank.ap, v3(wbt.ap[:, 0:8192], 8)[:, kc % 8, d * 128:(d + 1) * 128], v3(AB.ap, KAC)[:, kc, :],
                                                  start=(kc == 0), stop=(kc == KAC - 1)), reads=[wbt, AB], writes=[bank], pe_acc=(kc > 0))
                k.op("DVE", lambda e: e.tensor_tensor(out=h3[:, d, :], in0=h3[:, d, :], in1=bank.ap, op=ALU.add), reads=[bank, ht], writes=[ht])
            k.dma("SP", H1[:, cs].rearrange("(a p) n -> p a n", p=128), h3, reads=[ht], writes=[H1_t], part=True)
            rmsnorm(ht, h3, 0, ut, u3, PBK[0])
            for b in range(4):
                i = t * 4 + b
                bs = slice(b * 128, (b + 1) * 128)
                plg_b, prs = PG[b % 2], PU[b % 2]
                for kc in range(8):
                    k.op("PE", lambda e: e.matmul(plg_b.ap[:, 0:72], h3[:, kc, bs], wr_sb.ap[:, kc * 72:(kc + 1) * 72], start=(kc == 0), stop=(kc == 7)),
                         reads=[ht, wr_sb], writes=[plg_b], pe_acc=(kc > 0))
                k.op("PE", lambda e: e.matmul(prs.ap[:, 0:2], RSTD.ap[:, bs], ident[:, 0:2], start=True, stop=True), reads=[RSTD, cf], writes=[prs])
                rt, gmax, ngmax, gsum, gw, m1, m2, dd, ex, w1, w2 = r_s[0:11]
                k.op("DVE", lambda e: e.tensor_copy(out=rt.ap, in_=prs.ap[:, 0:1]), reads=[prs], writes=[rt])
                k.op("DVE", lambda e: e.scalar_tensor_tensor(out=r_lg.ap, in0=plg_b.ap[:, 0:72], scalar=rt.ap, in1=br_bc.ap, op0=ALU.mult, op1=ALU.add),
                     reads=[plg_b, rt, br_bc], writes=[r_lg])
                gl = r_lg.ap[:, 0:8]
                el = r_lg.ap[:, 8:72]
                k.op("DVE", lambda e: e.tensor_reduce(out=gmax.ap, in_=gl, axis=AX.X, op=ALU.max), reads=[r_lg], writes=[gmax])
                k.op("DVE", lambda e: e.tensor_scalar(out=r_goh.ap, in0=gl, scalar1=gmax.ap, scalar2=None, op0=ALU.is_equal), reads=[r_lg, gmax], writes=[r_goh])
                k.op("DVE", lambda e: e.tensor_scalar(out=ngmax.ap, in0=gmax.ap, scalar1=-1.0, scalar2=None, op0=ALU.mult), reads=[gmax], writes=[ngmax])
                k.op("ACT", lambda e: e.activation(out=r_a.ap, in_=gl, func=AF.Exp, bias=ngmax.ap, scale=1.0, accum_out=gsum.ap), reads=[r_lg, ngmax], writes=[r_a, gsum])
                k.op("DVE", lambda e: e.reciprocal(out=gw.ap, in_=gsum.ap), reads=[gsum], writes=[gw])
                k.op("DVE", lambda e: e.tensor_tensor(out=v3(r_t64.ap, 8), in0=v3(el, 8), in1=r_goh.ap.unsqueeze(2).to_broadcast([128, 8, 8]), op=ALU.mult),
                     reads=[r_lg, r_goh], writes=[r_t64])
                k.op("DVE", lambda e: e.tensor_reduce(out=r_es.ap, in_=v3(r_t64.ap, 8).rearrange("p g j -> p j g"), axis=AX.X, op=ALU.add), reads=[r_t64], writes=[r_es])
                k.op("DVE", lambda e: e.tensor_reduce(out=m1.ap, in_=r_es.ap, axis=AX.X, op=ALU.max), reads=[r_es], writes=[m1])
                k.op("DVE", lambda e: e.tensor_scalar(out=r_oh1.ap, in0=r_es.ap, scalar1=m1.ap, scalar2=None, op0=ALU.is_equal), reads=[r_es, m1], writes=[r_oh1])
                k.op("DVE", lambda e: e.scalar_tensor_tensor(out=r_es2.ap, in0=r_oh1.ap, scalar=-1e30, in1=r_es.ap, op0=ALU.mult, op1=ALU.add), reads=[r_oh1, r_es], writes=[r_es2])
                k.op("DVE", lambda e: e.tensor_reduce(out=m2.ap, in_=r_es2.ap, axis=AX.X, op=ALU.max), reads=[r_es2], writes=[m2])
                k.op("DVE", lambda e: e.tensor_scalar(out=r_oh2.ap, in0=r_es2.ap, scalar1=m2.ap, scalar2=None, op0=ALU.is_equal), reads=[r_es2, m2], writes=[r_oh2])
                k.op("DVE", lambda e: e.tensor_tensor(out=dd.ap, in0=m2.ap, in1=m1.ap, op=ALU.subtract), reads=[m1, m2], writes=[dd])
                k.op("ACT", lambda e: e.activation(out=ex.ap, in_=dd.ap, func=AF.Exp), reads=[dd], writes=[ex])
                k.op("DVE", lambda e: e.tensor_scalar(out=w1.ap, in0=ex.ap, scalar1=1.0, scalar2=None, op0=ALU.add), reads=[ex], writes=[w1])
                k.op("DVE", lambda e: e.reciprocal(out=w1.ap, in_=w1.ap), reads=[w1], writes=[w1])
                k.op("DVE", lambda e: e.tensor_tensor(out=w2.ap, in0=ex.ap, in1=w1.ap, op=ALU.mult), reads=[ex, w1], writes=[w2])
                k.op("DVE", lambda e: e.tensor_tensor(out=GA.ap[:, i:i + 1], in0=w1.ap, in1=gw.ap, op=ALU.mult), reads=[w1, gw], writes=[GA])
                k.op("DVE", lambda e: e.tensor_tensor(out=GB.ap[:, i:i + 1], in0=w2.ap, in1=gw.ap, op=ALU.mult), reads=[w2, gw], writes=[GB])
                k.op("DVE", lambda e: e.tensor_tensor(out=v3(OHC.ap[:, 0:64], 8), in0=r_goh.ap.unsqueeze(2).to_broadcast([128, 8, 8]),
                                                      in1=r_oh1.ap.unsqueeze(1).to_broadcast([128, 8, 8]), op=ALU.mult), reads=[r_goh, r_oh1], writes=[OHC])
                k.op("DVE", lambda e: e.tensor_tensor(out=v3(OHC.ap[:, 64:128], 8), in0=r_goh.ap.unsqueeze(2).to_broadcast([128, 8, 8]),
                                                      in1=r_oh2.ap.unsqueeze(1).to_broadcast([128, 8, 8]), op=ALU.mult), reads=[r_goh, r_oh2, OHC], writes=[OHC])
                k.op("DVE", lambda e: e.tensor_copy(out=OHS3[:, i, :], in_=OHC.ap), reads=[OHC], writes=[OHS])
                ppr, pcs = PD[0], PD[1]
                k.op("PE", lambda e: e.matmul(ppr.ap[:, 0:128], SLb.ap, OHC.ap, start=True, stop=True), reads=[SLb, OHC], writes=[ppr])
                k.op("PE", lambda e: e.matmul(pcs.ap[:, 0:128], ones_bf.ap, OHC.ap, start=True, stop=True), reads=[ones_bf, OHC], writes=[pcs])
                k.op("DVE", lambda e: e.tensor_tensor(out=PRS.ap, in0=ppr.ap[:, 0:128], in1=RUN.ap, op=ALU.add), reads=[ppr, RUN], writes=[PRS])
                k.op("DVE", lambda e: e.tensor_tensor(out=PRS.ap, in0=PRS.ap, in1=OHC.ap, op=ALU.mult), reads=[PRS, OHC], writes=[PRS])
                k.op("DVE", lambda e: e.tensor_reduce(out=RK0.ap[:, i:i + 1], in_=PRS.ap[:, 0:64], axis=AX.X, op=ALU.add), reads=[PRS], writes=[RK0])
                k.op("DVE", lambda e: e.tensor_reduce(out=RK1.ap[:, i:i + 1], in_=PRS.ap[:, 64:128], axis=AX.X, op=ALU.add), reads=[PRS], writes=[RK1])
                k.op("DVE", lambda e: e.tensor_tensor(out=RUN.ap, in0=RUN.ap, in1=pcs.ap[:, 0:128], op=ALU.add), reads=[pcs, RUN], writes=[RUN])
                trb = PBK[1].ap.bitcast(BF16)
                for kc in range(8):
                    k.op("PE", lambda e: e.transpose(trb[:, kc * 128:(kc + 1) * 128], u3[:, kc, bs], identb.ap), reads=[ut, identb], writes=[PBK[1]])
                k.op("ACT", lambda e: e.activation(out=XR3[:, i, :], in_=trb, func=AF.Copy), reads=[PBK[1]], writes=[XROWS])

        k.alias([BIG, IGF], ABS)
        cnt1, cnt2 = RUN.ap[:, 0:64], RUN.ap[:, 64:128]
        k.op("DVE", lambda e: e.tensor_tensor(out=CNT.ap, in0=cnt1, in1=cnt2, op=ALU.add), reads=[RUN], writes=[CNT])
        big_cm = BIG.ap[:, 0:2048].rearrange("p (a m) -> p a m", a=64)
        for mh in range(2):
            k.op("DVE", lambda e: e.tensor_tensor(out=big_cm, in0=CNT.ap.unsqueeze(2).to_broadcast([128, 64, 32]),
                                                  in1=THR[:, mh * 32:(mh + 1) * 32].unsqueeze(1).to_broadcast([128, 64, 32]), op=ALU.is_gt), reads=[CNT, cf], writes=[BIG])
            dstn = NB_ if mh == 0 else BASE1
            k.op("DVE", lambda e: e.tensor_reduce(out=dstn.ap, in_=big_cm, axis=AX.X, op=ALU.add), reads=[BIG], writes=[dstn])
        k.op("DVE", lambda e: e.tensor_tensor(out=NB_.ap, in0=NB_.ap, in1=BASE1.ap, op=ALU.add), reads=[NB_, BASE1], writes=[NB_])
        k.op("DVE", lambda e: e.tensor_scalar(out=NB_.ap, in0=NB_.ap, scalar1=float(MB), scalar2=None, op0=ALU.mult), reads=[NB_], writes=[NB_])
        k.op("DVE", lambda e: e.tensor_copy(out=PE_[0].ap, in_=NB_.ap), reads=[NB_], writes=[PE_[0]])
        cur = 0
        for sft in (1, 2, 4, 8, 16, 32):
            a_, b_ = PE_[cur], PE_[1 - cur]
            k.op("DVE", lambda e: e.tensor_copy(out=b_.ap[:, 0:sft], in_=a_.ap[:, 0:sft]), reads=[a_], writes=[b_])
            k.op("DVE", lambda e: e.tensor_tensor(out=b_.ap[:, sft:64], in0=a_.ap[:, sft:64], in1=a_.ap[:, 0:64 - sft], op=ALU.add), reads=[a_, b_], writes=[b_])
            cur = 1 - cur
        PEND = PE_[cur]
        k.op("DVE", lambda e: e.tensor_tensor(out=BASE1.ap, in0=PEND.ap, in1=NB_.ap, op=ALU.subtract), reads=[PEND, NB_], writes=[BASE1])
        k.op("DVE", lambda e: e.tensor_tensor(out=BASE2.ap, in0=BASE1.ap, in1=cnt1, op=ALU.add), reads=[BASE1, RUN], writes=[BASE2])
        TBC = 2048 // 64
        for (base, rk, di, lo) in ((BASE1, RK0, DI0, 0), (BASE2, RK1, DI1, 64)):
            for c0 in range(0, NTB, TBC):
                nb = min(TBC, NTB - c0)
                big_t = BIG.ap[:, 0:nb * 64].rearrange("p (a m) -> p a m", a=nb)
                k.op("DVE", lambda e: e.tensor_tensor(out=big_t, in0=OHS3[:, c0:c0 + nb, lo:lo + 64], in1=base.ap.unsqueeze(1).to_broadcast([128, nb, 64]), op=ALU.mult),
                     reads=[OHS, base], writes=[BIG])
                k.op("DVE", lambda e: e.tensor_reduce(out=DF.ap[:, c0:c0 + nb], in_=big_t, axis=AX.X, op=ALU.add), reads=[BIG], writes=[DF])
            k.op("DVE", lambda e: e.tensor_tensor(out=DF.ap, in0=DF.ap, in1=rk.ap, op=ALU.add), reads=[DF, rk], writes=[DF])
            k.op("DVE", lambda e: e.tensor_copy(out=di.ap, in_=DF.ap), reads=[DF], writes=[di])
        for c0 in range(0, NBLK, 32):
            nb = min(32, NBLK - c0)
            big_j = BIG.ap[:, 0:nb * 64].rearrange("p (a m) -> p a m", a=nb)
            k.op("DVE", lambda e: e.tensor_tensor(out=big_j, in0=PEND.ap.unsqueeze(1).to_broadcast([128, nb, 64]), in1=JB[:, c0:c0 + nb].unsqueeze(2).to_broadcast([128, nb, 64]), op=ALU.is_le),
                 reads=[PEND, cf], writes=[BIG])
            k.op("DVE", lambda e: e.tensor_reduce(out=EJ.ap[:, c0:c0 + nb], in_=big_j, axis=AX.X, op=ALU.add), reads=[BIG], writes=[EJ])
        k.op("DVE", lambda e: e.tensor_scalar(out=EJ.ap, in0=EJ.ap, scalar1=63.0, scalar2=None, op0=ALU.min), reads=[EJ], writes=[EJ])
        NST = 4
        HB = NBLK // NST
        k.op("DVE", lambda e: e.memset(SAME.ap, 0.0), writes=[SAME])
        for s0 in range(0, NBLK, HB):
            k.op("DVE", lambda e: e.tensor_tensor(out=SAME.ap[:, s0 + 1:s0 + HB], in0=EJ.ap[:, s0 + 1:s0 + HB], in1=EJ.ap[:, s0:s0 + HB - 1], op=ALU.is_equal),
                 reads=[EJ, SAME], writes=[SAME])
        k.op("DVE", lambda e: e.tensor_scalar(out=SAME.ap, in0=SAME.ap, scalar1=float(1 << 22), scalar2=None, op0=ALU.mult), reads=[SAME], writes=[SAME])
        igf3 = v3(IGF.ap[:, 0:NBLK * 2], NBLK)
        k.op("DVE", lambda e: e.scalar_tensor_tensor(out=BIG.ap[:, 0:NBLK], in0=EJ.ap, scalar=256.0, in1=SAME.ap, op0=ALU.mult, op1=ALU.add), reads=[EJ, SAME], writes=[BIG])
        k.op("DVE", lambda e: e.tensor_tensor(out=igf3, in0=BIG.ap[:, 0:NBLK].unsqueeze(2).to_broadcast([128, NBLK, 2]), in1=KP[:, 0:2].unsqueeze(1).to_broadcast([128, NBLK, 2]), op=ALU.add),
             reads=[BIG, cf], writes=[IGF])
        k.op("DVE", lambda e: e.tensor_copy(out=IGI.ap[:, 0:NBLK * 2], in_=IGF.ap[:, 0:NBLK * 2]), reads=[IGF], writes=[IGI])

        for i in range(NTB):
            k.idma(Xs, XR3[:, i, :], DI0, DI0.ap[:, i:i + 1], gather=False, reads=[XROWS], writes=[Xs_t], part=True)
            k.idma(Xs, XR3[:, i, :], DI1, DI1.ap[:, i:i + 1], gather=False, reads=[XROWS], writes=[Xs_t], part=True)

        k.alias(XB + XTB + YB, [BIG, IGF] + ABS)
        k.alias(WBX, [XROWS])
        WB4 = WB + WBX

        bnd = nc.gpsimd.to_reg(64 * 256 - 1)
        igi2 = v3(IGI.ap[:, 0:NBLK * 2], NBLK)

        def load_blk(j, n_):
            wb = WB4[n_ % NST]
            first = True
            for (src, base) in ((wg_r, 0), (wu_r, 4096), (wd_r, 8192)):
                for h in range(2):
                    k.idma(wb.ap[:, base + h * 2048:base + (h + 1) * 2048], src, IGI, igi2[:, j, h:h + 1], gather=True, writes=[wb], part=(not first), bound=bnd)
                    first = False

        def load_xb(j, n_):
            k.dma("SP", XB[n_ % 2].ap, Xs[j * MB:(j + 1) * MB, :], reads=[Xs_t], writes=[XB[n_ % 2]])

        def front(j, n_):
            wb, xb, xt = WB4[n_ % NST], XB[n_ % 2], XTB[n_ % 2]
            trb0, trb1 = PBK[0].ap.bitcast(BF16), PBK[1].ap.bitcast(BF16)
            for kc in range(8):
                dst = (trb0 if kc < 4 else trb1)[:, (kc % 4) * 128:(kc % 4 + 1) * 128]
                k.op("PE", lambda e: e.transpose(dst, xb.ap.rearrange("p (m c) -> p c m", c=8)[:, kc, :], identb.ap), reads=[xb, identb], writes=[PBK[0] if kc < 4 else PBK[1]])
            k.op("ACT", lambda e: e.activation(out=xt.ap[:, 0:512], in_=trb0[:, 0:512], func=AF.Copy), reads=[PBK[0]], writes=[xt])
            k.op("ACT", lambda e: e.activation(out=xt.ap[:, 512:1024], in_=trb1[:, 0:512], func=AF.Copy), reads=[PBK[1], xt], writes=[xt])
            Wg3, Wu3 = v3(wb.ap[:, 0:4096], 8), v3(wb.ap[:, 4096:8192], 8)
            pg, pu = PG[n_ % 2], PU[n_ % 2]
            for f in range(4):
                for kc in range(8):
                    k.op("PE", lambda e: e.matmul(pg.ap[:, f * 128:(f + 1) * 128], Wg3[:, kc, :].rearrange("p (m c) -> p c m", c=4)[:, f, :], xt.ap[:, kc * 128:(kc + 1) * 128],
                                                  start=(kc == 0), stop=(kc == 7)), reads=[wb, xt], writes=[pg], pe_acc=(kc > 0 or f > 0))
            for f in range(4):
                for kc in range(8):
                    k.op("PE", lambda e: e.matmul(pu.ap[:, f * 128:(f + 1) * 128], Wu3[:, kc, :].rearrange("p (m c) -> p c m", c=4)[:, f, :], xt.ap[:, kc * 128:(kc + 1) * 128],
                                                  start=(kc == 0), stop=(kc == 7)), reads=[wb, xt], writes=[pu], pe_acc=(kc > 0 or f > 0))
            k.op("ACT", lambda e: e.activation(out=SS[n_ % 2].ap, in_=pg.ap, func=AF.Silu), reads=[pg], writes=[SS[n_ % 2]])
            k.op("DVE", lambda e: e.tensor_tensor(out=HH[n_ % 2].ap, in0=SS[n_ % 2].ap, in1=pu.ap, op=ALU.mult), reads=[SS[n_ % 2], pu], writes=[HH[n_ % 2]])

        def back(j, n_):
            wb, hh, yb = WB4[n_ % NST], HH[n_ % 2], YB[n_ % 2]
            Wd3 = v3(wb.ap[:, 8192:12288], 4)
            for dh in range(2):
                pd = PD[dh]
                for f in range(4):
                    k.op("PE", lambda e: e.matmul(pd.ap, hh.ap[:, f * 128:(f + 1) * 128], Wd3[:, f, dh * 512:(dh + 1) * 512], start=(f == 0), stop=(f == 3)),
                         reads=[wb, hh], writes=[pd], pe_acc=(f > 0))
                if dh == 0:
                    k.op("ACT", lambda e: e.activation(out=yb.ap[:, 0:512], in_=pd.ap, func=AF.Copy), reads=[pd], writes=[yb])
                else:
                    k.op("DVE", lambda e: e.tensor_copy(out=yb.ap[:, 512:1024], in_=pd.ap), reads=[pd, yb], writes=[yb])
            k.dma("SP", Ys[j * MB:(j + 1) * MB, :], yb.ap, reads=[yb], writes=[Ys_t], part=True)

        order = []
        for q in range(HB):
            order += [s_ * HB + q for s_ in range(NST)]
        for n_ in range(NST):
            load_blk(order[n_], n_)
        for n_ in range(2):
            load_xb(order[n_], n_)
        for n_, j in enumerate(order):
            front(j, n_)
            back(j, n_)
            if n_ + NST < NBLK:
                load_blk(order[n_ + NST], n_ + NST)
            if n_ + 2 < NBLK:
                load_xb(order[n_ + 2], n_ + 2)

        k.dma("POOL", v3(WB[0].ap[:, 0:8192], 8), plg.rearrange("(a p) n -> p a n", p=128), writes=[WB[0]])
        k.dma("POOL", v3(WB[1].ap[:, 0:2048], 2), plp.rearrange("(a p) n -> p a n", p=128), writes=[WB[1]])
        k.alias(WQ + Y0 + Y1, [XROWS] + WBX)
        if mode == "A":
            k.dma("POOL", v3(WQ[0].ap, 8), wqkv[:, 0:1536].rearrange("(a p) n -> p a n", p=128), writes=[WQ[0]])
            k.dma("POOL", v3(WQ[1].ap, 8), wqkv[:, 1536:3072].rearrange("(a p) n -> p a n", p=128), writes=[WQ[1]])
        for t in range(NTILE):
            cs = slice(t * 512, (t + 1) * 512)
            ht, ut = HTL[t % 2], UTL[t % 2]
            h3, u3 = v3(ht.ap, 8), v3(ut.ap, 8)
            k.dma("SP", h3, H1[:, cs].rearrange("(a p) n -> p a n", p=128), reads=[H1_t], writes=[ht])
            k.dma("POOL", v3(PBt.ap, 2), pT[:, cs].rearrange("(a p) n -> p a n", p=128), writes=[PBt])
            for b in range(4):
                i = t * 4 + b
                bs = slice(b * 128, (b + 1) * 128)
                y0, y1 = Y0[i % 2], Y1[i % 2]
                k.idma(y0.ap, Ys, DI0, DI0.ap[:, i:i + 1], gather=True, reads=[Ys_t], writes=[y0])
                k.idma(y1.ap, Ys, DI1, DI1.ap[:, i:i + 1], gather=True, reads=[Ys_t], writes=[y1])
                k.op("DVE", lambda e: e.tensor_scalar(out=y0.ap, in0=y0.ap, scalar1=GA.ap[:, i:i + 1], scalar2=None, op0=ALU.mult), reads=[y0, GA], writes=[y0])
                k.op("DVE", lambda e: e.scalar_tensor_tensor(out=y0.ap, in0=y1.ap, scalar=GB.ap[:, i:i + 1], in1=y0.ap, op0=ALU.mult, op1=ALU.add),
                     reads=[y1, GB, y0], writes=[y0])
                for half in range(2):
                    bank = PD[half]
                    for dq in range(4):
                        d = half * 4 + dq
                        k.op("PE", lambda e: e.transpose(bank.ap[:, dq * 128:(dq + 1) * 128], y0.ap[:, d * 128:(d + 1) * 128], ident), reads=[y0, cf], writes=[bank])
                    k.op("DVE", lambda e: e.tensor_tensor(out=h3[:, half * 4:(half + 1) * 4, bs], in0=h3[:, half * 4:(half + 1) * 4, bs],
                                                          in1=bank.ap.rearrange("p (a n) -> p a n", a=4), op=ALU.add), reads=[bank, ht], writes=[ht])
            rmsnorm(ht, h3, 8, ut, u3, PBK[0])
            for d in range(8):
                pg, pu = PG[d % 2], PU[d % 2]
                for kc in range(8):
                    k.op("PE", lambda e: e.matmul(pg.ap, v3(WB[0].ap[:, 0:8192], 8)[:, kc, d * 128:(d + 1) * 128], u3[:, kc, :], start=(kc == 0), stop=(kc == 7)),
                         reads=[WB[0], ut], writes=[pg], pe_acc=(kc > 0))
                for kc in range(2):
                    k.op("PE", lambda e: e.matmul(pu.ap, v3(WB[1].ap[:, 0:2048], 2)[:, kc, d * 128:(d + 1) * 128], v3(PBt.ap, 2)[:, kc, :], start=(kc == 0), stop=(kc == 1)),
                         reads=[WB[1], PBt], writes=[pu], pe_acc=(kc > 0))
                sf = STF[d % 2]
                k.op("ACT", lambda e: e.activation(out=sf.ap, in_=pg.ap, func=AF.Sigmoid, bias=vec.ap[:, 16 + d:17 + d], scale=1.0), reads=[pg, vec], writes=[sf])
                k.op("DVE", lambda e: e.tensor_tensor(out=sf.ap, in0=sf.ap, in1=pu.ap, op=ALU.mult), reads=[sf, pu], writes=[sf])
                k.op("DVE", lambda e: e.tensor_tensor(out=h3[:, d, :], in0=h3[:, d, :], in1=sf.ap, op=ALU.add), reads=[sf, ht], writes=[ht])
            if mode == "A":
                k.dma("SP", hT_o[:, cs].rearrange("(a p) n -> p a n", p=128), h3, reads=[ht], writes=[ho_t], part=True)
                rmsnorm(ht, h3, 24, ut, u3, PBK[0])
                for n in range(16):
                    col = n * 128
                    wbt = WQ[0] if col < 1536 else WQ[1]
                    lc = col if col < 1536 else col - 1536
                    bank = PG[n % 2]
                    for kc in range(8):
                        k.op("PE", lambda e: e.matmul(bank.ap, v3(wbt.ap, 8)[:, kc, lc:lc + 128], u3[:, kc, :], start=(kc == 0), stop=(kc == 7)),
                             reads=[wbt, ut], writes=[bank], pe_acc=(kc > 0))
                    sg = STG[n % 2]
                    sc_ = (1.0 / np.sqrt(128.0)) if n < 8 else 1.0
                    k.op("ACT", lambda e: e.activation(out=sg.ap, in_=bank.ap, func=AF.Identity, scale=float(sc_)), reads=[bank], writes=[sg])
                    if n < 8:
                        k.dma("SP", qT_o[n * 128:(n + 1) * 128, cs], sg.ap, reads=[sg], writes=[qT_t], part=True)
                    else:
                        k.dma("SP", kT_o[(n - 8) * 128:(n - 7) * 128, cs], sg.ap, reads=[sg], writes=[kT_t], part=True)
                for b in range(4):
                    for hh_ in range(2):
                        bank = PU[hh_]
                        for kc in range(8):
                            k.op("PE", lambda e: e.matmul(bank.ap, u3[:, kc, b * 128:(b + 1) * 128], v3(WQ[1].ap, 8)[:, kc, 512 + hh_ * 512:1024 + hh_ * 512],
                                                          start=(kc == 0), stop=(kc == 7)), reads=[WQ[1], ut], writes=[bank], pe_acc=(kc > 0))
                        sg = STG[hh_]
                        k.op("ACT", lambda e: e.activation(out=sg.ap, in_=bank.ap, func=AF.Copy), reads=[bank], writes=[sg])
                        r0 = t * 512 + b * 128
                        k.dma("SP", v_o[r0:r0 + 128, hh_ * 512:(hh_ + 1) * 512], sg.ap, reads=[sg], writes=[v_t], part=True)
            else:
                k.op("ACT", lambda e: e.activation(out=u3, in_=h3, func=AF.Square), reads=[ht], writes=[ut])
                bank = PBK[0]
                for kc in range(8):
                    k.op("PE", lambda e: e.matmul(bank.ap, ones_bf.ap, u3[:, kc, :], start=(kc == 0), stop=(kc == 7)),
                         reads=[ut, ones_bf], writes=[bank], pe_acc=(kc > 0))
                k.op("ACT", lambda e: e.activation(out=LNV.ap, in_=bank.ap, func=AF.Ln, scale=1.0 / 1024, bias=EPS), reads=[bank], writes=[LNV])
                k.op("ACT", lambda e: e.activation(out=RSTD.ap, in_=LNV.ap, func=AF.Exp, scale=-0.5), reads=[LNV], writes=[RSTD])
                for d in range(8):
                    k.op("DVE", lambda e: e.scalar_tensor_tensor(out=h3[:, d, :], in0=h3[:, d, :], scalar=vec.ap[:, 24 + d:25 + d], in1=RSTD.ap,
                                                                 op0=ALU.mult, op1=ALU.mult), reads=[ht, RSTD, vec], writes=[ht])
                k.dma("SP", out_o[:, cs].rearrange("(a p) n -> p a n", p=128), h3, reads=[ht], writes=[out_t], part=True)
        k.finish("SP", [qT_t, kT_t, v_t, ho_t] if mode == "A" else [out_t])
        print("tok2 ninst", k.ninst)
    return nc
```

```python
import numpy as np
import ml_dtypes
from contextlib import ExitStack
import concourse.bass as bass
import concourse.mybir as mybir
from concourse.bass_utils import run_bass_kernel_spmd

F32 = mybir.dt.float32
BF16 = mybir.dt.bfloat16
AF = mybir.ActivationFunctionType
ALU = mybir.AluOpType
AX = mybir.AxisListType
EPS = 1e-6
NPBF = ml_dtypes.bfloat16


class T:
    __slots__ = ("ap", "w", "r", "dsem", "dcnt", "name")

    def __init__(self, ap, name=""):
        self.ap = ap
        self.w = None
        self.r = {}
        self.dsem = None
        self.dcnt = 0
        self.name = name


class K:
    def __init__(self, nc, stack):
        self.nc = nc
        self.stack = stack
        self.eng = {"PE": nc.tensor, "ACT": nc.scalar, "DVE": nc.vector, "POOL": nc.gpsimd, "SP": nc.sync}
        self.sem = {}
        self.cnt = {}
        for e in ("PE", "ACT", "DVE", "POOL"):
            self.sem[e] = stack.enter_context(nc.semaphore("s_" + e))
            self.cnt[e] = 0
        self.seen = {e: {} for e in self.eng}
        self.semobj = dict(self.sem)
        self.ndsem = 0
        self.ninst = 0
        self.nalloc = 0

    def sb(self, shape, dt, name=None):
        self.nalloc += 1
        return self.stack.enter_context(self.nc.sbuf_tensor(name or ("sb%d" % self.nalloc), list(shape), dt))

    def ps(self, shape, dt, name=None):
        self.nalloc += 1
        return self.stack.enter_context(self.nc.psum_tensor(name or ("ps%d" % self.nalloc), list(shape), dt))

    def sbt(self, shape, dt, name=None):
        return T(self.sb(shape, dt, name)[:])

    def _need(self, e, key, val):
        if val <= 0 or self.seen[e].get(key, 0) >= val:
            return
        self.eng[e].wait_ge(self.semobj[key], val)
        self.seen[e][key] = val

    def op(self, e, fn, reads=(), writes=(), pe_acc=False):
        for t in reads:
            if t.w is not None:
                self._need(e, *t.w)
        for t in writes:
            if t.w is not None and not (pe_acc and t.w[0] == "PE"):
                self._need(e, *t.w)
            for kk, v in t.r.items():
                self._need(e, kk, v)
        ins = fn(self.eng[e])
        self.cnt[e] += 1
        c = self.cnt[e]
        ins.then_inc(self.sem[e], 1)
        for t in reads:
            t.r[e] = c
        for t in writes:
            t.w = (e, c)
            t.r = {}
        self.ninst += 1
        return ins

    def dma(self, q, out_ap, in_ap, reads=(), writes=(), part=False, **kw):
        assert len(writes) == 1
        t = writes[0]
        if t.dsem is None:
            key = "d%d" % self.ndsem
            t.dsem = self.stack.enter_context(self.nc.semaphore(key))
            self.semobj[key] = t.dsem
            t.name = key
            self.ndsem += 1
        key = t.name
        for r in reads:
            if r.w is not None:
                self._need(q, *r.w)
        if t.w is not None and not (part and t.w[0] == key):
            self._need(q, *t.w)
        for kk, v in t.r.items():
            self._need(q, kk, v)
        ins = self.eng[q].dma_start(out=out_ap, in_=in_ap, **kw)
        t.dcnt += 16
        ins.then_inc(t.dsem, 16)
        for r in reads:
            r.r[key] = max(r.r.get(key, 0), t.dcnt)
        t.w = (key, t.dcnt)
        if not part:
            t.r = {}
        self.ninst += 1
        return ins

    def alias(self, new, old):
        for n in new:
            for o in old:
                for kk, v in o.r.items():
                    n.r[kk] = max(n.r.get(kk, 0), v)
                if o.w is not None:
                    n.r[o.w[0]] = max(n.r.get(o.w[0], 0), o.w[1])

    def finish(self, e, tiles):
        for t in tiles:
            if t.w is not None:
                self._need(e, *t.w)


def v3(ap, a):
    return ap.rearrange("p (a n) -> p a n", a=a)


def build_tok(NT, TH, KA, mode, NE=64):
    nc = bass.Bass("TRN2", target_bir_lowering=False)

    def din(name, shape, dt=F32):
        return nc.dram_tensor(name, list(shape), dt, kind="ExternalInput").ap()

    def dout(name, shape, dt=F32):
        return nc.dram_tensor(name, list(shape), dt, kind="ExternalOutput").ap()

    hT = din("hT", [1024, NT])
    aT = din("aT", [KA, NT], BF16)
    w_a = din("w_a", [KA, 1024])
    vecs = din("vecs", [128, 32])
    wr = din("wr", [1024, 72])
    br = din("br", [1, 72])
    wg = din("wg", [64, 1024, 512])
    wu = din("wu", [64, 1024, 512])
    wd = din("wd", [64, 512, 1024])
    plg = din("plg", [1024, 1024])
    plp = din("plp", [256, 1024])
    pT = din("pT", [256, NT])
    ident_d = din("ident", [128, 128])
    if mode == "A":
        wqkv = din("wqkv", [1024, 3072])
        hT_o = dout("hT_o", [1024, NT])
        qT_o = dout("qT_o", [1024, NT], BF16)
        kT_o = dout("kT_o", [1024, NT], BF16)
        v_o = dout("v_o", [NT, 1024], BF16)
    else:
        out_o = dout("out_o", [1024, NT])
    KAC = KA // 128
    NTL = TH // 512
    NH = NT // TH

    with ExitStack() as st:
        k = K(nc, st)
        h_sb = k.sb([128, 8 * TH], F32, "h_sb")
        h3 = v3(h_sb[:], 8)
        HT = [[T(h3[:, d, t * 512:(t + 1) * 512]) for d in range(8)] for t in range(NTL)]
        u_sb = k.sb([128, 8 * TH], BF16, "u_sb")
        u3 = v3(u_sb[:], 8)
        UT = [T(u3[:, :, t * 512:(t + 1) * 512]) for t in range(NTL)]
        WB = [k.sbt([128, 12288], BF16, "wb%d" % i) for i in range(2)]
        GT = k.sbt([64, TH], F32, "gt")
        NAB = max(1, min(2, (8 * TH) // (KAC * 512)))
        ABS = [T(u_sb[:, i * KAC * 512:(i + 1) * KAC * 512]) for i in range(NAB)]
        PB = k.sbt([128, 2 * 512], BF16, "pb")
        SQ = k.sbt([128, 8 * 512], BF16, "sq")
        LNV = k.sbt([128, 512], F32, "lnv")
        RSTD = k.sbt([128, 512], F32, "rstd")
        vec = k.sbt([128, 32], F32, "vec_sb")
        ident = k.sbt([128, 128], F32, "ident_sb")
        ones_bf = k.sbt([128, 128], BF16, "ones")
        wr_sb = k.sbt([128, 8 * 72], F32, "wr_sb")
        br_bc = k.sbt([128, 72], F32, "brbc")
        S_ = [k.sbt([128, 512], BF16, "s%d" % i) for i in range(2)]
        TT = [k.sbt([128, 512], BF16, "tt%d" % i) for i in range(2)]
        HS = [[k.sbt([128, 512], BF16, "hs%d_%d" % (i, f)) for f in range(4)] for i in range(2)]
        EB = [k.sbt([64, 128], F32, "eb%d" % i) for i in range(2)]
        STG = [k.sbt([128, 512], BF16, "stg%d" % i) for i in range(2)]
        STF = [k.sbt([128, 512], F32, "stf%d" % i) for i in range(2)]
        r_lg = k.sbt([128, 72], F32, "r_lg")
        r_a = k.sbt([128, 8], F32, "r_a")
        r_goh = k.sbt([128, 8], F32, "r_goh")
        r_t64 = k.sbt([128, 64], F32, "r_t64")
        r_es = k.sbt([128, 8], F32, "r_es")
        r_es2 = k.sbt([128, 8], F32, "r_es2")
        r_oh1 = k.sbt([128, 8], F32, "r_oh1")
        r_oh2 = k.sbt([128, 8], F32, "r_oh2")
        r_ew = k.sbt([128, 8], F32, "r_ew")
        r_G = k.sbt([128, 64], F32, "r_G")
        r_s = [k.sbt([128, 1], F32, "r_s%d" % i) for i in range(12)]
        BK = [T(k.ps([128, 512], F32, "bk%d" % i)[:]) for i in range(8)]
        PG, PU, PD, PBK = BK[0:2], BK[2:4], BK[4:6], BK[6:8]

        k.dma("SP", vec.ap, vecs, writes=[vec])
        k.dma("SP", ident.ap, ident_d, writes=[ident])
        k.dma("SP", v3(wr_sb.ap, 8), wr.rearrange("(a p) n -> p a n", p=128), writes=[wr_sb])
        k.dma("SP", br_bc.ap, br.partition_broadcast(128).rearrange("p o n -> p (o n)"), writes=[br_bc])
        k.op("DVE", lambda e: e.memset(ones_bf.ap, 1.0), writes=[ones_bf])
        for kc in range(8):
            k.op("DVE", lambda e: e.tensor_scalar(out=wr_sb.ap[:, kc * 72:(kc + 1) * 72], in0=wr_sb.ap[:, kc * 72:(kc + 1) * 72],
                                                  scalar1=vec.ap[:, kc:kc + 1], scalar2=None, op0=ALU.mult),
                 reads=[vec, wr_sb], writes=[wr_sb])

        def rmsnorm(src_tiles, src_ap, gcol, dst_t, dst_ap, bank):
            k.op("ACT", lambda e: e.activation(out=v3(SQ.ap, 8), in_=src_ap, func=AF.Square), reads=src_tiles, writes=[SQ])
            for kc in range(8):
                k.op("PE", lambda e: e.matmul(bank.ap, ones_bf.ap, v3(SQ.ap, 8)[:, kc, :], start=(kc == 0), stop=(kc == 7)),
                     reads=[SQ, ones_bf], writes=[bank], pe_acc=(kc > 0))
            k.op("ACT", lambda e: e.activation(out=LNV.ap, in_=bank.ap, func=AF.Ln, scale=1.0 / 1024, bias=EPS), reads=[bank], writes=[LNV])
            k.op("ACT", lambda e: e.activation(out=RSTD.ap, in_=LNV.ap, func=AF.Exp, scale=-0.5), reads=[LNV], writes=[RSTD])
            for kc in range(8):
                k.op("DVE", lambda e: e.scalar_tensor_tensor(out=dst_ap[:, kc, :], in0=src_ap[:, kc, :], scalar=vec.ap[:, gcol + kc:gcol + kc + 1],
                                                             in1=RSTD.ap, op0=ALU.mult, op1=ALU.mult),
                     reads=list(src_tiles) + [RSTD, vec], writes=[dst_t])

        if mode == "A":
            qT_t, kT_t, v_t, ho_t = T(qT_o), T(kT_o), T(v_o), T(hT_o)
        else:
            out_t = T(out_o)
        for hf in range(NH):
            c0 = hf * TH
            k.alias(ABS, UT)
            for kp in range(KAC // 8):
                k.dma("POOL", v3(WB[kp].ap[:, 0:8192], 8), w_a[kp * 1024:(kp + 1) * 1024, :].rearrange("(a p) n -> p a n", p=128), writes=[WB[kp]])
            for t in range(NTL):
                cs = slice(c0 + t * 512, c0 + (t + 1) * 512)
                for d in range(8):
                    k.dma("SP", HT[t][d].ap, hT[d * 128:(d + 1) * 128, cs], writes=[HT[t][d]])
                AB = ABS[t % NAB]
                k.dma("SP", v3(AB.ap, KAC), aT[:, cs].rearrange("(a p) n -> p a n", p=128), writes=[AB])
                for d in range(8):
                    bank = PD[d % 2]
                    for kc in range(KAC):
                        wbt = WB[kc // 8]
                        k.op("PE", lambda e: e.matmul(bank.ap, v3(wbt.ap[:, 0:8192], 8)[:, kc % 8, d * 128:(d + 1) * 128], v3(AB.ap, KAC)[:, kc, :],
                                                      start=(kc == 0), stop=(kc == KAC - 1)),
                             reads=[wbt, AB], writes=[bank], pe_acc=(kc > 0))
                    k.op("DVE", lambda e: e.tensor_tensor(out=HT[t][d].ap, in0=HT[t][d].ap, in1=bank.ap, op=ALU.add),
                         reads=[bank, HT[t][d]], writes=[HT[t][d]])
            k.alias(UT, ABS)
            for t in range(NTL):
                hap = h3[:, :, t * 512:(t + 1) * 512]
                rmsnorm(HT[t], hap, 0, UT[t], UT[t].ap, PBK[0])
                for b in range(4):
                    bs = slice(t * 512 + b * 128, t * 512 + (b + 1) * 128)
                    plg_b, prs = PG[b % 2], PU[b % 2]
                    for kc in range(8):
                        k.op("PE", lambda e: e.matmul(plg_b.ap[:, 0:72], h3[:, kc, bs], wr_sb.ap[:, kc * 72:(kc + 1) * 72], start=(kc == 0), stop=(kc == 7)),
                             reads=[HT[t][kc], wr_sb], writes=[plg_b], pe_acc=(kc > 0))
                    k.op("PE", lambda e: e.matmul(prs.ap[:, 0:2], RSTD.ap[:, b * 128:(b + 1) * 128], ident.ap[:, 0:2], start=True, stop=True),
                         reads=[RSTD, ident], writes=[prs])
                    rt, gmax, ngmax, gsum, gw, m1, m2, dd, ex, w1, w2 = r_s[0:11]
                    k.op("DVE", lambda e: e.tensor_copy(out=rt.ap, in_=prs.ap[:, 0:1]), reads=[prs], writes=[rt])
                    k.op("DVE", lambda e: e.scalar_tensor_tensor(out=r_lg.ap, in0=plg_b.ap[:, 0:72], scalar=rt.ap, in1=br_bc.ap, op0=ALU.mult, op1=ALU.add),
                         reads=[plg_b, rt, br_bc], writes=[r_lg])
                    gl = r_lg.ap[:, 0:8]
                    el = r_lg.ap[:, 8:72]
                    k.op("DVE", lambda e: e.tensor_reduce(out=gmax.ap, in_=gl, axis=AX.X, op=ALU.max), reads=[r_lg], writes=[gmax])
                    k.op("DVE", lambda e: e.tensor_scalar(out=r_goh.ap, in0=gl, scalar1=gmax.ap, scalar2=None, op0=ALU.is_equal), reads=[r_lg, gmax], writes=[r_goh])
                    k.op("DVE", lambda e: e.tensor_scalar(out=ngmax.ap, in0=gmax.ap, scalar1=-1.0, scalar2=None, op0=ALU.mult), reads=[gmax], writes=[ngmax])
                    k.op("ACT", lambda e: e.activation(out=r_a.ap, in_=gl, func=AF.Exp, bias=ngmax.ap, scale=1.0, accum_out=gsum.ap), reads=[r_lg, ngmax], writes=[r_a, gsum])
                    k.op("DVE", lambda e: e.reciprocal(out=gw.ap, in_=gsum.ap), reads=[gsum], writes=[gw])
                    k.op("DVE", lambda e: e.tensor_tensor(out=v3(r_t64.ap, 8), in0=v3(el, 8), in1=r_goh.ap.unsqueeze(2).to_broadcast([128, 8, 8]), op=ALU.mult),
                         reads=[r_lg, r_goh], writes=[r_t64])
                    k.op("DVE", lambda e: e.tensor_reduce(out=r_es.ap, in_=v3(r_t64.ap, 8).rearrange("p g j -> p j g"), axis=AX.X, op=ALU.add), reads=[r_t64], writes=[r_es])
                    k.op("DVE", lambda e: e.tensor_reduce(out=m1.ap, in_=r_es.ap, axis=AX.X, op=ALU.max), reads=[r_es], writes=[m1])
                    k.op("DVE", lambda e: e.tensor_scalar(out=r_oh1.ap, in0=r_es.ap, scalar1=m1.ap, scalar2=None, op0=ALU.is_equal), reads=[r_es, m1], writes=[r_oh1])
                    k.op("DVE", lambda e: e.scalar_tensor_tensor(out=r_es2.ap, in0=r_oh1.ap, scalar=-1e30, in1=r_es.ap, op0=ALU.mult, op1=ALU.add), reads=[r_oh1, r_es], writes=[r_es2])
                    k.op("DVE", lambda e: e.tensor_reduce(out=m2.ap, in_=r_es2.ap, axis=AX.X, op=ALU.max), reads=[r_es2], writes=[m2])
                    k.op("DVE", lambda e: e.tensor_scalar(out=r_oh2.ap, in0=r_es2.ap, scalar1=m2.ap, scalar2=None, op0=ALU.is_equal), reads=[r_es2, m2], writes=[r_oh2])
                    k.op("DVE", lambda e: e.tensor_tensor(out=dd.ap, in0=m2.ap, in1=m1.ap, op=ALU.subtract), reads=[m1, m2], writes=[dd])
                    k.op("ACT", lambda e: e.activation(out=ex.ap, in_=dd.ap, func=AF.Exp), reads=[dd], writes=[ex])
                    k.op("DVE", lambda e: e.tensor_scalar(out=w1.ap, in0=ex.ap, scalar1=1.0, scalar2=None, op0=ALU.add), reads=[ex], writes=[w1])
                    k.op("DVE", lambda e: e.reciprocal(out=w1.ap, in_=w1.ap), reads=[w1], writes=[w1])
                    k.op("DVE", lambda e: e.tensor_tensor(out=w2.ap, in0=ex.ap, in1=w1.ap, op=ALU.mult), reads=[ex, w1], writes=[w2])
                    k.op("DVE", lambda e: e.tensor_tensor(out=w1.ap, in0=w1.ap, in1=gw.ap, op=ALU.mult), reads=[w1, gw], writes=[w1])
                    k.op("DVE", lambda e: e.tensor_tensor(out=w2.ap, in0=w2.ap, in1=gw.ap, op=ALU.mult), reads=[w2, gw], writes=[w2])
                    k.op("DVE", lambda e: e.tensor_scalar(out=r_ew.ap, in0=r_oh1.ap, scalar1=w1.ap, scalar2=None, op0=ALU.mult), reads=[r_oh1, w1], writes=[r_ew])
                    k.op("DVE", lambda e: e.scalar_tensor_tensor(out=r_ew.ap, in0=r_oh2.ap, scalar=w2.ap, in1=r_ew.ap, op0=ALU.mult, op1=ALU.add), reads=[r_oh2, w2, r_ew], writes=[r_ew])
                    k.op("DVE", lambda e: e.tensor_tensor(out=v3(r_G.ap, 8), in0=r_goh.ap.unsqueeze(2).to_broadcast([128, 8, 8]),
                                                          in1=r_ew.ap.unsqueeze(1).to_broadcast([128, 8, 8]), op=ALU.mult), reads=[r_goh, r_ew], writes=[r_G])
                    ptr = PD[b % 2]
                    k.op("PE", lambda e: e.transpose(ptr.ap[0:64, 0:128], r_G.ap, ident.ap), reads=[r_G, ident], writes=[ptr])
                    k.op("DVE", lambda e: e.tensor_copy(out=GT.ap[:, bs], in_=ptr.ap[0:64, 0:128]), reads=[ptr], writes=[GT])
            def load_expert(e_):
                wb = WB[e_ % 2]
                k.dma("POOL", v3(wb.ap[:, 0:4096], 8), wg[e_].rearrange("(a p) n -> p a n", p=128), writes=[wb])
                k.dma("POOL", v3(wb.ap[:, 4096:8192], 8), wu[e_].rearrange("(a p) n -> p a n", p=128), writes=[wb], part=True)
                k.dma("POOL", v3(wb.ap[:, 8192:12288], 4), wd[e_].rearrange("(a p) n -> p a n", p=128), writes=[wb], part=True)

            def emit_gu(e_, t, i):
                wb = WB[e_ % 2]
                Wg3 = v3(wb.ap[:, 0:4096], 8)
                Wu3 = v3(wb.ap[:, 4096:8192], 8)
                pb = PBK[i % 2]
                eb = EB[i % 2]
                k.op("DVE", lambda e: e.tensor_copy(out=eb.ap, in_=ident.ap[0:64, e_:e_ + 1].to_broadcast([64, 128])), reads=[ident], writes=[eb])
                k.op("PE", lambda e: e.matmul(pb.ap, eb.ap, GT.ap[:, t * 512:(t + 1) * 512], start=True, stop=True), reads=[eb, GT], writes=[pb])
                for f in range(4):
                    pg, pu = PG[f % 2], PU[f % 2]
                    for kc in range(8):
                        k.op("PE", lambda e: e.matmul(pg.ap, Wg3[:, kc, f * 128:(f + 1) * 128], UT[t].ap[:, kc, :], start=(kc == 0), stop=(kc == 7)),
                             reads=[wb, UT[t]], writes=[pg], pe_acc=(kc > 0))
                    for kc in range(8):
                        k.op("PE", lambda e: e.matmul(pu.ap, Wu3[:, kc, f * 128:(f + 1) * 128], UT[t].ap[:, kc, :], start=(kc == 0), stop=(kc == 7)),
                             reads=[wb, UT[t]], writes=[pu], pe_acc=(kc > 0))
                    s_, tt = S_[f % 2], TT[f % 2]
                    k.op("ACT", lambda e: e.activation(out=s_.ap, in_=pg.ap, func=AF.Silu), reads=[pg], writes=[s_])
                    k.op("DVE", lambda e: e.tensor_tensor(out=tt.ap, in0=s_.ap, in1=pu.ap, op=ALU.mult), reads=[s_, pu], writes=[tt])
                    k.op("DVE", lambda e: e.tensor_tensor(out=HS[i % 2][f].ap, in0=tt.ap, in1=pb.ap, op=ALU.mult), reads=[tt, pb], writes=[HS[i % 2][f]])

            def emit_dn(e_, t, i):
                wb = WB[e_ % 2]
                Wd3 = v3(wb.ap[:, 8192:12288], 4)
                for d in range(8):
                    pd = PD[d % 2]
                    for f in range(4):
                        k.op("PE", lambda e: e.matmul(pd.ap, Wd3[:, f, d * 128:(d + 1) * 128], HS[i % 2][f].ap, start=(f == 0), stop=(f == 3)),
                             reads=[wb, HS[i % 2][f]], writes=[pd], pe_acc=(f > 0))
                    k.op("DVE", lambda e: e.tensor_tensor(out=HT[t][d].ap, in0=HT[t][d].ap, in1=pd.ap, op=ALU.add), reads=[pd, HT[t][d]], writes=[HT[t][d]])

            seq = [(e_, t) for e_ in range(NE) for t in range(NTL)]
            load_expert(0)
            prev = None
            for i, (e_, t) in enumerate(seq):
                emit_gu(e_, t, i)
                if prev is not None:
                    emit_dn(*prev)
                if t == 0 and e_ + 1 < NE:
                    load_expert(e_ + 1)
                prev = (e_, t, i)
            emit_dn(*prev)
            k.dma("POOL", v3(WB[0].ap[:, 0:8192], 8), plg.rearrange("(a p) n -> p a n", p=128), writes=[WB[0]])
            k.dma("POOL", v3(WB[1].ap[:, 0:2048], 2), plp.rearrange("(a p) n -> p a n", p=128), writes=[WB[1]])
            for t in range(NTL):
                cs = slice(c0 + t * 512, c0 + (t + 1) * 512)
                hap = h3[:, :, t * 512:(t + 1) * 512]
                k.dma("POOL", v3(PB.ap, 2), pT[:, cs].rearrange("(a p) n -> p a n", p=128), writes=[PB])
                rmsnorm(HT[t], hap, 8, UT[t], UT[t].ap, PBK[0])
                for d in range(8):
                    pg, pu = PG[d % 2], PU[d % 2]
                    for kc in range(8):
                        k.op("PE", lambda e: e.matmul(pg.ap, v3(WB[0].ap[:, 0:8192], 8)[:, kc, d * 128:(d + 1) * 128], UT[t].ap[:, kc, :],
                                                      start=(kc == 0), stop=(kc == 7)), reads=[WB[0], UT[t]], writes=[pg], pe_acc=(kc > 0))
                    for kc in range(2):
                        k.op("PE", lambda e: e.matmul(pu.ap, v3(WB[1].ap[:, 0:2048], 2)[:, kc, d * 128:(d + 1) * 128], v3(PB.ap, 2)[:, kc, :],
                                                      start=(kc == 0), stop=(kc == 1)), reads=[WB[1], PB], writes=[pu], pe_acc=(kc > 0))
                    sf = STF[d % 2]
                    k.op("ACT", lambda e: e.activation(out=sf.ap, in_=pg.ap, func=AF.Sigmoid, bias=vec.ap[:, 16 + d:17 + d], scale=1.0), reads=[pg, vec], writes=[sf])
                    k.op("DVE", lambda e: e.tensor_tensor(out=sf.ap, in0=sf.ap, in1=pu.ap, op=ALU.mult), reads=[sf, pu], writes=[sf])
                    k.op("DVE", lambda e: e.tensor_tensor(out=HT[t][d].ap, in0=HT[t][d].ap, in1=sf.ap, op=ALU.add), reads=[sf, HT[t][d]], writes=[HT[t][d]])
            if mode == "A":
                k.dma("POOL", v3(WB[0].ap, 8), wqkv[:, 0:1536].rearrange("(a p) n -> p a n", p=128), writes=[WB[0]])
                k.dma("POOL", v3(WB[1].ap, 8), wqkv[:, 1536:3072].rearrange("(a p) n -> p a n", p=128), writes=[WB[1]])
                for t in range(NTL):
                    cs = slice(c0 + t * 512, c0 + (t + 1) * 512)
                    hap = h3[:, :, t * 512:(t + 1) * 512]
                    for d in range(8):
                        k.dma("SP", hT_o[d * 128:(d + 1) * 128, cs], HT[t][d].ap, reads=[HT[t][d]], writes=[ho_t], part=True)
                    rmsnorm(HT[t], hap, 24, UT[t], UT[t].ap, PBK[0])
                    U3 = UT[t].ap
                    for n in range(16):
                        col = n * 128
                        wbt = WB[0] if col < 1536 else WB[1]
                        lc = col if col < 1536 else col - 1536
                        bank = PG[n % 2]
                        for kc in range(8):
                            k.op("PE", lambda e: e.matmul(bank.ap, v3(wbt.ap, 8)[:, kc, lc:lc + 128], U3[:, kc, :], start=(kc == 0), stop=(kc == 7)),
                                 reads=[wbt, UT[t]], writes=[bank], pe_acc=(kc > 0))
                        sg = STG[n % 2]
                        sc = (1.0 / np.sqrt(128.0)) if n < 8 else 1.0
                        k.op("ACT", lambda e: e.activation(out=sg.ap, in_=bank.ap, func=AF.Identity, scale=float(sc)), reads=[bank], writes=[sg])
                        if n < 8:
                            k.dma("SP", qT_o[n * 128:(n + 1) * 128, cs], sg.ap, reads=[sg], writes=[qT_t], part=True)
                        else:
                            k.dma("SP", kT_o[(n - 8) * 128:(n - 7) * 128, cs], sg.ap, reads=[sg], writes=[kT_t], part=True)
                    for b in range(4):
                        for hh in range(2):
                            bank = PU[hh]
                            for kc in range(8):
                                k.op("PE", lambda e: e.matmul(bank.ap, U3[:, kc, b * 128:(b + 1) * 128], v3(WB[1].ap, 8)[:, kc, 512 + hh * 512:1024 + hh * 512],
                                                              start=(kc == 0), stop=(kc == 7)), reads=[WB[1], UT[t]], writes=[bank], pe_acc=(kc > 0))
                            sg = STG[hh]
                            k.op("ACT", lambda e: e.activation(out=sg.ap, in_=bank.ap, func=AF.Copy), reads=[bank], writes=[sg])
                            r0 = c0 + t * 512 + b * 128
                            k.dma("SP", v_o[r0:r0 + 128, hh * 512:(hh + 1) * 512], sg.ap, reads=[sg], writes=[v_t], part=True)
                fin = [qT_t, kT_t, v_t, ho_t]
            else:
                for t in range(NTL):
                    cs = slice(c0 + t * 512, c0 + (t + 1) * 512)
                    hap = h3[:, :, t * 512:(t + 1) * 512]
                    k.op("ACT", lambda e: e.activation(out=v3(SQ.ap, 8), in_=hap, func=AF.Square), reads=HT[t], writes=[SQ])
                    bank = PBK[0]
                    for kc in range(8):
                        k.op("PE", lambda e: e.matmul(bank.ap, ones_bf.ap, v3(SQ.ap, 8)[:, kc, :], start=(kc == 0), stop=(kc == 7)),
                             reads=[SQ, ones_bf], writes=[bank], pe_acc=(kc > 0))
                    k.op("ACT", lambda e: e.activation(out=LNV.ap, in_=bank.ap, func=AF.Ln, scale=1.0 / 1024, bias=EPS), reads=[bank], writes=[LNV])
                    k.op("ACT", lambda e: e.activation(out=RSTD.ap, in_=LNV.ap, func=AF.Exp, scale=-0.5), reads=[LNV], writes=[RSTD])
                    for d in range(8):
                        k.op("DVE", lambda e: e.scalar_tensor_tensor(out=HT[t][d].ap, in0=HT[t][d].ap, scalar=vec.ap[:, 24 + d:25 + d], in1=RSTD.ap,
                                                                     op0=ALU.mult, op1=ALU.mult), reads=[HT[t][d], RSTD, vec], writes=[HT[t][d]])
                        k.dma("SP", out_o[d * 128:(d + 1) * 128, cs], HT[t][d].ap, reads=[HT[t][d]], writes=[out_t], part=True)
                fin = [out_t]
            if hf == NH - 1:
                k.finish("SP", fin)
        print("tok ninst", k.ninst)
    return nc


def build_attn(L, NHD=2):
    nc = bass.Bass("TRN2", target_bir_lowering=False)

    def din(name, shape, dt=F32):
        return nc.dram_tensor(name, list(shape), dt, kind="ExternalInput").ap()

    qT = din("qT", [NHD, 128, L], BF16)
    kT = din("kT", [NHD, 128, L], BF16)
    vP = din("vP", [NHD, 128, L // 128, 128], BF16)
    cst = din("cst", [128, 256 + 4 * 512])
    oT = nc.dram_tensor("oT", [NHD, 128, L], BF16, kind="ExternalOutput").ap()
    NQT = L // 512
    NB = L // 128
    with ExitStack() as st:
        k = K(nc, st)
        Qs = k.sbt([128, L], BF16, "Qs")
        Ks = k.sbt([128, L], BF16, "Ks")
        Vs = k.sbt([128, L], BF16, "Vs")
        V3 = v3(Vs.ap, NB)
        cf = k.sbt([128, 256 + 2048], F32, "cf")
        trin = k.sbt([128, 128], BF16, "trin")
        onen = k.sbt([128, 128], BF16, "onen")
        m01 = k.sbt([128, 2048], BF16, "m01")
        mneg = k.sbt([128, 2048], F32, "mneg")
        E_ = [k.sbt([128, 512], F32, "E%d" % i) for i in range(3)]
        SPf = [k.sbt([128, 512], BF16, "SPf%d" % i) for i in range(3)]
        SPm = [k.sbt([128, 512], BF16, "SPm%d" % i) for i in range(3)]
        TMP = [k.sbt([128, 512], F32, "TMP%d" % i) for i in range(3)]
        AT = [k.sbt([128, 512], BF16, "AT%d" % i) for i in range(3)]
        OFFS = [k.sbt([128, 512], F32, "OFF%d" % i) for i in range(2)]
        offi = [0]
        OST = [k.sbt([128, 512], BF16, "OST%d" % i) for i in range(2)]
        BK = [T(k.ps([128, 512], F32, "bk%d" % i)[:]) for i in range(8)]
        PA, PBb, PO = BK[0:3], BK[3:6], BK[6:8]
        oT_t = T(oT)

        k.dma("SP", cf.ap, cst, writes=[cf])
        k.op("DVE", lambda e: e.tensor_copy(out=trin.ap, in_=cf.ap[:, 0:128]), reads=[cf], writes=[trin])
        k.op("DVE", lambda e: e.tensor_copy(out=onen.ap, in_=cf.ap[:, 128:256]), reads=[cf], writes=[onen])
        k.op("DVE", lambda e: e.tensor_copy(out=m01.ap, in_=cf.ap[:, 256:2304]), reads=[cf], writes=[m01])
        k.op("DVE", lambda e: e.tensor_scalar(out=mneg.ap, in0=cf.ap[:, 256:2304], scalar1=-1.0, scalar2=30000.0, op0=ALU.add, op1=ALU.mult),
             reads=[cf], writes=[mneg])

        jobs = []
        for hd in range(NHD):
            for J in range(NQT):
                nb = 4 * J + 4
                for i, n in enumerate(range(nb - 1, -1, -1)):
                    jobs.append(dict(hd=hd, J=J, n=n, first=(i == 0), last=(n == 0), r=(n - 4 * J) if n >= 4 * J else -1))

        def load_head(hd):
            k.dma("SP", Qs.ap, qT[hd], writes=[Qs])
            k.dma("SP", Ks.ap, kT[hd], writes=[Ks])
            k.dma("SP", V3, vP[hd], writes=[Vs])

        def s1(j, i):
            if j["first"] and j["J"] == 0:
                load_head(j["hd"])
            A = PA[i % 3]
            qs = slice(j["J"] * 512, (j["J"] + 1) * 512)
            ks = slice(j["n"] * 128, (j["n"] + 1) * 128)
            k.op("PE", lambda e: e.matmul(A.ap, Ks.ap[:, ks], Qs.ap[:, qs], start=True, stop=False), reads=[Ks, Qs], writes=[A])
            k.op("ACT", lambda e: e.activation(out=E_[i % 3].ap, in_=A.ap, func=AF.Exp), reads=[A], writes=[E_[i % 3]])

        def s1b(j, i):
            k.op("ACT", lambda e: e.activation(out=SPf[i % 3].ap, in_=E_[i % 3].ap, func=AF.Ln, bias=1.0, scale=1.0), reads=[E_[i % 3]], writes=[SPf[i % 3]])
            if j["r"] >= 0:
                r = j["r"]
                k.op("POOL", lambda e: e.tensor_tensor(out=SPm[i % 3].ap, in0=SPf[i % 3].ap, in1=m01.ap[:, r * 512:(r + 1) * 512], op=ALU.mult),
                     reads=[SPf[i % 3], m01], writes=[SPm[i % 3]])

        def s2(j, i):
            A, B = PA[i % 3], PBb[i % 3]
            sp = SPm[i % 3] if j["r"] >= 0 else SPf[i % 3]
            OFF = OFFS[offi[0] % 2]
            if j["first"]:
                k.op("DVE", lambda e: e.memset(OFF.ap, 0.0), writes=[OFF])
            k.op("PE", lambda e: e.matmul(A.ap, trin.ap, sp.ap, start=False, stop=True), reads=[trin, sp], writes=[A], pe_acc=True)
            k.op("PE", lambda e: e.matmul(B.ap, onen.ap, sp.ap, start=True, stop=True), reads=[onen, sp], writes=[B])
            tm = TMP[i % 3]
            k.op("DVE", lambda e: e.tensor_tensor(out=tm.ap, in0=A.ap, in1=OFF.ap, op=ALU.add), reads=[A, OFF], writes=[tm])
            if j["r"] >= 0:
                r = j["r"]
                k.op("DVE", lambda e: e.tensor_tensor(out=tm.ap, in0=tm.ap, in1=mneg.ap[:, r * 512:(r + 1) * 512], op=ALU.add), reads=[tm, mneg], writes=[tm])
            k.op("ACT", lambda e: e.activation(out=AT[i % 3].ap, in_=tm.ap, func=AF.Exp), reads=[tm], writes=[AT[i % 3]])
            if not j["last"]:
                OFN = OFFS[(offi[0] + 1) % 2]
                k.op("DVE", lambda e: e.tensor_tensor(out=OFN.ap, in0=OFF.ap, in1=B.ap, op=ALU.add), reads=[B, OFF], writes=[OFN])
            offi[0] += 1

        grp = [0]

        def s3(j, i):
            O = PO[grp[0] % 2]
            k.op("PE", lambda e: e.matmul(O.ap, V3[:, j["n"], :], AT[i % 3].ap, start=j["first"], stop=j["last"]),
                 reads=[Vs, AT[i % 3]], writes=[O], pe_acc=(not j["first"]))
            if j["last"]:
                og = OST[grp[0] % 2]
                k.op("ACT", lambda e: e.activation(out=og.ap, in_=O.ap, func=AF.Copy), reads=[O], writes=[og])
                k.dma("SP", oT[j["hd"], :, j["J"] * 512:(j["J"] + 1) * 512], og.ap, reads=[og], writes=[oT_t], part=True)
                grp[0] += 1

        n = len(jobs)
        stages = [s1, s1b, s2, s3]
        done = [0] * n

        def run_next(jj):
            stages[done[jj]](jobs[jj], jj)
            done[jj] += 1

        for i in range(n + 3):
            if i < n and jobs[i]["first"] and jobs[i]["J"] == 0 and i > 0:
                for jj in range(max(0, i - 3), i):
                    while done[jj] < 4:
                        run_next(jj)
            for d_ in range(4):
                jj = i - d_
                if 0 <= jj < n and done[jj] == d_:
                    run_next(jj)
        assert all(x == 4 for x in done)
        k.finish("SP", [oT_t])
        print("attn ninst", k.ninst, "jobs", n)
    return nc


def attn_consts():
    s = np.arange(128)
    tri_neg = -(s[:, None] >= s[None, :]).astype(np.float32)
    ones_neg = -np.ones((128, 128), np.float32)
    q = np.arange(512)
    masks = [(q[None, :] > (s[:, None] + 128 * r)).astype(np.float32) for r in range(4)]
    return np.ascontiguousarray(np.concatenate([tri_neg, ones_neg] + masks, axis=1))


def build_ssd(L):
    nc = bass.Bass("TRN2", target_bir_lowering=False)

    def din(name, shape, dt=F32):
        return nc.dram_tensor(name, list(shape), dt, kind="ExternalInput").ap()

    xT = din("xT", [1024, L])
    w_in = din("w_in", [1024, 1288])
    vecs = din("vecs", [128, 38])
    rowc = din("rowc", [1, 1040])
    cst = din("cst", [128, 640])
    ynT = nc.dram_tensor("ynT", [512, L], BF16, kind="ExternalOutput").ap()
    NTL = L // 512
    with ExitStack() as st:
        k = K(nc, st)
        W = k.sbt([128, 8 * 1288], BF16, "W")
        W3 = v3(W.ap, 8)
        XT = [k.sbt([128, 8 * 512], F32, "XT%d" % i) for i in range(2)]
        UT = [k.sbt([128, 8 * 512], BF16, "UT%d" % i) for i in range(2)]
        SQ = k.sbt([128, 8 * 512], BF16, "SQ")
        LNV = k.sbt([128, 512], F32, "LNV")
        RSTD = k.sbt([128, 512], F32, "RSTD")
        vec = k.sbt([128, 38], F32, "vec_sb")
        bc = k.sbt([128, 1040], F32, "bc")
        cf = k.sbt([128, 640], F32, "cf")
        identb = k.sbt([128, 128], BF16, "identb")
        ones_bf = k.sbt([128, 128], BF16, "ones_bf")
        A_bc = k.sbt([128, 8], F32, "A_bc")
        XR = [k.sbt([128, 515], F32, "XR%d" % i) for i in range(6)]
        ACC = [k.sbt([128, 512], F32, "ACC%d" % i) for i in range(2)]
        XC = [k.sbt([128, 6 * 512], BF16, "XC%d" % i) for i in range(2)]
        ZS_2 = [k.sbt([128, 512], F32, "ZS_%d" % i) for i in range(3)]
        DTt_2 = [k.sbt([128, 8], F32, "DTt_%d" % i) for i in range(3)]
        DT_2 = [k.sbt([128, 8], F32, "DT_%d" % i) for i in range(3)]
        AA_2 = [k.sbt([128, 8], F32, "AA_%d" % i) for i in range(3)]
        EXPS_2 = [k.sbt([128, 24], F32, "EXPS_%d" % i) for i in range(3)]
        LH = [k.sbt([128, 128], F32, "LH%d" % i) for i in range(2)]
        DEC_2 = [k.sbt([128, 1024], F32, "DEC_%d" % i) for i in range(3)]
        CBm_2 = [k.sbt([128, 128], F32, "CBm_%d" % i) for i in range(3)]
        WT_2 = [k.sbt([128, 1024], BF16, "WT_%d" % i) for i in range(3)]
        XTOK_2 = [k.sbt([128, 512], BF16, "XTOK_%d" % i) for i in range(3)]
        BTOK_2 = [k.sbt([128, 128], BF16, "BTOK_%d" % i) for i in range(3)]
        XDT_2 = [k.sbt([128, 512], BF16, "XDT_%d" % i) for i in range(3)]
        XW_2 = [k.sbt([128, 512], BF16, "XW_%d" % i) for i in range(3)]
        Y1_2 = [k.sbt([128, 512], F32, "Y1_%d" % i) for i in range(3)]
        Y2_2 = [k.sbt([128, 512], F32, "Y2_%d" % i) for i in range(3)]
        YZ_2 = [k.sbt([128, 512], F32, "YZ_%d" % i) for i in range(3)]
        YSQ_2 = [k.sbt([128, 512], F32, "YSQ_%d" % i) for i in range(3)]
        YN_2 = [k.sbt([128, 512], BF16, "YN_%d" % i) for i in range(3)]
        SF = k.sbt([128, 512], F32, "SF")
        SBF = k.sbt([128, 512], BF16, "SBF")
        sc_2 = [[k.sbt([128, 1], F32, "sc%d_%d" % (j, i)) for i in range(3)] for j in range(3)]
        LH4 = [k.sbt([128, 128], F32, "LHx%d" % i) for i in range(2)]
        YNT = [k.sbt([128, 4 * 512], BF16, "YNT%d" % i) for i in range(2)]
        BK = [T(k.ps([128, 512], F32, "bk%d" % i)[:]) for i in range(8)]
        P_proj, P_st, P_sm, P_sg0, P_sg1, P_tr, P_yd, P_yo = BK
        ynT_t = T(ynT)

        k.dma("SP", vec.ap, vecs, writes=[vec])
        k.dma("SP", cf.ap, cst, writes=[cf])
        k.dma("SP", bc.ap, rowc.partition_broadcast(128).rearrange("p o n -> p (o n)"), writes=[bc])
        k.dma("POOL", W3, w_in.rearrange("(a p) n -> p a n", p=128), writes=[W])
        ident = cf.ap[:, 0:128]
        tri = cf.ap[:, 128:256]
        Um = cf.ap[:, 256:384]
        maskU = cf.ap[:, 384:512]
        onesf = cf.ap[:, 512:640]
        k.op("DVE", lambda e: e.tensor_copy(out=identb.ap, in_=ident), reads=[cf], writes=[identb])
        k.op("DVE", lambda e: e.memset(ones_bf.ap, 1.0), writes=[ones_bf])
        k.op("ACT", lambda e: e.activation(out=A_bc.ap, in_=bc.ap[:, 8:16], func=AF.Exp), reads=[bc], writes=[A_bc])
        k.op("DVE", lambda e: e.tensor_scalar(out=A_bc.ap, in0=A_bc.ap, scalar1=-1.0, scalar2=None, op0=ALU.mult), reads=[A_bc], writes=[A_bc])
        for cc in range(6):
            k.op("DVE", lambda e: e.memset(XR[cc].ap[:, 0:3], 0.0), writes=[XR[cc]])
        k.op("DVE", lambda e: e.memset(SF.ap, 0.0), writes=[SF])
        k.op("DVE", lambda e: e.memset(SBF.ap, 0.0), writes=[SBF])
        dtb, Dx, gn = bc.ap[:, 0:8], bc.ap[:, 16:528], bc.ap[:, 528:1040]

        def load_x(t):
            k.dma("SP", v3(XT[t % 2].ap, 8), xT[:, t * 512:(t + 1) * 512].rearrange("(a p) n -> p a n", p=128), writes=[XT[t % 2]])

        def stage_a(t):
            if t + 1 < NTL:
                load_x(t + 1)
            xt, ut, xc = XT[t % 2], UT[t % 2], XC[t % 2]
            x3, u3, xc3 = v3(xt.ap, 8), v3(ut.ap, 8), v3(xc.ap, 6)
            k.op("ACT", lambda e: e.activation(out=v3(SQ.ap, 8), in_=x3, func=AF.Square), reads=[xt], writes=[SQ])
            for kc in range(8):
                k.op("PE", lambda e: e.matmul(P_proj.ap, ones_bf.ap, v3(SQ.ap, 8)[:, kc, :], start=(kc == 0), stop=(kc == 7)),
                     reads=[SQ, ones_bf], writes=[P_proj], pe_acc=(kc > 0))
            k.op("ACT", lambda e: e.activation(out=LNV.ap, in_=P_proj.ap, func=AF.Ln, scale=1.0 / 1024, bias=EPS), reads=[P_proj], writes=[LNV])
            k.op("ACT", lambda e: e.activation(out=RSTD.ap, in_=LNV.ap, func=AF.Exp, scale=-0.5), reads=[LNV], writes=[RSTD])
            for kc in range(8):
                k.op("DVE", lambda e: e.scalar_tensor_tensor(out=u3[:, kc, :], in0=x3[:, kc, :], scalar=vec.ap[:, kc:kc + 1], in1=RSTD.ap,
                                                             op0=ALU.mult, op1=ALU.mult), reads=[xt, RSTD, vec], writes=[ut])
            for cc in range(6):
                for kc in range(8):
                    k.op("PE", lambda e: e.matmul(P_proj.ap, W3[:, kc, 512 + cc * 128:512 + (cc + 1) * 128], u3[:, kc, :], start=(kc == 0), stop=(kc == 7)),
                         reads=[W, ut], writes=[P_proj], pe_acc=(kc > 0))
                xr, acc = XR[cc], ACC[cc % 2]
                k.op("ACT", lambda e: e.activation(out=xr.ap[:, 3:515], in_=P_proj.ap, func=AF.Copy), reads=[P_proj], writes=[xr])
                k.op("DVE", lambda e: e.tensor_scalar(out=acc.ap, in0=xr.ap[:, 0:512], scalar1=vec.ap[:, 8 + cc * 4:9 + cc * 4], scalar2=None, op0=ALU.mult),
                     reads=[xr, vec], writes=[acc])
                for kk in range(1, 4):
                    k.op("DVE", lambda e: e.scalar_tensor_tensor(out=acc.ap, in0=xr.ap[:, kk:kk + 512], scalar=vec.ap[:, 8 + cc * 4 + kk:9 + cc * 4 + kk],
                                                                 in1=acc.ap, op0=ALU.mult, op1=ALU.add), reads=[xr, vec, acc], writes=[acc])
                k.op("ACT", lambda e: e.activation(out=xc3[:, cc, :], in_=acc.ap, func=AF.Silu, bias=vec.ap[:, 32 + cc:33 + cc], scale=1.0),
                     reads=[acc, vec], writes=[xc])
                k.op("DVE", lambda e: e.tensor_copy(out=xr.ap[:, 0:3], in_=xr.ap[:, 512:515]), reads=[xr], writes=[xr])

        def chunk_gen(t, q):
            ut, xc = UT[t % 2], XC[t % 2]
            u3, xc3 = v3(ut.ap, 8), v3(xc.ap, 6)
            ynt = YNT[t % 2]
            cs = slice(q * 128, (q + 1) * 128)
            ci = t * 4 + q
            ZS = ZS_2[ci % 3]
            DTt = DTt_2[ci % 3]
            DT = DT_2[ci % 3]
            AA = AA_2[ci % 3]
            EXPS = EXPS_2[ci % 3]
            DEC = DEC_2[ci % 3]
            CBm = CBm_2[ci % 3]
            WT = WT_2[ci % 3]
            XTOK = XTOK_2[ci % 3]
            BTOK = BTOK_2[ci % 3]
            XDT = XDT_2[ci % 3]
            XW = XW_2[ci % 3]
            Y1 = Y1_2[ci % 3]
            Y2 = Y2_2[ci % 3]
            YZ = YZ_2[ci % 3]
            YSQ = YSQ_2[ci % 3]
            YN = YN_2[ci % 3]
            sc = sc_2[ci % 3]
            for kc in range(8):
                k.op("PE", lambda e: e.matmul(P_proj.ap, u3[:, kc, cs], W3[:, kc, 0:512], start=(kc == 0), stop=(kc == 7)),
                     reads=[W, ut], writes=[P_proj], pe_acc=(kc > 0))
            k.op("ACT", lambda e: e.activation(out=ZS.ap, in_=P_proj.ap, func=AF.Silu), reads=[P_proj], writes=[ZS])
            yield
            for kc in range(8):
                k.op("PE", lambda e: e.matmul(P_sm.ap[:, 0:8], u3[:, kc, cs], W3[:, kc, 1280:1288], start=(kc == 0), stop=(kc == 7)),
                     reads=[W, ut], writes=[P_sm], pe_acc=(kc > 0))
            k.op("DVE", lambda e: e.tensor_tensor(out=DTt.ap, in0=P_sm.ap[:, 0:8], in1=dtb, op=ALU.add), reads=[P_sm, bc], writes=[DTt])
            k.op("ACT", lambda e: e.activation(out=DTt.ap, in_=DTt.ap, func=AF.Exp), reads=[DTt], writes=[DTt])
            k.op("ACT", lambda e: e.activation(out=DT.ap, in_=DTt.ap, func=AF.Ln, bias=1.0, scale=1.0), reads=[DTt], writes=[DT])
            k.op("DVE", lambda e: e.tensor_tensor(out=AA.ap, in0=DT.ap, in1=A_bc.ap, op=ALU.mult), reads=[DT, A_bc], writes=[AA])
            yield
            trb = P_tr.ap.bitcast(BF16)
            for cc in range(4):
                k.op("PE", lambda e: e.transpose(trb[:, cc * 128:(cc + 1) * 128], xc3[:, cc, cs], identb.ap), reads=[xc, identb], writes=[P_tr])
            k.op("PE", lambda e: e.transpose(trb[:, 512:640], xc3[:, 4, cs], identb.ap), reads=[xc, identb], writes=[P_tr])
            k.op("ACT", lambda e: e.activation(out=XTOK.ap, in_=trb[:, 0:512], func=AF.Copy), reads=[P_tr], writes=[XTOK])
            k.op("ACT", lambda e: e.activation(out=BTOK.ap, in_=trb[:, 512:640], func=AF.Copy), reads=[P_tr], writes=[BTOK])
            yield
            k.op("PE", lambda e: e.matmul(P_sm.ap[:, 32:40], tri, AA.ap, start=True, stop=True), reads=[cf, AA], writes=[P_sm])
            k.op("PE", lambda e: e.matmul(P_sm.ap[:, 40:48], Um, AA.ap, start=True, stop=True), reads=[cf, AA], writes=[P_sm])
            k.op("PE", lambda e: e.matmul(P_sm.ap[:, 48:56], onesf, AA.ap, start=True, stop=True), reads=[cf, AA], writes=[P_sm])
            k.op("ACT", lambda e: e.activation(out=EXPS.ap, in_=P_sm.ap[:, 32:56], func=AF.Exp), reads=[P_sm], writes=[EXPS])
            e_l, dte, cd = EXPS.ap[:, 0:8], EXPS.ap[:, 8:16], EXPS.ap[:, 16:24]
            yield
            k.op("PE", lambda e: e.matmul(P_sm.ap[:, 128:256], xc3[:, 4, cs], xc3[:, 5, cs], start=True, stop=True), reads=[xc], writes=[P_sm])
            k.op("DVE", lambda e: e.tensor_tensor(out=CBm.ap, in0=P_sm.ap[:, 128:256], in1=maskU, op=ALU.mult), reads=[P_sm, cf], writes=[CBm])
            yield
            for hh in range(8):
                lh = (LH + LH4)[hh % 4]
                k.op("DVE", lambda e: e.tensor_scalar(out=lh.ap, in0=Um, scalar1=AA.ap[:, hh:hh + 1], scalar2=None, op0=ALU.mult), reads=[cf, AA], writes=[lh])
                bank = P_sg0 if hh < 4 else P_sg1
                k.op("PE", lambda e: e.matmul(bank.ap[:, (hh % 4) * 128:(hh % 4 + 1) * 128], lh.ap, tri, start=True, stop=True), reads=[lh, cf], writes=[bank])
            k.op("ACT", lambda e: e.activation(out=DEC.ap[:, 0:512], in_=P_sg0.ap, func=AF.Exp), reads=[P_sg0], writes=[DEC])
            yield
            k.op("ACT", lambda e: e.activation(out=DEC.ap[:, 512:1024], in_=P_sg1.ap, func=AF.Exp), reads=[P_sg1], writes=[DEC])
            k.op("DVE", lambda e: e.tensor_tensor(out=v3(WT.ap, 8), in0=v3(DEC.ap, 8), in1=CBm.ap.unsqueeze(1).to_broadcast([128, 8, 128]), op=ALU.mult),
                 reads=[DEC, CBm], writes=[WT])
            k.op("DVE", lambda e: e.tensor_tensor(out=v3(XDT.ap, 8), in0=v3(XTOK.ap, 8), in1=DT.ap.unsqueeze(2).to_broadcast([128, 8, 64]), op=ALU.mult),
                 reads=[XTOK, DT], writes=[XDT])
            k.op("DVE", lambda e: e.tensor_tensor(out=v3(XW.ap, 8), in0=v3(XDT.ap, 8), in1=dte.unsqueeze(2).to_broadcast([128, 8, 64]), op=ALU.mult),
                 reads=[XDT, EXPS], writes=[XW])
            yield
            for hh in range(8):
                k.op("PE", lambda e: e.matmul(P_yd.ap[:, hh * 64:(hh + 1) * 64], v3(WT.ap, 8)[:, hh, :], v3(XDT.ap, 8)[:, hh, :], start=True, stop=True),
                     reads=[WT, XDT], writes=[P_yd])
            k.op("PE", lambda e: e.matmul(P_yo.ap, xc3[:, 5, cs], SBF.ap, start=True, stop=True), reads=[xc, SBF], writes=[P_yo])
            k.op("DVE", lambda e: e.tensor_tensor(out=v3(Y1.ap, 8), in0=v3(P_yo.ap, 8), in1=e_l.unsqueeze(2).to_broadcast([128, 8, 64]), op=ALU.mult),
                 reads=[P_yo, EXPS], writes=[Y1])
            k.op("DVE", lambda e: e.tensor_tensor(out=Y1.ap, in0=Y1.ap, in1=P_yd.ap, op=ALU.add), reads=[Y1, P_yd], writes=[Y1])
            yield
            k.op("PE", lambda e: e.matmul(P_st.ap, BTOK.ap, XW.ap, start=True, stop=True), reads=[BTOK, XW], writes=[P_st])
            k.op("DVE", lambda e: e.tensor_tensor(out=v3(SF.ap, 8), in0=v3(SF.ap, 8), in1=cd.unsqueeze(2).to_broadcast([128, 8, 64]), op=ALU.mult),
                 reads=[SF, EXPS], writes=[SF])
            k.op("DVE", lambda e: e.tensor_tensor(out=SF.ap, in0=SF.ap, in1=P_st.ap, op=ALU.add), reads=[SF, P_st], writes=[SF])
            k.op("ACT", lambda e: e.activation(out=SBF.ap, in_=SF.ap, func=AF.Copy), reads=[SF], writes=[SBF])
            yield
            k.op("DVE", lambda e: e.tensor_tensor(out=Y2.ap, in0=XTOK.ap, in1=Dx, op=ALU.mult), reads=[XTOK, bc], writes=[Y2])
            k.op("DVE", lambda e: e.tensor_tensor(out=Y2.ap, in0=Y2.ap, in1=Y1.ap, op=ALU.add), reads=[Y2, Y1], writes=[Y2])
            k.op("DVE", lambda e: e.tensor_tensor(out=YZ.ap, in0=Y2.ap, in1=ZS.ap, op=ALU.mult), reads=[Y2, ZS], writes=[YZ])
            k.op("ACT", lambda e: e.activation(out=YSQ.ap, in_=YZ.ap, func=AF.Square, accum_out=sc[0].ap), reads=[YZ], writes=[YSQ, sc[0]])
            k.op("ACT", lambda e: e.activation(out=sc[1].ap, in_=sc[0].ap, func=AF.Ln, scale=1.0 / 512, bias=EPS), reads=[sc[0]], writes=[sc[1]])
            k.op("ACT", lambda e: e.activation(out=sc[2].ap, in_=sc[1].ap, func=AF.Exp, scale=-0.5), reads=[sc[1]], writes=[sc[2]])
            k.op("DVE", lambda e: e.scalar_tensor_tensor(out=YN.ap, in0=YZ.ap, scalar=sc[2].ap, in1=gn, op0=ALU.mult, op1=ALU.mult),
                 reads=[YZ, sc[2], bc], writes=[YN])
            yield
            for cc in range(4):
                k.op("PE", lambda e: e.transpose(trb[:, cc * 128:(cc + 1) * 128], YN.ap[:, cc * 128:(cc + 1) * 128], identb.ap), reads=[YN, identb], writes=[P_tr])
            k.op("ACT", lambda e: e.activation(out=v3(ynt.ap, 4)[:, :, cs], in_=trb[:, 0:512].rearrange("p (a n) -> p a n", a=4), func=AF.Copy),
                 reads=[P_tr], writes=[ynt])
            if q == 3:
                k.dma("SP", ynT[:, t * 512:(t + 1) * 512].rearrange("(a p) n -> p a n", p=128), v3(ynt.ap, 4), reads=[ynt], writes=[ynT_t], part=True)


        load_x(0)
        NS, PER = 12, 6
        chunks = [(t, q) for t in range(NTL) for q in range(4)]
        gens = {}
        nslot = (len(chunks) - 1) * PER + NS + 2
        for tau in range(nslot):
            for ci, (t, q) in enumerate(chunks):
                st_ = tau - ci * PER
                if st_ < 0:
                    break
                if ci in gens and gens[ci] is None:
                    continue
                if ci not in gens:
                    if q == 0:
                        stage_a(t)
                    gens[ci] = chunk_gen(t, q)
                try:
                    next(gens[ci])
                except StopIteration:
                    gens[ci] = None
        k.finish("SP", [ynT_t])
        print("ssd ninst", k.ninst)
    return nc


def ssd_consts():
    s = np.arange(128)
    ident = np.eye(128, dtype=np.float32)
    tri = (s[:, None] <= s[None, :]).astype(np.float32)
    U = (s[:, None] > s[None, :]).astype(np.float32)
    maskU = (s[None, :] >= s[:, None]).astype(np.float32)
    ones = np.ones((128, 128), np.float32)
    return np.ascontiguousarray(np.concatenate([ident, tri, U, maskU, ones], axis=1))


def ssd_host_inputs(g, ssd_norm, w_in, conv_w, conv_b, dt_bias, a_log, d_skip, gnorm):
    cols = np.concatenate([np.arange(g * 512, (g + 1) * 512), 2048 + np.arange(g * 512, (g + 1) * 512),
                           4096 + np.arange(g * 128, (g + 1) * 128), 4096 + 512 + np.arange(g * 128, (g + 1) * 128),
                           5120 + np.arange(g * 8, (g + 1) * 8)])
    w = np.ascontiguousarray(w_in[:, cols])
    ch = np.concatenate([np.arange(g * 512, (g + 1) * 512), 2048 + np.arange(g * 128, (g + 1) * 128), 2048 + 512 + np.arange(g * 128, (g + 1) * 128)])
    cw = conv_w[:, ch]
    cb = conv_b[ch]
    vecs = np.zeros((128, 38), np.float32)
    vecs[:, 0:8] = ssd_norm.reshape(8, 128).T
    for cc in range(6):
        vecs[:, 8 + cc * 4:12 + cc * 4] = cw[:, cc * 128:(cc + 1) * 128].T
        vecs[:, 32 + cc] = cb[cc * 128:(cc + 1) * 128]
    rowc = np.concatenate([dt_bias[g * 8:(g + 1) * 8], a_log[g * 8:(g + 1) * 8], np.repeat(d_skip[g * 8:(g + 1) * 8], 64),
                           gnorm[g * 512:(g + 1) * 512]])[None, :].astype(np.float32)
    return w, vecs, np.ascontiguousarray(rowc)


_CACHE = {}


def _prog(name, fn, *a):
    key = (name,) + a
    if key not in _CACHE:
        _CACHE[key] = fn(*a)
    return _CACHE[key]


def _vcol(v):
    return np.asarray(v, np.float32).reshape(8, 128).T


def kernel(x, p, ssd_norm, ssd_w_in, ssd_conv_w, ssd_conv_b, ssd_dt_bias, ssd_a_log, ssd_d, ssd_gnorm, ssd_w_out,
           sb_norm, sb_w_qkv, sb_w_o, moe_norm, moe_w_rg, moe_b_rg, moe_w_re, moe_b_re, moe_w_gate, moe_w_up, moe_w_down,
           ple_norm, ple_w_gate, ple_b_gate, ple_w_proj, final_norm):
    f32 = lambda a: np.ascontiguousarray(np.asarray(a, dtype=np.float32))
    x, p = f32(x), f32(p)
    Bsz, L, D = x.shape
    NC = 8
    NT = (Bsz * L) // NC
    PB = NC // Bsz
    cores = list(range(NC))
    ident = np.eye(128, dtype=np.float32)

    nc1 = _prog("ssd", build_ssd, L)
    cst1 = ssd_consts()
    xTb = [np.ascontiguousarray(x[b].T) for b in range(Bsz)]
    maps = []
    for c in cores:
        b, g = c // PB, c % PB
        w, vecs, rowc = ssd_host_inputs(g, f32(ssd_norm[0]), f32(ssd_w_in[0]), f32(ssd_conv_w[0]), f32(ssd_conv_b[0]),
                                        f32(ssd_dt_bias[0]), f32(ssd_a_log[0]), f32(ssd_d[0]), f32(ssd_gnorm[0]))
        maps.append(dict(xT=xTb[b], w_in=w, vecs=vecs, rowc=rowc, cst=cst1))
    r1 = run_bass_kernel_spmd(nc1, maps, core_ids=cores).results
    ynT = [np.concatenate([r1[b * PB + g]["ynT"] for g in range(PB)], axis=0) for b in range(Bsz)]

    mcst = moe_consts(NT)

    def tok_maps(i, hT_list, aT_list, w_a, tail_norm, extra):
        vecs = np.ascontiguousarray(np.concatenate([_vcol(moe_norm[i]), _vcol(ple_norm[i]), _vcol(ple_b_gate[i]), _vcol(tail_norm)], axis=1))
        wr = np.ascontiguousarray(np.concatenate([f32(moe_w_rg[i]), f32(moe_w_re[i])], axis=1))
        br = np.ascontiguousarray(np.concatenate([f32(moe_b_rg[i]), f32(moe_b_re[i])])[None, :])
        wg_, wu_, wd_ = f32(moe_w_gate[i]), f32(moe_w_up[i]), f32(moe_w_down[i])
        plg_, plp_ = f32(ple_w_gate[i]), f32(ple_w_proj[i])
        out = []
        for c in cores:
            b, j = c // PB, c % PB
            sl = slice(j * NT, (j + 1) * NT)
            m = dict(hT=hT_list[c], aT=np.ascontiguousarray(aT_list[b][:, sl]), w_a=w_a, vecs=vecs, wr=wr, br=br, wg=wg_, wu=wu_, wd=wd_,
                     plg=plg_, plp=plp_, pT=np.ascontiguousarray(p[i, b, sl].T), mcst=mcst)
            m.update(extra)
            out.append(m)
        return out

    nc2 = _prog("tokA", build_tok2, NT, 2048, "A")
    hT0 = [np.ascontiguousarray(x[c // PB, (c % PB) * NT:(c % PB + 1) * NT].T) for c in cores]
    r2 = run_bass_kernel_spmd(nc2, tok_maps(0, hT0, ynT, f32(ssd_w_out[0]), f32(sb_norm[0]), dict(wqkv=f32(sb_w_qkv[0]))), core_ids=cores).results
    hT1 = [r2[c]["hT_o"] for c in cores]
    qTb = [np.concatenate([r2[b * PB + j]["qT_o"] for j in range(PB)], axis=1) for b in range(Bsz)]
    kTb = [np.concatenate([r2[b * PB + j]["kT_o"] for j in range(PB)], axis=1) for b in range(Bsz)]
    vb = [np.concatenate([r2[b * PB + j]["v_o"] for j in range(PB)], axis=0) for b in range(Bsz)]

    nc3 = _prog("attn", build_attn2, L)
    cst3 = attn_consts()
    maps = []
    for c in cores:
        b, hp = c // PB, c % PB
        rows = slice(hp * 256, (hp + 1) * 256)
        vP = np.ascontiguousarray(vb[b][:, rows].reshape(L // 128, 128, 2, 128).transpose(2, 1, 0, 3))
        maps.append(dict(qT=np.ascontiguousarray(qTb[b][rows].reshape(2, 128, L)), kT=np.ascontiguousarray(kTb[b][rows].reshape(2, 128, L)),
                         vP=vP, cst=cst3))
    r3 = run_bass_kernel_spmd(nc3, maps, core_ids=cores).results
    oT = [np.concatenate([r3[b * PB + hp]["oT"].reshape(256, L) for hp in range(PB)], axis=0) for b in range(Bsz)]

    nc4 = _prog("tokB", build_tok2, NT, 1024, "B")
    r4 = run_bass_kernel_spmd(nc4, tok_maps(1, hT1, oT, f32(sb_w_o[0]), f32(final_norm), {}), core_ids=cores).results
    out = np.empty((Bsz, L, D), np.float32)
    for c in cores:
        b, j = c // PB, c % PB
        out[b, j * NT:(j + 1) * NT, :] = r4[c]["out_o"].T
    return out


I32 = mybir.dt.int32
MB = 128
NBLK_OF = lambda NT: (2 * NT) // MB + 64


def moe_consts(NT):
    nblk = NBLK_OF(NT)
    s = np.arange(128)
    SL = (s[:, None] < s[None, :]).astype(np.float32)
    THR = np.tile((np.arange(64) * MB).astype(np.float32)[None, :], (128, 1))
    JB = np.tile((np.arange(nblk) * MB).astype(np.float32)[None, :], (128, 1))
    KP = (np.arange(8)[None, :] + 2 * s[:, None]).astype(np.float32)
    return np.ascontiguousarray(np.concatenate([np.eye(128, dtype=np.float32), SL, THR, JB, KP], axis=1))


def idma(self, out_ap, in_ap, idx_t, idx_ap, gather, reads=(), writes=(), part=False, bound=None):
    t = writes[0]
    if t.dsem is None:
        key = "d%d" % self.ndsem
        t.dsem = self.stack.enter_context(self.nc.semaphore(key))
        self.semobj[key] = t.dsem
        t.name = key
        self.ndsem += 1
    key = t.name
    for r in list(reads) + [idx_t]:
        if r.w is not None:
            self._need("POOL", *r.w)
    if t.w is not None and not (part and t.w[0] == key):
        self._need("POOL", *t.w)
    for kk, v in t.r.items():
        self._need("POOL", kk, v)
    off = bass.IndirectOffsetOnAxis(ap=idx_ap, axis=0)
    if gather and bound is not None:
        ins = self.nc.gpsimd.indirect_dma_start(out=out_ap, out_offset=None, in_=in_ap, in_offset=off, bounds_check=bound, oob_is_err=False)
    elif gather:
        ins = self.nc.gpsimd.indirect_dma_start(out=out_ap, out_offset=None, in_=in_ap, in_offset=off)
    else:
        ins = self.nc.gpsimd.indirect_dma_start(out=out_ap, out_offset=off, in_=in_ap, in_offset=None)
    t.dcnt += 16
    ins.then_inc(t.dsem, 16)
    for r in list(reads) + [idx_t]:
        r.r[key] = max(r.r.get(key, 0), t.dcnt)
    t.w = (key, t.dcnt)
    if not part:
        t.r = {}
    self.ninst += 1
    return ins


K.idma = idma


def build_tok2(NT, KA, mode):
    nc = bass.Bass("TRN2", target_bir_lowering=False)

    def din(name, shape, dt=F32):
        return nc.dram_tensor(name, list(shape), dt, kind="ExternalInput").ap()

    def dout(name, shape, dt=F32):
        return nc.dram_tensor(name, list(shape), dt, kind="ExternalOutput").ap()

    NBLK = NBLK_OF(NT)
    NROWS = NBLK * MB
    NTB = NT // 128
    NTILE = NT // 512
    hT = din("hT", [1024, NT])
    aT = din("aT", [KA, NT], BF16)
    w_a = din("w_a", [KA, 1024])
    vecs = din("vecs", [128, 32])
    wr = din("wr", [1024, 72])
    br = din("br", [1, 72])
    wg = din("wg", [64, 1024, 512])
    wu = din("wu", [64, 1024, 512])
    wd = din("wd", [64, 512, 1024])
    plg = din("plg", [1024, 1024])
    plp = din("plp", [256, 1024])
    pT = din("pT", [256, NT])
    NCST = 256 + 64 + NBLK + 8
    mcst = din("mcst", [128, NCST])
    if mode == "A":
        wqkv = din("wqkv", [1024, 3072])
        hT_o = dout("hT_o", [1024, NT])
        qT_o = dout("qT_o", [1024, NT], BF16)
        kT_o = dout("kT_o", [1024, NT], BF16)
        v_o = dout("v_o", [NT, 1024], BF16)
    else:
        out_o = dout("out_o", [1024, NT])
    H1 = nc.dram_tensor("H1s", [1024, NT], F32, kind="Internal").ap()
    Xs = nc.dram_tensor("Xs", [NROWS, 1024], BF16, kind="Internal").ap()
    Ys = nc.dram_tensor("Ys", [NROWS, 1024], F32, kind="Internal").ap()
    wg_r = wg.rearrange("e (p h r) n -> (e p h) (r n)", p=128, h=2, r=4)
    wu_r = wu.rearrange("e (p h r) n -> (e p h) (r n)", p=128, h=2, r=4)
    wd_r = wd.rearrange("e (p h r) n -> (e p h) (r n)", p=128, h=2, r=2)
    KAC = KA // 128

    with ExitStack() as st:
        k = K(nc, st)
        HTL = [k.sbt([128, 8 * 512], F32, "htl%d" % i) for i in range(1)] * 2
        UTL = [k.sbt([128, 8 * 512], BF16, "utl%d" % i) for i in range(1)] * 2
        WB = [k.sbt([128, 12288], BF16, "wb%d" % i) for i in range(2)]
        RA = k.sb([128, 8192], BF16, "regA")
        RAf = RA[:].bitcast(F32)
        ABS = [T(RA[:, 0:KAC * 512])]
        BIG = T(RAf[:, 0:2048])
        IGF = T(RAf[:, 2048:3072])
        XB = [T(RA[:, i * 1024:(i + 1) * 1024]) for i in range(2)]
        XTB = [T(RA[:, 2048 + i * 1024:2048 + (i + 1) * 1024]) for i in range(2)]
        YB = [T(RAf[:, 2048 + i * 1024:2048 + (i + 1) * 1024]) for i in range(2)]
        NRX = max(NTB * 1024, 32768)
        RX = k.sb([128, NRX], BF16, "regX")
        RXf = RX[:].bitcast(F32)
        XROWS = T(RX[:, 0:NTB * 1024])
        XR3 = v3(XROWS.ap, NTB)
        WQ = [T(RX[:, i * 12288:(i + 1) * 12288]) for i in range(2)]
        WBX = [T(RX[:, i * 12288:(i + 1) * 12288]) for i in range(2)]
        Y0 = [T(RXf[:, 12288 + i * 1024:12288 + (i + 1) * 1024]) for i in range(2)]
        Y1 = [T(RXf[:, 14336 + i * 1024:14336 + (i + 1) * 1024]) for i in range(2)]
        PBt = k.sbt([128, 2 * 512], BF16, "pbt")
        LNV = k.sbt([128, 512], F32, "lnv")
        RSTD = k.sbt([128, 512], F32, "rstd")
        vec = k.sbt([128, 32], F32, "vec_sb")
        cf = k.sbt([128, NCST], F32, "cf")
        ident = cf.ap[:, 0:128]
        identb = k.sbt([128, 128], BF16, "identb")
        SLb = k.sbt([128, 128], BF16, "SLb")
        ones_bf = k.sbt([128, 128], BF16, "ones_bf")
        wr_sb = k.sbt([128, 8 * 72], F32, "wr_sb")
        br_bc = k.sbt([128, 72], F32, "brbc")
        STG = [k.sbt([128, 512], BF16, "stg%d" % i) for i in range(2)]
        STF = [k.sbt([128, 512], F32, "stf%d" % i) for i in range(2)]
        r_lg = k.sbt([128, 72], F32, "r_lg")
        r_a = k.sbt([128, 8], F32, "r_a")
        r_goh = k.sbt([128, 8], F32, "r_goh")
        r_t64 = k.sbt([128, 64], F32, "r_t64")
        r_es = k.sbt([128, 8], F32, "r_es")
        r_es2 = k.sbt([128, 8], F32, "r_es2")
        r_oh1 = k.sbt([128, 8], F32, "r_oh1")
        r_oh2 = k.sbt([128, 8], F32, "r_oh2")
        r_s = [k.sbt([128, 1], F32, "r_s%d" % i) for i in range(12)]
        OHC = k.sbt([128, 128], BF16, "ohc")
        OHS = k.sbt([128, NTB * 128], BF16, "ohs")
        OHS3 = v3(OHS.ap, NTB)
        RUN = k.sbt([128, 128], F32, "run")
        PRS = k.sbt([128, 128], F32, "prs")
        JNK = k.sbt([128, 64], F32, "jnk")
        RK0 = k.sbt([128, NTB], F32, "rk0")
        RK1 = k.sbt([128, NTB], F32, "rk1")
        GA = k.sbt([128, NTB], F32, "ga")
        GB = k.sbt([128, NTB], F32, "gb")
        DI0 = k.sbt([128, NTB], I32, "di0")
        DI1 = k.sbt([128, NTB], I32, "di1")
        CNT = k.sbt([128, 64], F32, "cnt")
        NB_ = k.sbt([128, 64], F32, "nb_")
        PE_ = [k.sbt([128, 64], F32, "pe%d" % i) for i in range(2)]
        BASE1 = k.sbt([128, 64], F32, "base1")
        BASE2 = k.sbt([128, 64], F32, "base2")
        DF = k.sbt([128, NTB], F32, "df")
        EJ = k.sbt([128, NBLK], F32, "ej")
        SAME = k.sbt([128, NBLK], F32, "same")
        IGI = k.sbt([128, NBLK * 8], I32, "igi")
        IDI = k.sbt([128, NBLK * 4], I32, "idi")
        SS = [k.sbt([128, 512], BF16, "ss%d" % i) for i in range(2)]
        HH = [k.sbt([128, 512], BF16, "hh%d" % i) for i in range(2)]
        BK = [T(k.ps([128, 512], F32, "bk%d" % i)[:]) for i in range(8)]
        PG, PU, PD, PBK = BK[0:2], BK[2:4], BK[4:6], BK[6:8]
        H1_t, Xs_t, Ys_t = T(H1), T(Xs), T(Ys)
        if mode == "A":
            qT_t, kT_t, v_t, ho_t = T(qT_o), T(kT_o), T(v_o), T(hT_o)
        else:
            out_t = T(out_o)

        k.dma("SP", vec.ap, vecs, writes=[vec])
        k.dma("SP", cf.ap, mcst, writes=[cf])
        k.dma("SP", v3(wr_sb.ap, 8), wr.rearrange("(a p) n -> p a n", p=128), writes=[wr_sb])
        k.dma("SP", br_bc.ap, br.partition_broadcast(128).rearrange("p o n -> p (o n)"), writes=[br_bc])
        k.op("DVE", lambda e: e.memset(ones_bf.ap, 1.0), writes=[ones_bf])
        k.op("DVE", lambda e: e.memset(RUN.ap, 0.0), writes=[RUN])
        k.op("DVE", lambda e: e.tensor_copy(out=identb.ap, in_=ident), reads=[cf], writes=[identb])
        k.op("DVE", lambda e: e.tensor_copy(out=SLb.ap, in_=cf.ap[:, 128:256]), reads=[cf], writes=[SLb])
        THR = cf.ap[:, 256:320]
        JB = cf.ap[:, 320:320 + NBLK]
        KP = cf.ap[:, 320 + NBLK:328 + NBLK]
        for kc in range(8):
            k.op("DVE", lambda e: e.tensor_scalar(out=wr_sb.ap[:, kc * 72:(kc + 1) * 72], in0=wr_sb.ap[:, kc * 72:(kc + 1) * 72],
                                                  scalar1=vec.ap[:, kc:kc + 1], scalar2=None, op0=ALU.mult), reads=[vec, wr_sb], writes=[wr_sb])

        def rmsnorm(src_t, src_ap, gcol, dst_t, dst_ap, bank):
            k.op("ACT", lambda e: e.activation(out=dst_ap, in_=src_ap, func=AF.Square), reads=[src_t], writes=[dst_t])
            for kc in range(8):
                k.op("PE", lambda e: e.matmul(bank.ap, ones_bf.ap, dst_ap[:, kc, :], start=(kc == 0), stop=(kc == 7)),
                     reads=[dst_t, ones_bf], writes=[bank], pe_acc=(kc > 0))
            k.op("ACT", lambda e: e.activation(out=LNV.ap, in_=bank.ap, func=AF.Ln, scale=1.0 / 1024, bias=EPS), reads=[bank], writes=[LNV])
            k.op("ACT", lambda e: e.activation(out=RSTD.ap, in_=LNV.ap, func=AF.Exp, scale=-0.5), reads=[LNV], writes=[RSTD])
            for kc in range(8):
                k.op("DVE", lambda e: e.scalar_tensor_tensor(out=dst_ap[:, kc, :], in0=src_ap[:, kc, :], scalar=vec.ap[:, gcol + kc:gcol + kc + 1],
                                                             in1=RSTD.ap, op0=ALU.mult, op1=ALU.mult), reads=[src_t, RSTD, vec], writes=[dst_t])

        for kp in range(KAC // 8):
            k.dma("POOL", v3(WB[kp].ap[:, 0:8192], 8), w_a[kp * 1024:(kp + 1) * 1024, :].rearrange("(a p) n -> p a n", p=128), writes=[WB[kp]])
        for t in range(NTILE):
            cs = slice(t * 512, (t + 1) * 512)
            ht, ut, AB = HTL[t % 2], UTL[t % 2], ABS[0]
            h3, u3 = v3(ht.ap, 8), v3(ut.ap, 8)
            k.dma("SP", h3, hT[:, cs].rearrange("(a p) n -> p a n", p=128), writes=[ht])
            k.dma("SP", v3(AB.ap, KAC), aT[:, cs].rearrange("(a p) n -> p a n", p=128), writes=[AB])
            for d in range(8):
                bank = PD[d % 2]
                for kc in range(KAC):
                    wbt = WB[kc // 8]
                    k.op("PE", lambda e: e.matmul(bank.ap, v3(wbt.ap[:, 0:8192], 8)[:, kc % 8, d * 128:(d + 1) * 128], v3(AB.ap, KAC)[:, kc, :],
                                                  start=(kc == 0), stop=(kc == KAC - 1)), reads=[wbt, AB], writes=[bank], pe_acc=(kc > 0))
                k.op("DVE", lambda e: e.tensor_tensor(out=h3[:, d, :], in0=h3[:, d, :], in1=bank.ap, op=ALU.add), reads=[bank, ht], writes=[ht])
            k.dma("SP", H1[:, cs].rearrange("(a p) n -> p a n", p=128), h3, reads=[ht], writes=[H1_t], part=True)
            rmsnorm(ht, h3, 0, ut, u3, PBK[0])
            for b in range(4):
                i = t * 4 + b
                bs = slice(b * 128, (b + 1) * 128)
                plg_b, prs = PG[b % 2], PU[b % 2]
                for kc in range(8):
                    k.op("PE", lambda e: e.matmul(plg_b.ap[:, 0:72], h3[:, kc, bs], wr_sb.ap[:, kc * 72:(kc + 1) * 72], start=(kc == 0), stop=(kc == 7)),
                         reads=[ht, wr_sb], writes=[plg_b], pe_acc=(kc > 0))
                k.op("PE", lambda e: e.matmul(prs.ap[:, 0:2], RSTD.ap[:, bs], ident[:, 0:2], start=True, stop=True), reads=[RSTD, cf], writes=[prs])
                rt, gmax, ngmax, gsum, gw, m1, m2, dd, ex, w1, w2 = r_s[0:11]
                k.op("DVE", lambda e: e.tensor_copy(out=rt.ap, in_=prs.ap[:, 0:1]), reads=[prs], writes=[rt])
                k.op("DVE", lambda e: e.scalar_tensor_tensor(out=r_lg.ap, in0=plg_b.ap[:, 0:72], scalar=rt.ap, in1=br_bc.ap, op0=ALU.mult, op1=ALU.add),
                     reads=[plg_b, rt, br_bc], writes=[r_lg])
                gl = r_lg.ap[:, 0:8]
                el = r_lg.ap[:, 8:72]
                k.op("DVE", lambda e: e.tensor_reduce(out=gmax.ap, in_=gl, axis=AX.X, op=ALU.max), reads=[r_lg], writes=[gmax])
                k.op("DVE", lambda e: e.tensor_scalar(out=r_goh.ap, in0=gl, scalar1=gmax.ap, scalar2=None, op0=ALU.is_equal), reads=[r_lg, gmax], writes=[r_goh])
                k.op("DVE", lambda e: e.tensor_scalar(out=ngmax.ap, in0=gmax.ap, scalar1=-1.0, scalar2=None, op0=ALU.mult), reads=[gmax], writes=[ngmax])
                k.op("ACT", lambda e: e.activation(out=r_a.ap, in_=gl, func=AF.Exp, bias=ngmax.ap, scale=1.0, accum_out=gsum.ap), reads=[r_lg, ngmax], writes=[r_a, gsum])
                k.op("DVE", lambda e: e.reciprocal(out=gw.ap, in_=gsum.ap), reads=[gsum], writes=[gw])
                k.op("DVE", lambda e: e.tensor_tensor(out=v3(r_t64.ap, 8), in0=v3(el, 8), in1=r_goh.ap.unsqueeze(2).to_broadcast([128, 8, 8]), op=ALU.mult),
                     reads=[r_lg, r_goh], writes=[r_t64])
                k.op("DVE", lambda e: e.tensor_reduce(out=r_es.ap, in_=v3(r_t64.ap, 8).rearrange("p g j -> p j g"), axis=AX.X, op=ALU.add), reads=[r_t64], writes=[r_es])
                k.op("DVE", lambda e: e.tensor_reduce(out=m1.ap, in_=r_es.ap, axis=AX.X, op=ALU.max), reads=[r_es], writes=[m1])
                k.op("DVE", lambda e: e.tensor_scalar(out=r_oh1.ap, in0=r_es.ap, scalar1=m1.ap, scalar2=None, op0=ALU.is_equal), reads=[r_es, m1], writes=[r_oh1])
                k.op("DVE", lambda e: e.scalar_tensor_tensor(out=r_es2.ap, in0=r_oh1.ap, scalar=-1e30, in1=r_es.ap, op0=ALU.mult, op1=ALU.add), reads=[r_oh1, r_es], writes=[r_es2])
                k.op("DVE", lambda e: e.tensor_reduce(out=m2.ap, in_=r_es2.ap, axis=AX.X, op=ALU.max), reads=[r_es2], writes=[m2])
                k.op("DVE", lambda e: e.tensor_scalar(out=r_oh2.ap, in0=r_es2.ap, scalar1=m2.ap, scalar2=None, op0=ALU.is_equal), reads=[r_es2, m2], writes=[r_oh2])
                k.op("DVE", lambda e: e.tensor_tensor(out=dd.ap, in0=m2.ap, in1=m1.ap, op=ALU.subtract), reads=[m1, m2], writes=[dd])
                k.op("ACT", lambda e: e.activation(out=ex.ap, in_=dd.ap, func=AF.Exp), reads=[dd], writes=[ex])
                k.op("DVE", lambda e: e.tensor_scalar(out=w1.ap, in0=ex.ap, scalar1=1.0, scalar2=None, op0=ALU.add), reads=[ex], writes=[w1])
                k.op("DVE", lambda e: e.reciprocal(out=w1.ap, in_=w1.ap), reads=[w1], writes=[w1])
                k.op("DVE", lambda e: e.tensor_tensor(out=w2.ap, in0=ex.ap, in1=w1.ap, op=ALU.mult), reads=[ex, w1], writes=[w2])
                k.op("DVE", lambda e: e.tensor_tensor(out=GA.ap[:, i:i + 1], in0=w1.ap, in1=gw.ap, op=ALU.mult), reads=[w1, gw], writes=[GA])
                k.op("DVE", lambda e: e.tensor_tensor(out=GB.ap[:, i:i + 1], in0=w2.ap, in1=gw.ap, op=ALU.mult), reads=[w2, gw], writes=[GB])
                k.op("DVE", lambda e: e.tensor_tensor(out=v3(OHC.ap[:, 0:64], 8), in0=r_goh.ap.unsqueeze(2).to_broadcast([128, 8, 8]),
                                                      in1=r_oh1.ap.unsqueeze(1).to_broadcast([128, 8, 8]), op=ALU.mult), reads=[r_goh, r_oh1], writes=[OHC])
                k.op("DVE", lambda e: e.tensor_tensor(out=v3(OHC.ap[:, 64:128], 8), in0=r_goh.ap.unsqueeze(2).to_broadcast([128, 8, 8]),
                                                      in1=r_oh2.ap.unsqueeze(1).to_broadcast([128, 8, 8]), op=ALU.mult), reads=[r_goh, r_oh2, OHC], writes=[OHC])
                k.op("DVE", lambda e: e.tensor_copy(out=OHS3[:, i, :], in_=OHC.ap), reads=[OHC], writes=[OHS])
                ppr, pcs = PD[0], PD[1]
                k.op("PE", lambda e: e.matmul(ppr.ap[:, 0:128], SLb.ap, OHC.ap, start=True, stop=True), reads=[SLb, OHC], writes=[ppr])
                k.op("PE", lambda e: e.matmul(pcs.ap[:, 0:128], ones_bf.ap, OHC.ap, start=True, stop=True), reads=[ones_bf, OHC], writes=[pcs])
                k.op("DVE", lambda e: e.tensor_tensor(out=PRS.ap, in0=ppr.ap[:, 0:128], in1=RUN.ap, op=ALU.add), reads=[ppr, RUN], writes=[PRS])
                k.op("DVE", lambda e: e.tensor_tensor(out=PRS.ap, in0=PRS.ap, in1=OHC.ap, op=ALU.mult), reads=[PRS, OHC], writes=[PRS])
                k.op("DVE", lambda e: e.tensor_reduce(out=RK0.ap[:, i:i + 1], in_=PRS.ap[:, 0:64], axis=AX.X, op=ALU.add), reads=[PRS], writes=[RK0])
                k.op("DVE", lambda e: e.tensor_reduce(out=RK1.ap[:, i:i + 1], in_=PRS.ap[:, 64:128], axis=AX.X, op=ALU.add), reads=[PRS], writes=[RK1])
                k.op("DVE", lambda e: e.tensor_tensor(out=RUN.ap, in0=RUN.ap, in1=pcs.ap[:, 0:128], op=ALU.add), reads=[pcs, RUN], writes=[RUN])
                trb = PBK[1].ap.bitcast(BF16)
                for kc in range(8):
                    k.op("PE", lambda e: e.transpose(trb[:, kc * 128:(kc + 1) * 128], u3[:, kc, bs], identb.ap), reads=[ut, identb], writes=[PBK[1]])
                k.op("ACT", lambda e: e.activation(out=XR3[:, i, :], in_=trb, func=AF.Copy), reads=[PBK[1]], writes=[XROWS])

        k.alias([BIG, IGF], ABS)
        cnt1, cnt2 = RUN.ap[:, 0:64], RUN.ap[:, 64:128]
        k.op("DVE", lambda e: e.tensor_tensor(out=CNT.ap, in0=cnt1, in1=cnt2, op=ALU.add), reads=[RUN], writes=[CNT])
        big_cm = BIG.ap[:, 0:2048].rearrange("p (a m) -> p a m", a=64)
        for mh in range(2):
            k.op("DVE", lambda e: e.tensor_tensor(out=big_cm, in0=CNT.ap.unsqueeze(2).to_broadcast([128, 64, 32]),
                                                  in1=THR[:, mh * 32:(mh + 1) * 32].unsqueeze(1).to_broadcast([128, 64, 32]), op=ALU.is_gt), reads=[CNT, cf], writes=[BIG])
            dstn = NB_ if mh == 0 else BASE1
            k.op("DVE", lambda e: e.tensor_reduce(out=dstn.ap, in_=big_cm, axis=AX.X, op=ALU.add), reads=[BIG], writes=[dstn])
        k.op("DVE", lambda e: e.tensor_tensor(out=NB_.ap, in0=NB_.ap, in1=BASE1.ap, op=ALU.add), reads=[NB_, BASE1], writes=[NB_])
        k.op("DVE", lambda e: e.tensor_scalar(out=NB_.ap, in0=NB_.ap, scalar1=float(MB), scalar2=None, op0=ALU.mult), reads=[NB_], writes=[NB_])
        k.op("DVE", lambda e: e.tensor_copy(out=PE_[0].ap, in_=NB_.ap), reads=[NB_], writes=[PE_[0]])
        cur = 0
        for sft in (1, 2, 4, 8, 16, 32):
            a_, b_ = PE_[cur], PE_[1 - cur]
            k.op("DVE", lambda e: e.tensor_copy(out=b_.ap[:, 0:sft], in_=a_.ap[:, 0:sft]), reads=[a_], writes=[b_])
            k.op("DVE", lambda e: e.tensor_tensor(out=b_.ap[:, sft:64], in0=a_.ap[:, sft:64], in1=a_.ap[:, 0:64 - sft], op=ALU.add), reads=[a_, b_], writes=[b_])
            cur = 1 - cur
        PEND = PE_[cur]
        k.op("DVE", lambda e: e.tensor_tensor(out=BASE1.ap, in0=PEND.ap, in1=NB_.ap, op=ALU.subtract), reads=[PEND, NB_], writes=[BASE1])
        k.op("DVE", lambda e: e.tensor_tensor(out=BASE2.ap, in0=BASE1.ap, in1=cnt1, op=ALU.add), reads=[BASE1, RUN], writes=[BASE2])
        TBC = 2048 // 64
        for (base, rk, di, lo) in ((BASE1, RK0, DI0, 0), (BASE2, RK1, DI1, 64)):
            for c0 in range(0, NTB, TBC):
                nb = min(TBC, NTB - c0)
                big_t = BIG.ap[:, 0:nb * 64].rearrange("p (a m) -> p a m", a=nb)
                k.op("DVE", lambda e: e.tensor_tensor(out=big_t, in0=OHS3[:, c0:c0 + nb, lo:lo + 64], in1=base.ap.unsqueeze(1).to_broadcast([128, nb, 64]), op=ALU.mult),
                     reads=[OHS, base], writes=[BIG])
                k.op("DVE", lambda e: e.tensor_reduce(out=DF.ap[:, c0:c0 + nb], in_=big_t, axis=AX.X, op=ALU.add), reads=[BIG], writes=[DF])
            k.op("DVE", lambda e: e.tensor_tensor(out=DF.ap, in0=DF.ap, in1=rk.ap, op=ALU.add), reads=[DF, rk], writes=[DF])
            k.op("DVE", lambda e: e.tensor_copy(out=di.ap, in_=DF.ap), reads=[DF], writes=[di])
        for c0 in range(0, NBLK, 32):
            nb = min(32, NBLK - c0)
            big_j = BIG.ap[:, 0:nb * 64].rearrange("p (a m) -> p a m", a=nb)
            k.op("DVE", lambda e: e.tensor_tensor(out=big_j, in0=PEND.ap.unsqueeze(1).to_broadcast([128, nb, 64]), in1=JB[:, c0:c0 + nb].unsqueeze(2).to_broadcast([128, nb, 64]), op=ALU.is_le),
                 reads=[PEND, cf], writes=[BIG])
            k.op("DVE", lambda e: e.tensor_reduce(out=EJ.ap[:, c0:c0 + nb], in_=big_j, axis=AX.X, op=ALU.add), reads=[BIG], writes=[EJ])
        k.op("DVE", lambda e: e.tensor_scalar(out=EJ.ap, in0=EJ.ap, scalar1=63.0, scalar2=None, op0=ALU.min), reads=[EJ], writes=[EJ])
        NST = 4
        HB = NBLK // NST
        k.op("DVE", lambda e: e.memset(SAME.ap, 0.0), writes=[SAME])
        for s0 in range(0, NBLK, HB):
            k.op("DVE", lambda e: e.tensor_tensor(out=SAME.ap[:, s0 + 1:s0 + HB], in0=EJ.ap[:, s0 + 1:s0 + HB], in1=EJ.ap[:, s0:s0 + HB - 1], op=ALU.is_equal),
                 reads=[EJ, SAME], writes=[SAME])
        k.op("DVE", lambda e: e.tensor_scalar(out=SAME.ap, in0=SAME.ap, scalar1=float(1 << 22), scalar2=None, op0=ALU.mult), reads=[SAME], writes=[SAME])
        igf3 = v3(IGF.ap[:, 0:NBLK * 2], NBLK)
        k.op("DVE", lambda e: e.scalar_tensor_tensor(out=BIG.ap[:, 0:NBLK], in0=EJ.ap, scalar=256.0, in1=SAME.ap, op0=ALU.mult, op1=ALU.add), reads=[EJ, SAME], writes=[BIG])
        k.op("DVE", lambda e: e.tensor_tensor(out=igf3, in0=BIG.ap[:, 0:NBLK].unsqueeze(2).to_broadcast([128, NBLK, 2]), in1=KP[:, 0:2].unsqueeze(1).to_broadcast([128, NBLK, 2]), op=ALU.add),
             reads=[BIG, cf], writes=[IGF])
        k.op("DVE", lambda e: e.tensor_copy(out=IGI.ap[:, 0:NBLK * 2], in_=IGF.ap[:, 0:NBLK * 2]), reads=[IGF], writes=[IGI])

        for i in range(NTB):
            k.idma(Xs, XR3[:, i, :], DI0, DI0.ap[:, i:i + 1], gather=False, reads=[XROWS], writes=[Xs_t], part=True)
            k.idma(Xs, XR3[:, i, :], DI1, DI1.ap[:, i:i + 1], gather=False, reads=[XROWS], writes=[Xs_t], part=True)

        k.alias(XB + XTB + YB, [BIG, IGF] + ABS)
        k.alias(WBX, [XROWS])
        WB4 = WB + WBX

        bnd = nc.gpsimd.to_reg(64 * 256 - 1)
        igi2 = v3(IGI.ap[:, 0:NBLK * 2], NBLK)

        def load_blk(j, n_):
            wb = WB4[n_ % NST]
            first = True
            for (src, base) in ((wg_r, 0), (wu_r, 4096), (wd_r, 8192)):
                for h in range(2):
                    k.idma(wb.ap[:, base + h * 2048:base + (h + 1) * 2048], src, IGI, igi2[:, j, h:h + 1], gather=True, writes=[wb], part=(not first), bound=bnd)
                    first = False

        def load_xb(j, n_):
            k.dma("SP", XB[n_ % 2].ap, Xs[j * MB:(j + 1) * MB, :], reads=[Xs_t], writes=[XB[n_ % 2]])

        def front(j, n_):
            wb, xb, xt = WB4[n_ % NST], XB[n_ % 2], XTB[n_ % 2]
            trb0, trb1 = PBK[0].ap.bitcast(BF16), PBK[1].ap.bitcast(BF16)
            for kc in range(8):
                dst = (trb0 if kc < 4 else trb1)[:, (kc % 4) * 128:(kc % 4 + 1) * 128]
                k.op("PE", lambda e: e.transpose(dst, xb.ap.rearrange("p (m c) -> p c m", c=8)[:, kc, :], identb.ap), reads=[xb, identb], writes=[PBK[0] if kc < 4 else PBK[1]])
            k.op("ACT", lambda e: e.activation(out=xt.ap[:, 0:512], in_=trb0[:, 0:512], func=AF.Copy), reads=[PBK[0]], writes=[xt])
            k.op("ACT", lambda e: e.activation(out=xt.ap[:, 512:1024], in_=trb1[:, 0:512], func=AF.Copy), reads=[PBK[1], xt], writes=[xt])
            Wg3, Wu3 = v3(wb.ap[:, 0:4096], 8), v3(wb.ap[:, 4096:8192], 8)
            pg, pu = PG[n_ % 2], PU[n_ % 2]
            for f in range(4):
                for kc in range(8):
                    k.op("PE", lambda e: e.matmul(pg.ap[:, f * 128:(f + 1) * 128], Wg3[:, kc, :].rearrange("p (m c) -> p c m", c=4)[:, f, :], xt.ap[:, kc * 128:(kc + 1) * 128],
                                                  start=(kc == 0), stop=(kc == 7)), reads=[wb, xt], writes=[pg], pe_acc=(kc > 0 or f > 0))
            for f in range(4):
                for kc in range(8):
                    k.op("PE", lambda e: e.matmul(pu.ap[:, f * 128:(f + 1) * 128], Wu3[:, kc, :].rearrange("p (m c) -> p c m", c=4)[:, f, :], xt.ap[:, kc * 128:(kc + 1) * 128],
                                                  start=(kc == 0), stop=(kc == 7)), reads=[wb, xt], writes=[pu], pe_acc=(kc > 0 or f > 0))
            k.op("ACT", lambda e: e.activation(out=SS[n_ % 2].ap, in_=pg.ap, func=AF.Silu), reads=[pg], writes=[SS[n_ % 2]])
            k.op("DVE", lambda e: e.tensor_tensor(out=HH[n_ % 2].ap, in0=SS[n_ % 2].ap, in1=pu.ap, op=ALU.mult), reads=[SS[n_ % 2], pu], writes=[HH[n_ % 2]])

        def back(j, n_):
            wb, hh, yb = WB4[n_ % NST], HH[n_ % 2], YB[n_ % 2]
            Wd3 = v3(wb.ap[:, 8192:12288], 4)
            for dh in range(2):
                pd = PD[dh]
                for f in range(4):
                    k.op("PE", lambda e: e.matmul(pd.ap, hh.ap[:, f * 128:(f + 1) * 128], Wd3[:, f, dh * 512:(dh + 1) * 512], start=(f == 0), stop=(f == 3)),
                         reads=[wb, hh], writes=[pd], pe_acc=(f > 0))
                if dh == 0:
                    k.op("ACT", lambda e: e.activation(out=yb.ap[:, 0:512], in_=pd.ap, func=AF.Copy), reads=[pd], writes=[yb])
                else:
                    k.op("DVE", lambda e: e.tensor_copy(out=yb.ap[:, 512:1024], in_=pd.ap), reads=[pd, yb], writes=[yb])
            k.dma("SP", Ys[j * MB:(j + 1) * MB, :], yb.ap, reads=[yb], writes=[Ys_t], part=True)

        order = []
        for q in range(HB):
            order += [s_ * HB + q for s_ in range(NST)]
        for n_ in range(NST):
            load_blk(order[n_], n_)
        for n_ in range(2):
            load_xb(order[n_], n_)
        for n_, j in enumerate(order):
            front(j, n_)
            back(j, n_)
            if n_ + NST < NBLK:
                load_blk(order[n_ + NST], n_ + NST)
            if n_ + 2 < NBLK:
                load_xb(order[n_ + 2], n_ + 2)

        k.dma("POOL", v3(WB[0].ap[:, 0:8192], 8), plg.rearrange("(a p) n -> p a n", p=128), writes=[WB[0]])
        k.dma("POOL", v3(WB[1].ap[:, 0:2048], 2), plp.rearrange("(a p) n -> p a n", p=128), writes=[WB[1]])
        k.alias(WQ + Y0 + Y1, [XROWS] + WBX)
        if mode == "A":
            k.dma("POOL", v3(WQ[0].ap, 8), wqkv[:, 0:1536].rearrange("(a p) n -> p a n", p=128), writes=[WQ[0]])
            k.dma("POOL", v3(WQ[1].ap, 8), wqkv[:, 1536:3072].rearrange("(a p) n -> p a n", p=128), writes=[WQ[1]])
        for t in range(NTILE):
            cs = slice(t * 512, (t + 1) * 512)
            ht, ut = HTL[t % 2], UTL[t % 2]
            h3, u3 = v3(ht.ap, 8), v3(ut.ap, 8)
            k.dma("SP", h3, H1[:, cs].rearrange("(a p) n -> p a n", p=128), reads=[H1_t], writes=[ht])
            k.dma("POOL", v3(PBt.ap, 2), pT[:, cs].rearrange("(a p) n -> p a n", p=128), writes=[PBt])
            for b in range(4):
                i = t * 4 + b
                bs = slice(b * 128, (b + 1) * 128)
                y0, y1 = Y0[i % 2], Y1[i % 2]
                k.idma(y0.ap, Ys, DI0, DI0.ap[:, i:i + 1], gather=True, reads=[Ys_t], writes=[y0])
                k.idma(y1.ap, Ys, DI1, DI1.ap[:, i:i + 1], gather=True, reads=[Ys_t], writes=[y1])
                k.op("DVE", lambda e: e.tensor_scalar(out=y0.ap, in0=y0.ap, scalar1=GA.ap[:, i:i + 1], scalar2=None, op0=ALU.mult), reads=[y0, GA], writes=[y0])
                k.op("DVE", lambda e: e.scalar_tensor_tensor(out=y0.ap, in0=y1.ap, scalar=GB.ap[:, i:i + 1], in1=y0.ap, op0=ALU.mult, op1=ALU.add),
                     reads=[y1, GB, y0], writes=[y0])
                for half in range(2):
                    bank = PD[half]
                    for dq in range(4):
                        d = half * 4 + dq
                        k.op("PE", lambda e: e.transpose(bank.ap[:, dq * 128:(dq + 1) * 128], y0.ap[:, d * 128:(d + 1) * 128], ident), reads=[y0, cf], writes=[bank])
                    k.op("DVE", lambda e: e.tensor_tensor(out=h3[:, half * 4:(half + 1) * 4, bs], in0=h3[:, half * 4:(half + 1) * 4, bs],
                                                          in1=bank.ap.rearrange("p (a n) -> p a n", a=4), op=ALU.add), reads=[bank, ht], writes=[ht])
            rmsnorm(ht, h3, 8, ut, u3, PBK[0])
            for d in range(8):
                pg, pu = PG[d % 2], PU[d % 2]
                for kc in range(8):
                    k.op("PE", lambda e: e.matmul(pg.ap, v3(WB[0].ap[:, 0:8192], 8)[:, kc, d * 128:(d + 1) * 128], u3[:, kc, :], start=(kc == 0), stop=(kc == 7)),
                         reads=[WB[0], ut], writes=[pg], pe_acc=(kc > 0))
                for kc in range(2):
                    k.op("PE", lambda e: e.matmul(pu.ap, v3(WB[1].ap[:, 0:2048], 2)[:, kc, d * 128:(d + 1) * 128], v3(PBt.ap, 2)[:, kc, :], start=(kc == 0), stop=(kc == 1)),
                         reads=[WB[1], PBt], writes=[pu], pe_acc=(kc > 0))
                sf = STF[d % 2]
                k.op("ACT", lambda e: e.activation(out=sf.ap, in_=pg.ap, func=AF.Sigmoid, bias=vec.ap[:, 16 + d:17 + d], scale=1.0), reads=[pg, vec], writes=[sf])
                k.op("DVE", lambda e: e.tensor_tensor(out=sf.ap, in0=sf.ap, in1=pu.ap, op=ALU.mult), reads=[sf, pu], writes=[sf])
                k.op("DVE", lambda e: e.tensor_tensor(out=h3[:, d, :], in0=h3[:, d, :], in1=sf.ap, op=ALU.add), reads=[sf, ht], writes=[ht])
            if mode == "A":
                k.dma("SP", hT_o[:, cs].rearrange("(a p) n -> p a n", p=128), h3, reads=[ht], writes=[ho_t], part=True)
                rmsnorm(ht, h3, 24, ut, u3, PBK[0])
                for n in range(16):
                    col = n * 128
                    wbt = WQ[0] if col < 1536 else WQ[1]
                    lc = col if col < 1536 else col - 1536
                    bank = PG[n % 2]
                    for kc in range(8):
                        k.op("PE", lambda e: e.matmul(bank.ap, v3(wbt.ap, 8)[:, kc, lc:lc + 128], u3[:, kc, :], start=(kc == 0), stop=(kc == 7)),
                             reads=[wbt, ut], writes=[bank], pe_acc=(kc > 0))
                    sg = STG[n % 2]
                    sc_ = (1.0 / np.sqrt(128.0)) if n < 8 else 1.0
                    k.op("ACT", lambda e: e.activation(out=sg.ap, in_=bank.ap, func=AF.Identity, scale=float(sc_)), reads=[bank], writes=[sg])
                    if n < 8:
                        k.dma("SP", qT_o[n * 128:(n + 1) * 128, cs], sg.ap, reads=[sg], writes=[qT_t], part=True)
                    else:
                        k.dma("SP", kT_o[(n - 8) * 128:(n - 7) * 128, cs], sg.ap, reads=[sg], writes=[kT_t], part=True)
                for b in range(4):
                    for hh_ in range(2):
                        bank = PU[hh_]
                        for kc in range(8):
                            k.op("PE", lambda e: e.matmul(bank.ap, u3[:, kc, b * 128:(b + 1) * 128], v3(WQ[1].ap, 8)[:, kc, 512 + hh_ * 512:1024 + hh_ * 512],
                                                          start=(kc == 0), stop=(kc == 7)), reads=[WQ[1], ut], writes=[bank], pe_acc=(kc > 0))
                        sg = STG[hh_]
                        k.op("ACT", lambda e: e.activation(out=sg.ap, in_=bank.ap, func=AF.Copy), reads=[bank], writes=[sg])
                        r0 = t * 512 + b * 128
                        k.dma("SP", v_o[r0:r0 + 128, hh_ * 512:(hh_ + 1) * 512], sg.ap, reads=[sg], writes=[v_t], part=True)
            else:
                k.op("ACT", lambda e: e.activation(out=u3, in_=h3, func=AF.Square), reads=[ht], writes=[ut])
                bank = PBK[0]
                for kc in range(8):
                    k.op("PE", lambda e: e.matmul(bank.ap, ones_bf.ap, u3[:, kc, :], start=(kc == 0), stop=(kc == 7)),
                         reads=[ut, ones_bf], writes=[bank], pe_acc=(kc > 0))
                k.op("ACT", lambda e: e.activation(out=LNV.ap, in_=bank.ap, func=AF.Ln, scale=1.0 / 1024, bias=EPS), reads=[bank], writes=[LNV])
                k.op("ACT", lambda e: e.activation(out=RSTD.ap, in_=LNV.ap, func=AF.Exp, scale=-0.5), reads=[LNV], writes=[RSTD])
                for d in range(8):
                    k.op("DVE", lambda e: e.scalar_tensor_tensor(out=h3[:, d, :], in0=h3[:, d, :], scalar=vec.ap[:, 24 + d:25 + d], in1=RSTD.ap,
                                                                 op0=ALU.mult, op1=ALU.mult), reads=[ht, RSTD, vec], writes=[ht])
                k.dma("SP", out_o[:, cs].rearrange("(a p) n -> p a n", p=128), h3, reads=[ht], writes=[out_t], part=True)
        k.finish("SP", [qT_t, kT_t, v_t, ho_t] if mode == "A" else [out_t])
        print("tok2 ninst", k.ninst)
    return nc


def build_attn2(L, NHD=2):
    nc = bass.Bass("TRN2", target_bir_lowering=False)

    def din(name, shape, dt=F32):
        return nc.dram_tensor(name, list(shape), dt, kind="ExternalInput").ap()

    qT = din("qT", [NHD, 128, L], BF16)
    kT = din("kT", [NHD, 128, L], BF16)
    vP = din("vP", [NHD, 128, L // 128, 128], BF16)
    cst = din("cst", [128, 256 + 4 * 512])
    oT = nc.dram_tensor("oT", [NHD, 128, L], BF16, kind="ExternalOutput").ap()
    NQT = L // 512
    NB = L // 128
    with ExitStack() as st:
        k = K(nc, st)
        Qs = k.sbt([128, L], BF16, "Qs")
        Ks = k.sbt([128, L], BF16, "Ks")
        Vs = k.sbt([128, L], BF16, "Vs")
        V3 = v3(Vs.ap, NB)
        cf = k.sbt([128, 256 + 2048], F32, "cf")
        trin = k.sbt([128, 128], BF16, "trin")
        onen = k.sbt([128, 128], BF16, "onen")
        m01 = k.sbt([128, 2048], BF16, "m01")
        mneg = k.sbt([128, 2048], F32, "mneg")
        E_ = [k.sbt([128, 1024], F32, "E%d" % i) for i in range(3)]
        SPf = [k.sbt([128, 1024], BF16, "SPf%d" % i) for i in range(3)]
        SPm = [k.sbt([128, 1024], BF16, "SPm%d" % i) for i in range(3)]
        TMP = [k.sbt([128, 1024], F32, "TMP%d" % i) for i in range(3)]
        AT = [k.sbt([128, 1024], BF16, "AT%d" % i) for i in range(3)]
        OFFS = [k.sbt([128, 512], F32, "OFF%d" % i) for i in range(2)]
        offi = [0]
        OST = [k.sbt([128, 512], BF16, "OST%d" % i) for i in range(2)]
        PA = [T(k.ps([128, 1024], F32, "pa%d" % i)[:]) for i in range(3)]
        PBb = T(k.ps([128, 512], F32, "pbb")[:])
        PO = T(k.ps([128, 512], F32, "po")[:])
        oT_t = T(oT)

        k.dma("SP", cf.ap, cst, writes=[cf])
        k.op("DVE", lambda e: e.tensor_copy(out=trin.ap, in_=cf.ap[:, 0:128]), reads=[cf], writes=[trin])
        k.op("DVE", lambda e: e.tensor_copy(out=onen.ap, in_=cf.ap[:, 128:256]), reads=[cf], writes=[onen])
        for pos, r in enumerate((3, 2, 1, 0)):
            src = cf.ap[:, 256 + r * 512:256 + (r + 1) * 512]
            k.op("DVE", lambda e: e.tensor_copy(out=m01.ap[:, pos * 512:(pos + 1) * 512], in_=src), reads=[cf, m01], writes=[m01])
            k.op("DVE", lambda e: e.tensor_scalar(out=mneg.ap[:, pos * 512:(pos + 1) * 512], in0=src, scalar1=-1.0, scalar2=30000.0, op0=ALU.add, op1=ALU.mult),
                 reads=[cf, mneg], writes=[mneg])

        jobs = []
        for hd in range(NHD):
            for J in range(NQT):
                nb = 4 * J + 4
                for pi, n1 in enumerate(range(nb - 1, 0, -2)):
                    n0 = n1 - 1
                    dg = -1
                    if n1 == 4 * J + 3:
                        dg = 0
                    elif n1 == 4 * J + 1:
                        dg = 1
                    jobs.append(dict(hd=hd, J=J, n1=n1, n0=n0, first=(pi == 0), last=(n0 == 0), dg=dg))

        def load_head(hd):
            k.dma("SP", Qs.ap, qT[hd], writes=[Qs])
            k.dma("SP", Ks.ap, kT[hd], writes=[Ks])
            k.dma("SP", V3, vP[hd], writes=[Vs])

        def s1(j, i):
            if j["first"] and j["J"] == 0:
                load_head(j["hd"])
            A = PA[i % 3]
            qs = slice(j["J"] * 512, (j["J"] + 1) * 512)
            for h_, n in enumerate((j["n1"], j["n0"])):
                k.op("PE", lambda e: e.matmul(A.ap[:, h_ * 512:(h_ + 1) * 512], Ks.ap[:, n * 128:(n + 1) * 128], Qs.ap[:, qs], start=True, stop=False),
                     reads=[Ks, Qs], writes=[A], pe_acc=(h_ > 0))
            k.op("ACT", lambda e: e.activation(out=E_[i % 3].ap, in_=A.ap, func=AF.Exp), reads=[A], writes=[E_[i % 3]])

        def s1b(j, i):
            k.op("ACT", lambda e: e.activation(out=SPf[i % 3].ap, in_=E_[i % 3].ap, func=AF.Ln, bias=1.0, scale=1.0), reads=[E_[i % 3]], writes=[SPf[i % 3]])
            if j["dg"] >= 0:
                d_ = j["dg"]
                k.op("POOL", lambda e: e.tensor_tensor(out=SPm[i % 3].ap, in0=SPf[i % 3].ap, in1=m01.ap[:, d_ * 1024:(d_ + 1) * 1024], op=ALU.mult),
                     reads=[SPf[i % 3], m01], writes=[SPm[i % 3]])

        def s2(j, i):
            A, B = PA[i % 3], PBb
            sp = SPm[i % 3] if j["dg"] >= 0 else SPf[i % 3]
            OFF = OFFS[offi[0] % 2]
            if j["first"]:
                k.op("DVE", lambda e: e.memset(OFF.ap, 0.0), writes=[OFF])
            k.op("PE", lambda e: e.matmul(A.ap[:, 0:512], trin.ap, sp.ap[:, 0:512], start=False, stop=True), reads=[trin, sp], writes=[A], pe_acc=True)
            k.op("PE", lambda e: e.matmul(A.ap[:, 512:1024], trin.ap, sp.ap[:, 512:1024], start=False, stop=False), reads=[trin, sp], writes=[A], pe_acc=True)
            k.op("PE", lambda e: e.matmul(A.ap[:, 512:1024], onen.ap, sp.ap[:, 0:512], start=False, stop=True), reads=[onen, sp], writes=[A], pe_acc=True)
            tm = TMP[i % 3]
            k.op("DVE", lambda e: e.tensor_tensor(out=v3(tm.ap, 2), in0=v3(A.ap, 2), in1=OFF.ap.unsqueeze(1).to_broadcast([128, 2, 512]), op=ALU.add),
                 reads=[A, OFF], writes=[tm])
            if j["dg"] >= 0:
                d_ = j["dg"]
                k.op("DVE", lambda e: e.tensor_tensor(out=tm.ap, in0=tm.ap, in1=mneg.ap[:, d_ * 1024:(d_ + 1) * 1024], op=ALU.add), reads=[tm, mneg], writes=[tm])
            k.op("ACT", lambda e: e.activation(out=AT[i % 3].ap, in_=tm.ap, func=AF.Exp), reads=[tm], writes=[AT[i % 3]])
            if not j["last"]:
                k.op("PE", lambda e: e.matmul(B.ap, onen.ap, sp.ap[:, 0:512], start=True, stop=False), reads=[onen, sp], writes=[B])
                k.op("PE", lambda e: e.matmul(B.ap, onen.ap, sp.ap[:, 512:1024], start=False, stop=True), reads=[onen, sp], writes=[B], pe_acc=True)
                OFN = OFFS[(offi[0] + 1) % 2]
                k.op("DVE", lambda e: e.tensor_tensor(out=OFN.ap, in0=OFF.ap, in1=B.ap, op=ALU.add), reads=[B, OFF], writes=[OFN])
            offi[0] += 1

        grp = [0]

        def s3(j, i):
            O = PO
            k.op("PE", lambda e: e.matmul(O.ap, V3[:, j["n1"], :], AT[i % 3].ap[:, 0:512], start=j["first"], stop=False),
                 reads=[Vs, AT[i % 3]], writes=[O], pe_acc=(not j["first"]))
            k.op("PE", lambda e: e.matmul(O.ap, V3[:, j["n0"], :], AT[i % 3].ap[:, 512:1024], start=False, stop=j["last"]),
                 reads=[Vs, AT[i % 3]], writes=[O], pe_acc=True)
            if j["last"]:
                og = OST[grp[0] % 2]
                k.op("ACT", lambda e: e.activation(out=og.ap, in_=O.ap, func=AF.Copy), reads=[O], writes=[og])
                k.dma("SP", oT[j["hd"], :, j["J"] * 512:(j["J"] + 1) * 512], og.ap, reads=[og], writes=[oT_t], part=True)
                grp[0] += 1

        n = len(jobs)
        stages = [s1, s1b, s2, s3]
        done = [0] * n

        def run_next(jj):
            stages[done[jj]](jobs[jj], jj)
            done[jj] += 1

        for i in range(n + 3):
            if i < n and jobs[i]["first"] and jobs[i]["J"] == 0 and i > 0:
                for jj in range(max(0, i - 3), i):
                    while done[jj] < 4:
                        run_next(jj)
            for d_ in range(4):
                jj = i - d_
                if 0 <= jj < n and done[jj] == d_:
                    run_next(jj)
        assert all(x == 4 for x in done)
        k.finish("SP", [oT_t])
        print("attn2 ninst", k.ninst, "pairs", n)
    return nc
```

```python
import numpy as np
import ml_dtypes
from contextlib import ExitStack
import concourse.bass as bass
import concourse.mybir as mybir
from concourse.bass_utils import run_bass_kernel_spmd

F32 = mybir.dt.float32
BF16 = mybir.dt.bfloat16
AF = mybir.ActivationFunctionType
ALU = mybir.AluOpType
AX = mybir.AxisListType
EPS = 1e-6
NPBF = ml_dtypes.bfloat16


class T:
    __slots__ = ("ap", "w", "r", "dsem", "dcnt", "name", "ws")

    def __init__(self, ap, name=""):
        self.ap = ap
        self.w = None
        self.ws = {}
        self.r = {}
        self.dsem = None
        self.dcnt = 0
        self.name = name


class K:
    def __init__(self, nc, stack):
        self.nc = nc
        self.stack = stack
        self.eng = {"PE": nc.tensor, "ACT": nc.scalar, "DVE": nc.vector, "POOL": nc.gpsimd, "SP": nc.sync}
        self.sem = {}
        self.cnt = {}
        for e in ("PE", "ACT", "DVE", "POOL"):
            self.sem[e] = stack.enter_context(nc.semaphore("s_" + e))
            self.cnt[e] = 0
        self.seen = {e: {} for e in self.eng}
        self.semobj = dict(self.sem)
        self.ndsem = 0
        self.ninst = 0
        self.nalloc = 0

    def sb(self, shape, dt, name=None):
        self.nalloc += 1
        return self.stack.enter_context(self.nc.sbuf_tensor(name or ("sb%d" % self.nalloc), list(shape), dt))

    def ps(self, shape, dt, name=None):
        self.nalloc += 1
        return self.stack.enter_context(self.nc.psum_tensor(name or ("ps%d" % self.nalloc), list(shape), dt))

    def sbt(self, shape, dt, name=None):
        return T(self.sb(shape, dt, name)[:])

    def _need(self, e, key, val):
        if val <= 0 or self.seen[e].get(key, 0) >= val:
            return
        self.eng[e].wait_ge(self.semobj[key], val)
        self.seen[e][key] = val

    def _wait_written(self, e, t):
        if t.w is not None:
            self._need(e, *t.w)
        for kk, v in t.ws.items():
            self._need(e, kk, v)

    def op(self, e, fn, reads=(), writes=(), pe_acc=False):
        for t in reads:
            self._wait_written(e, t)
        for t in writes:
            if t.w is not None and not (pe_acc and t.w[0] == "PE"):
                self._need(e, *t.w)
            for kk, v in t.ws.items():
                self._need(e, kk, v)
            for kk, v in t.r.items():
                self._need(e, kk, v)
        ins = fn(self.eng[e])
        self.cnt[e] += 1
        c = self.cnt[e]
        ins.then_inc(self.sem[e], 1)
        for t in reads:
            t.r[e] = c
        for t in writes:
            t.w = (e, c)
            t.r = {}
        self.ninst += 1
        return ins

    def _own_sem(self, t):
        if t.dsem is None:
            key = "d%d" % self.ndsem
            t.dsem = self.stack.enter_context(self.nc.semaphore(key))
            self.semobj[key] = t.dsem
            t.name = key
            self.ndsem += 1
        return t.name

    def dma(self, q, out_ap, in_ap, reads=(), writes=(), part=False, sem_of=None, **kw):
        assert len(writes) == 1
        t = writes[0]
        owner = sem_of if sem_of is not None else t
        key = self._own_sem(owner)
        for r in reads:
            self._wait_written(q, r)
        if sem_of is None:
            if t.w is not None and not (part and t.w[0] == key):
                self._need(q, *t.w)
            for kk, v in t.ws.items():
                self._need(q, kk, v)
        for kk, v in t.r.items():
            self._need(q, kk, v)
        ins = self.eng[q].dma_start(out=out_ap, in_=in_ap, **kw)
        owner.dcnt += 16
        ins.then_inc(owner.dsem, 16)
        for r in reads:
            r.r[key] = max(r.r.get(key, 0), owner.dcnt)
        if sem_of is None:
            t.w = (key, t.dcnt)
            if not part:
                t.r = {}
                t.ws = {}
        else:
            t.ws[key] = owner.dcnt
        self.ninst += 1
        return ins

    def alias(self, new, old):
        for n in new:
            for o in old:
                for kk, v in o.r.items():
                    n.r[kk] = max(n.r.get(kk, 0), v)
                if o.w is not None:
                    n.r[o.w[0]] = max(n.r.get(o.w[0], 0), o.w[1])

    def finish(self, e, tiles):
        for t in tiles:
            self._wait_written(e, t)


def v3(ap, a):
    return ap.rearrange("p (a n) -> p a n", a=a)


def build_tok(NT, TH, KA, mode, NE=64):
    nc = bass.Bass("TRN2", target_bir_lowering=False)

    def din(name, shape, dt=F32):
        return nc.dram_tensor(name, list(shape), dt, kind="ExternalInput").ap()

    def dout(name, shape, dt=F32):
        return nc.dram_tensor(name, list(shape), dt, kind="ExternalOutput").ap()

    hT = din("hT", [1024, NT])
    aT = din("aT", [KA, NT], BF16)
    w_a = din("w_a", [KA, 1024])
    vecs = din("vecs", [128, 32])
    wr = din("wr", [1024, 72])
    br = din("br", [1, 72])
    wg = din("wg", [64, 1024, 512])
    wu = din("wu", [64, 1024, 512])
    wd = din("wd", [64, 512, 1024])
    plg = din("plg", [1024, 1024])
    plp = din("plp", [256, 1024])
    pT = din("pT", [256, NT])
    ident_d = din("ident", [128, 128])
    if mode == "A":
        wqkv = din("wqkv", [1024, 3072])
        hT_o = dout("hT_o", [1024, NT])
        qT_o = dout("qT_o", [1024, NT], BF16)
        kT_o = dout("kT_o", [1024, NT], BF16)
        v_o = dout("v_o", [NT, 1024], BF16)
    else:
        out_o = dout("out_o", [1024, NT])
    KAC = KA // 128
    NTL = TH // 512
    NH = NT // TH

    with ExitStack() as st:
        k = K(nc, st)
        h_sb = k.sb([128, 8 * TH], F32, "h_sb")
        h3 = v3(h_sb[:], 8)
        HT = [[T(h3[:, d, t * 512:(t + 1) * 512]) for d in range(8)] for t in range(NTL)]
        u_sb = k.sb([128, 8 * TH], BF16, "u_sb")
        u3 = v3(u_sb[:], 8)
        UT = [T(u3[:, :, t * 512:(t + 1) * 512]) for t in range(NTL)]
        WB = [k.sbt([128, 12288], BF16, "wb%d" % i) for i in range(2)]
        GT = k.sbt([64, TH], F32, "gt")
        NAB = max(1, min(2, (8 * TH) // (KAC * 512)))
        ABS = [T(u_sb[:, i * KAC * 512:(i + 1) * KAC * 512]) for i in range(NAB)]
        PB = k.sbt([128, 2 * 512], BF16, "pb")
        SQ = k.sbt([128, 8 * 512], BF16, "sq")
        LNV = k.sbt([128, 512], F32, "lnv")
        RSTD = k.sbt([128, 512], F32, "rstd")
        vec = k.sbt([128, 32], F32, "vec_sb")
        ident = k.sbt([128, 128], F32, "ident_sb")
        ones_bf = k.sbt([128, 128], BF16, "ones")
        wr_sb = k.sbt([128, 8 * 72], F32, "wr_sb")
        br_bc = k.sbt([128, 72], F32, "brbc")
        S_ = [k.sbt([128, 512], BF16, "s%d" % i) for i in range(2)]
        TT = [k.sbt([128, 512], BF16, "tt%d" % i) for i in range(2)]
        HS = [[k.sbt([128, 512], BF16, "hs%d_%d" % (i, f)) for f in range(4)] for i in range(2)]
        EB = [k.sbt([64, 128], F32, "eb%d" % i) for i in range(2)]
        STG = [k.sbt([128, 512], BF16, "stg%d" % i) for i in range(2)]
        STF = [k.sbt([128, 512], F32, "stf%d" % i) for i in range(2)]
        r_lg = k.sbt([128, 72], F32, "r_lg")
        r_a = k.sbt([128, 8], F32, "r_a")
        r_goh = k.sbt([128, 8], F32, "r_goh")
        r_t64 = k.sbt([128, 64], F32, "r_t64")
        r_es = k.sbt([128, 8], F32, "r_es")
        r_es2 = k.sbt([128, 8], F32, "r_es2")
        r_oh1 = k.sbt([128, 8], F32, "r_oh1")
        r_oh2 = k.sbt([128, 8], F32, "r_oh2")
        r_ew = k.sbt([128, 8], F32, "r_ew")
        r_G = k.sbt([128, 64], F32, "r_G")
        r_s = [k.sbt([128, 1], F32, "r_s%d" % i) for i in range(12)]
        BK = [T(k.ps([128, 512], F32, "bk%d" % i)[:]) for i in range(8)]
        PG, PU, PD, PBK = BK[0:2], BK[2:4], BK[4:6], BK[6:8]

        k.dma("SP", vec.ap, vecs, writes=[vec])
        k.dma("SP", ident.ap, ident_d, writes=[ident])
        k.dma("SP", v3(wr_sb.ap, 8), wr.rearrange("(a p) n -> p a n", p=128), writes=[wr_sb])
        k.dma("SP", br_bc.ap, br.partition_broadcast(128).rearrange("p o n -> p (o n)"), writes=[br_bc])
        k.op("DVE", lambda e: e.memset(ones_bf.ap, 1.0), writes=[ones_bf])
        for kc in range(8):
            k.op("DVE", lambda e: e.tensor_scalar(out=wr_sb.ap[:, kc * 72:(kc + 1) * 72], in0=wr_sb.ap[:, kc * 72:(kc + 1) * 72],
                                                  scalar1=vec.ap[:, kc:kc + 1], scalar2=None, op0=ALU.mult),
                 reads=[vec, wr_sb], writes=[wr_sb])

        def rmsnorm(src_tiles, src_ap, gcol, dst_t, dst_ap, bank):
            k.op("ACT", lambda e: e.activation(out=v3(SQ.ap, 8), in_=src_ap, func=AF.Square), reads=src_tiles, writes=[SQ])
            for kc in range(8):
                k.op("PE", lambda e: e.matmul(bank.ap, ones_bf.ap, v3(SQ.ap, 8)[:, kc, :], start=(kc == 0), stop=(kc == 7)),
                     reads=[SQ, ones_bf], writes=[bank], pe_acc=(kc > 0))
            k.op("ACT", lambda e: e.activation(out=LNV.ap, in_=bank.ap, func=AF.Ln, scale=1.0 / 1024, bias=EPS), reads=[bank], writes=[LNV])
            k.op("ACT", lambda e: e.activation(out=RSTD.ap, in_=LNV.ap, func=AF.Exp, scale=-0.5), reads=[LNV], writes=[RSTD])
            for kc in range(8):
                k.op("DVE", lambda e: e.scalar_tensor_tensor(out=dst_ap[:, kc, :], in0=src_ap[:, kc, :], scalar=vec.ap[:, gcol + kc:gcol + kc + 1],
                                                             in1=RSTD.ap, op0=ALU.mult, op1=ALU.mult),
                     reads=list(src_tiles) + [RSTD, vec], writes=[dst_t])

        if mode == "A":
            qT_t, kT_t, v_t, ho_t = T(qT_o), T(kT_o), T(v_o), T(hT_o)
        else:
            out_t = T(out_o)
        for hf in range(NH):
            c0 = hf * TH
            k.alias(ABS, UT)
            for kp in range(KAC // 8):
                k.dma("POOL", v3(WB[kp].ap[:, 0:8192], 8), w_a[kp * 1024:(kp + 1) * 1024, :].rearrange("(a p) n -> p a n", p=128), writes=[WB[kp]])
            for t in range(NTL):
                cs = slice(c0 + t * 512, c0 + (t + 1) * 512)
                for d in range(8):
                    k.dma("SP", HT[t][d].ap, hT[d * 128:(d + 1) * 128, cs], writes=[HT[t][d]])
                AB = ABS[t % NAB]
                k.dma("SP", v3(AB.ap, KAC), aT[:, cs].rearrange("(a p) n -> p a n", p=128), writes=[AB])
                for d in range(8):
                    bank = PD[d % 2]
                    for kc in range(KAC):
                        wbt = WB[kc // 8]
                        k.op("PE", lambda e: e.matmul(bank.ap, v3(wbt.ap[:, 0:8192], 8)[:, kc % 8, d * 128:(d + 1) * 128], v3(AB.ap, KAC)[:, kc, :],
                                                      start=(kc == 0), stop=(kc == KAC - 1)),
                             reads=[wbt, AB], writes=[bank], pe_acc=(kc > 0))
                    k.op("DVE", lambda e: e.tensor_tensor(out=HT[t][d].ap, in0=HT[t][d].ap, in1=bank.ap, op=ALU.add),
                         reads=[bank, HT[t][d]], writes=[HT[t][d]])
            k.alias(UT, ABS)
            for t in range(NTL):
                hap = h3[:, :, t * 512:(t + 1) * 512]
                rmsnorm(HT[t], hap, 0, UT[t], UT[t].ap, PBK[0])
                for b in range(4):
                    bs = slice(t * 512 + b * 128, t * 512 + (b + 1) * 128)
                    plg_b, prs = PG[b % 2], PU[b % 2]
                    for kc in range(8):
                        k.op("PE", lambda e: e.matmul(plg_b.ap[:, 0:72], h3[:, kc, bs], wr_sb.ap[:, kc * 72:(kc + 1) * 72], start=(kc == 0), stop=(kc == 7)),
                             reads=[HT[t][kc], wr_sb], writes=[plg_b], pe_acc=(kc > 0))
                    k.op("PE", lambda e: e.matmul(prs.ap[:, 0:2], RSTD.ap[:, b * 128:(b + 1) * 128], ident.ap[:, 0:2], start=True, stop=True),
                         reads=[RSTD, ident], writes=[prs])
                    rt, gmax, ngmax, gsum, gw, m1, m2, dd, ex, w1, w2 = r_s[0:11]
                    k.op("DVE", lambda e: e.tensor_copy(out=rt.ap, in_=prs.ap[:, 0:1]), reads=[prs], writes=[rt])
                    k.op("DVE", lambda e: e.scalar_tensor_tensor(out=r_lg.ap, in0=plg_b.ap[:, 0:72], scalar=rt.ap, in1=br_bc.ap, op0=ALU.mult, op1=ALU.add),
                         reads=[plg_b, rt, br_bc], writes=[r_lg])
                    gl = r_lg.ap[:, 0:8]
                    el = r_lg.ap[:, 8:72]
                    k.op("DVE", lambda e: e.tensor_reduce(out=gmax.ap, in_=gl, axis=AX.X, op=ALU.max), reads=[r_lg], writes=[gmax])
                    k.op("DVE", lambda e: e.tensor_scalar(out=r_goh.ap, in0=gl, scalar1=gmax.ap, scalar2=None, op0=ALU.is_equal), reads=[r_lg, gmax], writes=[r_goh])
                    k.op("DVE", lambda e: e.tensor_scalar(out=ngmax.ap, in0=gmax.ap, scalar1=-1.0, scalar2=None, op0=ALU.mult), reads=[gmax], writes=[ngmax])
                    k.op("ACT", lambda e: e.activation(out=r_a.ap, in_=gl, func=AF.Exp, bias=ngmax.ap, scale=1.0, accum_out=gsum.ap), reads=[r_lg, ngmax], writes=[r_a, gsum])
                    k.op("DVE", lambda e: e.reciprocal(out=gw.ap, in_=gsum.ap), reads=[gsum], writes=[gw])
                    k.op("DVE", lambda e: e.tensor_tensor(out=v3(r_t64.ap, 8), in0=v3(el, 8), in1=r_goh.ap.unsqueeze(2).to_broadcast([128, 8, 8]), op=ALU.mult),
                         reads=[r_lg, r_goh], writes=[r_t64])
                    k.op("DVE", lambda e: e.tensor_reduce(out=r_es.ap, in_=v3(r_t64.ap, 8).rearrange("p g j -> p j g"), axis=AX.X, op=ALU.add), reads=[r_t64], writes=[r_es])
                    k.op("DVE", lambda e: e.tensor_reduce(out=m1.ap, in_=r_es.ap, axis=AX.X, op=ALU.max), reads=[r_es], writes=[m1])
                    k.op("DVE", lambda e: e.tensor_scalar(out=r_oh1.ap, in0=r_es.ap, scalar1=m1.ap, scalar2=None, op0=ALU.is_equal), reads=[r_es, m1], writes=[r_oh1])
                    k.op("DVE", lambda e: e.scalar_tensor_tensor(out=r_es2.ap, in0=r_oh1.ap, scalar=-1e30, in1=r_es.ap, op0=ALU.mult, op1=ALU.add), reads=[r_oh1, r_es], writes=[r_es2])
                    k.op("DVE", lambda e: e.tensor_reduce(out=m2.ap, in_=r_es2.ap, axis=AX.X, op=ALU.max), reads=[r_es2], writes=[m2])
                    k.op("DVE", lambda e: e.tensor_scalar(out=r_oh2.ap, in0=r_es2.ap, scalar1=m2.ap, scalar2=None, op0=ALU.is_equal), reads=[r_es2, m2], writes=[r_oh2])
                    k.op("DVE", lambda e: e.tensor_tensor(out=dd.ap, in0=m2.ap, in1=m1.ap, op=ALU.subtract), reads=[m1, m2], writes=[dd])
                    k.op("ACT", lambda e: e.activation(out=ex.ap, in_=dd.ap, func=AF.Exp), reads=[dd], writes=[ex])
                    k.op("DVE", lambda e: e.tensor_scalar(out=w1.ap, in0=ex.ap, scalar1=1.0, scalar2=None, op0=ALU.add), reads=[ex], writes=[w1])
                    k.op("DVE", lambda e: e.reciprocal(out=w1.ap, in_=w1.ap), reads=[w1], writes=[w1])
                    k.op("DVE", lambda e: e.tensor_tensor(out=w2.ap, in0=ex.ap, in1=w1.ap, op=ALU.mult), reads=[ex, w1], writes=[w2])
                    k.op("DVE", lambda e: e.tensor_tensor(out=w1.ap, in0=w1.ap, in1=gw.ap, op=ALU.mult), reads=[w1, gw], writes=[w1])
                    k.op("DVE", lambda e: e.tensor_tensor(out=w2.ap, in0=w2.ap, in1=gw.ap, op=ALU.mult), reads=[w2, gw], writes=[w2])
                    k.op("DVE", lambda e: e.tensor_scalar(out=r_ew.ap, in0=r_oh1.ap, scalar1=w1.ap, scalar2=None, op0=ALU.mult), reads=[r_oh1, w1], writes=[r_ew])
                    k.op("DVE", lambda e: e.scalar_tensor_tensor(out=r_ew.ap, in0=r_oh2.ap, scalar=w2.ap, in1=r_ew.ap, op0=ALU.mult, op1=ALU.add), reads=[r_oh2, w2, r_ew], writes=[r_ew])
                    k.op("DVE", lambda e: e.tensor_tensor(out=v3(r_G.ap, 8), in0=r_goh.ap.unsqueeze(2).to_broadcast([128, 8, 8]),
                                                          in1=r_ew.ap.unsqueeze(1).to_broadcast([128, 8, 8]), op=ALU.mult), reads=[r_goh, r_ew], writes=[r_G])
                    ptr = PD[b % 2]
                    k.op("PE", lambda e: e.transpose(ptr.ap[0:64, 0:128], r_G.ap, ident.ap), reads=[r_G, ident], writes=[ptr])
                    k.op("DVE", lambda e: e.tensor_copy(out=GT.ap[:, bs], in_=ptr.ap[0:64, 0:128]), reads=[ptr], writes=[GT])
            def load_expert(e_):
                wb = WB[e_ % 2]
                k.dma("POOL", v3(wb.ap[:, 0:4096], 8), wg[e_].rearrange("(a p) n -> p a n", p=128), writes=[wb])
                k.dma("POOL", v3(wb.ap[:, 4096:8192], 8), wu[e_].rearrange("(a p) n -> p a n", p=128), writes=[wb], part=True)
                k.dma("POOL", v3(wb.ap[:, 8192:12288], 4), wd[e_].rearrange("(a p) n -> p a n", p=128), writes=[wb], part=True)

            def emit_gu(e_, t, i):
                wb = WB[e_ % 2]
                Wg3 = v3(wb.ap[:, 0:4096], 8)
                Wu3 = v3(wb.ap[:, 4096:8192], 8)
                pb = PBK[i % 2]
                eb = EB[i % 2]
                k.op("DVE", lambda e: e.tensor_copy(out=eb.ap, in_=ident.ap[0:64, e_:e_ + 1].to_broadcast([64, 128])), reads=[ident], writes=[eb])
                k.op("PE", lambda e: e.matmul(pb.ap, eb.ap, GT.ap[:, t * 512:(t + 1) * 512], start=True, stop=True), reads=[eb, GT], writes=[pb])
                for f in range(4):
                    pg, pu = PG[f % 2], PU[f % 2]
                    for kc in range(8):
                        k.op("PE", lambda e: e.matmul(pg.ap, Wg3[:, kc, f * 128:(f + 1) * 128], UT[t].ap[:, kc, :], start=(kc == 0), stop=(kc == 7)),
                             reads=[wb, UT[t]], writes=[pg], pe_acc=(kc > 0))
                    for kc in range(8):
                        k.op("PE", lambda e: e.matmul(pu.ap, Wu3[:, kc, f * 128:(f + 1) * 128], UT[t].ap[:, kc, :], start=(kc == 0), stop=(kc == 7)),
                             reads=[wb, UT[t]], writes=[pu], pe_acc=(kc > 0))
                    s_, tt = S_[f % 2], TT[f % 2]
                    k.op("ACT", lambda e: e.activation(out=s_.ap, in_=pg.ap, func=AF.Silu), reads=[pg], writes=[s_])
                    k.op("DVE", lambda e: e.tensor_tensor(out=tt.ap, in0=s_.ap, in1=pu.ap, op=ALU.mult), reads=[s_, pu], writes=[tt])
                    k.op("DVE", lambda e: e.tensor_tensor(out=HS[i % 2][f].ap, in0=tt.ap, in1=pb.ap, op=ALU.mult), reads=[tt, pb], writes=[HS[i % 2][f]])

            def emit_dn(e_, t, i):
                wb = WB[e_ % 2]
                Wd3 = v3(wb.ap[:, 8192:12288], 4)
                for d in range(8):
                    pd = PD[d % 2]
                    for f in range(4):
                        k.op("PE", lambda e: e.matmul(pd.ap, Wd3[:, f, d * 128:(d + 1) * 128], HS[i % 2][f].ap, start=(f == 0), stop=(f == 3)),
                             reads=[wb, HS[i % 2][f]], writes=[pd], pe_acc=(f > 0))
                    k.op("DVE", lambda e: e.tensor_tensor(out=HT[t][d].ap, in0=HT[t][d].ap, in1=pd.ap, op=ALU.add), reads=[pd, HT[t][d]], writes=[HT[t][d]])

            seq = [(e_, t) for e_ in range(NE) for t in range(NTL)]
            load_expert(0)
            prev = None
            for i, (e_, t) in enumerate(seq):
                emit_gu(e_, t, i)
                if prev is not None:
                    emit_dn(*prev)
                if t == 0 and e_ + 1 < NE:
                    load_expert(e_ + 1)
                prev = (e_, t, i)
            emit_dn(*prev)
            k.dma("POOL", v3(WB[0].ap[:, 0:8192], 8), plg.rearrange("(a p) n -> p a n", p=128), writes=[WB[0]])
            k.dma("POOL", v3(WB[1].ap[:, 0:2048], 2), plp.rearrange("(a p) n -> p a n", p=128), writes=[WB[1]])
            for t in range(NTL):
                cs = slice(c0 + t * 512, c0 + (t + 1) * 512)
                hap = h3[:, :, t * 512:(t + 1) * 512]
                k.dma("POOL", v3(PB.ap, 2), pT[:, cs].rearrange("(a p) n -> p a n", p=128), writes=[PB])
                rmsnorm(HT[t], hap, 8, UT[t], UT[t].ap, PBK[0])
                for d in range(8):
                    pg, pu = PG[d % 2], PU[d % 2]
                    for kc in range(8):
                        k.op("PE", lambda e: e.matmul(pg.ap, v3(WB[0].ap[:, 0:8192], 8)[:, kc, d * 128:(d + 1) * 128], UT[t].ap[:, kc, :],
                                                      start=(kc == 0), stop=(kc == 7)), reads=[WB[0], UT[t]], writes=[pg], pe_acc=(kc > 0))
                    for kc in range(2):
                        k.op("PE", lambda e: e.matmul(pu.ap, v3(WB[1].ap[:, 0:2048], 2)[:, kc, d * 128:(d + 1) * 128], v3(PB.ap, 2)[:, kc, :],
                                                      start=(kc == 0), stop=(kc == 1)), reads=[WB[1], PB], writes=[pu], pe_acc=(kc > 0))
                    sf = STF[d % 2]
                    k.op("ACT", lambda e: e.activation(out=sf.ap, in_=pg.ap, func=AF.Sigmoid, bias=vec.ap[:, 16 + d:17 + d], scale=1.0), reads=[pg, vec], writes=[sf])
                    k.op("DVE", lambda e: e.tensor_tensor(out=sf.ap, in0=sf.ap, in1=pu.ap, op=ALU.mult), reads=[sf, pu], writes=[sf])
                    k.op("DVE", lambda e: e.tensor_tensor(out=HT[t][d].ap, in0=HT[t][d].ap, in1=sf.ap, op=ALU.add), reads=[sf, HT[t][d]], writes=[HT[t][d]])
            if mode == "A":
                k.dma("POOL", v3(WB[0].ap, 8), wqkv[:, 0:1536].rearrange("(a p) n -> p a n", p=128), writes=[WB[0]])
                k.dma("POOL", v3(WB[1].ap, 8), wqkv[:, 1536:3072].rearrange("(a p) n -> p a n", p=128), writes=[WB[1]])
                for t in range(NTL):
                    cs = slice(c0 + t * 512, c0 + (t + 1) * 512)
                    hap = h3[:, :, t * 512:(t + 1) * 512]
                    for d in range(8):
                        k.dma("SP", hT_o[d * 128:(d + 1) * 128, cs], HT[t][d].ap, reads=[HT[t][d]], writes=[ho_t], part=True)
                    rmsnorm(HT[t], hap, 24, UT[t], UT[t].ap, PBK[0])
                    U3 = UT[t].ap
                    for n in range(16):
                        col = n * 128
                        wbt = WB[0] if col < 1536 else WB[1]
                        lc = col if col < 1536 else col - 1536
                        bank = PG[n % 2]
                        for kc in range(8):
                            k.op("PE", lambda e: e.matmul(bank.ap, v3(wbt.ap, 8)[:, kc, lc:lc + 128], U3[:, kc, :], start=(kc == 0), stop=(kc == 7)),
                                 reads=[wbt, UT[t]], writes=[bank], pe_acc=(kc > 0))
                        sg = STG[n % 2]
                        sc = (1.0 / np.sqrt(128.0)) if n < 8 else 1.0
                        k.op("ACT", lambda e: e.activation(out=sg.ap, in_=bank.ap, func=AF.Identity, scale=float(sc)), reads=[bank], writes=[sg])
                        if n < 8:
                            k.dma("SP", qT_o[n * 128:(n + 1) * 128, cs], sg.ap, reads=[sg], writes=[qT_t], part=True, sem_of=sg)
                        else:
                            k.dma("SP", kT_o[(n - 8) * 128:(n - 7) * 128, cs], sg.ap, reads=[sg], writes=[kT_t], part=True, sem_of=sg)
                    for b in range(4):
                        for hh in range(2):
                            bank = PU[hh]
                            for kc in range(8):
                                k.op("PE", lambda e: e.matmul(bank.ap, U3[:, kc, b * 128:(b + 1) * 128], v3(WB[1].ap, 8)[:, kc, 512 + hh * 512:1024 + hh * 512],
                                                              start=(kc == 0), stop=(kc == 7)), reads=[WB[1], UT[t]], writes=[bank], pe_acc=(kc > 0))
                            sg = STG[hh]
                            k.op("ACT", lambda e: e.activation(out=sg.ap, in_=bank.ap, func=AF.Copy), reads=[bank], writes=[sg])
                            r0 = c0 + t * 512 + b * 128
                            k.dma("SP", v_o[r0:r0 + 128, hh * 512:(hh + 1) * 512], sg.ap, reads=[sg], writes=[v_t], part=True, sem_of=sg)
                fin = [qT_t, kT_t, v_t, ho_t]
            else:
                for t in range(NTL):
                    cs = slice(c0 + t * 512, c0 + (t + 1) * 512)
                    hap = h3[:, :, t * 512:(t + 1) * 512]
                    k.op("ACT", lambda e: e.activation(out=v3(SQ.ap, 8), in_=hap, func=AF.Square), reads=HT[t], writes=[SQ])
                    bank = PBK[0]
                    for kc in range(8):
                        k.op("PE", lambda e: e.matmul(bank.ap, ones_bf.ap, v3(SQ.ap, 8)[:, kc, :], start=(kc == 0), stop=(kc == 7)),
                             reads=[SQ, ones_bf], writes=[bank], pe_acc=(kc > 0))
                    k.op("ACT", lambda e: e.activation(out=LNV.ap, in_=bank.ap, func=AF.Ln, scale=1.0 / 1024, bias=EPS), reads=[bank], writes=[LNV])
                    k.op("ACT", lambda e: e.activation(out=RSTD.ap, in_=LNV.ap, func=AF.Exp, scale=-0.5), reads=[LNV], writes=[RSTD])
                    for d in range(8):
                        k.op("DVE", lambda e: e.scalar_tensor_tensor(out=HT[t][d].ap, in0=HT[t][d].ap, scalar=vec.ap[:, 24 + d:25 + d], in1=RSTD.ap,
                                                                     op0=ALU.mult, op1=ALU.mult), reads=[HT[t][d], RSTD, vec], writes=[HT[t][d]])
                        k.dma("SP", out_o[d * 128:(d + 1) * 128, cs], HT[t][d].ap, reads=[HT[t][d]], writes=[out_t], part=True)
                fin = [out_t]
            if hf == NH - 1:
                k.finish("SP", fin)
        print("tok ninst", k.ninst)
    return nc


def build_attn(L, NHD=2):
    nc = bass.Bass("TRN2", target_bir_lowering=False)

    def din(name, shape, dt=F32):
        return nc.dram_tensor(name, list(shape), dt, kind="ExternalInput").ap()

    qT = din("qT", [NHD, 128, L], BF16)
    kT = din("kT", [NHD, 128, L], BF16)
    vP = din("vP", [NHD, 128, L // 128, 128], BF16)
    cst = din("cst", [128, 256 + 4 * 512])
    oT = nc.dram_tensor("oT", [NHD, 128, L], BF16, kind="ExternalOutput").ap()
    NQT = L // 512
    NB = L // 128
    with ExitStack() as st:
        k = K(nc, st)
        Qs = k.sbt([128, L], BF16, "Qs")
        Ks = k.sbt([128, L], BF16, "Ks")
        Vs = k.sbt([128, L], BF16, "Vs")
        V3 = v3(Vs.ap, NB)
        cf = k.sbt([128, 256 + 2048], F32, "cf")
        trin = k.sbt([128, 128], BF16, "trin")
        onen = k.sbt([128, 128], BF16, "onen")
        m01 = k.sbt([128, 2048], BF16, "m01")
        mneg = k.sbt([128, 2048], F32, "mneg")
        E_ = [k.sbt([128, 512], F32, "E%d" % i) for i in range(3)]
        SPf = [k.sbt([128, 512], BF16, "SPf%d" % i) for i in range(3)]
        SPm = [k.sbt([128, 512], BF16, "SPm%d" % i) for i in range(3)]
        TMP = [k.sbt([128, 512], F32, "TMP%d" % i) for i in range(3)]
        AT = [k.sbt([128, 512], BF16, "AT%d" % i) for i in range(3)]
        OFFS = [k.sbt([128, 512], F32, "OFF%d" % i) for i in range(2)]
        offi = [0]
        OST = [k.sbt([128, 512], BF16, "OST%d" % i) for i in range(2)]
        BK = [T(k.ps([128, 512], F32, "bk%d" % i)[:]) for i in range(8)]
        PA, PBb, PO = BK[0:3], BK[3:6], BK[6:8]
        oT_t = T(oT)

        k.dma("SP", cf.ap, cst, writes=[cf])
        k.op("DVE", lambda e: e.tensor_copy(out=trin.ap, in_=cf.ap[:, 0:128]), reads=[cf], writes=[trin])
        k.op("DVE", lambda e: e.tensor_copy(out=onen.ap, in_=cf.ap[:, 128:256]), reads=[cf], writes=[onen])
        k.op("DVE", lambda e: e.tensor_copy(out=m01.ap, in_=cf.ap[:, 256:2304]), reads=[cf], writes=[m01])
        k.op("DVE", lambda e: e.tensor_scalar(out=mneg.ap, in0=cf.ap[:, 256:2304], scalar1=-1.0, scalar2=30000.0, op0=ALU.add, op1=ALU.mult),
             reads=[cf], writes=[mneg])

        jobs = []
        for hd in range(NHD):
            for J in range(NQT):
                nb = 4 * J + 4
                for i, n in enumerate(range(nb - 1, -1, -1)):
                    jobs.append(dict(hd=hd, J=J, n=n, first=(i == 0), last=(n == 0), r=(n - 4 * J) if n >= 4 * J else -1))

        def load_head(hd):
            k.dma("SP", Qs.ap, qT[hd], writes=[Qs])
            k.dma("SP", Ks.ap, kT[hd], writes=[Ks])
            k.dma("SP", V3, vP[hd], writes=[Vs])

        def s1(j, i):
            if j["first"] and j["J"] == 0:
                load_head(j["hd"])
            A = PA[i % 3]
            qs = slice(j["J"] * 512, (j["J"] + 1) * 512)
            ks = slice(j["n"] * 128, (j["n"] + 1) * 128)
            k.op("PE", lambda e: e.matmul(A.ap, Ks.ap[:, ks], Qs.ap[:, qs], start=True, stop=False), reads=[Ks, Qs], writes=[A])
            k.op("ACT", lambda e: e.activation(out=E_[i % 3].ap, in_=A.ap, func=AF.Exp), reads=[A], writes=[E_[i % 3]])

        def s1b(j, i):
            k.op("ACT", lambda e: e.activation(out=SPf[i % 3].ap, in_=E_[i % 3].ap, func=AF.Ln, bias=1.0, scale=1.0), reads=[E_[i % 3]], writes=[SPf[i % 3]])
            if j["r"] >= 0:
                r = j["r"]
                k.op("POOL", lambda e: e.tensor_tensor(out=SPm[i % 3].ap, in0=SPf[i % 3].ap, in1=m01.ap[:, r * 512:(r + 1) * 512], op=ALU.mult),
                     reads=[SPf[i % 3], m01], writes=[SPm[i % 3]])

        def s2(j, i):
            A, B = PA[i % 3], PBb[i % 3]
            sp = SPm[i % 3] if j["r"] >= 0 else SPf[i % 3]
            OFF = OFFS[offi[0] % 2]
            if j["first"]:
                k.op("DVE", lambda e: e.memset(OFF.ap, 0.0), writes=[OFF])
            k.op("PE", lambda e: e.matmul(A.ap, trin.ap, sp.ap, start=False, stop=True), reads=[trin, sp], writes=[A], pe_acc=True)
            k.op("PE", lambda e: e.matmul(B.ap, onen.ap, sp.ap, start=True, stop=True), reads=[onen, sp], writes=[B])
            tm = TMP[i % 3]
            k.op("DVE", lambda e: e.tensor_tensor(out=tm.ap, in0=A.ap, in1=OFF.ap, op=ALU.add), reads=[A, OFF], writes=[tm])
            if j["r"] >= 0:
                r = j["r"]
                k.op("DVE", lambda e: e.tensor_tensor(out=tm.ap, in0=tm.ap, in1=mneg.ap[:, r * 512:(r + 1) * 512], op=ALU.add), reads=[tm, mneg], writes=[tm])
            k.op("ACT", lambda e: e.activation(out=AT[i % 3].ap, in_=tm.ap, func=AF.Exp), reads=[tm], writes=[AT[i % 3]])
            if not j["last"]:
                OFN = OFFS[(offi[0] + 1) % 2]
                k.op("DVE", lambda e: e.tensor_tensor(out=OFN.ap, in0=OFF.ap, in1=B.ap, op=ALU.add), reads=[B, OFF], writes=[OFN])
            offi[0] += 1

        grp = [0]

        def s3(j, i):
            O = PO[grp[0] % 2]
            k.op("PE", lambda e: e.matmul(O.ap, V3[:, j["n"], :], AT[i % 3].ap, start=j["first"], stop=j["last"]),
                 reads=[Vs, AT[i % 3]], writes=[O], pe_acc=(not j["first"]))
            if j["last"]:
                og = OST[grp[0] % 2]
                k.op("ACT", lambda e: e.activation(out=og.ap, in_=O.ap, func=AF.Copy), reads=[O], writes=[og])
                k.dma("SP", oT[j["hd"], :, j["J"] * 512:(j["J"] + 1) * 512], og.ap, reads=[og], writes=[oT_t], part=True, sem_of=og)
                grp[0] += 1

        n = len(jobs)
        stages = [s1, s1b, s2, s3]
        done = [0] * n

        def run_next(jj):
            stages[done[jj]](jobs[jj], jj)
            done[jj] += 1

        for i in range(n + 3):
            if i < n and jobs[i]["first"] and jobs[i]["J"] == 0 and i > 0:
                for jj in range(max(0, i - 3), i):
                    while done[jj] < 4:
                        run_next(jj)
            for d_ in range(4):
                jj = i - d_
                if 0 <= jj < n and done[jj] == d_:
                    run_next(jj)
        assert all(x == 4 for x in done)
        k.finish("SP", [oT_t])
        print("attn ninst", k.ninst, "jobs", n)
    return nc


def attn_consts():
    s = np.arange(128)
    tri_neg = -(s[:, None] >= s[None, :]).astype(np.float32)
    ones_neg = -np.ones((128, 128), np.float32)
    q = np.arange(512)
    masks = [(q[None, :] > (s[:, None] + 128 * r)).astype(np.float32) for r in range(4)]
    return np.ascontiguousarray(np.concatenate([tri_neg, ones_neg] + masks, axis=1))


def build_ssd(L):
    nc = bass.Bass("TRN2", target_bir_lowering=False)

    def din(name, shape, dt=F32):
        return nc.dram_tensor(name, list(shape), dt, kind="ExternalInput").ap()

    xT = din("xT", [1024, L])
    w_in = din("w_in", [1024, 1288])
    vecs = din("vecs", [128, 38])
    rowc = din("rowc", [1, 1040])
    cst = din("cst", [128, 640])
    ynT = nc.dram_tensor("ynT", [512, L], BF16, kind="ExternalOutput").ap()
    NTL = L // 512
    with ExitStack() as st:
        k = K(nc, st)
        W = k.sbt([128, 8 * 1288], BF16, "W")
        W3 = v3(W.ap, 8)
        XT = [k.sbt([128, 8 * 512], F32, "XT%d" % i) for i in range(2)]
        UT = [k.sbt([128, 8 * 512], BF16, "UT%d" % i) for i in range(2)]
        SQ = k.sbt([128, 8 * 512], BF16, "SQ")
        LNV = k.sbt([128, 512], F32, "LNV")
        RSTD = k.sbt([128, 512], F32, "RSTD")
        vec = k.sbt([128, 38], F32, "vec_sb")
        bc = k.sbt([128, 1040], F32, "bc")
        cf = k.sbt([128, 640], F32, "cf")
        identb = k.sbt([128, 128], BF16, "identb")
        ones_bf = k.sbt([128, 128], BF16, "ones_bf")
        A_bc = k.sbt([128, 8], F32, "A_bc")
        XR = [k.sbt([128, 515], F32, "XR%d" % i) for i in range(6)]
        ACC = [k.sbt([128, 512], F32, "ACC%d" % i) for i in range(2)]
        XC = [k.sbt([128, 6 * 512], BF16, "XC%d" % i) for i in range(2)]
        ZS_2 = [k.sbt([128, 512], F32, "ZS_%d" % i) for i in range(3)]
        DTt_2 = [k.sbt([128, 8], F32, "DTt_%d" % i) for i in range(3)]
        DT_2 = [k.sbt([128, 8], F32, "DT_%d" % i) for i in range(3)]
        AA_2 = [k.sbt([128, 8], F32, "AA_%d" % i) for i in range(3)]
        EXPS_2 = [k.sbt([128, 24], F32, "EXPS_%d" % i) for i in range(3)]
        LH = [k.sbt([128, 128], F32, "LH%d" % i) for i in range(2)]
        DEC_2 = [k.sbt([128, 1024], F32, "DEC_%d" % i) for i in range(3)]
        CBm_2 = [k.sbt([128, 128], F32, "CBm_%d" % i) for i in range(3)]
        WT_2 = [k.sbt([128, 1024], BF16, "WT_%d" % i) for i in range(3)]
        XTOK_2 = [k.sbt([128, 512], BF16, "XTOK_%d" % i) for i in range(3)]
        BTOK_2 = [k.sbt([128, 128], BF16, "BTOK_%d" % i) for i in range(3)]
        XDT_2 = [k.sbt([128, 512], BF16, "XDT_%d" % i) for i in range(3)]
        XW_2 = [k.sbt([128, 512], BF16, "XW_%d" % i) for i in range(3)]
        Y1_2 = [k.sbt([128, 512], F32, "Y1_%d" % i) for i in range(3)]
        Y2_2 = [k.sbt([128, 512], F32, "Y2_%d" % i) for i in range(3)]
        YZ_2 = [k.sbt([128, 512], F32, "YZ_%d" % i) for i in range(3)]
        YSQ_2 = [k.sbt([128, 512], F32, "YSQ_%d" % i) for i in range(3)]
        YN_2 = [k.sbt([128, 512], BF16, "YN_%d" % i) for i in range(3)]
        SF = k.sbt([128, 512], F32, "SF")
        SBF = k.sbt([128, 512], BF16, "SBF")
        sc_2 = [[k.sbt([128, 1], F32, "sc%d_%d" % (j, i)) for i in range(3)] for j in range(3)]
        LH4 = [k.sbt([128, 128], F32, "LHx%d" % i) for i in range(2)]
        YNT = [k.sbt([128, 4 * 512], BF16, "YNT%d" % i) for i in range(2)]
        BK = [T(k.ps([128, 512], F32, "bk%d" % i)[:]) for i in range(8)]
        P_proj, P_st, P_sm, P_sg0, P_sg1, P_tr, P_yd, P_yo = BK
        ynT_t = T(ynT)

        k.dma("SP", vec.ap, vecs, writes=[vec])
        k.dma("SP", cf.ap, cst, writes=[cf])
        k.dma("SP", bc.ap, rowc.partition_broadcast(128).rearrange("p o n -> p (o n)"), writes=[bc])
        k.dma("POOL", W3, w_in.rearrange("(a p) n -> p a n", p=128), writes=[W])
        ident = cf.ap[:, 0:128]
        tri = cf.ap[:, 128:256]
        Um = cf.ap[:, 256:384]
        maskU = cf.ap[:, 384:512]
        onesf = cf.ap[:, 512:640]
        k.op("DVE", lambda e: e.tensor_copy(out=identb.ap, in_=ident), reads=[cf], writes=[identb])
        k.op("DVE", lambda e: e.memset(ones_bf.ap, 1.0), writes=[ones_bf])
        k.op("ACT", lambda e: e.activation(out=A_bc.ap, in_=bc.ap[:, 8:16], func=AF.Exp), reads=[bc], writes=[A_bc])
        k.op("DVE", lambda e: e.tensor_scalar(out=A_bc.ap, in0=A_bc.ap, scalar1=-1.0, scalar2=None, op0=ALU.mult), reads=[A_bc], writes=[A_bc])
        for cc in range(6):
            k.op("DVE", lambda e: e.memset(XR[cc].ap[:, 0:3], 0.0), writes=[XR[cc]])
        k.op("DVE", lambda e: e.memset(SF.ap, 0.0), writes=[SF])
        k.op("DVE", lambda e: e.memset(SBF.ap, 0.0), writes=[SBF])
        dtb, Dx, gn = bc.ap[:, 0:8], bc.ap[:, 16:528], bc.ap[:, 528:1040]

        def load_x(t):
            k.dma("SP", v3(XT[t % 2].ap, 8), xT[:, t * 512:(t + 1) * 512].rearrange("(a p) n -> p a n", p=128), writes=[XT[t % 2]])

        def stage_a(t):
            if t + 1 < NTL:
                load_x(t + 1)
            xt, ut, xc = XT[t % 2], UT[t % 2], XC[t % 2]
            x3, u3, xc3 = v3(xt.ap, 8), v3(ut.ap, 8), v3(xc.ap, 6)
            k.op("ACT", lambda e: e.activation(out=v3(SQ.ap, 8), in_=x3, func=AF.Square), reads=[xt], writes=[SQ])
            for kc in range(8):
                k.op("PE", lambda e: e.matmul(P_proj.ap, ones_bf.ap, v3(SQ.ap, 8)[:, kc, :], start=(kc == 0), stop=(kc == 7)),
                     reads=[SQ, ones_bf], writes=[P_proj], pe_acc=(kc > 0))
            k.op("ACT", lambda e: e.activation(out=LNV.ap, in_=P_proj.ap, func=AF.Ln, scale=1.0 / 1024, bias=EPS), reads=[P_proj], writes=[LNV])
            k.op("ACT", lambda e: e.activation(out=RSTD.ap, in_=LNV.ap, func=AF.Exp, scale=-0.5), reads=[LNV], writes=[RSTD])
            for kc in range(8):
                k.op("DVE", lambda e: e.scalar_tensor_tensor(out=u3[:, kc, :], in0=x3[:, kc, :], scalar=vec.ap[:, kc:kc + 1], in1=RSTD.ap,
                                                             op0=ALU.mult, op1=ALU.mult), reads=[xt, RSTD, vec], writes=[ut])
            for cc in range(6):
                for kc in range(8):
                    k.op("PE", lambda e: e.matmul(P_proj.ap, W3[:, kc, 512 + cc * 128:512 + (cc + 1) * 128], u3[:, kc, :], start=(kc == 0), stop=(kc == 7)),
                         reads=[W, ut], writes=[P_proj], pe_acc=(kc > 0))
                xr, acc = XR[cc], ACC[cc % 2]
                k.op("ACT", lambda e: e.activation(out=xr.ap[:, 3:515], in_=P_proj.ap, func=AF.Copy), reads=[P_proj], writes=[xr])
                k.op("DVE", lambda e: e.tensor_scalar(out=acc.ap, in0=xr.ap[:, 0:512], scalar1=vec.ap[:, 8 + cc * 4:9 + cc * 4], scalar2=None, op0=ALU.mult),
                     reads=[xr, vec], writes=[acc])
                for kk in range(1, 4):
                    k.op("DVE", lambda e: e.scalar_tensor_tensor(out=acc.ap, in0=xr.ap[:, kk:kk + 512], scalar=vec.ap[:, 8 + cc * 4 + kk:9 + cc * 4 + kk],
                                                                 in1=acc.ap, op0=ALU.mult, op1=ALU.add), reads=[xr, vec, acc], writes=[acc])
                k.op("ACT", lambda e: e.activation(out=xc3[:, cc, :], in_=acc.ap, func=AF.Silu, bias=vec.ap[:, 32 + cc:33 + cc], scale=1.0),
                     reads=[acc, vec], writes=[xc])
                k.op("DVE", lambda e: e.tensor_copy(out=xr.ap[:, 0:3], in_=xr.ap[:, 512:515]), reads=[xr], writes=[xr])

        def chunk_gen(t, q):
            ut, xc = UT[t % 2], XC[t % 2]
            u3, xc3 = v3(ut.ap, 8), v3(xc.ap, 6)
            ynt = YNT[t % 2]
            cs = slice(q * 128, (q + 1) * 128)
            ci = t * 4 + q
            ZS = ZS_2[ci % 3]
            DTt = DTt_2[ci % 3]
            DT = DT_2[ci % 3]
            AA = AA_2[ci % 3]
            EXPS = EXPS_2[ci % 3]
            DEC = DEC_2[ci % 3]
            CBm = CBm_2[ci % 3]
            WT = WT_2[ci % 3]
            XTOK = XTOK_2[ci % 3]
            BTOK = BTOK_2[ci % 3]
            XDT = XDT_2[ci % 3]
            XW = XW_2[ci % 3]
            Y1 = Y1_2[ci % 3]
            Y2 = Y2_2[ci % 3]
            YZ = YZ_2[ci % 3]
            YSQ = YSQ_2[ci % 3]
            YN = YN_2[ci % 3]
            sc = sc_2[ci % 3]
            for kc in range(8):
                k.op("PE", lambda e: e.matmul(P_proj.ap, u3[:, kc, cs], W3[:, kc, 0:512], start=(kc == 0), stop=(kc == 7)),
                     reads=[W, ut], writes=[P_proj], pe_acc=(kc > 0))
            k.op("ACT", lambda e: e.activation(out=ZS.ap, in_=P_proj.ap, func=AF.Silu), reads=[P_proj], writes=[ZS])
            yield
            for kc in range(8):
                k.op("PE", lambda e: e.matmul(P_sm.ap[:, 0:8], u3[:, kc, cs], W3[:, kc, 1280:1288], start=(kc == 0), stop=(kc == 7)),
                     reads=[W, ut], writes=[P_sm], pe_acc=(kc > 0))
            k.op("DVE", lambda e: e.tensor_tensor(out=DTt.ap, in0=P_sm.ap[:, 0:8], in1=dtb, op=ALU.add), reads=[P_sm, bc], writes=[DTt])
            k.op("ACT", lambda e: e.activation(out=DTt.ap, in_=DTt.ap, func=AF.Exp), reads=[DTt], writes=[DTt])
            k.op("ACT", lambda e: e.activation(out=DT.ap, in_=DTt.ap, func=AF.Ln, bias=1.0, scale=1.0), reads=[DTt], writes=[DT])
            k.op("DVE", lambda e: e.tensor_tensor(out=AA.ap, in0=DT.ap, in1=A_bc.ap, op=ALU.mult), reads=[DT, A_bc], writes=[AA])
            yield
            trb = P_tr.ap.bitcast(BF16)
            for cc in range(4):
                k.op("PE", lambda e: e.transpose(trb[:, cc * 128:(cc + 1) * 128], xc3[:, cc, cs], identb.ap), reads=[xc, identb], writes=[P_tr])
            k.op("PE", lambda e: e.transpose(trb[:, 512:640], xc3[:, 4, cs], identb.ap), reads=[xc, identb], writes=[P_tr])
            k.op("ACT", lambda e: e.activation(out=XTOK.ap, in_=trb[:, 0:512], func=AF.Copy), reads=[P_tr], writes=[XTOK])
            k.op("ACT", lambda e: e.activation(out=BTOK.ap, in_=trb[:, 512:640], func=AF.Copy), reads=[P_tr], writes=[BTOK])
            yield
            k.op("PE", lambda e: e.matmul(P_sm.ap[:, 32:40], tri, AA.ap, start=True, stop=True), reads=[cf, AA], writes=[P_sm])
            k.op("PE", lambda e: e.matmul(P_sm.ap[:, 40:48], Um, AA.ap, start=True, stop=True), reads=[cf, AA], writes=[P_sm])
            k.op("PE", lambda e: e.matmul(P_sm.ap[:, 48:56], onesf, AA.ap, start=True, stop=True), reads=[cf, AA], writes=[P_sm])
            k.op("ACT", lambda e: e.activation(out=EXPS.ap, in_=P_sm.ap[:, 32:56], func=AF.Exp), reads=[P_sm], writes=[EXPS])
            e_l, dte, cd = EXPS.ap[:, 0:8], EXPS.ap[:, 8:16], EXPS.ap[:, 16:24]
            yield
            k.op("PE", lambda e: e.matmul(P_sm.ap[:, 128:256], xc3[:, 4, cs], xc3[:, 5, cs], start=True, stop=True), reads=[xc], writes=[P_sm])
            k.op("DVE", lambda e: e.tensor_tensor(out=CBm.ap, in0=P_sm.ap[:, 128:256], in1=maskU, op=ALU.mult), reads=[P_sm, cf], writes=[CBm])
            yield
            for hh in range(8):
                lh = (LH + LH4)[hh % 4]
                k.op("DVE", lambda e: e.tensor_scalar(out=lh.ap, in0=Um, scalar1=AA.ap[:, hh:hh + 1], scalar2=None, op0=ALU.mult), reads=[cf, AA], writes=[lh])
                bank = P_sg0 if hh < 4 else P_sg1
                k.op("PE", lambda e: e.matmul(bank.ap[:, (hh % 4) * 128:(hh % 4 + 1) * 128], lh.ap, tri, start=True, stop=True), reads=[lh, cf], writes=[bank])
            k.op("ACT", lambda e: e.activation(out=DEC.ap[:, 0:512], in_=P_sg0.ap, func=AF.Exp), reads=[P_sg0], writes=[DEC])
            yield
            k.op("ACT", lambda e: e.activation(out=DEC.ap[:, 512:1024], in_=P_sg1.ap, func=AF.Exp), reads=[P_sg1], writes=[DEC])
            k.op("DVE", lambda e: e.tensor_tensor(out=v3(WT.ap, 8), in0=v3(DEC.ap, 8), in1=CBm.ap.unsqueeze(1).to_broadcast([128, 8, 128]), op=ALU.mult),
                 reads=[DEC, CBm], writes=[WT])
            k.op("DVE", lambda e: e.tensor_tensor(out=v3(XDT.ap, 8), in0=v3(XTOK.ap, 8), in1=DT.ap.unsqueeze(2).to_broadcast([128, 8, 64]), op=ALU.mult),
                 reads=[XTOK, DT], writes=[XDT])
            k.op("DVE", lambda e: e.tensor_tensor(out=v3(XW.ap, 8), in0=v3(XDT.ap, 8), in1=dte.unsqueeze(2).to_broadcast([128, 8, 64]), op=ALU.mult),
                 reads=[XDT, EXPS], writes=[XW])
            yield
            for hh in range(8):
                k.op("PE", lambda e: e.matmul(P_yd.ap[:, hh * 64:(hh + 1) * 64], v3(WT.ap, 8)[:, hh, :], v3(XDT.ap, 8)[:, hh, :], start=True, stop=True),
                     reads=[WT, XDT], writes=[P_yd])
            k.op("PE", lambda e: e.matmul(P_yo.ap, xc3[:, 5, cs], SBF.ap, start=True, stop=True), reads=[xc, SBF], writes=[P_yo])
            k.op("DVE", lambda e: e.tensor_tensor(out=v3(Y1.ap, 8), in0=v3(P_yo.ap, 8), in1=e_l.unsqueeze(2).to_broadcast([128, 8, 64]), op=ALU.mult),
                 reads=[P_yo, EXPS], writes=[Y1])
            k.op("DVE", lambda e: e.tensor_tensor(out=Y1.ap, in0=Y1.ap, in1=P_yd.ap, op=ALU.add), reads=[Y1, P_yd], writes=[Y1])
            yield
            k.op("PE", lambda e: e.matmul(P_st.ap, BTOK.ap, XW.ap, start=True, stop=True), reads=[BTOK, XW], writes=[P_st])
            k.op("DVE", lambda e: e.tensor_tensor(out=v3(SF.ap, 8), in0=v3(SF.ap, 8), in1=cd.unsqueeze(2).to_broadcast([128, 8, 64]), op=ALU.mult),
                 reads=[SF, EXPS], writes=[SF])
            k.op("DVE", lambda e: e.tensor_tensor(out=SF.ap, in0=SF.ap, in1=P_st.ap, op=ALU.add), reads=[SF, P_st], writes=[SF])
            k.op("ACT", lambda e: e.activation(out=SBF.ap, in_=SF.ap, func=AF.Copy), reads=[SF], writes=[SBF])
            yield
            k.op("DVE", lambda e: e.tensor_tensor(out=Y2.ap, in0=XTOK.ap, in1=Dx, op=ALU.mult), reads=[XTOK, bc], writes=[Y2])
            k.op("DVE", lambda e: e.tensor_tensor(out=Y2.ap, in0=Y2.ap, in1=Y1.ap, op=ALU.add), reads=[Y2, Y1], writes=[Y2])
            k.op("DVE", lambda e: e.tensor_tensor(out=YZ.ap, in0=Y2.ap, in1=ZS.ap, op=ALU.mult), reads=[Y2, ZS], writes=[YZ])
            k.op("ACT", lambda e: e.activation(out=YSQ.ap, in_=YZ.ap, func=AF.Square, accum_out=sc[0].ap), reads=[YZ], writes=[YSQ, sc[0]])
            k.op("ACT", lambda e: e.activation(out=sc[1].ap, in_=sc[0].ap, func=AF.Ln, scale=1.0 / 512, bias=EPS), reads=[sc[0]], writes=[sc[1]])
            k.op("ACT", lambda e: e.activation(out=sc[2].ap, in_=sc[1].ap, func=AF.Exp, scale=-0.5), reads=[sc[1]], writes=[sc[2]])
            k.op("DVE", lambda e: e.scalar_tensor_tensor(out=YN.ap, in0=YZ.ap, scalar=sc[2].ap, in1=gn, op0=ALU.mult, op1=ALU.mult),
                 reads=[YZ, sc[2], bc], writes=[YN])
            yield
            for cc in range(4):
                k.op("PE", lambda e: e.transpose(trb[:, cc * 128:(cc + 1) * 128], YN.ap[:, cc * 128:(cc + 1) * 128], identb.ap), reads=[YN, identb], writes=[P_tr])
            k.op("ACT", lambda e: e.activation(out=v3(ynt.ap, 4)[:, :, cs], in_=trb[:, 0:512].rearrange("p (a n) -> p a n", a=4), func=AF.Copy),
                 reads=[P_tr], writes=[ynt])
            if q == 3:
                k.dma("SP", ynT[:, t * 512:(t + 1) * 512].rearrange("(a p) n -> p a n", p=128), v3(ynt.ap, 4), reads=[ynt], writes=[ynT_t], part=True, sem_of=ynt)


        load_x(0)
        NS, PER = 12, 6
        chunks = [(t, q) for t in range(NTL) for q in range(4)]
        gens = {}
        nslot = (len(chunks) - 1) * PER + NS + 2
        for tau in range(nslot):
            for ci, (t, q) in enumerate(chunks):
                st_ = tau - ci * PER
                if st_ < 0:
                    break
                if ci in gens and gens[ci] is None:
                    continue
                if ci not in gens:
                    if q == 0:
                        stage_a(t)
                    gens[ci] = chunk_gen(t, q)
                try:
                    next(gens[ci])
                except StopIteration:
                    gens[ci] = None
        k.finish("SP", [ynT_t])
        print("ssd ninst", k.ninst)
    return nc


def ssd_consts():
    s = np.arange(128)
    ident = np.eye(128, dtype=np.float32)
    tri = (s[:, None] <= s[None, :]).astype(np.float32)
    U = (s[:, None] > s[None, :]).astype(np.float32)
    maskU = (s[None, :] >= s[:, None]).astype(np.float32)
    ones = np.ones((128, 128), np.float32)
    return np.ascontiguousarray(np.concatenate([ident, tri, U, maskU, ones], axis=1))


def ssd_host_inputs(g, ssd_norm, w_in, conv_w, conv_b, dt_bias, a_log, d_skip, gnorm):
    cols = np.concatenate([np.arange(g * 512, (g + 1) * 512), 2048 + np.arange(g * 512, (g + 1) * 512),
                           4096 + np.arange(g * 128, (g + 1) * 128), 4096 + 512 + np.arange(g * 128, (g + 1) * 128),
                           5120 + np.arange(g * 8, (g + 1) * 8)])
    w = np.ascontiguousarray(w_in[:, cols])
    ch = np.concatenate([np.arange(g * 512, (g + 1) * 512), 2048 + np.arange(g * 128, (g + 1) * 128), 2048 + 512 + np.arange(g * 128, (g + 1) * 128)])
    cw = conv_w[:, ch]
    cb = conv_b[ch]
    vecs = np.zeros((128, 38), np.float32)
    vecs[:, 0:8] = ssd_norm.reshape(8, 128).T
    for cc in range(6):
        vecs[:, 8 + cc * 4:12 + cc * 4] = cw[:, cc * 128:(cc + 1) * 128].T
        vecs[:, 32 + cc] = cb[cc * 128:(cc + 1) * 128]
    rowc = np.concatenate([dt_bias[g * 8:(g + 1) * 8], a_log[g * 8:(g + 1) * 8], np.repeat(d_skip[g * 8:(g + 1) * 8], 64),
                           gnorm[g * 512:(g + 1) * 512]])[None, :].astype(np.float32)
    return w, vecs, np.ascontiguousarray(rowc)


_CACHE = {}


def _prog(name, fn, *a):
    key = (name,) + a
    if key not in _CACHE:
        _CACHE[key] = fn(*a)
    return _CACHE[key]


def _vcol(v):
    return np.asarray(v, np.float32).reshape(8, 128).T


def kernel(x, p, ssd_norm, ssd_w_in, ssd_conv_w, ssd_conv_b, ssd_dt_bias, ssd_a_log, ssd_d, ssd_gnorm, ssd_w_out,
           sb_norm, sb_w_qkv, sb_w_o, moe_norm, moe_w_rg, moe_b_rg, moe_w_re, moe_b_re, moe_w_gate, moe_w_up, moe_w_down,
           ple_norm, ple_w_gate, ple_b_gate, ple_w_proj, final_norm):
    f32 = lambda a: np.ascontiguousarray(np.asarray(a, dtype=np.float32))
    x, p = f32(x), f32(p)
    Bsz, L, D = x.shape
    NC = 8
    NT = (Bsz * L) // NC
    PB = NC // Bsz
    cores = list(range(NC))
    ident = np.eye(128, dtype=np.float32)

    nc1 = _prog("ssd", build_ssd, L)
    cst1 = ssd_consts()
    xTb = [np.ascontiguousarray(x[b].T) for b in range(Bsz)]
    maps = []
    for c in cores:
        b, g = c // PB, c % PB
        w, vecs, rowc = ssd_host_inputs(g, f32(ssd_norm[0]), f32(ssd_w_in[0]), f32(ssd_conv_w[0]), f32(ssd_conv_b[0]),
                                        f32(ssd_dt_bias[0]), f32(ssd_a_log[0]), f32(ssd_d[0]), f32(ssd_gnorm[0]))
        maps.append(dict(xT=xTb[b], w_in=w, vecs=vecs, rowc=rowc, cst=cst1))
    r1 = run_bass_kernel_spmd(nc1, maps, core_ids=cores).results
    ynT = [np.concatenate([r1[b * PB + g]["ynT"] for g in range(PB)], axis=0) for b in range(Bsz)]

    mcst = moe_consts(NT)

    def tok_maps(i, hT_list, aT_list, w_a, tail_norm, extra):
        vecs = np.ascontiguousarray(np.concatenate([_vcol(moe_norm[i]), _vcol(ple_norm[i]), _vcol(ple_b_gate[i]), _vcol(tail_norm)], axis=1))
        wr = np.ascontiguousarray(np.concatenate([f32(moe_w_rg[i]), f32(moe_w_re[i])], axis=1))
        br = np.ascontiguousarray(np.concatenate([f32(moe_b_rg[i]), f32(moe_b_re[i])])[None, :])
        wg_, wu_, wd_ = f32(moe_w_gate[i]), f32(moe_w_up[i]), f32(moe_w_down[i])
        plg_, plp_ = f32(ple_w_gate[i]), f32(ple_w_proj[i])
        out = []
        for c in cores:
            b, j = c // PB, c % PB
            sl = slice(j * NT, (j + 1) * NT)
            m = dict(hT=hT_list[c], aT=np.ascontiguousarray(aT_list[b][:, sl]), w_a=w_a, vecs=vecs, wr=wr, br=br, wg=wg_, wu=wu_, wd=wd_,
                     plg=plg_, plp=plp_, pT=np.ascontiguousarray(p[i, b, sl].T), mcst=mcst)
            m.update(extra)
            out.append(m)
        return out

    nc2 = _prog("tokA", build_tok2, NT, 2048, "A")
    hT0 = [np.ascontiguousarray(x[c // PB, (c % PB) * NT:(c % PB + 1) * NT].T) for c in cores]
    r2 = run_bass_kernel_spmd(nc2, tok_maps(0, hT0, ynT, f32(ssd_w_out[0]), f32(sb_norm[0]), dict(wqkv=f32(sb_w_qkv[0]))), core_ids=cores).results
    hT1 = [r2[c]["hT_o"] for c in cores]
    qTb = [np.concatenate([r2[b * PB + j]["qT_o"] for j in range(PB)], axis=1) for b in range(Bsz)]
    kTb = [np.concatenate([r2[b * PB + j]["kT_o"] for j in range(PB)], axis=1) for b in range(Bsz)]
    vb = [np.concatenate([r2[b * PB + j]["v_o"] for j in range(PB)], axis=0) for b in range(Bsz)]

    nc3 = _prog("attn", build_attn2, L)
    cst3 = attn_consts()
    maps = []
    for c in cores:
        b, hp = c // PB, c % PB
        rows = slice(hp * 256, (hp + 1) * 256)
        vP = np.ascontiguousarray(vb[b][:, rows].reshape(L // 128, 128, 2, 128).transpose(2, 1, 0, 3))
        maps.append(dict(qT=np.ascontiguousarray(qTb[b][rows].reshape(2, 128, L)), kT=np.ascontiguousarray(kTb[b][rows].reshape(2, 128, L)),
                         vP=vP, cst=cst3))
    r3 = run_bass_kernel_spmd(nc3, maps, core_ids=cores).results
    oT = [np.concatenate([r3[b * PB + hp]["oT"].reshape(256, L) for hp in range(PB)], axis=0) for b in range(Bsz)]

    nc4 = _prog("tokB", build_tok2, NT, 1024, "B")
    r4 = run_bass_kernel_spmd(nc4, tok_maps(1, hT1, oT, f32(sb_w_o[0]), f32(final_norm), {}), core_ids=cores).results
    out = np.empty((Bsz, L, D), np.float32)
    for c in cores:
        b, j = c // PB, c % PB
        out[b, j * NT:(j + 1) * NT, :] = r4[c]["out_o"].T
    return out


I32 = mybir.dt.int32
MB = 128
NBLK_OF = lambda NT: (2 * NT) // MB + 64


def moe_consts(NT):
    nblk = NBLK_OF(NT)
    s = np.arange(128)
    SL = (s[:, None] < s[None, :]).astype(np.float32)
    THR = np.tile((np.arange(64) * MB).astype(np.float32)[None, :], (128, 1))
    JB = np.tile((np.arange(nblk) * MB).astype(np.float32)[None, :], (128, 1))
    KP = (np.arange(8)[None, :] + 2 * s[:, None]).astype(np.float32)
    return np.ascontiguousarray(np.concatenate([np.eye(128, dtype=np.float32), SL, THR, JB, KP], axis=1))


def idma(self, out_ap, in_ap, idx_t, idx_ap, gather, reads=(), writes=(), part=False, bound=None):
    t = writes[0]
    if t.dsem is None:
        key = "d%d" % self.ndsem
        t.dsem = self.stack.enter_context(self.nc.semaphore(key))
        self.semobj[key] = t.dsem
        t.name = key
        self.ndsem += 1
    key = t.name
    for r in list(reads) + [idx_t]:
        self._wait_written("POOL", r)
    if t.w is not None and not (part and t.w[0] == key):
        self._need("POOL", *t.w)
    for kk, v in t.r.items():
        self._need("POOL", kk, v)
    off = bass.IndirectOffsetOnAxis(ap=idx_ap, axis=0)
    if gather and bound is not None:
        ins = self.nc.gpsimd.indirect_dma_start(out=out_ap, out_offset=None, in_=in_ap, in_offset=off, bounds_check=bound, oob_is_err=False)
    elif gather:
        ins = self.nc.gpsimd.indirect_dma_start(out=out_ap, out_offset=None, in_=in_ap, in_offset=off)
    else:
        ins = self.nc.gpsimd.indirect_dma_start(out=out_ap, out_offset=off, in_=in_ap, in_offset=None)
    t.dcnt += 16
    ins.then_inc(t.dsem, 16)
    for r in list(reads) + [idx_t]:
        r.r[key] = max(r.r.get(key, 0), t.dcnt)
    t.w = (key, t.dcnt)
    if not part:
        t.r = {}
    self.ninst += 1
    return ins


K.idma = idma


def build_tok2(NT, KA, mode):
    nc = bass.Bass("TRN2", target_bir_lowering=False)

    def din(name, shape, dt=F32):
        return nc.dram_tensor(name, list(shape), dt, kind="ExternalInput").ap()

    def dout(name, shape, dt=F32):
        return nc.dram_tensor(name, list(shape), dt, kind="ExternalOutput").ap()

    NBLK = NBLK_OF(NT)
    NROWS = NBLK * MB
    NTB = NT // 128
    NTILE = NT // 512
    hT = din("hT", [1024, NT])
    aT = din("aT", [KA, NT], BF16)
    w_a = din("w_a", [KA, 1024])
    vecs = din("vecs", [128, 32])
    wr = din("wr", [1024, 72])
    br = din("br", [1, 72])
    wg = din("wg", [64, 1024, 512])
    wu = din("wu", [64, 1024, 512])
    wd = din("wd", [64, 512, 1024])
    plg = din("plg", [1024, 1024])
    plp = din("plp", [256, 1024])
    pT = din("pT", [256, NT])
    NCST = 256 + 64 + NBLK + 8
    mcst = din("mcst", [128, NCST])
    if mode == "A":
        wqkv = din("wqkv", [1024, 3072])
        hT_o = dout("hT_o", [1024, NT])
        qT_o = dout("qT_o", [1024, NT], BF16)
        kT_o = dout("kT_o", [1024, NT], BF16)
        v_o = dout("v_o", [NT, 1024], BF16)
    else:
        out_o = dout("out_o", [1024, NT])
    H1 = nc.dram_tensor("H1s", [1024, NT], F32, kind="Internal").ap()
    Xs = nc.dram_tensor("Xs", [NROWS, 1024], BF16, kind="Internal").ap()
    Ys = nc.dram_tensor("Ys", [NROWS, 1024], F32, kind="Internal").ap()
    wg_r = wg.rearrange("e (p h r) n -> (e p h) (r n)", p=128, h=2, r=4)
    wu_r = wu.rearrange("e (p h r) n -> (e p h) (r n)", p=128, h=2, r=4)
    wd_r = wd.rearrange("e (p h r) n -> (e p h) (r n)", p=128, h=2, r=2)
    KAC = KA // 128

    with ExitStack() as st:
        k = K(nc, st)
        HTL = [k.sbt([128, 8 * 512], F32, "htl%d" % i) for i in range(1)] * 2
        UTL = [k.sbt([128, 8 * 512], BF16, "utl%d" % i) for i in range(1)] * 2
        WB = [k.sbt([128, 12288], BF16, "wb%d" % i) for i in range(2)]
        RA = k.sb([128, 8192], BF16, "regA")
        RAf = RA[:].bitcast(F32)
        ABS = [T(RA[:, 0:KAC * 512])]
        BIG = T(RAf[:, 0:2048])
        IGF = T(RAf[:, 2048:3072])
        XB = [T(RA[:, i * 1024:(i + 1) * 1024]) for i in range(2)]
        XTB = [T(RA[:, 2048 + i * 1024:2048 + (i + 1) * 1024]) for i in range(2)]
        YB = [T(RAf[:, 2048 + i * 1024:2048 + (i + 1) * 1024]) for i in range(2)]
        NRX = max(NTB * 1024, 32768)
        RX = k.sb([128, NRX], BF16, "regX")
        RXf = RX[:].bitcast(F32)
        XROWS = T(RX[:, 0:NTB * 1024])
        XR3 = v3(XROWS.ap, NTB)
        WQ = [T(RX[:, i * 12288:(i + 1) * 12288]) for i in range(2)]
        WBX = [T(RX[:, i * 12288:(i + 1) * 12288]) for i in range(2)]
        Y0 = [T(RXf[:, 12288 + i * 1024:12288 + (i + 1) * 1024]) for i in range(2)]
        Y1 = [T(RXf[:, 14336 + i * 1024:14336 + (i + 1) * 1024]) for i in range(2)]
        PBt = k.sbt([128, 2 * 512], BF16, "pbt")
        LNV = k.sbt([128, 512], F32, "lnv")
        RSTD = k.sbt([128, 512], F32, "rstd")
        vec = k.sbt([128, 32], F32, "vec_sb")
        cf = k.sbt([128, NCST], F32, "cf")
        ident = cf.ap[:, 0:128]
        identb = k.sbt([128, 128], BF16, "identb")
        SLb = k.sbt([128, 128], BF16, "SLb")
        ones_bf = k.sbt([128, 128], BF16, "ones_bf")
        wr_sb = k.sbt([128, 8 * 72], F32, "wr_sb")
        br_bc = k.sbt([128, 72], F32, "brbc")
        STG = [k.sbt([128, 512], BF16, "stg%d" % i) for i in range(2)]
        STF = [k.sbt([128, 512], F32, "stf%d" % i) for i in range(2)]
        r_lg = k.sbt([128, 72], F32, "r_lg")
        r_a = k.sbt([128, 8], F32, "r_a")
        r_goh = k.sbt([128, 8], F32, "r_goh")
        r_t64 = k.sbt([128, 64], F32, "r_t64")
        r_es = k.sbt([128, 8], F32, "r_es")
        r_es2 = k.sbt([128, 8], F32, "r_es2")
        r_oh1 = k.sbt([128, 8], F32, "r_oh1")
        r_oh2 = k.sbt([128, 8], F32, "r_oh2")
        r_s = [k.sbt([128, 1], F32, "r_s%d" % i) for i in range(12)]
        OHC = k.sbt([128, 128], BF16, "ohc")
        OHS = k.sbt([128, NTB * 128], BF16, "ohs")
        OHS3 = v3(OHS.ap, NTB)
        RUN = k.sbt([128, 128], F32, "run")
        PRS = k.sbt([128, 128], F32, "prs")
        JNK = k.sbt([128, 64], F32, "jnk")
        RK0 = k.sbt([128, NTB], F32, "rk0")
        RK1 = k.sbt([128, NTB], F32, "rk1")
        GA = k.sbt([128, NTB], F32, "ga")
        GB = k.sbt([128, NTB], F32, "gb")
        DI0 = k.sbt([128, NTB], I32, "di0")
        DI1 = k.sbt([128, NTB], I32, "di1")
        CNT = k.sbt([128, 64], F32, "cnt")
        NB_ = k.sbt([128, 64], F32, "nb_")
        PE_ = [k.sbt([128, 64], F32, "pe%d" % i) for i in range(2)]
        BASE1 = k.sbt([128, 64], F32, "base1")
        BASE2 = k.sbt([128, 64], F32, "base2")
        DF = k.sbt([128, NTB], F32, "df")
        EJ = k.sbt([128, NBLK], F32, "ej")
        SAME = k.sbt([128, NBLK], F32, "same")
        IGI = k.sbt([128, NBLK * 8], I32, "igi")
        IDI = k.sbt([128, NBLK * 4], I32, "idi")
        SS = [k.sbt([128, 512], BF16, "ss%d" % i) for i in range(2)]
        HH = [k.sbt([128, 512], BF16, "hh%d" % i) for i in range(2)]
        BK = [T(k.ps([128, 512], F32, "bk%d" % i)[:]) for i in range(8)]
        PG, PU, PD, PBK = BK[0:2], BK[2:4], BK[4:6], BK[6:8]
        H1_t, Xs_t, Ys_t = T(H1), T(Xs), T(Ys)
        if mode == "A":
            qT_t, kT_t, v_t, ho_t = T(qT_o), T(kT_o), T(v_o), T(hT_o)
        else:
            out_t = T(out_o)

        k.dma("SP", vec.ap, vecs, writes=[vec])
        k.dma("SP", cf.ap, mcst, writes=[cf])
        k.dma("SP", v3(wr_sb.ap, 8), wr.rearrange("(a p) n -> p a n", p=128), writes=[wr_sb])
        k.dma("SP", br_bc.ap, br.partition_broadcast(128).rearrange("p o n -> p (o n)"), writes=[br_bc])
        k.op("DVE", lambda e: e.memset(ones_bf.ap, 1.0), writes=[ones_bf])
        k.op("DVE", lambda e: e.memset(RUN.ap, 0.0), writes=[RUN])
        k.op("DVE", lambda e: e.tensor_copy(out=identb.ap, in_=ident), reads=[cf], writes=[identb])
        k.op("DVE", lambda e: e.tensor_copy(out=SLb.ap, in_=cf.ap[:, 128:256]), reads=[cf], writes=[SLb])
        THR = cf.ap[:, 256:320]
        JB = cf.ap[:, 320:320 + NBLK]
        KP = cf.ap[:, 320 + NBLK:328 + NBLK]
        for kc in range(8):
            k.op("DVE", lambda e: e.tensor_scalar(out=wr_sb.ap[:, kc * 72:(kc + 1) * 72], in0=wr_sb.ap[:, kc * 72:(kc + 1) * 72],
                                                  scalar1=vec.ap[:, kc:kc + 1], scalar2=None, op0=ALU.mult), reads=[vec, wr_sb], writes=[wr_sb])

        def rmsnorm(src_t, src_ap, gcol, dst_t, dst_ap, bank):
            k.op("ACT", lambda e: e.activation(out=dst_ap, in_=src_ap, func=AF.Square), reads=[src_t], writes=[dst_t])
            for kc in range(8):
                k.op("PE", lambda e: e.matmul(bank.ap, ones_bf.ap, dst_ap[:, kc, :], start=(kc == 0), stop=(kc == 7)),
                     reads=[dst_t, ones_bf], writes=[bank], pe_acc=(kc > 0))
            k.op("ACT", lambda e: e.activation(out=LNV.ap, in_=bank.ap, func=AF.Ln, scale=1.0 / 1024, bias=EPS), reads=[bank], writes=[LNV])
            k.op("ACT", lambda e: e.activation(out=RSTD.ap, in_=LNV.ap, func=AF.Exp, scale=-0.5), reads=[LNV], writes=[RSTD])
            for kc in range(8):
                k.op("DVE", lambda e: e.scalar_tensor_tensor(out=dst_ap[:, kc, :], in0=src_ap[:, kc, :], scalar=vec.ap[:, gcol + kc:gcol + kc + 1],
                                                             in1=RSTD.ap, op0=ALU.mult, op1=ALU.mult), reads=[src_t, RSTD, vec], writes=[dst_t])

        for kp in range(KAC // 8):
            k.dma("POOL", v3(WB[kp].ap[:, 0:8192], 8), w_a[kp * 1024:(kp + 1) * 1024, :].rearrange("(a p) n -> p a n", p=128), writes=[WB[kp]])
        for t in range(NTILE):
            cs = slice(t * 512, (t + 1) * 512)
            ht, ut, AB = HTL[t % 2], UTL[t % 2], ABS[0]
            h3, u3 = v3(ht.ap, 8), v3(ut.ap, 8)
            k.dma("SP", h3, hT[:, cs].rearrange("(a p) n -> p a n", p=128), writes=[ht])
            k.dma("SP", v3(AB.ap, KAC), aT[:, cs].rearrange("(a p) n -> p a n", p=128), writes=[AB])
            for d in range(8):
                bank = PD[d % 2]
                for kc in range(KAC):
                    wbt = WB[kc // 8]
                    k.op("PE", lambda e: e.matmul(bank.ap, v3(wbt.ap[:, 0:8192], 8)[:, kc % 8, d * 128:(d + 1) * 128], v3(AB.ap, KAC)[:, kc, :],
                                                  start=(kc == 0), stop=(kc == KAC - 1)), reads=[wbt, AB], writes=[bank], pe_acc=(kc > 0))
                k.op("DVE", lambda e: e.tensor_tensor(out=h3[:, d, :], in0=h3[:, d, :], in1=bank.ap, op=ALU.add), reads=[bank, ht], writes=[ht])
            k.dma("SP", H1[:, cs].rearrange("(a p) n -> p a n", p=128), h3, reads=[ht], writes=[H1_t], part=True)
            rmsnorm(ht, h3, 0, ut, u3, PBK[0])
            for b in range(4):
                i = t * 4 + b
                bs = slice(b * 128, (b + 1) * 128)
                plg_b, prs = PG[b % 2], PU[b % 2]
                for kc in range(8):
                    k.op("PE", lambda e: e.matmul(plg_b.ap[:, 0:72], h3[:, kc, bs], wr_sb.ap[:, kc * 72:(kc + 1) * 72], start=(kc == 0), stop=(kc == 7)),
                         reads=[ht, wr_sb], writes=[plg_b], pe_acc=(kc > 0))
                k.op("PE", lambda e: e.matmul(prs.ap[:, 0:2], RSTD.ap[:, bs], ident[:, 0:2], start=True, stop=True), reads=[RSTD, cf], writes=[prs])
                rt, gmax, ngmax, gsum, gw, m1, m2, dd, ex, w1, w2 = r_s[0:11]
                k.op("DVE", lambda e: e.tensor_copy(out=rt.ap, in_=prs.ap[:, 0:1]), reads=[prs], writes=[rt])
                k.op("DVE", lambda e: e.scalar_tensor_tensor(out=r_lg.ap, in0=plg_b.ap[:, 0:72], scalar=rt.ap, in1=br_bc.ap, op0=ALU.mult, op1=ALU.add),
                     reads=[plg_b, rt, br_bc], writes=[r_lg])
                gl = r_lg.ap[:, 0:8]
                el = r_lg.ap[:, 8:72]
                k.op("DVE", lambda e: e.tensor_reduce(out=gmax.ap, in_=gl, axis=AX.X, op=ALU.max), reads=[r_lg], writes=[gmax])
                k.op("DVE", lambda e: e.tensor_scalar(out=r_goh.ap, in0=gl, scalar1=gmax.ap, scalar2=None, op0=ALU.is_equal), reads=[r_lg, gmax], writes=[r_goh])
                k.op("DVE", lambda e: e.tensor_scalar(out=ngmax.ap, in0=gmax.ap, scalar1=-1.0, scalar2=None, op0=ALU.mult), reads=[gmax], writes=[ngmax])
                k.op("ACT", lambda e: e.activation(out=r_a.ap, in_=gl, func=AF.Exp, bias=ngmax.ap, scale=1.0, accum_out=gsum.ap), reads=[r_lg, ngmax], writes=[r_a, gsum])
                k.op("DVE", lambda e: e.reciprocal(out=gw.ap, in_=gsum.ap), reads=[gsum], writes=[gw])
                k.op("DVE", lambda e: e.tensor_tensor(out=v3(r_t64.ap, 8), in0=v3(el, 8), in1=r_goh.ap.unsqueeze(2).to_broadcast([128, 8, 8]), op=ALU.mult),
                     reads=[r_lg, r_goh], writes=[r_t64])
                k.op("DVE", lambda e: e.tensor_reduce(out=r_es.ap, in_=v3(r_t64.ap, 8).rearrange("p g j -> p j g"), axis=AX.X, op=ALU.add), reads=[r_t64], writes=[r_es])
                k.op("DVE", lambda e: e.tensor_reduce(out=m1.ap, in_=r_es.ap, axis=AX.X, op=ALU.max), reads=[r_es], writes=[m1])
                k.op("DVE", lambda e: e.tensor_scalar(out=r_oh1.ap, in0=r_es.ap, scalar1=m1.ap, scalar2=None, op0=ALU.is_equal), reads=[r_es, m1], writes=[r_oh1])
                k.op("DVE", lambda e: e.scalar_tensor_tensor(out=r_es2.ap, in0=r_oh1.ap, scalar=-1e30, in1=r_es.ap, op0=ALU.mult, op1=ALU.add), reads=[r_oh1, r_es], writes=[r_es2])
                k.op("DVE", lambda e: e.tensor_reduce(out=m2.ap, in_=r_es2.ap, axis=AX.X, op=ALU.max), reads=[r_es2], writes=[m2])
                k.op("DVE", lambda e: e.tensor_scalar(out=r_oh2.ap, in0=r_es2.ap, scalar1=m2.ap, scalar2=None, op0=ALU.is_equal), reads=[r_es2, m2], writes=[r_oh2])
                k.op("DVE", lambda e: e.tensor_tensor(out=dd.ap, in0=m2.ap, in1=m1.ap, op=ALU.subtract), reads=[m1, m2], writes=[dd])
                k.op("ACT", lambda e: e.activation(out=ex.ap, in_=dd.ap, func=AF.Exp), reads=[dd], writes=[ex])
                k.op("DVE", lambda e: e.tensor_scalar(out=w1.ap, in0=ex.ap, scalar1=1.0, scalar2=None, op0=ALU.add), reads=[ex], writes=[w1])
                k.op("DVE", lambda e: e.reciprocal(out=w1.ap, in_=w1.ap), reads=[w1], writes=[w1])
                k.op("DVE", lambda e: e.tensor_tensor(out=w2.ap, in0=ex.ap, in1=w1.ap, op=ALU.mult), reads=[ex, w1], writes=[w2])
                k.op("DVE", lambda e: e.tensor_tensor(out=GA.ap[:, i:i + 1], in0=w1.ap, in1=gw.ap, op=ALU.mult), reads=[w1, gw], writes=[GA])
                k.op("DVE", lambda e: e.tensor_tensor(out=GB.ap[:, i:i + 1], in0=w2.ap, in1=gw.ap, op=ALU.mult), reads=[w2, gw], writes=[GB])
                k.op("DVE", lambda e: e.tensor_tensor(out=v3(OHC.ap[:, 0:64], 8), in0=r_goh.ap.unsqueeze(2).to_broadcast([128, 8, 8]),
                                                      in1=r_oh1.ap.unsqueeze(1).to_broadcast([128, 8, 8]), op=ALU.mult), reads=[r_goh, r_oh1], writes=[OHC])
                k.op("DVE", lambda e: e.tensor_tensor(out=v3(OHC.ap[:, 64:128], 8), in0=r_goh.ap.unsqueeze(2).to_broadcast([128, 8, 8]),
                                                      in1=r_oh2.ap.unsqueeze(1).to_broadcast([128, 8, 8]), op=ALU.mult), reads=[r_goh, r_oh2, OHC], writes=[OHC])
                k.op("DVE", lambda e: e.tensor_copy(out=OHS3[:, i, :], in_=OHC.ap), reads=[OHC], writes=[OHS])
                ppr, pcs = PD[0], PD[1]
                k.op("PE", lambda e: e.matmul(ppr.ap[:, 0:128], SLb.ap, OHC.ap, start=True, stop=True), reads=[SLb, OHC], writes=[ppr])
                k.op("PE", lambda e: e.matmul(pcs.ap[:, 0:128], ones_bf.ap, OHC.ap, start=True, stop=True), reads=[ones_bf, OHC], writes=[pcs])
                k.op("DVE", lambda e: e.tensor_tensor(out=PRS.ap, in0=ppr.ap[:, 0:128], in1=RUN.ap, op=ALU.add), reads=[ppr, RUN], writes=[PRS])
                k.op("DVE", lambda e: e.tensor_tensor(out=PRS.ap, in0=PRS.ap, in1=OHC.ap, op=ALU.mult), reads=[PRS, OHC], writes=[PRS])
                k.op("DVE", lambda e: e.tensor_reduce(out=RK0.ap[:, i:i + 1], in_=PRS.ap[:, 0:64], axis=AX.X, op=ALU.add), reads=[PRS], writes=[RK0])
                k.op("DVE", lambda e: e.tensor_reduce(out=RK1.ap[:, i:i + 1], in_=PRS.ap[:, 64:128], axis=AX.X, op=ALU.add), reads=[PRS], writes=[RK1])
                k.op("DVE", lambda e: e.tensor_tensor(out=RUN.ap, in0=RUN.ap, in1=pcs.ap[:, 0:128], op=ALU.add), reads=[pcs, RUN], writes=[RUN])
                trb = PBK[1].ap.bitcast(BF16)
                for kc in range(8):
                    k.op("PE", lambda e: e.transpose(trb[:, kc * 128:(kc + 1) * 128], u3[:, kc, bs], identb.ap), reads=[ut, identb], writes=[PBK[1]])
                k.op("ACT", lambda e: e.activation(out=XR3[:, i, :], in_=trb, func=AF.Copy), reads=[PBK[1]], writes=[XROWS])

        k.alias([BIG, IGF], ABS)
        cnt1, cnt2 = RUN.ap[:, 0:64], RUN.ap[:, 64:128]
        k.op("DVE", lambda e: e.tensor_tensor(out=CNT.ap, in0=cnt1, in1=cnt2, op=ALU.add), reads=[RUN], writes=[CNT])
        big_cm = BIG.ap[:, 0:2048].rearrange("p (a m) -> p a m", a=64)
        for mh in range(2):
            k.op("DVE", lambda e: e.tensor_tensor(out=big_cm, in0=CNT.ap.unsqueeze(2).to_broadcast([128, 64, 32]),
                                                  in1=THR[:, mh * 32:(mh + 1) * 32].unsqueeze(1).to_broadcast([128, 64, 32]), op=ALU.is_gt), reads=[CNT, cf], writes=[BIG])
            dstn = NB_ if mh == 0 else BASE1
            k.op("DVE", lambda e: e.tensor_reduce(out=dstn.ap, in_=big_cm, axis=AX.X, op=ALU.add), reads=[BIG], writes=[dstn])
        k.op("DVE", lambda e: e.tensor_tensor(out=NB_.ap, in0=NB_.ap, in1=BASE1.ap, op=ALU.add), reads=[NB_, BASE1], writes=[NB_])
        k.op("DVE", lambda e: e.tensor_scalar(out=NB_.ap, in0=NB_.ap, scalar1=float(MB), scalar2=None, op0=ALU.mult), reads=[NB_], writes=[NB_])
        k.op("DVE", lambda e: e.tensor_copy(out=PE_[0].ap, in_=NB_.ap), reads=[NB_], writes=[PE_[0]])
        cur = 0
        for sft in (1, 2, 4, 8, 16, 32):
            a_, b_ = PE_[cur], PE_[1 - cur]
            k.op("DVE", lambda e: e.tensor_copy(out=b_.ap[:, 0:sft], in_=a_.ap[:, 0:sft]), reads=[a_], writes=[b_])
            k.op("DVE", lambda e: e.tensor_tensor(out=b_.ap[:, sft:64], in0=a_.ap[:, sft:64], in1=a_.ap[:, 0:64 - sft], op=ALU.add), reads=[a_, b_], writes=[b_])
            cur = 1 - cur
        PEND = PE_[cur]
        k.op("DVE", lambda e: e.tensor_tensor(out=BASE1.ap, in0=PEND.ap, in1=NB_.ap, op=ALU.subtract), reads=[PEND, NB_], writes=[BASE1])
        k.op("DVE", lambda e: e.tensor_tensor(out=BASE2.ap, in0=BASE1.ap, in1=cnt1, op=ALU.add), reads=[BASE1, RUN], writes=[BASE2])
        TBC = 2048 // 64
        for (base, rk, di, lo) in ((BASE1, RK0, DI0, 0), (BASE2, RK1, DI1, 64)):
            for c0 in range(0, NTB, TBC):
                nb = min(TBC, NTB - c0)
                big_t = BIG.ap[:, 0:nb * 64].rearrange("p (a m) -> p a m", a=nb)
                k.op("DVE", lambda e: e.tensor_tensor(out=big_t, in0=OHS3[:, c0:c0 + nb, lo:lo + 64], in1=base.ap.unsqueeze(1).to_broadcast([128, nb, 64]), op=ALU.mult),
                     reads=[OHS, base], writes=[BIG])
                k.op("DVE", lambda e: e.tensor_reduce(out=DF.ap[:, c0:c0 + nb], in_=big_t, axis=AX.X, op=ALU.add), reads=[BIG], writes=[DF])
            k.op("DVE", lambda e: e.tensor_tensor(out=DF.ap, in0=DF.ap, in1=rk.ap, op=ALU.add), reads=[DF, rk], writes=[DF])
            k.op("DVE", lambda e: e.tensor_copy(out=di.ap, in_=DF.ap), reads=[DF], writes=[di])
        for c0 in range(0, NBLK, 32):
            nb = min(32, NBLK - c0)
            big_j = BIG.ap[:, 0:nb * 64].rearrange("p (a m) -> p a m", a=nb)
            k.op("DVE", lambda e: e.tensor_tensor(out=big_j, in0=PEND.ap.unsqueeze(1).to_broadcast([128, nb, 64]), in1=JB[:, c0:c0 + nb].unsqueeze(2).to_broadcast([128, nb, 64]), op=ALU.is_le),
                 reads=[PEND, cf], writes=[BIG])
            k.op("DVE", lambda e: e.tensor_reduce(out=EJ.ap[:, c0:c0 + nb], in_=big_j, axis=AX.X, op=ALU.add), reads=[BIG], writes=[EJ])
        k.op("DVE", lambda e: e.tensor_scalar(out=EJ.ap, in0=EJ.ap, scalar1=63.0, scalar2=None, op0=ALU.min), reads=[EJ], writes=[EJ])
        NST = 4
        HB = NBLK // NST
        k.op("DVE", lambda e: e.memset(SAME.ap, 0.0), writes=[SAME])
        for s0 in range(0, NBLK, HB):
            k.op("DVE", lambda e: e.tensor_tensor(out=SAME.ap[:, s0 + 1:s0 + HB], in0=EJ.ap[:, s0 + 1:s0 + HB], in1=EJ.ap[:, s0:s0 + HB - 1], op=ALU.is_equal),
                 reads=[EJ, SAME], writes=[SAME])
        k.op("DVE", lambda e: e.tensor_scalar(out=SAME.ap, in0=SAME.ap, scalar1=float(1 << 22), scalar2=None, op0=ALU.mult), reads=[SAME], writes=[SAME])
        igf3 = v3(IGF.ap[:, 0:NBLK * 2], NBLK)
        k.op("DVE", lambda e: e.scalar_tensor_tensor(out=BIG.ap[:, 0:NBLK], in0=EJ.ap, scalar=256.0, in1=SAME.ap, op0=ALU.mult, op1=ALU.add), reads=[EJ, SAME], writes=[BIG])
        k.op("DVE", lambda e: e.tensor_tensor(out=igf3, in0=BIG.ap[:, 0:NBLK].unsqueeze(2).to_broadcast([128, NBLK, 2]), in1=KP[:, 0:2].unsqueeze(1).to_broadcast([128, NBLK, 2]), op=ALU.add),
             reads=[BIG, cf], writes=[IGF])
        k.op("DVE", lambda e: e.tensor_copy(out=IGI.ap[:, 0:NBLK * 2], in_=IGF.ap[:, 0:NBLK * 2]), reads=[IGF], writes=[IGI])

        for i in range(NTB):
            k.idma(Xs, XR3[:, i, :], DI0, DI0.ap[:, i:i + 1], gather=False, reads=[XROWS], writes=[Xs_t], part=True)
            k.idma(Xs, XR3[:, i, :], DI1, DI1.ap[:, i:i + 1], gather=False, reads=[XROWS], writes=[Xs_t], part=True)

        k.alias(XB + XTB + YB, [BIG, IGF] + ABS)
        k.alias(WBX, [XROWS])
        WB4 = WB + WBX

        bnd = nc.gpsimd.to_reg(64 * 256 - 1)
        igi2 = v3(IGI.ap[:, 0:NBLK * 2], NBLK)

        def load_blk(j, n_):
            wb = WB4[n_ % NST]
            first = True
            for (src, base) in ((wg_r, 0), (wu_r, 4096), (wd_r, 8192)):
                for h in range(2):
                    k.idma(wb.ap[:, base + h * 2048:base + (h + 1) * 2048], src, IGI, igi2[:, j, h:h + 1], gather=True, writes=[wb], part=(not first), bound=bnd)
                    first = False

        def load_xb(j, n_):
            k.dma("SP", XB[n_ % 2].ap, Xs[j * MB:(j + 1) * MB, :], reads=[Xs_t], writes=[XB[n_ % 2]])

        def front(j, n_):
            wb, xb, xt = WB4[n_ % NST], XB[n_ % 2], XTB[n_ % 2]
            trb0, trb1 = PBK[0].ap.bitcast(BF16), PBK[1].ap.bitcast(BF16)
            for kc in range(8):
                dst = (trb0 if kc < 4 else trb1)[:, (kc % 4) * 128:(kc % 4 + 1) * 128]
                k.op("PE", lambda e: e.transpose(dst, xb.ap.rearrange("p (m c) -> p c m", c=8)[:, kc, :], identb.ap), reads=[xb, identb], writes=[PBK[0] if kc < 4 else PBK[1]])
            k.op("ACT", lambda e: e.activation(out=xt.ap[:, 0:512], in_=trb0[:, 0:512], func=AF.Copy), reads=[PBK[0]], writes=[xt])
            k.op("ACT", lambda e: e.activation(out=xt.ap[:, 512:1024], in_=trb1[:, 0:512], func=AF.Copy), reads=[PBK[1], xt], writes=[xt])
            Wg3, Wu3 = v3(wb.ap[:, 0:4096], 8), v3(wb.ap[:, 4096:8192], 8)
            pg, pu = PG[n_ % 2], PU[n_ % 2]
            for f in range(4):
                for kc in range(8):
                    k.op("PE", lambda e: e.matmul(pg.ap[:, f * 128:(f + 1) * 128], Wg3[:, kc, :].rearrange("p (m c) -> p c m", c=4)[:, f, :], xt.ap[:, kc * 128:(kc + 1) * 128],
                                                  start=(kc == 0), stop=(kc == 7)), reads=[wb, xt], writes=[pg], pe_acc=(kc > 0 or f > 0))
            for f in range(4):
                for kc in range(8):
                    k.op("PE", lambda e: e.matmul(pu.ap[:, f * 128:(f + 1) * 128], Wu3[:, kc, :].rearrange("p (m c) -> p c m", c=4)[:, f, :], xt.ap[:, kc * 128:(kc + 1) * 128],
                                                  start=(kc == 0), stop=(kc == 7)), reads=[wb, xt], writes=[pu], pe_acc=(kc > 0 or f > 0))
            k.op("ACT", lambda e: e.activation(out=SS[n_ % 2].ap, in_=pg.ap, func=AF.Silu), reads=[pg], writes=[SS[n_ % 2]])
            k.op("DVE", lambda e: e.tensor_tensor(out=HH[n_ % 2].ap, in0=SS[n_ % 2].ap, in1=pu.ap, op=ALU.mult), reads=[SS[n_ % 2], pu], writes=[HH[n_ % 2]])

        def back(j, n_):
            wb, hh, yb = WB4[n_ % NST], HH[n_ % 2], YB[n_ % 2]
            Wd3 = v3(wb.ap[:, 8192:12288], 4)
            for dh in range(2):
                pd = PD[dh]
                for f in range(4):
                    k.op("PE", lambda e: e.matmul(pd.ap, hh.ap[:, f * 128:(f + 1) * 128], Wd3[:, f, dh * 512:(dh + 1) * 512], start=(f == 0), stop=(f == 3)),
                         reads=[wb, hh], writes=[pd], pe_acc=(f > 0))
                if dh == 0:
                    k.op("ACT", lambda e: e.activation(out=yb.ap[:, 0:512], in_=pd.ap, func=AF.Copy), reads=[pd], writes=[yb])
                else:
                    k.op("DVE", lambda e: e.tensor_copy(out=yb.ap[:, 512:1024], in_=pd.ap), reads=[pd, yb], writes=[yb])
            k.dma("SP", Ys[j * MB:(j + 1) * MB, :], yb.ap, reads=[yb], writes=[Ys_t], part=True, sem_of=yb)

        order = []
        for q in range(HB):
            order += [s_ * HB + q for s_ in range(NST)]
        for n_ in range(NST):
            load_blk(order[n_], n_)
        for n_ in range(2):
            load_xb(order[n_], n_)
        for n_, j in enumerate(order):
            front(j, n_)
            back(j, n_)
            if n_ + NST < NBLK:
                load_blk(order[n_ + NST], n_ + NST)
            if n_ + 2 < NBLK:
                load_xb(order[n_ + 2], n_ + 2)

        k.dma("POOL", v3(WB[0].ap[:, 0:8192], 8), plg.rearrange("(a p) n -> p a n", p=128), writes=[WB[0]])
        k.dma("POOL", v3(WB[1].ap[:, 0:2048], 2), plp.rearrange("(a p) n -> p a n", p=128), writes=[WB[1]])
        k.alias(WQ + Y0 + Y1, [XROWS] + WBX)
        if mode == "A":
            k.dma("POOL", v3(WQ[0].ap, 8), wqkv[:, 0:1536].rearrange("(a p) n -> p a n", p=128), writes=[WQ[0]])
            k.dma("POOL", v3(WQ[1].ap, 8), wqkv[:, 1536:3072].rearrange("(a p) n -> p a n", p=128), writes=[WQ[1]])
        for t in range(NTILE):
            cs = slice(t * 512, (t + 1) * 512)
            ht, ut = HTL[t % 2], UTL[t % 2]
            h3, u3 = v3(ht.ap, 8), v3(ut.ap, 8)
            k.dma("SP", h3, H1[:, cs].rearrange("(a p) n -> p a n", p=128), reads=[H1_t], writes=[ht])
            k.dma("POOL", v3(PBt.ap, 2), pT[:, cs].rearrange("(a p) n -> p a n", p=128), writes=[PBt])
            for b in range(4):
                i = t * 4 + b
                bs = slice(b * 128, (b + 1) * 128)
                y0, y1 = Y0[i % 2], Y1[i % 2]
                k.idma(y0.ap, Ys, DI0, DI0.ap[:, i:i + 1], gather=True, reads=[Ys_t], writes=[y0])
                k.idma(y1.ap, Ys, DI1, DI1.ap[:, i:i + 1], gather=True, reads=[Ys_t], writes=[y1])
                k.op("DVE", lambda e: e.tensor_scalar(out=y0.ap, in0=y0.ap, scalar1=GA.ap[:, i:i + 1], scalar2=None, op0=ALU.mult), reads=[y0, GA], writes=[y0])
                k.op("DVE", lambda e: e.scalar_tensor_tensor(out=y0.ap, in0=y1.ap, scalar=GB.ap[:, i:i + 1], in1=y0.ap, op0=ALU.mult, op1=ALU.add),
                     reads=[y1, GB, y0], writes=[y0])
                for half in range(2):
                    bank = PD[half]
                    for dq in range(4):
                        d = half * 4 + dq
                        k.op("PE", lambda e: e.transpose(bank.ap[:, dq * 128:(dq + 1) * 128], y0.ap[:, d * 128:(d + 1) * 128], ident), reads=[y0, cf], writes=[bank])
                    k.op("DVE", lambda e: e.tensor_tensor(out=h3[:, half * 4:(half + 1) * 4, bs], in0=h3[:, half * 4:(half + 1) * 4, bs],
                                                          in1=bank.ap.rearrange("p (a n) -> p a n", a=4), op=ALU.add), reads=[bank, ht], writes=[ht])
            rmsnorm(ht, h3, 8, ut, u3, PBK[0])
            for d in range(8):
                pg, pu = PG[d % 2], PU[d % 2]
                for kc in range(8):
                    k.op("PE", lambda e: e.matmul(pg.ap, v3(WB[0].ap[:, 0:8192], 8)[:, kc, d * 128:(d + 1) * 128], u3[:, kc, :], start=(kc == 0), stop=(kc == 7)),
                         reads=[WB[0], ut], writes=[pg], pe_acc=(kc > 0))
                for kc in range(2):
                    k.op("PE", lambda e: e.matmul(pu.ap, v3(WB[1].ap[:, 0:2048], 2)[:, kc, d * 128:(d + 1) * 128], v3(PBt.ap, 2)[:, kc, :], start=(kc == 0), stop=(kc == 1)),
                         reads=[WB[1], PBt], writes=[pu], pe_acc=(kc > 0))
                sf = STF[d % 2]
                k.op("ACT", lambda e: e.activation(out=sf.ap, in_=pg.ap, func=AF.Sigmoid, bias=vec.ap[:, 16 + d:17 + d], scale=1.0), reads=[pg, vec], writes=[sf])
                k.op("DVE", lambda e: e.tensor_tensor(out=sf.ap, in0=sf.ap, in1=pu.ap, op=ALU.mult), reads=[sf, pu], writes=[sf])
                k.op("DVE", lambda e: e.tensor_tensor(out=h3[:, d, :], in0=h3[:, d, :], in1=sf.ap, op=ALU.add), reads=[sf, ht], writes=[ht])
            if mode == "A":
                k.dma("SP", hT_o[:, cs].rearrange("(a p) n -> p a n", p=128), h3, reads=[ht], writes=[ho_t], part=True)
                rmsnorm(ht, h3, 24, ut, u3, PBK[0])
                for n in range(16):
                    col = n * 128
                    wbt = WQ[0] if col < 1536 else WQ[1]
                    lc = col if col < 1536 else col - 1536
                    bank = PG[n % 2]
                    for kc in range(8):
                        k.op("PE", lambda e: e.matmul(bank.ap, v3(wbt.ap, 8)[:, kc, lc:lc + 128], u3[:, kc, :], start=(kc == 0), stop=(kc == 7)),
                             reads=[wbt, ut], writes=[bank], pe_acc=(kc > 0))
                    sg = STG[n % 2]
                    sc_ = (1.0 / np.sqrt(128.0)) if n < 8 else 1.0
                    k.op("ACT", lambda e: e.activation(out=sg.ap, in_=bank.ap, func=AF.Identity, scale=float(sc_)), reads=[bank], writes=[sg])
                    if n < 8:
                        k.dma("SP", qT_o[n * 128:(n + 1) * 128, cs], sg.ap, reads=[sg], writes=[qT_t], part=True, sem_of=sg)
                    else:
                        k.dma("SP", kT_o[(n - 8) * 128:(n - 7) * 128, cs], sg.ap, reads=[sg], writes=[kT_t], part=True, sem_of=sg)
                for b in range(4):
                    for hh_ in range(2):
                        bank = PU[hh_]
                        for kc in range(8):
                            k.op("PE", lambda e: e.matmul(bank.ap, u3[:, kc, b * 128:(b + 1) * 128], v3(WQ[1].ap, 8)[:, kc, 512 + hh_ * 512:1024 + hh_ * 512],
                                                          start=(kc == 0), stop=(kc == 7)), reads=[WQ[1], ut], writes=[bank], pe_acc=(kc > 0))
                        sg = STG[hh_]
                        k.op("ACT", lambda e: e.activation(out=sg.ap, in_=bank.ap, func=AF.Copy), reads=[bank], writes=[sg])
                        r0 = t * 512 + b * 128
                        k.dma("SP", v_o[r0:r0 + 128, hh_ * 512:(hh_ + 1) * 512], sg.ap, reads=[sg], writes=[v_t], part=True, sem_of=sg)
            else:
                k.op("ACT", lambda e: e.activation(out=u3, in_=h3, func=AF.Square), reads=[ht], writes=[ut])
                bank = PBK[0]
                for kc in range(8):
                    k.op("PE", lambda e: e.matmul(bank.ap, ones_bf.ap, u3[:, kc, :], start=(kc == 0), stop=(kc == 7)),
                         reads=[ut, ones_bf], writes=[bank], pe_acc=(kc > 0))
                k.op("ACT", lambda e: e.activation(out=LNV.ap, in_=bank.ap, func=AF.Ln, scale=1.0 / 1024, bias=EPS), reads=[bank], writes=[LNV])
                k.op("ACT", lambda e: e.activation(out=RSTD.ap, in_=LNV.ap, func=AF.Exp, scale=-0.5), reads=[LNV], writes=[RSTD])
                for d in range(8):
                    k.op("DVE", lambda e: e.scalar_tensor_tensor(out=h3[:, d, :], in0=h3[:, d, :], scalar=vec.ap[:, 24 + d:25 + d], in1=RSTD.ap,
                                                                 op0=ALU.mult, op1=ALU.mult), reads=[ht, RSTD, vec], writes=[ht])
                k.dma("SP", out_o[:, cs].rearrange("(a p) n -> p a n", p=128), h3, reads=[ht], writes=[out_t], part=True)
        k.finish("SP", [qT_t, kT_t, v_t, ho_t] if mode == "A" else [out_t])
        print("tok2 ninst", k.ninst)
    return nc


def build_attn2(L, NHD=2):
    nc = bass.Bass("TRN2", target_bir_lowering=False)

    def din(name, shape, dt=F32):
        return nc.dram_tensor(name, list(shape), dt, kind="ExternalInput").ap()

    qT = din("qT", [NHD, 128, L], BF16)
    kT = din("kT", [NHD, 128, L], BF16)
    vP = din("vP", [NHD, 128, L // 128, 128], BF16)
    cst = din("cst", [128, 256 + 4 * 512])
    oT = nc.dram_tensor("oT", [NHD, 128, L], BF16, kind="ExternalOutput").ap()
    NQT = L // 512
    NB = L // 128
    with ExitStack() as st:
        k = K(nc, st)
        Qs = k.sbt([128, L], BF16, "Qs")
        Ks = k.sbt([128, L], BF16, "Ks")
        Vs = k.sbt([128, L], BF16, "Vs")
        V3 = v3(Vs.ap, NB)
        cf = k.sbt([128, 256 + 2048], F32, "cf")
        trin = k.sbt([128, 128], BF16, "trin")
        onen = k.sbt([128, 128], BF16, "onen")
        m01 = k.sbt([128, 2048], BF16, "m01")
        mneg = k.sbt([128, 2048], F32, "mneg")
        E_ = [k.sbt([128, 1024], F32, "E%d" % i) for i in range(3)]
        SPf = [k.sbt([128, 1024], BF16, "SPf%d" % i) for i in range(3)]
        SPm = [k.sbt([128, 1024], BF16, "SPm%d" % i) for i in range(3)]
        TMP = [k.sbt([128, 1024], F32, "TMP%d" % i) for i in range(3)]
        AT = [k.sbt([128, 1024], BF16, "AT%d" % i) for i in range(3)]
        OFFS = [k.sbt([128, 512], F32, "OFF%d" % i) for i in range(2)]
        offi = [0]
        OST = [k.sbt([128, 512], BF16, "OST%d" % i) for i in range(2)]
        PA = [T(k.ps([128, 1024], F32, "pa%d" % i)[:]) for i in range(3)]
        PBb = T(k.ps([128, 512], F32, "pbb")[:])
        PO = T(k.ps([128, 512], F32, "po")[:])
        oT_t = T(oT)

        k.dma("SP", cf.ap, cst, writes=[cf])
        k.op("DVE", lambda e: e.tensor_copy(out=trin.ap, in_=cf.ap[:, 0:128]), reads=[cf], writes=[trin])
        k.op("DVE", lambda e: e.tensor_copy(out=onen.ap, in_=cf.ap[:, 128:256]), reads=[cf], writes=[onen])
        for pos, r in enumerate((3, 2, 1, 0)):
            src = cf.ap[:, 256 + r * 512:256 + (r + 1) * 512]
            k.op("DVE", lambda e: e.tensor_copy(out=m01.ap[:, pos * 512:(pos + 1) * 512], in_=src), reads=[cf, m01], writes=[m01])
            k.op("DVE", lambda e: e.tensor_scalar(out=mneg.ap[:, pos * 512:(pos + 1) * 512], in0=src, scalar1=-1.0, scalar2=30000.0, op0=ALU.add, op1=ALU.mult),
                 reads=[cf, mneg], writes=[mneg])

        jobs = []
        for hd in range(NHD):
            for J in range(NQT):
                nb = 4 * J + 4
                for pi, n1 in enumerate(range(nb - 1, 0, -2)):
                    n0 = n1 - 1
                    dg = -1
                    if n1 == 4 * J + 3:
                        dg = 0
                    elif n1 == 4 * J + 1:
                        dg = 1
                    jobs.append(dict(hd=hd, J=J, n1=n1, n0=n0, first=(pi == 0), last=(n0 == 0), dg=dg))

        def load_head(hd):
            k.dma("SP", Qs.ap, qT[hd], writes=[Qs])
            k.dma("SP", Ks.ap, kT[hd], writes=[Ks])
            k.dma("SP", V3, vP[hd], writes=[Vs])

        def s1(j, i):
            if j["first"] and j["J"] == 0:
                load_head(j["hd"])
            A = PA[i % 3]
            qs = slice(j["J"] * 512, (j["J"] + 1) * 512)
            for h_, n in enumerate((j["n1"], j["n0"])):
                k.op("PE", lambda e: e.matmul(A.ap[:, h_ * 512:(h_ + 1) * 512], Ks.ap[:, n * 128:(n + 1) * 128], Qs.ap[:, qs], start=True, stop=False),
                     reads=[Ks, Qs], writes=[A], pe_acc=(h_ > 0))
            k.op("ACT", lambda e: e.activation(out=E_[i % 3].ap, in_=A.ap, func=AF.Exp), reads=[A], writes=[E_[i % 3]])

        def s1b(j, i):
            k.op("ACT", lambda e: e.activation(out=SPf[i % 3].ap, in_=E_[i % 3].ap, func=AF.Ln, bias=1.0, scale=1.0), reads=[E_[i % 3]], writes=[SPf[i % 3]])
            if j["dg"] >= 0:
                d_ = j["dg"]
                k.op("POOL", lambda e: e.tensor_tensor(out=SPm[i % 3].ap, in0=SPf[i % 3].ap, in1=m01.ap[:, d_ * 1024:(d_ + 1) * 1024], op=ALU.mult),
                     reads=[SPf[i % 3], m01], writes=[SPm[i % 3]])

        def s2(j, i):
            A, B = PA[i % 3], PBb
            sp = SPm[i % 3] if j["dg"] >= 0 else SPf[i % 3]
            OFF = OFFS[offi[0] % 2]
            if j["first"]:
                k.op("DVE", lambda e: e.memset(OFF.ap, 0.0), writes=[OFF])
            k.op("PE", lambda e: e.matmul(A.ap[:, 0:512], trin.ap, sp.ap[:, 0:512], start=False, stop=True), reads=[trin, sp], writes=[A], pe_acc=True)
            k.op("PE", lambda e: e.matmul(A.ap[:, 512:1024], trin.ap, sp.ap[:, 512:1024], start=False, stop=False), reads=[trin, sp], writes=[A], pe_acc=True)
            k.op("PE", lambda e: e.matmul(A.ap[:, 512:1024], onen.ap, sp.ap[:, 0:512], start=False, stop=True), reads=[onen, sp], writes=[A], pe_acc=True)
            tm = TMP[i % 3]
            k.op("DVE", lambda e: e.tensor_tensor(out=v3(tm.ap, 2), in0=v3(A.ap, 2), in1=OFF.ap.unsqueeze(1).to_broadcast([128, 2, 512]), op=ALU.add),
                 reads=[A, OFF], writes=[tm])
            if j["dg"] >= 0:
                d_ = j["dg"]
                k.op("DVE", lambda e: e.tensor_tensor(out=tm.ap, in0=tm.ap, in1=mneg.ap[:, d_ * 1024:(d_ + 1) * 1024], op=ALU.add), reads=[tm, mneg], writes=[tm])
            k.op("ACT", lambda e: e.activation(out=AT[i % 3].ap, in_=tm.ap, func=AF.Exp), reads=[tm], writes=[AT[i % 3]])
            if not j["last"]:
                k.op("PE", lambda e: e.matmul(B.ap, onen.ap, sp.ap[:, 0:512], start=True, stop=False), reads=[onen, sp], writes=[B])
                k.op("PE", lambda e: e.matmul(B.ap, onen.ap, sp.ap[:, 512:1024], start=False, stop=True), reads=[onen, sp], writes=[B], pe_acc=True)
                OFN = OFFS[(offi[0] + 1) % 2]
                k.op("DVE", lambda e: e.tensor_tensor(out=OFN.ap, in0=OFF.ap, in1=B.ap, op=ALU.add), reads=[B, OFF], writes=[OFN])
            offi[0] += 1

        grp = [0]

        def s3(j, i):
            O = PO
            k.op("PE", lambda e: e.matmul(O.ap, V3[:, j["n1"], :], AT[i % 3].ap[:, 0:512], start=j["first"], stop=False),
                 reads=[Vs, AT[i % 3]], writes=[O], pe_acc=(not j["first"]))
            k.op("PE", lambda e: e.matmul(O.ap, V3[:, j["n0"], :], AT[i % 3].ap[:, 512:1024], start=False, stop=j["last"]),
                 reads=[Vs, AT[i % 3]], writes=[O], pe_acc=True)
            if j["last"]:
                og = OST[grp[0] % 2]
                k.op("ACT", lambda e: e.activation(out=og.ap, in_=O.ap, func=AF.Copy), reads=[O], writes=[og])
                k.dma("SP", oT[j["hd"], :, j["J"] * 512:(j["J"] + 1) * 512], og.ap, reads=[og], writes=[oT_t], part=True, sem_of=og)
                grp[0] += 1

        n = len(jobs)
        stages = [s1, s1b, s2, s3]
        done = [0] * n

        def run_next(jj):
            stages[done[jj]](jobs[jj], jj)
            done[jj] += 1

        for i in range(n + 3):
            if i < n and jobs[i]["first"] and jobs[i]["J"] == 0 and i > 0:
                for jj in range(max(0, i - 3), i):
                    while done[jj] < 4:
                        run_next(jj)
            for d_ in range(4):
                jj = i - d_
                if 0 <= jj < n and done[jj] == d_:
                    run_next(jj)
        assert all(x == 4 for x in done)
        k.finish("SP", [oT_t])
        print("attn2 ninst", k.ninst, "pairs", n)
    return nc
```
